# Optimizing a Trainium2 kernel written in Bass

```python
import math
import jax, jax.numpy as jnp
from jax import lax
import numpy as np

D_MODEL = 2048
BATCH = 4
SEQ = 2048
DEPTH = 4

M_HEADS = 8
M_QK = D_MODEL // 16
M_V = D_MODEL // 8
M_WIDTH = M_HEADS * M_V
M_CHUNK = 64
A_HEADS = 8
A_QK = D_MODEL // 16
A_V = 2 * A_QK
A_WIDTH = A_HEADS * A_V
ROPE_DIM = A_QK // 4
ROPE_THETA = 500000.0
Q_BLOCK = 128
D_FF = ((8 * D_MODEL // 3 + 255) // 256) * 256
CONV_W = 3
EPS = 1e-6
N_BRANCH = 2

SPLITS = [M_HEADS * M_QK, M_HEADS * M_QK, M_WIDTH, M_WIDTH, M_HEADS, M_HEADS,
          2 * A_HEADS * A_QK, 2 * A_HEADS * A_QK, A_WIDTH, N_BRANCH * D_MODEL]
OFFSETS = [int(o) for o in np.cumsum(SPLITS)[:-1]]
N_IN = int(sum(SPLITS))
F_GATE_OFF = OFFSETS[4]

kernel_name = "hybrid_mlstm_diffattn_convglu"


def rms_norm(x, g):
    xf = x.astype(jnp.float32)
    y = xf * lax.rsqrt(jnp.mean(xf * xf, axis=-1, keepdims=True) + EPS)
    return (y * g.astype(jnp.float32)).astype(x.dtype)


def partial_rotary(x, pos):
    half = ROPE_DIM // 2
    inv = ROPE_THETA ** (-jnp.arange(half, dtype=jnp.float32) / half)
    ang = pos.astype(jnp.float32)[:, None] * inv[None, :]
    cos = jnp.cos(ang)[None, :, None, None, :]
    sin = jnp.sin(ang)[None, :, None, None, :]
    xr = x[..., :ROPE_DIM].astype(jnp.float32)
    x1, x2 = xr[..., :half], xr[..., half:]
    rot = jnp.concatenate([x1 * cos - x2 * sin, x2 * cos + x1 * sin], axis=-1)
    return jnp.concatenate([rot.astype(x.dtype), x[..., ROPE_DIM:]], axis=-1)


def mlstm_chunkwise(q, k, v, i_pre, f_pre):
    B, H, S, dk = q.shape
    dv = v.shape[-1]
    L = M_CHUNK
    NC = S // L
    f32 = jnp.float32
    q = q.astype(f32)
    k = k.astype(f32) * (dk ** -0.5)
    v = v.astype(f32)
    log_f = jax.nn.log_sigmoid(f_pre.astype(f32))
    i_log = i_pre.astype(f32)

    def to_chunks(a):
        return jnp.moveaxis(a.reshape(B, H, NC, L, *a.shape[3:]), 2, 0)

    qc, kc, vc, ic = to_chunks(q), to_chunks(k), to_chunks(v), to_chunks(i_log)
    bc = jnp.cumsum(to_chunks(log_f), axis=-1)
    causal = jnp.tril(jnp.ones((L, L), dtype=bool))

    def step(carry, xs):
        C, n, m = carry
        q_, k_, v_, i_, b_ = xs
        log_d = b_[..., :, None] - b_[..., None, :] + i_[..., None, :]
        log_d = jnp.where(causal, log_d, -jnp.inf)
        inter_log = b_ + m[..., None]
        m_t = jnp.maximum(inter_log, jnp.max(log_d, axis=-1))
        d_mat = jnp.where(causal, jnp.exp(log_d - m_t[..., None]), 0.0)
        inter = jnp.exp(inter_log - m_t)
        s_mat = jnp.einsum('bhtd,bhsd->bhts', q_, k_) * d_mat
        num = (jnp.einsum('bhts,bhsv->bhtv', s_mat, v_)
               + inter[..., None] * jnp.einsum('bhtd,bhvd->bhtv', q_, C))
        den = s_mat.sum(-1) + inter * jnp.einsum('bhtd,bhd->bht', q_, n)
        h = num / jnp.maximum(jnp.abs(den), jnp.exp(-m_t))[..., None]
        g = b_[..., -1]
        w_log = g[..., None] - b_ + i_
        m_new = jnp.maximum(g + m, jnp.max(w_log, axis=-1))
        decay = jnp.exp(g + m - m_new)
        w = jnp.exp(w_log - m_new[..., None])
        C_new = decay[..., None, None] * C + jnp.einsum('bhs,bhsv,bhsd->bhvd', w, v_, k_)
        n_new = decay[..., None] * n + jnp.einsum('bhs,bhsd->bhd', w, k_)
        return (C_new, n_new, m_new), h

    init = (jnp.zeros((B, H, dv, dk), f32), jnp.zeros((B, H, dk), f32), jnp.zeros((B, H), f32))
    _, hs = lax.scan(step, init, (qc, kc, vc, ic, bc))
    return jnp.moveaxis(hs, 0, 2).reshape(B, H, S, dv)


def diff_attention(q, k, v, lam):
    B, H, _, S, d = q.shape
    dv = v.shape[-1]
    nb = S // Q_BLOCK
    scale = d ** -0.5
    kpos = jnp.arange(S)

    def block(i):
        start = i * Q_BLOCK
        qb = lax.dynamic_slice_in_dim(q, start, Q_BLOCK, axis=3)
        s = jnp.einsum('bhcqd,bhckd->bhcqk', qb, k).astype(jnp.float32) * scale
        qpos = start + jnp.arange(Q_BLOCK)
        s = jnp.where(kpos[None, :] <= qpos[:, None], s, -jnp.inf)
        p = jax.nn.softmax(s, axis=-1)
        a = p[:, :, 0] - lam * p[:, :, 1]
        return jnp.einsum('bhqk,bhkv->bhqv', a.astype(v.dtype), v)

    out = lax.map(block, jnp.arange(nb))
    return jnp.moveaxis(out, 0, 2).reshape(B, H, S, dv)


def hybrid_mixer(h, pos, layer, w_in, b_in, m_norm, a_norm, lam_qk, w_bm, w_bd, w_o):
    B, S, _ = h.shape
    z = h @ w_in + b_in
    mq, mk, mv, mo, mi, mf, aq, ak, av, gt = jnp.split(z, OFFSETS, axis=-1)

    q = mq.reshape(B, S, M_HEADS, M_QK).transpose(0, 2, 1, 3)
    k = mk.reshape(B, S, M_HEADS, M_QK).transpose(0, 2, 1, 3)
    v = mv.reshape(B, S, M_HEADS, M_V).transpose(0, 2, 1, 3)
    hm = mlstm_chunkwise(q, k, v, mi.transpose(0, 2, 1), mf.transpose(0, 2, 1))
    hm = hm.transpose(0, 2, 1, 3).astype(h.dtype)
    hm = rms_norm(hm, m_norm.reshape(M_HEADS, M_V)).reshape(B, S, M_WIDTH)
    hm = hm * jax.nn.sigmoid(mo)
    ym = hm @ w_bm

    lam_init = 0.8 - 0.6 * math.exp(-0.3 * layer)
    lq = lam_qk.astype(jnp.float32)
    lam = jnp.exp(jnp.sum(lq[0] * lq[1])) - jnp.exp(jnp.sum(lq[2] * lq[3])) + lam_init
    qa = partial_rotary(aq.reshape(B, S, A_HEADS, 2, A_QK), pos).transpose(0, 2, 3, 1, 4)
    ka = partial_rotary(ak.reshape(B, S, A_HEADS, 2, A_QK), pos).transpose(0, 2, 3, 1, 4)
    va = av.reshape(B, S, A_HEADS, A_V).transpose(0, 2, 1, 3)
    ha = diff_attention(qa, ka, va, lam).transpose(0, 2, 1, 3)
    ha = rms_norm(ha, a_norm.reshape(A_HEADS, A_V)) * (1.0 - lam_init)
    yd = ha.reshape(B, S, A_WIDTH) @ w_bd

    g_m, g_d = jnp.split(gt, N_BRANCH, axis=-1)
    y = jax.nn.sigmoid(g_m) * ym + jax.nn.sigmoid(g_d) * yd
    return y @ w_o


def conv_glu(h, w_up, conv_w, conv_b, w_down):
    S = h.shape[1]
    gate, val = jnp.split(h @ w_up, 2, axis=-1)
    gp = jnp.pad(gate, ((0, 0), (CONV_W - 1, 0), (0, 0)))
    conv = sum(conv_w[j] * gp[:, j:j + S] for j in range(CONV_W)) + conv_b
    return (jax.nn.silu(conv) * val) @ w_down


def setup_inputs(seed: int = 0) -> dict:
    key = jax.random.key(seed)
    ks = jax.random.split(key, 16)
    f32 = jnp.float32
    out_scale = (2.0 * DEPTH) ** -0.5

    def nrm(k, shape, scale):
        return jax.random.normal(k, shape, f32) * scale

    x = nrm(ks[0], (BATCH, SEQ, D_MODEL), 1.0)
    norm_mix = 1.0 + nrm(ks[1], (DEPTH, D_MODEL), 0.02)
    w_in = nrm(ks[2], (DEPTH, D_MODEL, N_IN), D_MODEL ** -0.5)
    b_in = nrm(ks[3], (DEPTH, N_IN), 0.01)
    b_in = b_in.at[:, F_GATE_OFF:F_GATE_OFF + M_HEADS].add(jnp.linspace(3.0, 6.0, M_HEADS, dtype=f32))
    m_norm = 1.0 + nrm(ks[4], (DEPTH, M_WIDTH), 0.02)
    a_norm = 1.0 + nrm(ks[5], (DEPTH, A_WIDTH), 0.02)
    lam_qk = nrm(ks[6], (DEPTH, 4, A_QK), 0.1)
    w_bm = nrm(ks[7], (DEPTH, M_WIDTH, D_MODEL), M_WIDTH ** -0.5)
    w_bd = nrm(ks[8], (DEPTH, A_WIDTH, D_MODEL), A_WIDTH ** -0.5)
    w_o = nrm(ks[9], (DEPTH, D_MODEL, D_MODEL), D_MODEL ** -0.5 * out_scale)
    norm_ffn = 1.0 + nrm(ks[10], (DEPTH, D_MODEL), 0.02)
    w_up = nrm(ks[11], (DEPTH, D_MODEL, 2 * D_FF), D_MODEL ** -0.5)
    conv_w = nrm(ks[12], (DEPTH, CONV_W, D_FF), CONV_W ** -0.5)
    conv_b = nrm(ks[13], (DEPTH, D_FF), 0.01)
    w_down = nrm(ks[14], (DEPTH, D_FF, D_MODEL), D_FF ** -0.5 * out_scale)
    norm_final = 1.0 + nrm(ks[15], (D_MODEL,), 0.02)
    return {"x": x, "norm_mix": norm_mix, "w_in": w_in, "b_in": b_in, "m_norm": m_norm,
            "a_norm": a_norm, "lam_qk": lam_qk, "w_bm": w_bm, "w_bd": w_bd, "w_o": w_o,
            "norm_ffn": norm_ffn, "w_up": w_up, "conv_w": conv_w, "conv_b": conv_b,
            "w_down": w_down, "norm_final": norm_final}


def reference(x, norm_mix, w_in, b_in, m_norm, a_norm, lam_qk, w_bm, w_bd, w_o,
              norm_ffn, w_up, conv_w, conv_b, w_down, norm_final):
    pos = jnp.arange(x.shape[1])
    h = x
    for l in range(DEPTH):
        h = h + hybrid_mixer(rms_norm(h, norm_mix[l]), pos, l, w_in[l], b_in[l], m_norm[l],
                             a_norm[l], lam_qk[l], w_bm[l], w_bd[l], w_o[l])
        h = h + conv_glu(rms_norm(h, norm_ffn[l]), w_up[l], conv_w[l], conv_b[l], w_down[l])
    return rms_norm(h, norm_final)
```

```python
import math
from contextlib import ExitStack

import numpy as np
import concourse.bass as bass
import concourse.mybir as mybir
from concourse.bass_utils import run_bass_kernel_spmd

F32 = mybir.dt.float32
BF16 = mybir.dt.bfloat16
AF = mybir.ActivationFunctionType
ALU = mybir.AluOpType

D = 2048
KC = 16
H = 8
NIN = 16400
DFF = 5632
FC = 44
EPS = 1e-6
O_MQ, O_MK, O_MV, O_MO, O_MI, O_AQ, O_AK, O_AV, O_GM, O_GD = (
    0, 1024, 2048, 4096, 6144, 6160, 8208, 10256, 12304, 14352)
ROPE_THETA = 500000.0


class Buf:
    __slots__ = ("w", "r", "dsem", "dcnt")

    def __init__(self):
        self.w = None
        self.r = {}
        self.dsem = None
        self.dcnt = 0


class Prog:
    ENGS = ("pe", "act", "dve", "pool", "sp")

    def __init__(self, nc, es, name):
        self.nc, self.es, self.name = nc, es, name
        self.q = {e: [] for e in self.ENGS}
        self.allsems = []
        self.sem = {e: self._newsem(f"{name}_{e}") for e in self.ENGS}
        self.cnt = {e: 0 for e in self.ENGS}
        self.waited = {e: {} for e in self.ENGS}
        self.dsems = []
        self.nd = 0

    def _newsem(self, name):
        h = self.nc.alloc_semaphore(name=name)
        self.allsems.append(h)
        return h

    def _wait(self, eng, ev):
        sem, val = ev
        if eng == "pe" and sem is self.sem["pe"]:
            return
        w = self.waited[eng]
        k = id(sem)
        if w.get(k, 0) >= val:
            return
        w[k] = val
        self.q[eng].append((0, sem, val))

    def _deps(self, eng, reads, writes):
        for b in reads:
            if b.w is not None:
                self._wait(eng, b.w)
        for b in writes:
            if b.w is not None:
                self._wait(eng, b.w)
            for ev in b.r.values():
                self._wait(eng, ev)

    @staticmethod
    def _commit(ev, reads, writes):
        for b in reads:
            b.r[id(ev[0])] = ev
        for b in writes:
            b.w = ev
            b.r = {}

    def op(self, eng, fn, reads=(), writes=()):
        self._deps(eng, reads, writes)
        self.cnt[eng] += 1
        ev = (self.sem[eng], self.cnt[eng])
        self.q[eng].append((1, fn, self.sem[eng], 1))
        self._commit(ev, reads, writes)
        return ev

    def dma(self, qeng, fn, sb, reads=(), writes=()):
        self._deps(qeng, reads, writes)
        if sb.dsem is None:
            self.nd += 1
            sb.dsem = self._newsem(f"{self.name}_d{self.nd}")
            sb.dcnt = 0
            self.dsems.append(sb)
        sb.dcnt += 16
        ev = (sb.dsem, sb.dcnt)
        self.q[qeng].append((1, fn, sb.dsem, 16))
        self._commit(ev, reads, writes)
        return ev

    def finish(self):
        for e in self.ENGS:
            for e2 in self.ENGS:
                if e2 != e and self.cnt[e2] > 0:
                    self._wait(e, (self.sem[e2], self.cnt[e2]))
            for sb in self.dsems:
                self._wait(e, (sb.dsem, sb.dcnt))
        for sb in self.dsems:
            sb.dsem = None
            sb.dcnt = 0
            sb.w = None
            sb.r = {}
        with self.nc.Block() as blk:
            blk.tensor(lambda e: self._emit("pe", e))
            blk.scalar(lambda e: self._emit("act", e))
            blk.vector(lambda e: self._emit("dve", e))
            blk.gpsimd(lambda e: self._emit("pool", e))
            blk.sync(lambda e: self._emit("sp", e))
        self.nc.all_engine_barrier()
        self.nc.clear_and_free_semaphores(self.allsems)
        self.nc.all_engine_barrier()

    def _emit(self, eng, e):
        for it in self.q[eng]:
            if it[0] == 0:
                e.wait_ge(it[1], it[2])
            else:
                it[1](e).then_inc(it[2], it[3])


class Rot:
    def __init__(self, items):
        self.items = items
        self.i = 0

    def next(self):
        x = self.items[self.i]
        self.i = (self.i + 1) % len(self.items)
        return x


class G:
    pass


_uid = [0]


def uid(p):
    _uid[0] += 1
    return f"{p}{_uid[0]}"


def sb(es, nc, shape, dt, name="t"):
    return es.enter_context(nc.sbuf_tensor(uid(name), list(shape), dt))


def pst(es, nc, shape, dt, name="p"):
    return es.enter_context(nc.psum_tensor(uid(name), list(shape), dt))


def sbufs(es, nc, n, shape, dt, name="t"):
    return [(sb(es, nc, shape, dt, name), Buf()) for _ in range(n)]


def psbufs(es, nc, n, shape, dt, name="p"):
    return [(pst(es, nc, shape, dt, name), Buf()) for _ in range(n)]


def fm(ap):
    return ap.rearrange("(k p) t -> p k t", p=128)


def ph_consts(g):
    nc = g.nc
    with ExitStack() as es:
        P = Prog(nc, es, uid("cst"))
        for t, src in ((g.identb, g.c_ident), (g.trib, g.c_tri), (g.trif, g.c_tri), (g.onesf, g.c_ones),
                       (g.pmf, g.c_pm)):
            P.dma("pool", lambda e, t=t, src=src: e.dma_start(out=t[:], in_=src), Buf(), writes=[])
        P.finish()


def ph_norm(g, src, gam_src, final_dst=None):
    nc, T = g.nc, g.T
    W = 256
    with ExitStack() as es:
        P = Prog(nc, es, uid("nrm"))
        hb = Rot(sbufs(es, nc, 2, [128, KC, W], F32, "hb"))
        sq = sb(es, nc, [128, KC, W], F32, "sq")
        sqA, sqB = Buf(), Buf()
        gm = sb(es, nc, [128, KC], F32, "gm")
        gmB = Buf()
        rs = Rot(sbufs(es, nc, 2, [128, W], F32, "rs"))
        rstd = Rot(sbufs(es, nc, 2, [128, W], F32, "rstd"))
        ps = Rot(psbufs(es, nc, 2, [128, W], F32, "nps"))
        ob = Rot(sbufs(es, nc, 2, [128, KC, W], F32, "ob")) if final_dst is not None else None
        P.dma("pool", lambda e: e.dma_start(out=gm[:], in_=gam_src), gmB, writes=[gmB])
        for j in range(T // W):
            ht, hB = hb.next()
            P.dma("pool", lambda e, ht=ht, j=j: e.dma_start(out=ht[:], in_=fm(src[:, j * W:(j + 1) * W])), hB,
                  writes=[hB])
            P.op("act", lambda e, ht=ht: e.activation(out=sq[:, 0:8, :], in_=ht[:, 0:8, :], func=AF.Square),
                 reads=[hB], writes=[sqA])
            P.op("dve", lambda e, ht=ht: e.tensor_tensor(out=sq[:, 8:16, :], in0=ht[:, 8:16, :], in1=ht[:, 8:16, :],
                                                         op=ALU.mult), reads=[hB], writes=[sqB])
            pt, pB = ps.next()

            def mm(e, pt=pt):
                for k in range(KC):
                    ins = e.matmul(pt[:], lhsT=g.onesf[:], rhs=sq[:, k, :], start=(k == 0), stop=(k == KC - 1))
                return ins
            P.op("pe", mm, reads=[sqA, sqB], writes=[pB])
            rt, rB = rs.next()
            P.op("act", lambda e, rt=rt, pt=pt: e.activation(out=rt[:], in_=pt[:], func=AF.Sqrt, bias=EPS,
                                                             scale=1.0 / D), reads=[pB], writes=[rB])
            st, sB = rstd.next()
            P.op("dve", lambda e, st=st, rt=rt: e.reciprocal(out=st[:], in_=rt[:]), reads=[rB], writes=[sB])
            if final_dst is None:
                def nrm(e, ht=ht, st=st, j=j):
                    for k in range(KC):
                        ins = e.scalar_tensor_tensor(out=g.hnT[:, k, j * W:(j + 1) * W], in0=ht[:, k, :],
                                                     scalar=gm[:, k:k + 1], in1=st[:], op0=ALU.mult, op1=ALU.mult)
                    return ins
                P.op("dve", nrm, reads=[hB, sB, gmB], writes=[Buf()])
            else:
                ot, oB = ob.next()

                def nrm(e, ht=ht, st=st, ot=ot):
                    for k in range(KC):
                        ins = e.scalar_tensor_tensor(out=ot[:, k, :], in0=ht[:, k, :],
                                                     scalar=gm[:, k:k + 1], in1=st[:], op0=ALU.mult, op1=ALU.mult)
                    return ins
                P.op("dve", nrm, reads=[hB, sB, gmB], writes=[oB])
                P.dma("sp", lambda e, ot=ot, j=j: e.dma_start(out=fm(final_dst[:, j * W:(j + 1) * W]), in_=ot[:]),
                      oB, reads=[oB])
        P.finish()


def slab_src(w2d, c0, cw):
    return w2d[:, c0:c0 + cw].rearrange("(k p) n -> p k n", p=128)


def ph_proj(g, l):
    nc, T = g.nc, g.T
    NQ, NT = T // 512, T // 128
    w = g.w_in[l]
    with ExitStack() as es:
        P = Prog(nc, es, uid("prj"))
        slabs = Rot(sbufs(es, nc, 3, [128, KC, 512], BF16, "wsl"))
        psr = Rot(psbufs(es, nc, 4, [128, 512], F32, "pp"))
        pxp = Rot(psbufs(es, nc, 2, [32, 512], F32, "px"))
        stg = Rot(sbufs(es, nc, 4, [128, 512], BF16, "stg"))
        xbr = Rot(sbufs(es, nc, 2, [128, 512], F32, "xb"))
        t1r = Rot(sbufs(es, nc, 2, [32, 512], F32, "t1"))
        t2r = Rot(sbufs(es, nc, 2, [32, 512], F32, "t2"))
        tmr = Rot(sbufs(es, nc, 2, [128, 512], F32, "tm"))
        cosT = sb(es, nc, [32, T], F32, "cos")
        sinT = sb(es, nc, [32, T], F32, "sin")
        bqk = sb(es, nc, [128, 48], F32, "bqk")
        brep = sb(es, nc, [128, 6144], F32, "brep")
        bif = sb(es, nc, [128, 16], F32, "bif")
        wif = sb(es, nc, [128, KC, 16], BF16, "wif")
        cB = Buf()
        for t, src in ((cosT, g.c_cos), (sinT, g.c_sin), (bqk, g.bqk[l]), (brep, g.btm[l].partition_broadcast(128)),
                       (bif, g.bif[l].partition_broadcast(128)), (wif, slab_src(w, O_MI, 16))):
            P.dma("pool", lambda e, t=t, src=src: e.dma_start(out=t[:], in_=src), cB, writes=[cB])
        jobs = []
        for s in range(2):
            jobs.append((O_MQ + s * 512, "fm", ("m", 0 + s * 4)))
        for s in range(2):
            jobs.append((O_MK + s * 512, "fm", ("m", 8 + s * 4)))
        for s in range(4):
            jobs.append((O_AQ + s * 512, "fm", ("aq", 16 + s * 4)))
        for s in range(4):
            jobs.append((O_AK + s * 512, "fm", ("ak", 32 + s * 4)))
        for s in range(4):
            jobs.append((O_MV + s * 512, "tm", ("mv", s)))
        for s in range(4):
            jobs.append((O_MO + s * 512, "tm", ("mo", s)))
        for s in range(4):
            jobs.append((O_AV + s * 512, "tm", ("av", s)))
        loaded = []

        def load(i):
            c0 = jobs[i][0]
            t, b = slabs.next()
            P.dma("pool", lambda e, t=t, c0=c0: e.dma_start(out=t[:], in_=slab_src(w, c0, 512)), b, writes=[b])
            loaded.append((t, b))
        load(0)
        load(1)
        for tt in range(NT):
            pt, pB = psr.next()

            def mm(e, pt=pt, tt=tt):
                for k in range(KC):
                    ins = e.matmul(pt[:, 0:16], lhsT=g.hnT[:, k, tt * 128:(tt + 1) * 128], rhs=wif[:, k, :],
                                   start=(k == 0), stop=(k == KC - 1))
                return ins
            P.op("pe", mm, reads=[cB], writes=[pB])
            P.op("dve", lambda e, pt=pt, tt=tt: e.tensor_tensor(out=g.IF[:, tt, :], in0=pt[:, 0:16], in1=bif[:],
                                                                op=ALU.add), reads=[pB, cB], writes=[Buf()])
        for i, (c0, kind, info) in enumerate(jobs):
            if i + 2 < len(jobs):
                load(i + 2)
            wt, wB = loaded[i]
            if kind == "fm":
                typ, ch0 = info
                for cc in range(4):
                    ch = ch0 + cc
                    for q in range(NQ):
                        pt, pB = psr.next()

                        def mm(e, pt=pt, wt=wt, cc=cc, q=q):
                            for k in range(KC):
                                ins = e.matmul(pt[:], lhsT=wt[:, k, cc * 128:(cc + 1) * 128],
                                               rhs=g.hnT[:, k, q * 512:(q + 1) * 512],
                                               start=(k == 0), stop=(k == KC - 1))
                            return ins
                        P.op("pe", mm, reads=[wB], writes=[pB])
                        st, sB = stg.next()
                        if typ == "m":
                            P.op("act", lambda e, st=st, pt=pt, ch=ch: e.activation(
                                out=st[:], in_=pt[:], func=AF.Identity, bias=bqk[:, ch:ch + 1]),
                                reads=[pB, cB], writes=[sB])
                        else:
                            xb, xB = xbr.next()
                            P.op("act", lambda e, xb=xb, pt=pt, ch=ch: e.activation(
                                out=xb[:], in_=pt[:], func=AF.Identity, bias=bqk[:, ch:ch + 1]),
                                reads=[pB, cB], writes=[xB])
                            px, pxB = pxp.next()
                            P.op("pe", lambda e, px=px, xb=xb: e.matmul(px[:], lhsT=g.pmf[:], rhs=xb[0:32, :],
                                                                        start=True, stop=True),
                                 reads=[xB], writes=[pxB])
                            t1, t1B = t1r.next()
                            t2, t2B = t2r.next()
                            P.op("dve", lambda e, t1=t1, px=px, q=q: e.tensor_tensor(
                                out=t1[:], in0=px[:], in1=sinT[:, q * 512:(q + 1) * 512], op=ALU.mult),
                                reads=[pxB, cB], writes=[t1B])
                            P.op("dve", lambda e, t2=t2, xb=xb, q=q: e.tensor_tensor(
                                out=t2[:], in0=xb[0:32, :], in1=cosT[:, q * 512:(q + 1) * 512], op=ALU.mult),
                                reads=[xB, cB], writes=[t2B])
                            P.op("dve", lambda e, t1=t1, t2=t2, xb=xb: e.tensor_tensor(
                                out=xb[0:32, :], in0=t1[:], in1=t2[:], op=ALU.add),
                                reads=[t1B, t2B], writes=[xB])
                            sc = (128.0 ** -0.5) if typ == "aq" else 1.0
                            P.op("act", lambda e, st=st, xb=xb, sc=sc: e.activation(
                                out=st[:], in_=xb[:], func=AF.Identity, scale=sc), reads=[xB], writes=[sB])
                        P.dma("sp", lambda e, st=st, ch=ch, q=q: e.dma_start(
                            out=g.QKs[ch * 128:(ch + 1) * 128, q * 512:(q + 1) * 512], in_=st[:]), sB, reads=[sB])
            else:
                typ, s = info
                dst = {"mv": g.MV, "mo": g.MO, "av": g.AV}[typ]
                boff = {"mv": 0, "mo": 2048, "av": 4096}[typ] + s * 512
                for tt in range(NT):
                    pt, pB = psr.next()

                    def mm(e, pt=pt, wt=wt, tt=tt):
                        for k in range(KC):
                            ins = e.matmul(pt[:], lhsT=g.hnT[:, k, tt * 128:(tt + 1) * 128], rhs=wt[:, k, :],
                                           start=(k == 0), stop=(k == KC - 1))
                        return ins
                    P.op("pe", mm, reads=[wB], writes=[pB])
                    st, sB = stg.next()
                    if typ == "mo":
                        tm, tB = tmr.next()
                        P.op("dve", lambda e, tm=tm, pt=pt, boff=boff: e.tensor_tensor(
                            out=tm[:], in0=pt[:], in1=brep[:, boff:boff + 512], op=ALU.add),
                            reads=[pB, cB], writes=[tB])
                        P.op("act", lambda e, st=st, tm=tm: e.activation(out=st[:], in_=tm[:], func=AF.Sigmoid),
                             reads=[tB], writes=[sB])
                    else:
                        P.op("dve", lambda e, st=st, pt=pt, boff=boff: e.tensor_tensor(
                            out=st[:], in0=pt[:], in1=brep[:, boff:boff + 512], op=ALU.add),
                            reads=[pB, cB], writes=[sB])
                    P.dma("sp", lambda e, st=st, dst=dst, tt=tt, s=s: e.dma_start(
                        out=dst[tt * 128:(tt + 1) * 128, s * 512:(s + 1) * 512], in_=st[:]), sB, reads=[sB])
        P.finish()


def tm_src(ap2d, h, NT):
    return ap2d[:, h * 256:(h + 1) * 256].rearrange("(t p) d -> p t d", p=128)


def ph_mlstm(g, l):
    nc, T = g.nc, g.T
    NT = T // 128
    with ExitStack() as es:
        P = Prog(nc, es, uid("mls"))
        NG = NT * 8
        def v3(ap):
            return ap.rearrange("p (c h) -> p c h", h=8)
        e1 = sb(es, nc, [128, NG], F32, "e1")
        l1 = sb(es, nc, [128, NG], F32, "l1")
        tg = sb(es, nc, [128, NG], F32, "tg")
        u = sb(es, nc, [128, NG], F32, "u")
        bnd = sb(es, nc, [128, NG], F32, "bnd")
        eg = sb(es, nc, [128, NG], F32, "eg")
        mrep = sb(es, nc, [128, 2048], F32, "mrep")
        cgp = pst(es, nc, [128, 2, NG], F32, "cgp")
        gB = Buf()
        P.dma("pool", lambda e: e.dma_start(out=mrep[:], in_=g.mnorm[l].partition_broadcast(128)), gB,
              writes=[gB])
        P.op("act", lambda e: e.activation(out=v3(e1[:]), in_=g.IF[:, :, 8:16], func=AF.Exp, scale=-1.0),
             writes=[gB])
        P.op("act", lambda e: e.activation(out=l1[:], in_=e1[:], func=AF.Ln, bias=1.0), reads=[gB], writes=[gB])

        def mmg(e):
            e.matmul(cgp[:, 0, :], lhsT=g.trif[:], rhs=l1[:], start=True, stop=True)
            return e.matmul(cgp[:, 1, :], lhsT=g.onesf[:], rhs=l1[:], start=True, stop=True)
        P.op("pe", mmg, reads=[gB], writes=[gB])
        P.op("dve", lambda e: e.tensor_tensor(out=v3(tg[:]), in0=v3(cgp[:, 0, :]), in1=g.IF[:, :, 0:8], op=ALU.add),
             reads=[gB], writes=[gB])

        def gex(e):
            e.activation(out=u[:], in_=tg[:], func=AF.Exp, bias=math.log(128.0 ** -0.5))
            e.activation(out=bnd[:], in_=cgp[:, 0, :], func=AF.Exp)
            return e.activation(out=eg[:], in_=cgp[:, 1, :], func=AF.Exp, scale=-1.0)
        P.op("act", gex, reads=[gB], writes=[gB])

        qkr = Rot([(sb(es, nc, [128, T], BF16, "qT"), sb(es, nc, [128, T], BF16, "kT"), Buf(), Buf())
                   for _ in range(2)])
        var = [(sb(es, nc, [128, NT, 257], BF16, "vA"), Buf()) for _ in range(2)]
        gr = Rot(sbufs(es, nc, 2, [128, NT, 256], BF16, "Gg"))
        hms = Rot(sbufs(es, nc, 2, [128, 2, T], BF16, "hms"))
        C = sb(es, nc, [128, 257], F32, "C")
        C1 = sb(es, nc, [128, 257], F32, "C1")
        Cb = sb(es, nc, [128, 257], BF16, "Cb")
        CB, C1B, CbB = Buf(), Buf(), Buf()
        ptr = Rot(sbufs(es, nc, 2, [128, 128], BF16, "PT"))
        kur = Rot(sbufs(es, nc, 2, [128, 128], BF16, "ku"))
        smr = Rot(sbufs(es, nc, 2, [128, 8], F32, "sm"))
        jkr = Rot(sbufs(es, nc, 2, [128, 256], F32, "jk"))
        hmr = Rot(sbufs(es, nc, 2, [128, 256], BF16, "hm"))
        stp = Rot(psbufs(es, nc, 2, [128, 128], F32, "stp"))
        ktp = Rot(psbufs(es, nc, 1, [128, 128], BF16, "ktp"))
        outp = Rot(psbufs(es, nc, 2, [128, 257], F32, "outp"))
        updp = Rot(psbufs(es, nc, 1, [128, 257], F32, "updp"))
        trp = Rot(psbufs(es, nc, 1, [128, 2, 128], BF16, "trp"))
        for vt, vB in var:
            P.op("dve", lambda e, vt=vt: e.memset(vt[:, :, 256:257], 1.0), writes=[vB])
        vr = Rot(var)
        for h in range(H):
            qT, kT, qB, kB = qkr.next()
            qkB = qB
            vA, vB = vr.next()
            Gt, GB = gr.next()
            hs, hsB = hms.next()
            P.dma("pool", lambda e, qT=qT, h=h: e.dma_start(out=qT[:], in_=g.QKs[h * 128:(h + 1) * 128, :]), qB,
                  writes=[qB])
            P.dma("pool", lambda e, kT=kT, h=h: e.dma_start(out=kT[:], in_=g.QKs[(8 + h) * 128:(9 + h) * 128, :]),
                  kB, writes=[kB])
            P.dma("pool", lambda e, vA=vA, h=h: e.dma_start(out=vA[:, :, 0:256], in_=tm_src(g.MV, h, NT)), vB,
                  writes=[vB])
            P.dma("pool", lambda e, Gt=Gt, h=h: e.dma_start(out=Gt[:], in_=tm_src(g.MO, h, NT)), GB,
                  writes=[GB])

            def gmul(e, Gt=Gt, h=h):
                for c in range(NT):
                    ins = e.tensor_tensor(out=Gt[:, c, :], in0=Gt[:, c, :], in1=mrep[:, h * 256:(h + 1) * 256],
                                          op=ALU.mult)
                return ins
            P.op("dve", gmul, reads=[gB], writes=[GB])
            for c in range(NT):
                cs = slice(c * 128, (c + 1) * 128)
                uc = u[:, c * 8 + h:c * 8 + h + 1]
                st, stB = stp.next()
                P.op("pe", lambda e, st=st, kT=kT, qT=qT, cs=cs: e.matmul(st[:], lhsT=kT[:, cs], rhs=qT[:, cs],
                                                                          start=True, stop=True),
                     reads=[qB, kB], writes=[stB])
                pt, ptB = ptr.next()
                P.op("dve", lambda e, pt=pt, st=st, uc=uc: e.scalar_tensor_tensor(
                    out=pt[:], in0=st[:], scalar=uc, in1=g.trif[:], op0=ALU.mult, op1=ALU.mult),
                    reads=[stB, gB], writes=[ptB])
                kt, ktB = ktp.next()
                P.op("pe", lambda e, kt=kt, kT=kT, cs=cs: e.transpose(kt[:], kT[:, cs], g.identb[:]),
                     reads=[kB], writes=[ktB])
                ku, kuB = kur.next()
                P.op("act", lambda e, ku=ku, kt=kt, uc=uc: e.activation(out=ku[:], in_=kt[:], func=AF.Identity,
                                                                        scale=uc),
                     reads=[ktB, gB], writes=[kuB])
                ot, oB = outp.next()

                def mmo(e, ot=ot, pt=pt, vA=vA, qT=qT, c=c, cs=cs):
                    ins = e.matmul(ot[:], lhsT=pt[:], rhs=vA[:, c, :], start=True, stop=(c == 0))
                    if c > 0:
                        ins = e.matmul(ot[:], lhsT=qT[:, cs], rhs=Cb[:], start=False, stop=True)
                    return ins
                P.op("pe", mmo, reads=[ptB, vB, qB] + ([CbB] if c > 0 else []), writes=[oB])
                if c < NT - 1:
                    up, upB = updp.next()
                    P.op("pe", lambda e, up=up, ku=ku, vA=vA, c=c: e.matmul(up[:], lhsT=ku[:], rhs=vA[:, c, :],
                                                                            start=True, stop=True),
                         reads=[kuB, vB], writes=[upB])
                    egc = eg[:, c * 8 + h:c * 8 + h + 1]
                    if c == 0:
                        P.op("dve", lambda e, up=up, egc=egc: e.tensor_scalar(
                            out=C[:], in0=up[:], scalar1=egc, scalar2=None, op0=ALU.mult),
                            reads=[upB, gB], writes=[CB])
                    else:
                        P.op("act", lambda e, egc=egc: e.activation(out=C1[:], in_=C[:], func=AF.Identity,
                                                                    scale=egc),
                             reads=[CB, gB], writes=[C1B])
                        P.op("dve", lambda e, up=up, egc=egc: e.scalar_tensor_tensor(
                            out=C[:], in0=up[:], scalar=egc, in1=C1[:], op0=ALU.mult, op1=ALU.add),
                            reads=[upB, C1B, gB], writes=[CB])
                    P.op("act", lambda e: e.activation(out=Cb[:], in_=C[:], func=AF.Identity),
                         reads=[CB], writes=[CbB])
                sm, smB = smr.next()
                P.op("act", lambda e, sm=sm, ot=ot: e.activation(out=sm[:, 0:1], in_=ot[:, 256:257], func=AF.Abs),
                     reads=[oB], writes=[smB])
                P.op("dve", lambda e, sm=sm, c=c, h=h: e.tensor_scalar(
                    out=sm[:, 1:2], in0=sm[:, 0:1], scalar1=bnd[:, c * 8 + h:c * 8 + h + 1], scalar2=None, op0=ALU.max),
                    reads=[smB, gB], writes=[smB])
                P.op("dve", lambda e, sm=sm: e.reciprocal(out=sm[:, 2:3], in_=sm[:, 1:2]),
                     reads=[smB], writes=[smB])
                jk, jkB = jkr.next()
                P.op("act", lambda e, jk=jk, ot=ot, sm=sm: e.activation(
                    out=jk[:], in_=ot[:, 0:256], func=AF.Square, scale=sm[:, 2:3], accum_out=sm[:, 3:4]),
                    reads=[oB, smB], writes=[jkB, smB])
                P.op("act", lambda e, sm=sm: e.activation(out=sm[:, 4:5], in_=sm[:, 3:4], func=AF.Ln, bias=EPS,
                                                          scale=1.0 / 256), reads=[smB], writes=[smB])
                P.op("act", lambda e, sm=sm: e.activation(out=sm[:, 5:6], in_=sm[:, 4:5], func=AF.Exp,
                                                          scale=-0.5), reads=[smB], writes=[smB])
                P.op("dve", lambda e, sm=sm: e.tensor_tensor(out=sm[:, 6:7], in0=sm[:, 5:6], in1=sm[:, 2:3],
                                                             op=ALU.mult), reads=[smB], writes=[smB])
                hm, hmB = hmr.next()
                P.op("dve", lambda e, hm=hm, ot=ot, sm=sm, Gt=Gt, c=c: e.scalar_tensor_tensor(
                    out=hm[:], in0=ot[:, 0:256], scalar=sm[:, 6:7], in1=Gt[:, c, :], op0=ALU.mult, op1=ALU.mult),
                    reads=[oB, smB, GB], writes=[hmB])
                tr, trB = trp.next()

                def trs(e, tr=tr, hm=hm):
                    e.transpose(tr[:, 0, :], hm[:, 0:128], g.identb[:])
                    return e.transpose(tr[:, 1, :], hm[:, 128:256], g.identb[:])
                P.op("pe", trs, reads=[hmB], writes=[trB])
                P.op("act", lambda e, hs=hs, tr=tr, cs=cs: e.activation(out=hs[:, :, cs], in_=tr[:],
                                                                        func=AF.Identity),
                     reads=[trB], writes=[hsB])
            P.dma("sp", lambda e, hs=hs, h=h: e.dma_start(
                out=g.HMT[h * 256:(h + 1) * 256, :].rearrange("(j p) t -> p j t", p=128), in_=hs[:]), hsB,
                reads=[hsB])
        P.finish()


def ph_attn(g, l):
    nc, T = g.nc, g.T
    NT, NG = T // 128, T // 256
    lam_init = 0.8 - 0.6 * math.exp(-0.3 * l)
    with ExitStack() as es:
        P = Prog(nc, es, uid("att"))
        lq = sb(es, nc, [128, 4, 128], F32, "lq")
        lp = sb(es, nc, [128, 2, 128], F32, "lp")
        ls = sb(es, nc, [128, 8], F32, "ls")
        anrep = sb(es, nc, [128, 2048], F32, "anrep")
        cB = Buf()
        P.dma("pool", lambda e: e.dma_start(out=lq[:].rearrange("p a b -> p (a b)"),
                                            in_=g.lamqk[l].partition_broadcast(128)), cB, writes=[cB])
        P.dma("pool", lambda e: e.dma_start(out=anrep[:], in_=g.anorm[l].partition_broadcast(128)), cB,
              writes=[cB])

        def lam1(e):
            e.tensor_tensor(out=lp[:, 0, :], in0=lq[:, 0, :], in1=lq[:, 1, :], op=ALU.mult)
            return e.tensor_tensor(out=lp[:, 1, :], in0=lq[:, 2, :], in1=lq[:, 3, :], op=ALU.mult)
        P.op("dve", lam1, reads=[cB], writes=[cB])

        def lam2(e):
            e.reduce_sum(out=ls[:, 0:1], in_=lp[:, 0, :], axis=mybir.AxisListType.X)
            return e.reduce_sum(out=ls[:, 1:2], in_=lp[:, 1, :], axis=mybir.AxisListType.X)
        P.op("dve", lam2, reads=[cB], writes=[cB])
        P.op("act", lambda e: e.activation(out=ls[:, 2:4], in_=ls[:, 0:2], func=AF.Exp), reads=[cB], writes=[cB])
        P.op("dve", lambda e: e.scalar_tensor_tensor(out=ls[:, 4:5], in0=ls[:, 3:4], scalar=-lam_init,
                                                     in1=ls[:, 2:3], op0=ALU.add, op1=ALU.subtract),
             reads=[cB], writes=[cB])
        P.op("dve", lambda e: e.tensor_scalar(out=anrep[:], in0=anrep[:], scalar1=(1.0 - lam_init), scalar2=None,
                                              op0=ALU.mult), reads=[cB], writes=[cB])
        nlam = ls[:, 4:5]

        qkr = Rot([(sb(es, nc, [128, 2, T], BF16, "q2"), sb(es, nc, [128, 2, T], BF16, "k2"), Buf(), Buf())
                   for _ in range(2)])
        var = [(sb(es, nc, [128, NT, 257], BF16, "vA"), Buf()) for _ in range(2)]
        has = Rot(sbufs(es, nc, 2, [128, 2, T], BF16, "has"))
        ptr = Rot(sbufs(es, nc, 3, [128, 2, 256], BF16, "PT"))
        rrr = Rot(sbufs(es, nc, 2, [128, 8], F32, "rr"))
        a0r = Rot(sbufs(es, nc, 2, [128, 256], F32, "a0"))
        a1r = Rot(sbufs(es, nc, 2, [128, 256], F32, "a1"))
        jkr = Rot(sbufs(es, nc, 2, [128, 256], F32, "jk"))
        habr = Rot(sbufs(es, nc, 2, [128, 256], BF16, "hab"))
        stp = Rot(psbufs(es, nc, 2, [128, 2, 256], F32, "stp"))
        outp = [[(pst(es, nc, [128, 257], F32, "outp"), Buf()) for j in range(2)] for m in range(2)]
        trp = Rot(psbufs(es, nc, 1, [128, 2, 128], BF16, "trp"))
        for vt, vB in var:
            P.op("dve", lambda e, vt=vt: e.memset(vt[:, :, 256:257], 1.0), writes=[vB])
        vr = Rot(var)
        for h in range(H):
            q2, k2, qB, kB = qkr.next()
            vA, vB = vr.next()
            hs, hsB = has.next()
            c0 = (16 + 2 * h) * 128
            P.dma("pool", lambda e, q2=q2, c0=c0: e.dma_start(
                out=q2[:], in_=g.QKs[c0:c0 + 256, :].rearrange("(c p) t -> p c t", p=128)), qB, writes=[qB])
            c1 = (32 + 2 * h) * 128
            P.dma("pool", lambda e, k2=k2, c1=c1: e.dma_start(
                out=k2[:], in_=g.QKs[c1:c1 + 256, :].rearrange("(c p) t -> p c t", p=128)), kB, writes=[kB])
            P.dma("pool", lambda e, vA=vA, h=h: e.dma_start(out=vA[:, :, 0:256], in_=tm_src(g.AV, h, NT)), vB,
                  writes=[vB])
            for gq in range(NG):
                qs = slice(gq * 256, (gq + 1) * 256)
                for kt in range(2 * gq + 2):
                    ks = slice(kt * 128, (kt + 1) * 128)
                    st, stB = stp.next()

                    def mmq(e, st=st, k2=k2, q2=q2, ks=ks, qs=qs):
                        e.matmul(st[:, 0, :], lhsT=k2[:, 0, ks], rhs=q2[:, 0, qs], start=True, stop=True)
                        return e.matmul(st[:, 1, :], lhsT=k2[:, 1, ks], rhs=q2[:, 1, qs], start=True, stop=True)
                    P.op("pe", mmq, reads=[qB, kB], writes=[stB])
                    pt, ptB = ptr.next()
                    P.op("act", lambda e, pt=pt, st=st: e.activation(out=pt[:], in_=st[:], func=AF.Exp),
                         reads=[stB], writes=[ptB])
                    if kt >= 2 * gq:
                        j0 = kt - 2 * gq

                        def msk(e, pt=pt, j0=j0):
                            e.tensor_tensor(out=pt[:, 0, j0 * 128:(j0 + 1) * 128],
                                            in0=pt[:, 0, j0 * 128:(j0 + 1) * 128], in1=g.trib[:], op=ALU.mult)
                            return e.tensor_tensor(out=pt[:, 1, j0 * 128:(j0 + 1) * 128],
                                                   in0=pt[:, 1, j0 * 128:(j0 + 1) * 128], in1=g.trib[:],
                                                   op=ALU.mult)
                        P.op("dve", msk, reads=[ptB], writes=[ptB])
                    js = [j for j in range(2) if kt <= 2 * gq + j]

                    def mmv(e, pt=pt, vA=vA, kt=kt, gq=gq, js=js):
                        for j in js:
                            for m in range(2):
                                ins = e.matmul(outp[m][j][0][:], lhsT=pt[:, m, j * 128:(j + 1) * 128],
                                               rhs=vA[:, kt, :], start=(kt == 0), stop=(kt == 2 * gq + j),
                                               skip_group_check=True)
                        return ins
                    P.op("pe", mmv, reads=[ptB, vB], writes=[outp[m][j][1] for j in js for m in range(2)])
                for j in range(2):
                    (o0, o0B), (o1, o1B) = outp[0][j], outp[1][j]
                    ts = slice((gq * 2 + j) * 128, (gq * 2 + j + 1) * 128)
                    rr, rB = rrr.next()

                    def rcp(e, rr=rr, o0=o0, o1=o1):
                        e.reciprocal(out=rr[:, 0:1], in_=o0[:, 256:257])
                        return e.reciprocal(out=rr[:, 1:2], in_=o1[:, 256:257])
                    P.op("dve", rcp, reads=[o0B, o1B], writes=[rB])
                    P.op("dve", lambda e, rr=rr: e.tensor_tensor(out=rr[:, 2:3], in0=rr[:, 1:2], in1=nlam,
                                                                 op=ALU.mult), reads=[rB, cB], writes=[rB])
                    a0, a0B = a0r.next()
                    P.op("act", lambda e, a0=a0, o0=o0, rr=rr: e.activation(out=a0[:], in_=o0[:, 0:256],
                                                                            func=AF.Identity, scale=rr[:, 0:1]),
                         reads=[o0B, rB], writes=[a0B])
                    a1, a1B = a1r.next()
                    P.op("dve", lambda e, a1=a1, o1=o1, rr=rr, a0=a0: e.scalar_tensor_tensor(
                        out=a1[:], in0=o1[:, 0:256], scalar=rr[:, 2:3], in1=a0[:], op0=ALU.mult, op1=ALU.add),
                        reads=[o1B, rB, a0B], writes=[a1B])
                    jk, jkB = jkr.next()
                    P.op("act", lambda e, jk=jk, a1=a1, rr=rr: e.activation(out=jk[:], in_=a1[:], func=AF.Square,
                                                                            accum_out=rr[:, 3:4]),
                         reads=[a1B, rB], writes=[jkB, rB])
                    P.op("act", lambda e, rr=rr: e.activation(out=rr[:, 4:5], in_=rr[:, 3:4], func=AF.Ln, bias=EPS,
                                                              scale=1.0 / 256), reads=[rB], writes=[rB])
                    P.op("act", lambda e, rr=rr: e.activation(out=rr[:, 5:6], in_=rr[:, 4:5], func=AF.Exp,
                                                              scale=-0.5), reads=[rB], writes=[rB])
                    hab, habB = habr.next()
                    P.op("dve", lambda e, hab=hab, a1=a1, rr=rr, h=h: e.scalar_tensor_tensor(
                        out=hab[:], in0=a1[:], scalar=rr[:, 5:6], in1=anrep[:, h * 256:(h + 1) * 256],
                        op0=ALU.mult, op1=ALU.mult), reads=[a1B, rB, cB], writes=[habB])
                    tr, trB = trp.next()

                    def trs(e, tr=tr, hab=hab):
                        e.transpose(tr[:, 0, :], hab[:, 0:128], g.identb[:])
                        return e.transpose(tr[:, 1, :], hab[:, 128:256], g.identb[:])
                    P.op("pe", trs, reads=[habB], writes=[trB])
                    P.op("act", lambda e, hs=hs, tr=tr, ts=ts: e.activation(out=hs[:, :, ts], in_=tr[:],
                                                                            func=AF.Identity),
                         reads=[trB], writes=[hsB])
            P.dma("sp", lambda e, hs=hs, h=h: e.dma_start(
                out=g.HAT[h * 256:(h + 1) * 256, :].rearrange("(j p) t -> p j t", p=128), in_=hs[:]), hsB,
                reads=[hsB])
        P.finish()


def ph_merge(g, l, which):
    nc, T = g.nc, g.T
    NQ = T // 512
    wa = (g.w_bm if which == 0 else g.w_bd)[l]
    wg = g.w_in[l]
    goff = O_GM if which == 0 else O_GD
    src = g.HMT if which == 0 else g.HAT
    with ExitStack() as es:
        P = Prog(nc, es, uid("mrg"))
        xT = sb(es, nc, [128, KC, T], BF16, "xT")
        xB = Buf()
        bg = sb(es, nc, [128, 32], F32, "bg")
        bgB = Buf()
        P.dma("pool", lambda e: e.dma_start(out=bg[:], in_=g.bgate[l]), bgB, writes=[bgB])
        P.dma("pool", lambda e: e.dma_start(out=xT[:], in_=fm(src)), xB, writes=[xB])
        SW = 256
        NS = 2048 // SW
        CPS = SW // 128
        slabs = Rot(sbufs(es, nc, 4, [128, KC, SW], BF16, "wsl"))
        psy = Rot(psbufs(es, nc, 3, [128, 512], F32, "py"))
        psg = Rot(psbufs(es, nc, 3, [128, 512], F32, "pg"))
        sgr = Rot(sbufs(es, nc, 2, [128, 512], F32, "sg"))
        y1r = Rot(sbufs(es, nc, 3, [128, 512], F32, "y1"))
        if which == 1:
            tr_ = Rot(sbufs(es, nc, 2, [128, 512], F32, "tt"))
            yor = Rot(sbufs(es, nc, 3, [128, 512], BF16, "yo"))
        loaded = []

        def load(s):
            ta, ba = slabs.next()
            P.dma("pool", lambda e, ta=ta, s=s: e.dma_start(out=ta[:], in_=slab_src(wa, s * SW, SW)), ba,
                  writes=[ba])
            tg, bgs = slabs.next()
            P.dma("pool", lambda e, tg=tg, s=s: e.dma_start(out=tg[:], in_=slab_src(wg, goff + s * SW, SW)), bgs,
                  writes=[bgs])
            loaded.append((ta, ba, tg, bgs))
        load(0)
        for s in range(NS):
            if s + 1 < NS:
                load(s + 1)
            ta, ba, tg, bgs = loaded[s]
            for cc in range(CPS):
                ch = s * CPS + cc
                for q in range(NQ):
                    qs = slice(q * 512, (q + 1) * 512)
                    py, pyB = psy.next()
                    pg, pgB = psg.next()

                    def mm(e, py=py, pg=pg, ta=ta, tg=tg, cc=cc, qs=qs):
                        for k in range(KC):
                            e.matmul(py[:], lhsT=ta[:, k, cc * 128:(cc + 1) * 128], rhs=xT[:, k, qs],
                                     start=(k == 0), stop=(k == KC - 1))
                        for k in range(KC):
                            ins = e.matmul(pg[:], lhsT=tg[:, k, cc * 128:(cc + 1) * 128], rhs=g.hnT[:, k, qs],
                                           start=(k == 0), stop=(k == KC - 1))
                        return ins
                    P.op("pe", mm, reads=[ba, bgs, xB], writes=[pyB, pgB])
                    sg, sgB = sgr.next()
                    bcol = which * 16 + ch
                    P.op("act", lambda e, sg=sg, pg=pg, bcol=bcol: e.activation(
                        out=sg[:], in_=pg[:], func=AF.Sigmoid, bias=bg[:, bcol:bcol + 1]),
                        reads=[pgB, bgB], writes=[sgB])
                    if which == 0:
                        y1, y1B = y1r.next()
                        P.op("dve", lambda e, y1=y1, py=py, sg=sg: e.tensor_tensor(out=y1[:], in0=py[:], in1=sg[:],
                                                                                   op=ALU.mult),
                             reads=[pyB, sgB], writes=[y1B])
                        P.dma("sp", lambda e, y1=y1, ch=ch, qs=qs: e.dma_start(
                            out=g.Y1[ch * 128:(ch + 1) * 128, qs], in_=y1[:]), y1B, reads=[y1B])
                    else:
                        y1, y1B = y1r.next()
                        P.dma("pool", lambda e, y1=y1, ch=ch, qs=qs: e.dma_start(
                            out=y1[:], in_=g.Y1[ch * 128:(ch + 1) * 128, qs]), y1B, writes=[y1B])
                        tt, ttB = tr_.next()
                        P.op("dve", lambda e, tt=tt, py=py, sg=sg: e.tensor_tensor(out=tt[:], in0=py[:], in1=sg[:],
                                                                                   op=ALU.mult),
                             reads=[pyB, sgB], writes=[ttB])
                        yo, yoB = yor.next()
                        P.op("dve", lambda e, yo=yo, tt=tt, y1=y1: e.tensor_tensor(out=yo[:], in0=tt[:], in1=y1[:],
                                                                                   op=ALU.add),
                             reads=[ttB, y1B], writes=[yoB])
                        P.dma("sp", lambda e, yo=yo, ch=ch, qs=qs: e.dma_start(
                            out=g.YT[ch * 128:(ch + 1) * 128, qs], in_=yo[:]), yoB, reads=[yoB])
        P.finish()


def ph_dump_hn(g, dst):
    nc = g.nc
    with ExitStack() as es:
        P = Prog(nc, es, uid("dmp"))
        b = Buf()
        for k in range(KC):
            P.dma("sp", lambda e, k=k: e.dma_start(out=dst[k * 128:(k + 1) * 128, :], in_=g.hnT[:, k, :]), b,
                  reads=[b])
        P.finish()


def ph_oproj(g, l, hsrc):
    nc, T = g.nc, g.T
    NQ = T // 512
    wo = g.w_o[l]
    with ExitStack() as es:
        P = Prog(nc, es, uid("opr"))
        xT = sb(es, nc, [128, KC, T], BF16, "xT")
        xB = Buf()
        P.dma("pool", lambda e: e.dma_start(out=xT[:], in_=fm(g.YT)), xB, writes=[xB])
        slabs = Rot(sbufs(es, nc, 3, [128, KC, 512], BF16, "wsl"))
        psr = Rot(psbufs(es, nc, 4, [128, 512], F32, "pp"))
        hir = Rot(sbufs(es, nc, 3, [128, 512], F32, "hi"))
        hor = Rot(sbufs(es, nc, 3, [128, 512], F32, "ho"))
        loaded = []

        def load(s):
            t, b = slabs.next()
            P.dma("pool", lambda e, t=t, s=s: e.dma_start(out=t[:], in_=slab_src(wo, s * 512, 512)), b, writes=[b])
            loaded.append((t, b))
        load(0)
        load(1)
        for s in range(4):
            if s + 2 < 4:
                load(s + 2)
            wt, wB = loaded[s]
            for cc in range(4):
                ch = s * 4 + cc
                for q in range(NQ):
                    qs = slice(q * 512, (q + 1) * 512)
                    hi, hiB = hir.next()
                    P.dma("pool", lambda e, hi=hi, ch=ch, qs=qs: e.dma_start(
                        out=hi[:], in_=hsrc[ch * 128:(ch + 1) * 128, qs]), hiB, writes=[hiB])
                    pt, pB = psr.next()

                    def mm(e, pt=pt, wt=wt, cc=cc, qs=qs):
                        for k in range(KC):
                            ins = e.matmul(pt[:], lhsT=wt[:, k, cc * 128:(cc + 1) * 128], rhs=xT[:, k, qs],
                                           start=(k == 0), stop=(k == KC - 1))
                        return ins
                    P.op("pe", mm, reads=[wB, xB], writes=[pB])
                    ho, hoB = hor.next()
                    P.op("dve", lambda e, ho=ho, pt=pt, hi=hi: e.tensor_tensor(out=ho[:], in0=pt[:], in1=hi[:],
                                                                               op=ALU.add),
                         reads=[pB, hiB], writes=[hoB])
                    P.dma("sp", lambda e, ho=ho, ch=ch, qs=qs: e.dma_start(
                        out=g.hT[ch * 128:(ch + 1) * 128, qs], in_=ho[:]), hoB, reads=[hoB])
                    if False:
                        P.dma("sp", lambda e, ho=ho, ch=ch, qs=qs: e.dma_start(
                            out=g.H1[ch * 128:(ch + 1) * 128, qs], in_=ho[:]), hoB, reads=[hoB])
        P.finish()


def ph_up(g, l):
    nc, T = g.nc, g.T
    NQ = T // 512
    wu = g.w_up[l]
    with ExitStack() as es:
        P = Prog(nc, es, uid("up"))
        cw = sb(es, nc, [128, FC, 3], F32, "cw")
        cb = sb(es, nc, [128, FC], F32, "cb")
        cB = Buf()
        P.dma("pool", lambda e: e.dma_start(out=cw[:], in_=g.cw[l]), cB, writes=[cB])
        P.dma("pool", lambda e: e.dma_start(out=cb[:], in_=g.cb[l]), cB, writes=[cB])
        slabs = Rot(sbufs(es, nc, 4, [128, KC, 512], BF16, "wsl"))
        psr = Rot(psbufs(es, nc, 6, [128, 512], F32, "pp"))
        gts = [(sb(es, nc, [128, T + 2], F32, "gt"), Buf()) for _ in range(2)]
        vsr = Rot(sbufs(es, nc, 2, [128, T], F32, "vs"))
        cvr = Rot(sbufs(es, nc, 2, [128, T], F32, "cv"))
        uor = Rot(sbufs(es, nc, 2, [128, T], BF16, "uo"))
        for gt, gB in gts:
            P.op("dve", lambda e, gt=gt: e.memset(gt[:, 0:2], 0.0), writes=[gB])
        gtr = Rot(gts)
        loaded = []

        def load(s):
            ta, ba = slabs.next()
            P.dma("pool", lambda e, ta=ta, s=s: e.dma_start(out=ta[:], in_=slab_src(wu, s * 512, 512)), ba,
                  writes=[ba])
            tv, bv = slabs.next()
            P.dma("pool", lambda e, tv=tv, s=s: e.dma_start(out=tv[:], in_=slab_src(wu, DFF + s * 512, 512)), bv,
                  writes=[bv])
            loaded.append((ta, ba, tv, bv))
        load(0)
        for s in range(11):
            if s + 1 < 11:
                load(s + 1)
            ta, ba, tv, bv = loaded[s]
            for cc in range(4):
                j = s * 4 + cc
                gt, gB = gtr.next()
                vs, vB = vsr.next()
                for q in range(NQ):
                    qs = slice(q * 512, (q + 1) * 512)
                    pa, paB = psr.next()
                    pv, pvB = psr.next()

                    def mm(e, pa=pa, pv=pv, ta=ta, tv=tv, cc=cc, qs=qs):
                        for k in range(KC):
                            e.matmul(pa[:], lhsT=ta[:, k, cc * 128:(cc + 1) * 128], rhs=g.hnT[:, k, qs],
                                     start=(k == 0), stop=(k == KC - 1))
                        for k in range(KC):
                            ins = e.matmul(pv[:], lhsT=tv[:, k, cc * 128:(cc + 1) * 128], rhs=g.hnT[:, k, qs],
                                           start=(k == 0), stop=(k == KC - 1))
                        return ins
                    P.op("pe", mm, reads=[ba, bv], writes=[paB, pvB])
                    P.op("act", lambda e, gt=gt, pa=pa, q=q: e.activation(
                        out=gt[:, 2 + q * 512:2 + (q + 1) * 512], in_=pa[:], func=AF.Identity),
                        reads=[paB], writes=[gB])
                    P.op("act", lambda e, vs=vs, pv=pv, qs=qs: e.activation(out=vs[:, qs], in_=pv[:],
                                                                            func=AF.Identity),
                         reads=[pvB], writes=[vB])
                cv, cvB = cvr.next()

                P.op("dve", lambda e, cv=cv, gt=gt, j=j: e.tensor_scalar(
                    out=cv[:], in0=gt[:, 0:T], scalar1=cw[:, j, 0:1], scalar2=cb[:, j:j + 1], op0=ALU.mult,
                    op1=ALU.add), reads=[gB, cB], writes=[cvB])
                P.op("dve", lambda e, cv=cv, gt=gt, j=j: e.scalar_tensor_tensor(
                    out=cv[:], in0=gt[:, 1:T + 1], scalar=cw[:, j, 1:2], in1=cv[:], op0=ALU.mult, op1=ALU.add),
                    reads=[gB, cB, cvB], writes=[cvB])
                P.op("dve", lambda e, cv=cv, gt=gt, j=j: e.scalar_tensor_tensor(
                    out=cv[:], in0=gt[:, 2:T + 2], scalar=cw[:, j, 2:3], in1=cv[:], op0=ALU.mult, op1=ALU.add),
                    reads=[gB, cB, cvB], writes=[cvB])
                P.op("act", lambda e, cv=cv: e.activation(out=cv[:], in_=cv[:], func=AF.Silu),
                     reads=[cvB], writes=[cvB])
                uo, uoB = uor.next()
                P.op("dve", lambda e, uo=uo, cv=cv, vs=vs: e.tensor_tensor(out=uo[:], in0=cv[:], in1=vs[:],
                                                                           op=ALU.mult),
                     reads=[cvB, vB], writes=[uoB])
                P.dma("sp", lambda e, uo=uo, j=j: e.dma_start(out=g.UT[j * 128:(j + 1) * 128, :], in_=uo[:]), uoB,
                      reads=[uoB])
        P.finish()


def ph_down(g, l):
    nc, T = g.nc, g.T
    TT = min(T, 1024)
    NQ = TT // 512
    wd = g.w_down[l]
    with ExitStack() as es:
        P = Prog(nc, es, uid("dwn"))
        uT = sb(es, nc, [128, FC, TT], BF16, "uT")
        uB = Buf()
        slabs = Rot(sbufs(es, nc, 3, [128, FC, 128], BF16, "wsl"))
        psr = Rot(psbufs(es, nc, 4, [128, 512], F32, "pp"))
        hir = Rot(sbufs(es, nc, 3, [128, 512], F32, "hi"))
        hor = Rot(sbufs(es, nc, 3, [128, 512], F32, "ho"))
        for half in range(T // TT):
            t0 = half * TT
            P.dma("pool", lambda e, t0=t0: e.dma_start(out=uT[:], in_=fm(g.UT[:, t0:t0 + TT])), uB, writes=[uB])
            loaded = []

            def load(c):
                t, b = slabs.next()
                P.dma("pool", lambda e, t=t, c=c: e.dma_start(
                    out=t[:], in_=wd[:, c * 128:(c + 1) * 128].rearrange("(k p) n -> p k n", p=128)), b,
                    writes=[b])
                loaded.append((t, b))
            load(0)
            load(1)
            for c in range(KC):
                if c + 2 < KC:
                    load(c + 2)
                wt, wB = loaded[c]
                for q in range(NQ):
                    qs = slice(q * 512, (q + 1) * 512)
                    gs = slice(t0 + q * 512, t0 + (q + 1) * 512)
                    hi, hiB = hir.next()
                    P.dma("pool", lambda e, hi=hi, c=c, gs=gs: e.dma_start(
                        out=hi[:], in_=g.hT[c * 128:(c + 1) * 128, gs]), hiB, writes=[hiB])
                    pt, pB = psr.next()

                    def mm(e, pt=pt, wt=wt, qs=qs):
                        for k in range(FC):
                            ins = e.matmul(pt[:], lhsT=wt[:, k, :], rhs=uT[:, k, qs], start=(k == 0),
                                           stop=(k == FC - 1))
                        return ins
                    P.op("pe", mm, reads=[wB, uB], writes=[pB])
                    ho, hoB = hor.next()
                    P.op("dve", lambda e, ho=ho, pt=pt, hi=hi: e.tensor_tensor(out=ho[:], in0=pt[:], in1=hi[:],
                                                                               op=ALU.add),
                         reads=[pB, hiB], writes=[hoB])
                    P.dma("sp", lambda e, ho=ho, c=c, gs=gs: e.dma_start(
                        out=g.hT[c * 128:(c + 1) * 128, gs], in_=ho[:]), hoB, reads=[hoB])
        P.finish()


def build(T, L, debug=False):
    nc = bass.Bass("TRN2", target_bir_lowering=False)
    g = G()
    g.nc, g.T, g.L = nc, T, L
    g.debug = debug

    def din(name, shape, dt=F32):
        return nc.dram_tensor(name, list(shape), dt, kind="ExternalInput").ap()

    def scr(name, shape, dt):
        kind = "ExternalOutput" if debug else "Internal"
        return nc.dram_tensor(name, list(shape), dt, kind=kind).ap()
    g.xT = din("xT", [D, T])
    g.w_in = din("w_in", [L, D, NIN])
    g.w_bm = din("w_bm", [L, D, D])
    g.w_bd = din("w_bd", [L, D, D])
    g.w_o = din("w_o", [L, D, D])
    g.w_up = din("w_up", [L, D, 2 * DFF])
    g.w_down = din("w_down", [L, DFF, D])
    g.gmix = din("gmix", [L, 128, KC])
    g.gffn = din("gffn", [L, 128, KC])
    g.gfin = din("gfin", [128, KC])
    g.bqk = din("bqk", [L, 128, 48])
    g.bgate = din("bgate", [L, 128, 32])
    g.btm = din("btm", [L, 6144])
    g.bif = din("bif", [L, 16])
    g.mnorm = din("mnorm", [L, 2048])
    g.anorm = din("anorm", [L, 2048])
    g.lamqk = din("lamqk", [L, 512])
    g.cw = din("cw", [L, 128, FC, 3])
    g.cb = din("cb", [L, 128, FC])
    g.c_ident = din("c_ident", [128, 128])
    g.c_tri = din("c_tri", [128, 128])
    g.c_ones = din("c_ones", [128, 128])
    g.c_pm = din("c_pm", [32, 32])
    g.c_cos = din("c_cos", [32, T])
    g.c_sin = din("c_sin", [32, T])
    g.outT = nc.dram_tensor("outT", [D, T], F32, kind="ExternalOutput").ap()
    g.hT = scr("hT", [D, T], F32)
    g.QKs = scr("QKs", [48 * 128, T], BF16)
    g.MV = scr("MV", [T, 2048], BF16)
    g.MO = scr("MO", [T, 2048], BF16)
    g.AV = scr("AV", [T, 2048], BF16)
    g.HMT = scr("HMT", [D, T], BF16)
    g.HAT = scr("HAT", [D, T], BF16)
    g.Y1 = scr("Y1", [D, T], F32)
    g.YT = scr("YT", [D, T], BF16)
    g.UT = scr("UT", [DFF, T], BF16)
    if debug:
        g.H1 = scr("H1", [D, T], F32)
        g.HN2 = scr("HN2", [D, T], BF16)
    with ExitStack() as es:
        g.hnT = sb(es, nc, [128, KC, T], BF16, "hnT")
        g.IF = sb(es, nc, [128, T // 128, 16], F32, "IF")
        g.identb = sb(es, nc, [128, 128], BF16, "identb")
        g.trib = sb(es, nc, [128, 128], BF16, "trib")
        g.trif = sb(es, nc, [128, 128], F32, "trif")
        g.onesf = sb(es, nc, [128, 128], F32, "onesf")
        g.pmf = sb(es, nc, [32, 32], F32, "pmf")
        ph_consts(g)
        for l in range(L):
            hsrc = g.xT if l == 0 else g.hT
            ph_norm(g, hsrc, g.gmix[l])
            ph_proj(g, l)
            ph_mlstm(g, l)
            ph_attn(g, l)
            ph_merge(g, l, 0)
            ph_merge(g, l, 1)
            ph_oproj(g, l, hsrc)
            if debug == 2:
                continue
            ph_norm(g, g.hT, g.gffn[l])
            ph_up(g, l)
            if debug == 3:
                continue
            ph_down(g, l)
        ph_norm(g, g.hT, g.gfin, final_dst=g.outT)
    return nc


def pcol(v, nch):
    v = np.asarray(v, dtype=np.float32)
    return np.ascontiguousarray(np.swapaxes(v.reshape(v.shape[:-1] + (nch, 128)), -1, -2))


def host_consts(T):
    s = np.arange(128)
    tri = (s[:, None] <= s[None, :]).astype(np.float32)
    pm = np.zeros((32, 32), np.float32)
    for j in range(16):
        pm[j + 16, j] = -1.0
        pm[j, j + 16] = 1.0
    half = 16
    inv = (np.float32(ROPE_THETA) ** (-np.arange(half, dtype=np.float32) / np.float32(half))).astype(np.float32)
    ang = np.arange(T, dtype=np.float32)[:, None] * inv[None, :]
    cos = np.cos(ang.astype(np.float64)).astype(np.float32).T
    sin = np.sin(ang.astype(np.float64)).astype(np.float32).T
    return {
        "c_ident": np.eye(128, dtype=np.float32), "c_tri": tri, "c_ones": np.ones((128, 128), np.float32),
        "c_pm": pm, "c_cos": np.ascontiguousarray(np.concatenate([cos, cos], 0)),
        "c_sin": np.ascontiguousarray(np.concatenate([sin, sin], 0)),
    }


def prep_shared(L, T, norm_mix, w_in, b_in, m_norm, a_norm, lam_qk, w_bm, w_bd, w_o, norm_ffn, w_up, conv_w, conv_b,
                w_down, norm_final):
    f = lambda a: np.ascontiguousarray(np.asarray(a, dtype=np.float32))
    b_in = f(b_in)
    bqk = np.concatenate([b_in[:, O_MQ:O_MQ + 2048], b_in[:, O_AQ:O_AQ + 4096]], axis=1)
    bgate = b_in[:, O_GM:O_GM + 4096]
    btm = np.concatenate([b_in[:, O_MV:O_MV + 4096], b_in[:, O_AV:O_AV + 2048]], axis=1)
    cwp = np.stack([pcol(f(conv_w)[:, i, :], FC) for i in range(3)], axis=-1)
    d = {
        "w_in": f(w_in)[:L], "w_bm": f(w_bm)[:L], "w_bd": f(w_bd)[:L], "w_o": f(w_o)[:L], "w_up": f(w_up)[:L],
        "w_down": f(w_down)[:L],
        "gmix": pcol(norm_mix, KC)[:L], "gffn": pcol(norm_ffn, KC)[:L], "gfin": pcol(norm_final, KC),
        "bqk": pcol(bqk, 48)[:L], "bgate": pcol(bgate, 32)[:L], "btm": f(btm)[:L],
        "bif": f(b_in[:, O_MI:O_MI + 16])[:L], "mnorm": f(m_norm)[:L], "anorm": f(a_norm)[:L],
        "lamqk": f(lam_qk).reshape(-1, 512)[:L], "cw": np.ascontiguousarray(cwp)[:L], "cb": pcol(conv_b, FC)[:L],
    }
    d.update(host_consts(T))
    return {k: np.ascontiguousarray(v) for k, v in d.items()}


def kernel(x, norm_mix, w_in, b_in, m_norm, a_norm, lam_qk, w_bm, w_bd, w_o, norm_ffn, w_up, conv_w, conv_b, w_down,
           norm_final):
    x = np.asarray(x, dtype=np.float32)
    B, S, _ = x.shape
    L = np.asarray(norm_mix).shape[0]
    shared = prep_shared(L, S, norm_mix, w_in, b_in, m_norm, a_norm, lam_qk, w_bm, w_bd, w_o, norm_ffn, w_up, conv_w,
                         conv_b, w_down, norm_final)
    nc = build(S, L)
    in_maps = []
    for b in range(B):
        m = dict(shared)
        m["xT"] = np.ascontiguousarray(x[b].T)
        in_maps.append(m)
    res = run_bass_kernel_spmd(nc, in_maps, core_ids=list(range(B)))
    out = np.stack([np.ascontiguousarray(res.results[b]["outT"].T) for b in range(B)], axis=0)
    return out.astype(np.float32)
```

```python
import math
from contextlib import ExitStack

import numpy as np
import concourse.bass as bass
import concourse.mybir as mybir
from concourse.bass_utils import run_bass_kernel_spmd

F32 = mybir.dt.float32
BF16 = mybir.dt.bfloat16
AF = mybir.ActivationFunctionType
ALU = mybir.AluOpType

D = 2048
KC = 16
H = 8
NIN = 16400
DFF = 5632
FC = 44
EPS = 1e-6
O_MQ, O_MK, O_MV, O_MO, O_MI, O_AQ, O_AK, O_AV, O_GM, O_GD = (
    0, 1024, 2048, 4096, 6144, 6160, 8208, 10256, 12304, 14352)
ROPE_THETA = 500000.0


class Buf:
    __slots__ = ("w", "r", "dsem", "dcnt")

    def __init__(self):
        self.w = None
        self.r = {}
        self.dsem = None
        self.dcnt = 0


class Prog:
    ENGS = ("pe", "act", "dve", "pool", "sp")

    def __init__(self, nc, es, name):
        self.nc, self.es, self.name = nc, es, name
        self.q = {e: [] for e in self.ENGS}
        self.allsems = []
        self.sem = {e: self._newsem(f"{name}_{e}") for e in self.ENGS}
        self.cnt = {e: 0 for e in self.ENGS}
        self.waited = {e: {} for e in self.ENGS}
        self.dsems = []
        self.nd = 0

    def _newsem(self, name):
        h = self.nc.alloc_semaphore(name=name)
        self.allsems.append(h)
        return h

    def _wait(self, eng, ev):
        sem, val = ev
        if eng == "pe" and sem is self.sem["pe"]:
            return
        w = self.waited[eng]
        k = id(sem)
        if w.get(k, 0) >= val:
            return
        w[k] = val
        self.q[eng].append((0, sem, val))

    def _deps(self, eng, reads, writes):
        for b in reads:
            if b.w is not None:
                self._wait(eng, b.w)
        for b in writes:
            if b.w is not None:
                self._wait(eng, b.w)
            for ev in b.r.values():
                self._wait(eng, ev)

    @staticmethod
    def _commit(ev, reads, writes):
        for b in reads:
            b.r[id(ev[0])] = ev
        for b in writes:
            b.w = ev
            b.r = {}

    def op(self, eng, fn, reads=(), writes=()):
        self._deps(eng, reads, writes)
        self.cnt[eng] += 1
        ev = (self.sem[eng], self.cnt[eng])
        self.q[eng].append((1, fn, self.sem[eng], 1))
        self._commit(ev, reads, writes)
        return ev

    def dma(self, qeng, fn, sb, reads=(), writes=()):
        self._deps(qeng, reads, writes)
        if sb.dsem is None:
            self.nd += 1
            sb.dsem = self._newsem(f"{self.name}_d{self.nd}")
            sb.dcnt = 0
            self.dsems.append(sb)
        sb.dcnt += 16
        ev = (sb.dsem, sb.dcnt)
        self.q[qeng].append((1, fn, sb.dsem, 16))
        self._commit(ev, reads, writes)
        return ev

    def finish(self):
        for e in self.ENGS:
            for e2 in self.ENGS:
                if e2 != e and self.cnt[e2] > 0:
                    self._wait(e, (self.sem[e2], self.cnt[e2]))
            for sb in self.dsems:
                self._wait(e, (sb.dsem, sb.dcnt))
        for sb in self.dsems:
            sb.dsem = None
            sb.dcnt = 0
            sb.w = None
            sb.r = {}
        with self.nc.Block() as blk:
            blk.tensor(lambda e: self._emit("pe", e))
            blk.scalar(lambda e: self._emit("act", e))
            blk.vector(lambda e: self._emit("dve", e))
            blk.gpsimd(lambda e: self._emit("pool", e))
            blk.sync(lambda e: self._emit("sp", e))
        self.nc.all_engine_barrier()
        self.nc.clear_and_free_semaphores(self.allsems)
        self.nc.all_engine_barrier()

    def _emit(self, eng, e):
        for it in self.q[eng]:
            if it[0] == 0:
                e.wait_ge(it[1], it[2])
            else:
                it[1](e).then_inc(it[2], it[3])


class Rot:
    def __init__(self, items):
        self.items = items
        self.i = 0

    def next(self):
        x = self.items[self.i]
        self.i = (self.i + 1) % len(self.items)
        return x


class G:
    pass


_uid = [0]


def uid(p):
    _uid[0] += 1
    return f"{p}{_uid[0]}"


def sb(es, nc, shape, dt, name="t"):
    return es.enter_context(nc.sbuf_tensor(uid(name), list(shape), dt))


def pst(es, nc, shape, dt, name="p"):
    return es.enter_context(nc.psum_tensor(uid(name), list(shape), dt))


def sbufs(es, nc, n, shape, dt, name="t"):
    return [(sb(es, nc, shape, dt, name), Buf()) for _ in range(n)]


def psbufs(es, nc, n, shape, dt, name="p"):
    return [(pst(es, nc, shape, dt, name), Buf()) for _ in range(n)]


def fm(ap):
    return ap.rearrange("(k p) t -> p k t", p=128)


def ph_consts(g):
    nc = g.nc
    with ExitStack() as es:
        P = Prog(nc, es, uid("cst"))
        for t, src in ((g.identb, g.c_ident), (g.trib, g.c_tri), (g.trif, g.c_tri), (g.onesf, g.c_ones),
                       (g.pmf, g.c_pm)):
            P.dma("pool", lambda e, t=t, src=src: e.dma_start(out=t[:], in_=src), Buf(), writes=[])
        P.finish()


def ph_norm(g, src, gam_src, final_dst=None):
    nc, T = g.nc, g.T
    W = 256
    with ExitStack() as es:
        P = Prog(nc, es, uid("nrm"))
        hb = Rot(sbufs(es, nc, 2, [128, KC, W], F32, "hb"))
        sq = sb(es, nc, [128, KC, W], F32, "sq")
        sqA, sqB = Buf(), Buf()
        gm = sb(es, nc, [128, KC], F32, "gm")
        gmB = Buf()
        rs = Rot(sbufs(es, nc, 2, [128, W], F32, "rs"))
        rstd = Rot(sbufs(es, nc, 2, [128, W], F32, "rstd"))
        ps = Rot(psbufs(es, nc, 2, [128, W], F32, "nps"))
        ob = Rot(sbufs(es, nc, 2, [128, KC, W], F32, "ob")) if final_dst is not None else None
        P.dma("pool", lambda e: e.dma_start(out=gm[:], in_=gam_src), gmB, writes=[gmB])
        for j in range(T // W):
            ht, hB = hb.next()
            P.dma("pool", lambda e, ht=ht, j=j: e.dma_start(out=ht[:], in_=fm(src[:, j * W:(j + 1) * W])), hB,
                  writes=[hB])
            P.op("act", lambda e, ht=ht: e.activation(out=sq[:, 0:8, :], in_=ht[:, 0:8, :], func=AF.Square),
                 reads=[hB], writes=[sqA])
            P.op("dve", lambda e, ht=ht: e.tensor_tensor(out=sq[:, 8:16, :], in0=ht[:, 8:16, :], in1=ht[:, 8:16, :],
                                                         op=ALU.mult), reads=[hB], writes=[sqB])
            pt, pB = ps.next()

            def mm(e, pt=pt):
                for k in range(KC):
                    ins = e.matmul(pt[:], lhsT=g.onesf[:], rhs=sq[:, k, :], start=(k == 0), stop=(k == KC - 1))
                return ins
            P.op("pe", mm, reads=[sqA, sqB], writes=[pB])
            rt, rB = rs.next()
            P.op("act", lambda e, rt=rt, pt=pt: e.activation(out=rt[:], in_=pt[:], func=AF.Sqrt, bias=EPS,
                                                             scale=1.0 / D), reads=[pB], writes=[rB])
            st, sB = rstd.next()
            P.op("dve", lambda e, st=st, rt=rt: e.reciprocal(out=st[:], in_=rt[:]), reads=[rB], writes=[sB])
            if final_dst is None:
                def nrm(e, ht=ht, st=st, j=j):
                    for k in range(KC):
                        ins = e.scalar_tensor_tensor(out=g.hnT[:, k, j * W:(j + 1) * W], in0=ht[:, k, :],
                                                     scalar=gm[:, k:k + 1], in1=st[:], op0=ALU.mult, op1=ALU.mult)
                    return ins
                P.op("dve", nrm, reads=[hB, sB, gmB], writes=[Buf()])
            else:
                ot, oB = ob.next()

                def nrm(e, ht=ht, st=st, ot=ot):
                    for k in range(KC):
                        ins = e.scalar_tensor_tensor(out=ot[:, k, :], in0=ht[:, k, :],
                                                     scalar=gm[:, k:k + 1], in1=st[:], op0=ALU.mult, op1=ALU.mult)
                    return ins
                P.op("dve", nrm, reads=[hB, sB, gmB], writes=[oB])
                P.dma("sp", lambda e, ot=ot, j=j: e.dma_start(out=fm(final_dst[:, j * W:(j + 1) * W]), in_=ot[:]),
                      oB, reads=[oB])
        P.finish()


def slab_src(w2d, c0, cw):
    return w2d[:, c0:c0 + cw].rearrange("(k p) n -> p k n", p=128)


def ph_proj(g, l):
    nc, T = g.nc, g.T
    NQ, NT = T // 512, T // 128
    w = g.w_in[l]
    with ExitStack() as es:
        P = Prog(nc, es, uid("prj"))
        slabs = Rot(sbufs(es, nc, 3, [128, KC, 512], BF16, "wsl"))
        psr = Rot(psbufs(es, nc, 4, [128, 512], F32, "pp"))
        pxp = Rot(psbufs(es, nc, 2, [32, 512], F32, "px"))
        stg = Rot(sbufs(es, nc, 4, [128, 512], BF16, "stg"))
        xbr = Rot(sbufs(es, nc, 2, [128, 512], F32, "xb"))
        t1r = Rot(sbufs(es, nc, 2, [32, 512], F32, "t1"))
        t2r = Rot(sbufs(es, nc, 2, [32, 512], F32, "t2"))
        tmr = Rot(sbufs(es, nc, 2, [128, 512], F32, "tm"))
        cosT = sb(es, nc, [32, T], F32, "cos")
        sinT = sb(es, nc, [32, T], F32, "sin")
        bqk = sb(es, nc, [128, 48], F32, "bqk")
        brep = sb(es, nc, [128, 6144], F32, "brep")
        bif = sb(es, nc, [128, 16], F32, "bif")
        wif = sb(es, nc, [128, KC, 16], BF16, "wif")
        cB = Buf()
        for t, src in ((cosT, g.c_cos), (sinT, g.c_sin), (bqk, g.bqk[l]), (brep, g.btm[l].partition_broadcast(128)),
                       (bif, g.bif[l].partition_broadcast(128)), (wif, slab_src(w, O_MI, 16))):
            P.dma("pool", lambda e, t=t, src=src: e.dma_start(out=t[:], in_=src), cB, writes=[cB])
        jobs = []
        for s in range(2):
            jobs.append((O_MQ + s * 512, "fm", ("m", 0 + s * 4)))
        for s in range(2):
            jobs.append((O_MK + s * 512, "fm", ("m", 8 + s * 4)))
        for s in range(4):
            jobs.append((O_AQ + s * 512, "fm", ("aq", 16 + s * 4)))
        for s in range(4):
            jobs.append((O_AK + s * 512, "fm", ("ak", 32 + s * 4)))
        for s in range(4):
            jobs.append((O_MV + s * 512, "tm", ("mv", s)))
        for s in range(4):
            jobs.append((O_MO + s * 512, "tm", ("mo", s)))
        for s in range(4):
            jobs.append((O_AV + s * 512, "tm", ("av", s)))
        loaded = []

        def load(i):
            c0 = jobs[i][0]
            t, b = slabs.next()
            P.dma("pool", lambda e, t=t, c0=c0: e.dma_start(out=t[:], in_=slab_src(w, c0, 512)), b, writes=[b])
            loaded.append((t, b))
        load(0)
        load(1)
        for tt in range(NT):
            pt, pB = psr.next()

            def mm(e, pt=pt, tt=tt):
                for k in range(KC):
                    ins = e.matmul(pt[:, 0:16], lhsT=g.hnT[:, k, tt * 128:(tt + 1) * 128], rhs=wif[:, k, :],
                                   start=(k == 0), stop=(k == KC - 1))
                return ins
            P.op("pe", mm, reads=[cB], writes=[pB])
            P.op("dve", lambda e, pt=pt, tt=tt: e.tensor_tensor(out=g.IF[:, tt, :], in0=pt[:, 0:16], in1=bif[:],
                                                                op=ALU.add), reads=[pB, cB], writes=[Buf()])
        for i, (c0, kind, info) in enumerate(jobs):
            if i + 2 < len(jobs):
                load(i + 2)
            wt, wB = loaded[i]
            if kind == "fm":
                typ, ch0 = info
                for cc in range(4):
                    ch = ch0 + cc
                    for q in range(NQ):
                        pt, pB = psr.next()

                        def mm(e, pt=pt, wt=wt, cc=cc, q=q):
                            for k in range(KC):
                                ins = e.matmul(pt[:], lhsT=wt[:, k, cc * 128:(cc + 1) * 128],
                                               rhs=g.hnT[:, k, q * 512:(q + 1) * 512],
                                               start=(k == 0), stop=(k == KC - 1))
                            return ins
                        P.op("pe", mm, reads=[wB], writes=[pB])
                        st, sB = stg.next()
                        if typ == "m":
                            P.op("act", lambda e, st=st, pt=pt, ch=ch: e.activation(
                                out=st[:], in_=pt[:], func=AF.Identity, bias=bqk[:, ch:ch + 1]),
                                reads=[pB, cB], writes=[sB])
                        else:
                            xb, xB = xbr.next()
                            P.op("act", lambda e, xb=xb, pt=pt, ch=ch: e.activation(
                                out=xb[:], in_=pt[:], func=AF.Identity, bias=bqk[:, ch:ch + 1]),
                                reads=[pB, cB], writes=[xB])
                            px, pxB = pxp.next()
                            P.op("pe", lambda e, px=px, xb=xb: e.matmul(px[:], lhsT=g.pmf[:], rhs=xb[0:32, :],
                                                                        start=True, stop=True),
                                 reads=[xB], writes=[pxB])
                            t1, t1B = t1r.next()
                            t2, t2B = t2r.next()
                            P.op("dve", lambda e, t1=t1, px=px, q=q: e.tensor_tensor(
                                out=t1[:], in0=px[:], in1=sinT[:, q * 512:(q + 1) * 512], op=ALU.mult),
                                reads=[pxB, cB], writes=[t1B])
                            P.op("dve", lambda e, t2=t2, xb=xb, q=q: e.tensor_tensor(
                                out=t2[:], in0=xb[0:32, :], in1=cosT[:, q * 512:(q + 1) * 512], op=ALU.mult),
                                reads=[xB, cB], writes=[t2B])
                            P.op("dve", lambda e, t1=t1, t2=t2, xb=xb: e.tensor_tensor(
                                out=xb[0:32, :], in0=t1[:], in1=t2[:], op=ALU.add),
                                reads=[t1B, t2B], writes=[xB])
                            sc = (128.0 ** -0.5) if typ == "aq" else 1.0
                            P.op("act", lambda e, st=st, xb=xb, sc=sc: e.activation(
                                out=st[:], in_=xb[:], func=AF.Identity, scale=sc), reads=[xB], writes=[sB])
                        P.dma("sp", lambda e, st=st, ch=ch, q=q: e.dma_start(
                            out=g.QKs[ch * 128:(ch + 1) * 128, q * 512:(q + 1) * 512], in_=st[:]), sB, reads=[sB])
            else:
                typ, s = info
                dst = {"mv": g.MV, "mo": g.MO, "av": g.AV}[typ]
                boff = {"mv": 0, "mo": 2048, "av": 4096}[typ] + s * 512
                for tt in range(NT):
                    pt, pB = psr.next()

                    def mm(e, pt=pt, wt=wt, tt=tt):
                        for k in range(KC):
                            ins = e.matmul(pt[:], lhsT=g.hnT[:, k, tt * 128:(tt + 1) * 128], rhs=wt[:, k, :],
                                           start=(k == 0), stop=(k == KC - 1))
                        return ins
                    P.op("pe", mm, reads=[wB], writes=[pB])
                    st, sB = stg.next()
                    if typ == "mo":
                        tm, tB = tmr.next()
                        P.op("dve", lambda e, tm=tm, pt=pt, boff=boff: e.tensor_tensor(
                            out=tm[:], in0=pt[:], in1=brep[:, boff:boff + 512], op=ALU.add),
                            reads=[pB, cB], writes=[tB])
                        P.op("act", lambda e, st=st, tm=tm: e.activation(out=st[:], in_=tm[:], func=AF.Sigmoid),
                             reads=[tB], writes=[sB])
                    else:
                        P.op("dve", lambda e, st=st, pt=pt, boff=boff: e.tensor_tensor(
                            out=st[:], in0=pt[:], in1=brep[:, boff:boff + 512], op=ALU.add),
                            reads=[pB, cB], writes=[sB])
                    P.dma("sp", lambda e, st=st, dst=dst, tt=tt, s=s: e.dma_start(
                        out=dst[tt * 128:(tt + 1) * 128, s * 512:(s + 1) * 512], in_=st[:]), sB, reads=[sB])
        P.finish()


def tm_src(ap2d, h, NT):
    return ap2d[:, h * 256:(h + 1) * 256].rearrange("(t p) d -> p t d", p=128)


def ph_mlstm(g, l):
    nc, T = g.nc, g.T
    NT = T // 128
    with ExitStack() as es:
        P = Prog(nc, es, uid("mls"))
        NG = NT * 8
        def v3(ap):
            return ap.rearrange("p (c h) -> p c h", h=8)
        e1 = sb(es, nc, [128, NG], F32, "e1")
        l1 = sb(es, nc, [128, NG], F32, "l1")
        tg = sb(es, nc, [128, NG], F32, "tg")
        u = sb(es, nc, [128, NG], F32, "u")
        bnd = sb(es, nc, [128, NG], F32, "bnd")
        eg = sb(es, nc, [128, NG], F32, "eg")
        mrep = sb(es, nc, [128, 2048], F32, "mrep")
        cgp = pst(es, nc, [128, 2, NG], F32, "cgp")
        gB = Buf()
        P.dma("pool", lambda e: e.dma_start(out=mrep[:], in_=g.mnorm[l].partition_broadcast(128)), gB,
              writes=[gB])
        P.op("act", lambda e: e.activation(out=v3(e1[:]), in_=g.IF[:, :, 8:16], func=AF.Exp, scale=-1.0),
             writes=[gB])
        P.op("act", lambda e: e.activation(out=l1[:], in_=e1[:], func=AF.Ln, bias=1.0), reads=[gB], writes=[gB])

        def mmg(e):
            e.matmul(cgp[:, 0, :], lhsT=g.trif[:], rhs=l1[:], start=True, stop=True)
            return e.matmul(cgp[:, 1, :], lhsT=g.onesf[:], rhs=l1[:], start=True, stop=True)
        P.op("pe", mmg, reads=[gB], writes=[gB])
        P.op("dve", lambda e: e.tensor_tensor(out=v3(tg[:]), in0=v3(cgp[:, 0, :]), in1=g.IF[:, :, 0:8], op=ALU.add),
             reads=[gB], writes=[gB])

        def gex(e):
            e.activation(out=u[:], in_=tg[:], func=AF.Exp, bias=math.log(128.0 ** -0.5))
            e.activation(out=bnd[:], in_=cgp[:, 0, :], func=AF.Exp)
            return e.activation(out=eg[:], in_=cgp[:, 1, :], func=AF.Exp, scale=-1.0)
        P.op("act", gex, reads=[gB], writes=[gB])

        qkr = Rot([(sb(es, nc, [128, T], BF16, "qT"), sb(es, nc, [128, T], BF16, "kT"), Buf(), Buf())
                   for _ in range(2)])
        var = [(sb(es, nc, [128, NT, 257], BF16, "vA"), Buf()) for _ in range(2)]
        gr = Rot(sbufs(es, nc, 2, [128, NT, 256], BF16, "Gg"))
        hms = Rot(sbufs(es, nc, 2, [128, 2, T], BF16, "hms"))
        C = sb(es, nc, [128, 257], F32, "C")
        C1 = sb(es, nc, [128, 257], F32, "C1")
        Cb = sb(es, nc, [128, 257], BF16, "Cb")
        CB, C1B, CbB = Buf(), Buf(), Buf()
        ptr = Rot(sbufs(es, nc, 2, [128, 128], BF16, "PT"))
        kur = Rot(sbufs(es, nc, 2, [128, 128], BF16, "ku"))
        smr = Rot(sbufs(es, nc, 2, [128, 8], F32, "sm"))
        jkr = Rot(sbufs(es, nc, 2, [128, 256], F32, "jk"))
        hmr = Rot(sbufs(es, nc, 2, [128, 256], BF16, "hm"))
        stp = Rot(psbufs(es, nc, 2, [128, 128], F32, "stp"))
        ktp = Rot(psbufs(es, nc, 1, [128, 128], BF16, "ktp"))
        outp = Rot(psbufs(es, nc, 2, [128, 257], F32, "outp"))
        updp = Rot(psbufs(es, nc, 1, [128, 257], F32, "updp"))
        trp = Rot(psbufs(es, nc, 1, [128, 2, 128], BF16, "trp"))
        for vt, vB in var:
            P.op("dve", lambda e, vt=vt: e.memset(vt[:, :, 256:257], 1.0), writes=[vB])
        vr = Rot(var)
        for h in range(H):
            qT, kT, qB, kB = qkr.next()
            qkB = qB
            vA, vB = vr.next()
            Gt, GB = gr.next()
            hs, hsB = hms.next()
            P.dma("pool", lambda e, qT=qT, h=h: e.dma_start(out=qT[:], in_=g.QKs[h * 128:(h + 1) * 128, :]), qB,
                  writes=[qB])
            P.dma("pool", lambda e, kT=kT, h=h: e.dma_start(out=kT[:], in_=g.QKs[(8 + h) * 128:(9 + h) * 128, :]),
                  kB, writes=[kB])
            P.dma("pool", lambda e, vA=vA, h=h: e.dma_start(out=vA[:, :, 0:256], in_=tm_src(g.MV, h, NT)), vB,
                  writes=[vB])
            P.dma("pool", lambda e, Gt=Gt, h=h: e.dma_start(out=Gt[:], in_=tm_src(g.MO, h, NT)), GB,
                  writes=[GB])

            def gmul(e, Gt=Gt, h=h):
                for c in range(NT):
                    ins = e.tensor_tensor(out=Gt[:, c, :], in0=Gt[:, c, :], in1=mrep[:, h * 256:(h + 1) * 256],
                                          op=ALU.mult)
                return ins
            P.op("dve", gmul, reads=[gB], writes=[GB])
            for c in range(NT):
                cs = slice(c * 128, (c + 1) * 128)
                uc = u[:, c * 8 + h:c * 8 + h + 1]
                st, stB = stp.next()
                P.op("pe", lambda e, st=st, kT=kT, qT=qT, cs=cs: e.matmul(st[:], lhsT=kT[:, cs], rhs=qT[:, cs],
                                                                          start=True, stop=True),
                     reads=[qB, kB], writes=[stB])
                pt, ptB = ptr.next()
                P.op("dve", lambda e, pt=pt, st=st, uc=uc: e.scalar_tensor_tensor(
                    out=pt[:], in0=st[:], scalar=uc, in1=g.trif[:], op0=ALU.mult, op1=ALU.mult),
                    reads=[stB, gB], writes=[ptB])
                kt, ktB = ktp.next()
                P.op("pe", lambda e, kt=kt, kT=kT, cs=cs: e.transpose(kt[:], kT[:, cs], g.identb[:]),
                     reads=[kB], writes=[ktB])
                ku, kuB = kur.next()
                P.op("act", lambda e, ku=ku, kt=kt, uc=uc: e.activation(out=ku[:], in_=kt[:], func=AF.Identity,
                                                                        scale=uc),
                     reads=[ktB, gB], writes=[kuB])
                ot, oB = outp.next()

                def mmo(e, ot=ot, pt=pt, vA=vA, qT=qT, c=c, cs=cs):
                    ins = e.matmul(ot[:], lhsT=pt[:], rhs=vA[:, c, :], start=True, stop=(c == 0))
                    if c > 0:
                        ins = e.matmul(ot[:], lhsT=qT[:, cs], rhs=Cb[:], start=False, stop=True)
                    return ins
                P.op("pe", mmo, reads=[ptB, vB, qB] + ([CbB] if c > 0 else []), writes=[oB])
                if c < NT - 1:
                    up, upB = updp.next()
                    P.op("pe", lambda e, up=up, ku=ku, vA=vA, c=c: e.matmul(up[:], lhsT=ku[:], rhs=vA[:, c, :],
                                                                            start=True, stop=True),
                         reads=[kuB, vB], writes=[upB])
                    egc = eg[:, c * 8 + h:c * 8 + h + 1]
                    if c == 0:
                        P.op("dve", lambda e, up=up, egc=egc: e.tensor_scalar(
                            out=C[:], in0=up[:], scalar1=egc, scalar2=None, op0=ALU.mult),
                            reads=[upB, gB], writes=[CB])
                    else:
                        P.op("act", lambda e, egc=egc: e.activation(out=C1[:], in_=C[:], func=AF.Identity,
                                                                    scale=egc),
                             reads=[CB, gB], writes=[C1B])
                        P.op("dve", lambda e, up=up, egc=egc: e.scalar_tensor_tensor(
                            out=C[:], in0=up[:], scalar=egc, in1=C1[:], op0=ALU.mult, op1=ALU.add),
                            reads=[upB, C1B, gB], writes=[CB])
                    P.op("act", lambda e: e.activation(out=Cb[:], in_=C[:], func=AF.Identity),
                         reads=[CB], writes=[CbB])
                sm, smB = smr.next()
                P.op("act", lambda e, sm=sm, ot=ot: e.activation(out=sm[:, 0:1], in_=ot[:, 256:257], func=AF.Abs),
                     reads=[oB], writes=[smB])
                P.op("dve", lambda e, sm=sm, c=c, h=h: e.tensor_scalar(
                    out=sm[:, 1:2], in0=sm[:, 0:1], scalar1=bnd[:, c * 8 + h:c * 8 + h + 1], scalar2=None, op0=ALU.max),
                    reads=[smB, gB], writes=[smB])
                P.op("dve", lambda e, sm=sm: e.reciprocal(out=sm[:, 2:3], in_=sm[:, 1:2]),
                     reads=[smB], writes=[smB])
                jk, jkB = jkr.next()
                P.op("act", lambda e, jk=jk, ot=ot, sm=sm: e.activation(
                    out=jk[:], in_=ot[:, 0:256], func=AF.Square, scale=sm[:, 2:3], accum_out=sm[:, 3:4]),
                    reads=[oB, smB], writes=[jkB, smB])
                P.op("act", lambda e, sm=sm: e.activation(out=sm[:, 4:5], in_=sm[:, 3:4], func=AF.Ln, bias=EPS,
                                                          scale=1.0 / 256), reads=[smB], writes=[smB])
                P.op("act", lambda e, sm=sm: e.activation(out=sm[:, 5:6], in_=sm[:, 4:5], func=AF.Exp,
                                                          scale=-0.5), reads=[smB], writes=[smB])
                P.op("dve", lambda e, sm=sm: e.tensor_tensor(out=sm[:, 6:7], in0=sm[:, 5:6], in1=sm[:, 2:3],
                                                             op=ALU.mult), reads=[smB], writes=[smB])
                hm, hmB = hmr.next()
                P.op("dve", lambda e, hm=hm, ot=ot, sm=sm, Gt=Gt, c=c: e.scalar_tensor_tensor(
                    out=hm[:], in0=ot[:, 0:256], scalar=sm[:, 6:7], in1=Gt[:, c, :], op0=ALU.mult, op1=ALU.mult),
                    reads=[oB, smB, GB], writes=[hmB])
                tr, trB = trp.next()

                def trs(e, tr=tr, hm=hm):
                    e.transpose(tr[:, 0, :], hm[:, 0:128], g.identb[:])
                    return e.transpose(tr[:, 1, :], hm[:, 128:256], g.identb[:])
                P.op("pe", trs, reads=[hmB], writes=[trB])
                P.op("act", lambda e, hs=hs, tr=tr, cs=cs: e.activation(out=hs[:, :, cs], in_=tr[:],
                                                                        func=AF.Identity),
                     reads=[trB], writes=[hsB])
            P.dma("sp", lambda e, hs=hs, h=h: e.dma_start(
                out=g.HMT[h * 256:(h + 1) * 256, :].rearrange("(j p) t -> p j t", p=128), in_=hs[:]), hsB,
                reads=[hsB])
        P.finish()


def ph_attn(g, l):
    nc, T = g.nc, g.T
    NT, NG = T // 128, T // 256
    lam_init = 0.8 - 0.6 * math.exp(-0.3 * l)
    with ExitStack() as es:
        P = Prog(nc, es, uid("att"))
        lq = sb(es, nc, [128, 4, 128], F32, "lq")
        lp = sb(es, nc, [128, 2, 128], F32, "lp")
        ls = sb(es, nc, [128, 8], F32, "ls")
        anrep = sb(es, nc, [128, 2048], F32, "anrep")
        cB = Buf()
        P.dma("pool", lambda e: e.dma_start(out=lq[:].rearrange("p a b -> p (a b)"),
                                            in_=g.lamqk[l].partition_broadcast(128)), cB, writes=[cB])
        P.dma("pool", lambda e: e.dma_start(out=anrep[:], in_=g.anorm[l].partition_broadcast(128)), cB,
              writes=[cB])

        def lam1(e):
            e.tensor_tensor(out=lp[:, 0, :], in0=lq[:, 0, :], in1=lq[:, 1, :], op=ALU.mult)
            return e.tensor_tensor(out=lp[:, 1, :], in0=lq[:, 2, :], in1=lq[:, 3, :], op=ALU.mult)
        P.op("dve", lam1, reads=[cB], writes=[cB])

        def lam2(e):
            e.reduce_sum(out=ls[:, 0:1], in_=lp[:, 0, :], axis=mybir.AxisListType.X)
            return e.reduce_sum(out=ls[:, 1:2], in_=lp[:, 1, :], axis=mybir.AxisListType.X)
        P.op("dve", lam2, reads=[cB], writes=[cB])
        P.op("act", lambda e: e.activation(out=ls[:, 2:4], in_=ls[:, 0:2], func=AF.Exp), reads=[cB], writes=[cB])
        P.op("dve", lambda e: e.scalar_tensor_tensor(out=ls[:, 4:5], in0=ls[:, 3:4], scalar=-lam_init,
                                                     in1=ls[:, 2:3], op0=ALU.add, op1=ALU.subtract),
             reads=[cB], writes=[cB])
        P.op("dve", lambda e: e.tensor_scalar(out=anrep[:], in0=anrep[:], scalar1=(1.0 - lam_init), scalar2=None,
                                              op0=ALU.mult), reads=[cB], writes=[cB])
        nlam = ls[:, 4:5]

        inb = []
        for i in range(2):
            vt = sb(es, nc, [128, NT, 257], BF16, "vA")
            vB = Buf()
            P.op("dve", lambda e, vt=vt: e.memset(vt[:, :, 256:257], 1.0), writes=[vB])
            inb.append(dict(q2=sb(es, nc, [128, 2, T], BF16, "q2"), k2=sb(es, nc, [128, 2, T], BF16, "k2"),
                            qB=Buf(), kB=Buf(), vA=vt, vB=vB, hs=sb(es, nc, [128, 2, T], BF16, "has"), hsB=Buf()))
        inr = Rot(inb)
        ptr = Rot(sbufs(es, nc, 4, [128, 2, 256], BF16, "PT"))
        rrr = Rot(sbufs(es, nc, 2, [128, 8], F32, "rr"))
        c0r = Rot(sbufs(es, nc, 2, [128, 257], F32, "c0"))
        c1r = Rot(sbufs(es, nc, 2, [128, 257], F32, "c1"))
        a0r = Rot(sbufs(es, nc, 2, [128, 256], F32, "a0"))
        a1r = Rot(sbufs(es, nc, 2, [128, 256], F32, "a1"))
        jkr = Rot(sbufs(es, nc, 2, [128, 256], F32, "jk"))
        habr = Rot(sbufs(es, nc, 2, [128, 256], BF16, "hab"))
        stp = Rot(psbufs(es, nc, 3, [128, 2, 256], F32, "stp"))
        outp = [[(pst(es, nc, [128, 257], F32, "outp"), Buf()) for j in range(2)] for m in range(2)]
        trp = Rot(psbufs(es, nc, 1, [128, 2, 128], BF16, "trp"))

        hb = {}

        def ensure_head(h):
            if h in hb or h >= H:
                return
            b = inr.next()
            c0 = (16 + 2 * h) * 128
            c1 = (32 + 2 * h) * 128
            P.dma("pool", lambda e: e.dma_start(
                out=b["q2"][:], in_=g.QKs[c0:c0 + 256, :].rearrange("(c p) t -> p c t", p=128)), b["qB"],
                writes=[b["qB"]])
            P.dma("pool", lambda e: e.dma_start(
                out=b["k2"][:], in_=g.QKs[c1:c1 + 256, :].rearrange("(c p) t -> p c t", p=128)), b["kB"],
                writes=[b["kB"]])
            P.dma("pool", lambda e: e.dma_start(out=b["vA"][:, :, 0:256], in_=tm_src(g.AV, h, NT)), b["vB"],
                  writes=[b["vB"]])
            hb[h] = b

        steps = [(h, gq, kt) for h in range(H) for gq in range(NG) for kt in range(2 * gq + 2)]
        LA = 2
        stq = {}

        def rec_qk(i):
            h, gq, kt = steps[i]
            ensure_head(h)
            b = hb[h]
            q2, k2 = b["q2"], b["k2"]
            qs = slice(gq * 256, (gq + 1) * 256)
            ks = slice(kt * 128, (kt + 1) * 128)
            st, stB = stp.next()

            def mmq(e):
                e.matmul(st[:, 0, :], lhsT=k2[:, 0, ks], rhs=q2[:, 0, qs], start=True, stop=True)
                return e.matmul(st[:, 1, :], lhsT=k2[:, 1, ks], rhs=q2[:, 1, qs], start=True, stop=True)
            P.op("pe", mmq, reads=[b["qB"], b["kB"]], writes=[stB])
            stq[i] = (st, stB)

        def epilogue(h, gq, j, b):
            (o0, o0B), (o1, o1B) = outp[0][j], outp[1][j]
            hs, hsB = b["hs"], b["hsB"]
            ts = slice((gq * 2 + j) * 128, (gq * 2 + j + 1) * 128)
            c0, c0B = c0r.next()
            c1, c1B = c1r.next()
            P.op("act", lambda e: e.activation(out=c0[:], in_=o0[:], func=AF.Identity), reads=[o0B], writes=[c0B])
            P.op("dve", lambda e: e.tensor_copy(out=c1[:], in_=o1[:]), reads=[o1B], writes=[c1B])
            rr, rB = rrr.next()

            def rcp(e):
                e.reciprocal(out=rr[:, 0:1], in_=c0[:, 256:257])
                return e.reciprocal(out=rr[:, 1:2], in_=c1[:, 256:257])
            P.op("dve", rcp, reads=[c0B, c1B], writes=[rB])
            P.op("dve", lambda e: e.tensor_tensor(out=rr[:, 2:3], in0=rr[:, 1:2], in1=nlam, op=ALU.mult),
                 reads=[rB, cB], writes=[rB])
            a0, a0B = a0r.next()
            P.op("act", lambda e: e.activation(out=a0[:], in_=c0[:, 0:256], func=AF.Identity, scale=rr[:, 0:1]),
                 reads=[c0B, rB], writes=[a0B])
            a1, a1B = a1r.next()
            P.op("dve", lambda e: e.scalar_tensor_tensor(out=a1[:], in0=c1[:, 0:256], scalar=rr[:, 2:3], in1=a0[:],
                                                         op0=ALU.mult, op1=ALU.add),
                 reads=[c1B, rB, a0B], writes=[a1B])
            jk, jkB = jkr.next()
            P.op("act", lambda e: e.activation(out=jk[:], in_=a1[:], func=AF.Square, accum_out=rr[:, 3:4]),
                 reads=[a1B, rB], writes=[jkB, rB])
            P.op("act", lambda e: e.activation(out=rr[:, 4:5], in_=rr[:, 3:4], func=AF.Ln, bias=EPS,
                                               scale=1.0 / 256), reads=[rB], writes=[rB])
            P.op("act", lambda e: e.activation(out=rr[:, 5:6], in_=rr[:, 4:5], func=AF.Exp, scale=-0.5),
                 reads=[rB], writes=[rB])
            hab, habB = habr.next()
            P.op("dve", lambda e: e.scalar_tensor_tensor(out=hab[:], in0=a1[:], scalar=rr[:, 5:6],
                                                         in1=anrep[:, h * 256:(h + 1) * 256], op0=ALU.mult,
                                                         op1=ALU.mult), reads=[a1B, rB, cB], writes=[habB])
            tr, trB = trp.next()

            def trs(e):
                e.transpose(tr[:, 0, :], hab[:, 0:128], g.identb[:])
                return e.transpose(tr[:, 1, :], hab[:, 128:256], g.identb[:])
            P.op("pe", trs, reads=[habB], writes=[trB])
            P.op("act", lambda e: e.activation(out=hs[:, :, ts], in_=tr[:], func=AF.Identity),
                 reads=[trB], writes=[hsB])

        for i in range(min(LA, len(steps))):
            rec_qk(i)
        for i, (h, gq, kt) in enumerate(steps):
            if gq == 0 and kt == 0:
                ensure_head(h + 1)
            if i + LA < len(steps):
                rec_qk(i + LA)
            b = hb[h]
            vA, vB = b["vA"], b["vB"]
            st, stB = stq.pop(i)
            pt, ptB = ptr.next()
            P.op("act", lambda e, pt=pt, st=st: e.activation(out=pt[:], in_=st[:], func=AF.Exp),
                 reads=[stB], writes=[ptB])
            if kt >= 2 * gq:
                j0 = kt - 2 * gq

                def msk(e, pt=pt, j0=j0):
                    e.tensor_tensor(out=pt[:, 0, j0 * 128:(j0 + 1) * 128], in0=pt[:, 0, j0 * 128:(j0 + 1) * 128],
                                    in1=g.trib[:], op=ALU.mult)
                    return e.tensor_tensor(out=pt[:, 1, j0 * 128:(j0 + 1) * 128],
                                           in0=pt[:, 1, j0 * 128:(j0 + 1) * 128], in1=g.trib[:], op=ALU.mult)
                P.op("dve", msk, reads=[ptB], writes=[ptB])
            js = [j for j in range(2) if kt <= 2 * gq + j]

            def mmv(e, pt=pt, vA=vA, kt=kt, gq=gq, js=js):
                for j in js:
                    for m in range(2):
                        ins = e.matmul(outp[m][j][0][:], lhsT=pt[:, m, j * 128:(j + 1) * 128], rhs=vA[:, kt, :],
                                       start=(kt == 0), stop=(kt == 2 * gq + j), skip_group_check=True)
                return ins
            P.op("pe", mmv, reads=[ptB, vB], writes=[outp[m][j][1] for j in js for m in range(2)])
            if kt == 2 * gq:
                epilogue(h, gq, 0, b)
            if kt == 2 * gq + 1:
                epilogue(h, gq, 1, b)
                if gq == NG - 1:
                    P.dma("sp", lambda e, b=b, h=h: e.dma_start(
                        out=g.HAT[h * 256:(h + 1) * 256, :].rearrange("(j p) t -> p j t", p=128), in_=b["hs"][:]),
                        b["hsB"], reads=[b["hsB"]])
                    del hb[h]
        P.finish()


def ph_merge(g, l, which):
    nc, T = g.nc, g.T
    NQ = T // 512
    wa = (g.w_bm if which == 0 else g.w_bd)[l]
    wg = g.w_in[l]
    goff = O_GM if which == 0 else O_GD
    src = g.HMT if which == 0 else g.HAT
    with ExitStack() as es:
        P = Prog(nc, es, uid("mrg"))
        xT = sb(es, nc, [128, KC, T], BF16, "xT")
        xB = Buf()
        bg = sb(es, nc, [128, 32], F32, "bg")
        bgB = Buf()
        P.dma("pool", lambda e: e.dma_start(out=bg[:], in_=g.bgate[l]), bgB, writes=[bgB])
        P.dma("pool", lambda e: e.dma_start(out=xT[:], in_=fm(src)), xB, writes=[xB])
        SW = 256
        NS = 2048 // SW
        CPS = SW // 128
        slabs = Rot(sbufs(es, nc, 4, [128, KC, SW], BF16, "wsl"))
        psy = Rot(psbufs(es, nc, 3, [128, 512], F32, "py"))
        psg = Rot(psbufs(es, nc, 3, [128, 512], F32, "pg"))
        sgr = Rot(sbufs(es, nc, 2, [128, 512], F32, "sg"))
        y1r = Rot(sbufs(es, nc, 3, [128, 512], F32, "y1"))
        if which == 1:
            tr_ = Rot(sbufs(es, nc, 2, [128, 512], F32, "tt"))
            yor = Rot(sbufs(es, nc, 3, [128, 512], BF16, "yo"))
        loaded = []

        def load(s):
            ta, ba = slabs.next()
            P.dma("pool", lambda e, ta=ta, s=s: e.dma_start(out=ta[:], in_=slab_src(wa, s * SW, SW)), ba,
                  writes=[ba])
            tg, bgs = slabs.next()
            P.dma("pool", lambda e, tg=tg, s=s: e.dma_start(out=tg[:], in_=slab_src(wg, goff + s * SW, SW)), bgs,
                  writes=[bgs])
            loaded.append((ta, ba, tg, bgs))
        load(0)
        for s in range(NS):
            if s + 1 < NS:
                load(s + 1)
            ta, ba, tg, bgs = loaded[s]
            for cc in range(CPS):
                ch = s * CPS + cc
                for q in range(NQ):
                    qs = slice(q * 512, (q + 1) * 512)
                    py, pyB = psy.next()
                    pg, pgB = psg.next()

                    def mm(e, py=py, pg=pg, ta=ta, tg=tg, cc=cc, qs=qs):
                        for k in range(KC):
                            e.matmul(py[:], lhsT=ta[:, k, cc * 128:(cc + 1) * 128], rhs=xT[:, k, qs],
                                     start=(k == 0), stop=(k == KC - 1))
                        for k in range(KC):
                            ins = e.matmul(pg[:], lhsT=tg[:, k, cc * 128:(cc + 1) * 128], rhs=g.hnT[:, k, qs],
                                           start=(k == 0), stop=(k == KC - 1))
                        return ins
                    P.op("pe", mm, reads=[ba, bgs, xB], writes=[pyB, pgB])
                    sg, sgB = sgr.next()
                    bcol = which * 16 + ch
                    P.op("act", lambda e, sg=sg, pg=pg, bcol=bcol: e.activation(
                        out=sg[:], in_=pg[:], func=AF.Sigmoid, bias=bg[:, bcol:bcol + 1]),
                        reads=[pgB, bgB], writes=[sgB])
                    if which == 0:
                        y1, y1B = y1r.next()
                        P.op("dve", lambda e, y1=y1, py=py, sg=sg: e.tensor_tensor(out=y1[:], in0=py[:], in1=sg[:],
                                                                                   op=ALU.mult),
                             reads=[pyB, sgB], writes=[y1B])
                        P.dma("sp", lambda e, y1=y1, ch=ch, qs=qs: e.dma_start(
                            out=g.Y1[ch * 128:(ch + 1) * 128, qs], in_=y1[:]), y1B, reads=[y1B])
                    else:
                        y1, y1B = y1r.next()
                        P.dma("pool", lambda e, y1=y1, ch=ch, qs=qs: e.dma_start(
                            out=y1[:], in_=g.Y1[ch * 128:(ch + 1) * 128, qs]), y1B, writes=[y1B])
                        tt, ttB = tr_.next()
                        P.op("dve", lambda e, tt=tt, py=py, sg=sg: e.tensor_tensor(out=tt[:], in0=py[:], in1=sg[:],
                                                                                   op=ALU.mult),
                             reads=[pyB, sgB], writes=[ttB])
                        yo, yoB = yor.next()
                        P.op("dve", lambda e, yo=yo, tt=tt, y1=y1: e.tensor_tensor(out=yo[:], in0=tt[:], in1=y1[:],
                                                                                   op=ALU.add),
                             reads=[ttB, y1B], writes=[yoB])
                        P.dma("sp", lambda e, yo=yo, ch=ch, qs=qs: e.dma_start(
                            out=g.YT[ch * 128:(ch + 1) * 128, qs], in_=yo[:]), yoB, reads=[yoB])
        P.finish()


def ph_dump_hn(g, dst):
    nc = g.nc
    with ExitStack() as es:
        P = Prog(nc, es, uid("dmp"))
        b = Buf()
        for k in range(KC):
            P.dma("sp", lambda e, k=k: e.dma_start(out=dst[k * 128:(k + 1) * 128, :], in_=g.hnT[:, k, :]), b,
                  reads=[b])
        P.finish()


def ph_oproj(g, l, hsrc):
    nc, T = g.nc, g.T
    NQ = T // 512
    wo = g.w_o[l]
    with ExitStack() as es:
        P = Prog(nc, es, uid("opr"))
        xT = sb(es, nc, [128, KC, T], BF16, "xT")
        xB = Buf()
        P.dma("pool", lambda e: e.dma_start(out=xT[:], in_=fm(g.YT)), xB, writes=[xB])
        slabs = Rot(sbufs(es, nc, 3, [128, KC, 512], BF16, "wsl"))
        psr = Rot(psbufs(es, nc, 4, [128, 512], F32, "pp"))
        hir = Rot(sbufs(es, nc, 3, [128, 512], F32, "hi"))
        hor = Rot(sbufs(es, nc, 3, [128, 512], F32, "ho"))
        loaded = []

        def load(s):
            t, b = slabs.next()
            P.dma("pool", lambda e, t=t, s=s: e.dma_start(out=t[:], in_=slab_src(wo, s * 512, 512)), b, writes=[b])
            loaded.append((t, b))
        load(0)
        load(1)
        for s in range(4):
            if s + 2 < 4:
                load(s + 2)
            wt, wB = loaded[s]
            for cc in range(4):
                ch = s * 4 + cc
                for q in range(NQ):
                    qs = slice(q * 512, (q + 1) * 512)
                    hi, hiB = hir.next()
                    P.dma("pool", lambda e, hi=hi, ch=ch, qs=qs: e.dma_start(
                        out=hi[:], in_=hsrc[ch * 128:(ch + 1) * 128, qs]), hiB, writes=[hiB])
                    pt, pB = psr.next()

                    def mm(e, pt=pt, wt=wt, cc=cc, qs=qs):
                        for k in range(KC):
                            ins = e.matmul(pt[:], lhsT=wt[:, k, cc * 128:(cc + 1) * 128], rhs=xT[:, k, qs],
                                           start=(k == 0), stop=(k == KC - 1))
                        return ins
                    P.op("pe", mm, reads=[wB, xB], writes=[pB])
                    ho, hoB = hor.next()
                    P.op("dve", lambda e, ho=ho, pt=pt, hi=hi: e.tensor_tensor(out=ho[:], in0=pt[:], in1=hi[:],
                                                                               op=ALU.add),
                         reads=[pB, hiB], writes=[hoB])
                    P.dma("sp", lambda e, ho=ho, ch=ch, qs=qs: e.dma_start(
                        out=g.hT[ch * 128:(ch + 1) * 128, qs], in_=ho[:]), hoB, reads=[hoB])
                    if False:
                        P.dma("sp", lambda e, ho=ho, ch=ch, qs=qs: e.dma_start(
                            out=g.H1[ch * 128:(ch + 1) * 128, qs], in_=ho[:]), hoB, reads=[hoB])
        P.finish()


def ph_up(g, l):
    nc, T = g.nc, g.T
    NQ = T // 512
    wu = g.w_up[l]
    with ExitStack() as es:
        P = Prog(nc, es, uid("up"))
        cw = sb(es, nc, [128, FC, 3], F32, "cw")
        cb = sb(es, nc, [128, FC], F32, "cb")
        cB = Buf()
        P.dma("pool", lambda e: e.dma_start(out=cw[:], in_=g.cw[l]), cB, writes=[cB])
        P.dma("pool", lambda e: e.dma_start(out=cb[:], in_=g.cb[l]), cB, writes=[cB])
        slabs = Rot(sbufs(es, nc, 4, [128, KC, 512], BF16, "wsl"))
        psr = Rot(psbufs(es, nc, 6, [128, 512], F32, "pp"))
        gts = [(sb(es, nc, [128, T + 2], F32, "gt"), Buf()) for _ in range(2)]
        vsr = Rot(sbufs(es, nc, 2, [128, T], F32, "vs"))
        cvr = Rot(sbufs(es, nc, 2, [128, T], F32, "cv"))
        uor = Rot(sbufs(es, nc, 2, [128, T], BF16, "uo"))
        for gt, gB in gts:
            P.op("dve", lambda e, gt=gt: e.memset(gt[:, 0:2], 0.0), writes=[gB])
        gtr = Rot(gts)
        loaded = []

        def load(s):
            ta, ba = slabs.next()
            P.dma("pool", lambda e, ta=ta, s=s: e.dma_start(out=ta[:], in_=slab_src(wu, s * 512, 512)), ba,
                  writes=[ba])
            tv, bv = slabs.next()
            P.dma("pool", lambda e, tv=tv, s=s: e.dma_start(out=tv[:], in_=slab_src(wu, DFF + s * 512, 512)), bv,
                  writes=[bv])
            loaded.append((ta, ba, tv, bv))
        load(0)
        for s in range(11):
            if s + 1 < 11:
                load(s + 1)
            ta, ba, tv, bv = loaded[s]
            for cc in range(4):
                j = s * 4 + cc
                gt, gB = gtr.next()
                vs, vB = vsr.next()
                for q in range(NQ):
                    qs = slice(q * 512, (q + 1) * 512)
                    pa, paB = psr.next()
                    pv, pvB = psr.next()

                    def mm(e, pa=pa, pv=pv, ta=ta, tv=tv, cc=cc, qs=qs):
                        for k in range(KC):
                            e.matmul(pa[:], lhsT=ta[:, k, cc * 128:(cc + 1) * 128], rhs=g.hnT[:, k, qs],
                                     start=(k == 0), stop=(k == KC - 1))
                        for k in range(KC):
                            ins = e.matmul(pv[:], lhsT=tv[:, k, cc * 128:(cc + 1) * 128], rhs=g.hnT[:, k, qs],
                                           start=(k == 0), stop=(k == KC - 1))
                        return ins
                    P.op("pe", mm, reads=[ba, bv], writes=[paB, pvB])
                    P.op("act", lambda e, gt=gt, pa=pa, q=q: e.activation(
                        out=gt[:, 2 + q * 512:2 + (q + 1) * 512], in_=pa[:], func=AF.Identity),
                        reads=[paB], writes=[gB])
                    P.op("act", lambda e, vs=vs, pv=pv, qs=qs: e.activation(out=vs[:, qs], in_=pv[:],
                                                                            func=AF.Identity),
                         reads=[pvB], writes=[vB])
                cv, cvB = cvr.next()

                P.op("dve", lambda e, cv=cv, gt=gt, j=j: e.tensor_scalar(
                    out=cv[:], in0=gt[:, 0:T], scalar1=cw[:, j, 0:1], scalar2=cb[:, j:j + 1], op0=ALU.mult,
                    op1=ALU.add), reads=[gB, cB], writes=[cvB])
                P.op("dve", lambda e, cv=cv, gt=gt, j=j: e.scalar_tensor_tensor(
                    out=cv[:], in0=gt[:, 1:T + 1], scalar=cw[:, j, 1:2], in1=cv[:], op0=ALU.mult, op1=ALU.add),
                    reads=[gB, cB, cvB], writes=[cvB])
                P.op("dve", lambda e, cv=cv, gt=gt, j=j: e.scalar_tensor_tensor(
                    out=cv[:], in0=gt[:, 2:T + 2], scalar=cw[:, j, 2:3], in1=cv[:], op0=ALU.mult, op1=ALU.add),
                    reads=[gB, cB, cvB], writes=[cvB])
                P.op("act", lambda e, cv=cv: e.activation(out=cv[:], in_=cv[:], func=AF.Silu),
                     reads=[cvB], writes=[cvB])
                uo, uoB = uor.next()
                P.op("dve", lambda e, uo=uo, cv=cv, vs=vs: e.tensor_tensor(out=uo[:], in0=cv[:], in1=vs[:],
                                                                           op=ALU.mult),
                     reads=[cvB, vB], writes=[uoB])
                P.dma("sp", lambda e, uo=uo, j=j: e.dma_start(out=g.UT[j * 128:(j + 1) * 128, :], in_=uo[:]), uoB,
                      reads=[uoB])
        P.finish()


def ph_down(g, l):
    nc, T = g.nc, g.T
    TT = min(T, 1024)
    NQ = TT // 512
    wd = g.w_down[l]
    with ExitStack() as es:
        P = Prog(nc, es, uid("dwn"))
        uT = sb(es, nc, [128, FC, TT], BF16, "uT")
        uB = Buf()
        slabs = Rot(sbufs(es, nc, 3, [128, FC, 128], BF16, "wsl"))
        psr = Rot(psbufs(es, nc, 4, [128, 512], F32, "pp"))
        hir = Rot(sbufs(es, nc, 3, [128, 512], F32, "hi"))
        hor = Rot(sbufs(es, nc, 3, [128, 512], F32, "ho"))
        for half in range(T // TT):
            t0 = half * TT
            P.dma("pool", lambda e, t0=t0: e.dma_start(out=uT[:], in_=fm(g.UT[:, t0:t0 + TT])), uB, writes=[uB])
            loaded = []

            def load(c):
                t, b = slabs.next()
                P.dma("pool", lambda e, t=t, c=c: e.dma_start(
                    out=t[:], in_=wd[:, c * 128:(c + 1) * 128].rearrange("(k p) n -> p k n", p=128)), b,
                    writes=[b])
                loaded.append((t, b))
            load(0)
            load(1)
            for c in range(KC):
                if c + 2 < KC:
                    load(c + 2)
                wt, wB = loaded[c]
                for q in range(NQ):
                    qs = slice(q * 512, (q + 1) * 512)
                    gs = slice(t0 + q * 512, t0 + (q + 1) * 512)
                    hi, hiB = hir.next()
                    P.dma("pool", lambda e, hi=hi, c=c, gs=gs: e.dma_start(
                        out=hi[:], in_=g.hT[c * 128:(c + 1) * 128, gs]), hiB, writes=[hiB])
                    pt, pB = psr.next()

                    def mm(e, pt=pt, wt=wt, qs=qs):
                        for k in range(FC):
                            ins = e.matmul(pt[:], lhsT=wt[:, k, :], rhs=uT[:, k, qs], start=(k == 0),
                                           stop=(k == FC - 1))
                        return ins
                    P.op("pe", mm, reads=[wB, uB], writes=[pB])
                    ho, hoB = hor.next()
                    P.op("dve", lambda e, ho=ho, pt=pt, hi=hi: e.tensor_tensor(out=ho[:], in0=pt[:], in1=hi[:],
                                                                               op=ALU.add),
                         reads=[pB, hiB], writes=[hoB])
                    P.dma("sp", lambda e, ho=ho, c=c, gs=gs: e.dma_start(
                        out=g.hT[c * 128:(c + 1) * 128, gs], in_=ho[:]), hoB, reads=[hoB])
        P.finish()


def build(T, L, debug=False):
    nc = bass.Bass("TRN2", target_bir_lowering=False)
    g = G()
    g.nc, g.T, g.L = nc, T, L
    g.debug = debug

    def din(name, shape, dt=F32):
        return nc.dram_tensor(name, list(shape), dt, kind="ExternalInput").ap()

    def scr(name, shape, dt):
        kind = "ExternalOutput" if debug else "Internal"
        return nc.dram_tensor(name, list(shape), dt, kind=kind).ap()
    g.xT = din("xT", [D, T])
    g.w_in = din("w_in", [L, D, NIN])
    g.w_bm = din("w_bm", [L, D, D])
    g.w_bd = din("w_bd", [L, D, D])
    g.w_o = din("w_o", [L, D, D])
    g.w_up = din("w_up", [L, D, 2 * DFF])
    g.w_down = din("w_down", [L, DFF, D])
    g.gmix = din("gmix", [L, 128, KC])
    g.gffn = din("gffn", [L, 128, KC])
    g.gfin = din("gfin", [128, KC])
    g.bqk = din("bqk", [L, 128, 48])
    g.bgate = din("bgate", [L, 128, 32])
    g.btm = din("btm", [L, 6144])
    g.bif = din("bif", [L, 16])
    g.mnorm = din("mnorm", [L, 2048])
    g.anorm = din("anorm", [L, 2048])
    g.lamqk = din("lamqk", [L, 512])
    g.cw = din("cw", [L, 128, FC, 3])
    g.cb = din("cb", [L, 128, FC])
    g.c_ident = din("c_ident", [128, 128])
    g.c_tri = din("c_tri", [128, 128])
    g.c_ones = din("c_ones", [128, 128])
    g.c_pm = din("c_pm", [32, 32])
    g.c_cos = din("c_cos", [32, T])
    g.c_sin = din("c_sin", [32, T])
    g.outT = nc.dram_tensor("outT", [D, T], F32, kind="ExternalOutput").ap()
    g.hT = scr("hT", [D, T], F32)
    g.QKs = scr("QKs", [48 * 128, T], BF16)
    g.MV = scr("MV", [T, 2048], BF16)
    g.MO = scr("MO", [T, 2048], BF16)
    g.AV = scr("AV", [T, 2048], BF16)
    g.HMT = scr("HMT", [D, T], BF16)
    g.HAT = scr("HAT", [D, T], BF16)
    g.Y1 = scr("Y1", [D, T], F32)
    g.YT = scr("YT", [D, T], BF16)
    g.UT = scr("UT", [DFF, T], BF16)
    if debug:
        g.H1 = scr("H1", [D, T], F32)
        g.HN2 = scr("HN2", [D, T], BF16)
    with ExitStack() as es:
        g.hnT = sb(es, nc, [128, KC, T], BF16, "hnT")
        g.IF = sb(es, nc, [128, T // 128, 16], F32, "IF")
        g.identb = sb(es, nc, [128, 128], BF16, "identb")
        g.trib = sb(es, nc, [128, 128], BF16, "trib")
        g.trif = sb(es, nc, [128, 128], F32, "trif")
        g.onesf = sb(es, nc, [128, 128], F32, "onesf")
        g.pmf = sb(es, nc, [32, 32], F32, "pmf")
        ph_consts(g)
        for l in range(L):
            hsrc = g.xT if l == 0 else g.hT
            ph_norm(g, hsrc, g.gmix[l])
            ph_proj(g, l)
            ph_mlstm(g, l)
            ph_attn(g, l)
            ph_merge(g, l, 0)
            ph_merge(g, l, 1)
            ph_oproj(g, l, hsrc)
            if debug == 2:
                continue
            ph_norm(g, g.hT, g.gffn[l])
            ph_up(g, l)
            if debug == 3:
                continue
            ph_down(g, l)
        ph_norm(g, g.hT, g.gfin, final_dst=g.outT)
    return nc


def pcol(v, nch):
    v = np.asarray(v, dtype=np.float32)
    return np.ascontiguousarray(np.swapaxes(v.reshape(v.shape[:-1] + (nch, 128)), -1, -2))


def host_consts(T):
    s = np.arange(128)
    tri = (s[:, None] <= s[None, :]).astype(np.float32)
    pm = np.zeros((32, 32), np.float32)
    for j in range(16):
        pm[j + 16, j] = -1.0
        pm[j, j + 16] = 1.0
    half = 16
    inv = (np.float32(ROPE_THETA) ** (-np.arange(half, dtype=np.float32) / np.float32(half))).astype(np.float32)
    ang = np.arange(T, dtype=np.float32)[:, None] * inv[None, :]
    cos = np.cos(ang.astype(np.float64)).astype(np.float32).T
    sin = np.sin(ang.astype(np.float64)).astype(np.float32).T
    return {
        "c_ident": np.eye(128, dtype=np.float32), "c_tri": tri, "c_ones": np.ones((128, 128), np.float32),
        "c_pm": pm, "c_cos": np.ascontiguousarray(np.concatenate([cos, cos], 0)),
        "c_sin": np.ascontiguousarray(np.concatenate([sin, sin], 0)),
    }


def prep_shared(L, T, norm_mix, w_in, b_in, m_norm, a_norm, lam_qk, w_bm, w_bd, w_o, norm_ffn, w_up, conv_w, conv_b,
                w_down, norm_final):
    f = lambda a: np.ascontiguousarray(np.asarray(a, dtype=np.float32))
    b_in = f(b_in)
    bqk = np.concatenate([b_in[:, O_MQ:O_MQ + 2048], b_in[:, O_AQ:O_AQ + 4096]], axis=1)
    bgate = b_in[:, O_GM:O_GM + 4096]
    btm = np.concatenate([b_in[:, O_MV:O_MV + 4096], b_in[:, O_AV:O_AV + 2048]], axis=1)
    cwp = np.stack([pcol(f(conv_w)[:, i, :], FC) for i in range(3)], axis=-1)
    d = {
        "w_in": f(w_in)[:L], "w_bm": f(w_bm)[:L], "w_bd": f(w_bd)[:L], "w_o": f(w_o)[:L], "w_up": f(w_up)[:L],
        "w_down": f(w_down)[:L],
        "gmix": pcol(norm_mix, KC)[:L], "gffn": pcol(norm_ffn, KC)[:L], "gfin": pcol(norm_final, KC),
        "bqk": pcol(bqk, 48)[:L], "bgate": pcol(bgate, 32)[:L], "btm": f(btm)[:L],
        "bif": f(b_in[:, O_MI:O_MI + 16])[:L], "mnorm": f(m_norm)[:L], "anorm": f(a_norm)[:L],
        "lamqk": f(lam_qk).reshape(-1, 512)[:L], "cw": np.ascontiguousarray(cwp)[:L], "cb": pcol(conv_b, FC)[:L],
    }
    d.update(host_consts(T))
    return {k: np.ascontiguousarray(v) for k, v in d.items()}


def kernel(x, norm_mix, w_in, b_in, m_norm, a_norm, lam_qk, w_bm, w_bd, w_o, norm_ffn, w_up, conv_w, conv_b, w_down,
           norm_final):
    x = np.asarray(x, dtype=np.float32)
    B, S, _ = x.shape
    L = np.asarray(norm_mix).shape[0]
    shared = prep_shared(L, S, norm_mix, w_in, b_in, m_norm, a_norm, lam_qk, w_bm, w_bd, w_o, norm_ffn, w_up, conv_w,
                         conv_b, w_down, norm_final)
    nc = build(S, L)
    in_maps = []
    for b in range(B):
        m = dict(shared)
        m["xT"] = np.ascontiguousarray(x[b].T)
        in_maps.append(m)
    res = run_bass_kernel_spmd(nc, in_maps, core_ids=list(range(B)))
    out = np.stack([np.ascontiguousarray(res.results[b]["outT"].T) for b in range(B)], axis=0)
    return out.astype(np.float32)
```

```python
import math
from contextlib import ExitStack

import numpy as np
import concourse.bass as bass
import concourse.mybir as mybir
from concourse.bass_utils import run_bass_kernel_spmd

F32 = mybir.dt.float32
BF16 = mybir.dt.bfloat16
AF = mybir.ActivationFunctionType
ALU = mybir.AluOpType

D = 2048
KC = 16
H = 8
NIN = 16400
DFF = 5632
FC = 44
EPS = 1e-6
O_MQ, O_MK, O_MV, O_MO, O_MI, O_AQ, O_AK, O_AV, O_GM, O_GD = (
    0, 1024, 2048, 4096, 6144, 6160, 8208, 10256, 12304, 14352)
ROPE_THETA = 500000.0


class Buf:
    __slots__ = ("w", "r", "dsem", "dcnt")

    def __init__(self):
        self.w = None
        self.r = {}
        self.dsem = None
        self.dcnt = 0


class Prog:
    ENGS = ("pe", "act", "dve", "pool", "sp")

    def __init__(self, nc, es, name):
        self.nc, self.es, self.name = nc, es, name
        self.q = {e: [] for e in self.ENGS}
        self.allsems = []
        self.sem = {e: self._newsem(f"{name}_{e}") for e in self.ENGS}
        self.cnt = {e: 0 for e in self.ENGS}
        self.waited = {e: {} for e in self.ENGS}
        self.dsems = []
        self.nd = 0

    def _newsem(self, name):
        h = self.nc.alloc_semaphore(name=name)
        self.allsems.append(h)
        return h

    def _wait(self, eng, ev):
        sem, val = ev
        if eng == "pe" and sem is self.sem["pe"]:
            return
        w = self.waited[eng]
        k = id(sem)
        if w.get(k, 0) >= val:
            return
        w[k] = val
        self.q[eng].append((0, sem, val))

    def _deps(self, eng, reads, writes):
        for b in reads:
            if b.w is not None:
                self._wait(eng, b.w)
        for b in writes:
            if b.w is not None:
                self._wait(eng, b.w)
            for ev in b.r.values():
                self._wait(eng, ev)

    @staticmethod
    def _commit(ev, reads, writes):
        for b in reads:
            b.r[id(ev[0])] = ev
        for b in writes:
            b.w = ev
            b.r = {}

    def op(self, eng, fn, reads=(), writes=()):
        self._deps(eng, reads, writes)
        self.cnt[eng] += 1
        ev = (self.sem[eng], self.cnt[eng])
        self.q[eng].append((1, fn, self.sem[eng], 1))
        self._commit(ev, reads, writes)
        return ev

    def dma(self, qeng, fn, sb, reads=(), writes=()):
        self._deps(qeng, reads, writes)
        if sb.dsem is None:
            self.nd += 1
            sb.dsem = self._newsem(f"{self.name}_d{self.nd}")
            sb.dcnt = 0
            self.dsems.append(sb)
        sb.dcnt += 16
        ev = (sb.dsem, sb.dcnt)
        self.q[qeng].append((1, fn, sb.dsem, 16))
        self._commit(ev, reads, writes)
        return ev

    def finish(self):
        for e in self.ENGS:
            for e2 in self.ENGS:
                if e2 != e and self.cnt[e2] > 0:
                    self._wait(e, (self.sem[e2], self.cnt[e2]))
            for sb in self.dsems:
                self._wait(e, (sb.dsem, sb.dcnt))
        for sb in self.dsems:
            sb.dsem = None
            sb.dcnt = 0
            sb.w = None
            sb.r = {}
        with self.nc.Block() as blk:
            blk.tensor(lambda e: self._emit("pe", e))
            blk.scalar(lambda e: self._emit("act", e))
            blk.vector(lambda e: self._emit("dve", e))
            blk.gpsimd(lambda e: self._emit("pool", e))
            blk.sync(lambda e: self._emit("sp", e))
        self.nc.all_engine_barrier()
        self.nc.clear_and_free_semaphores(self.allsems)
        self.nc.all_engine_barrier()

    def _emit(self, eng, e):
        for it in self.q[eng]:
            if it[0] == 0:
                e.wait_ge(it[1], it[2])
            else:
                it[1](e).then_inc(it[2], it[3])


class Rot:
    def __init__(self, items):
        self.items = items
        self.i = 0

    def next(self):
        x = self.items[self.i]
        self.i = (self.i + 1) % len(self.items)
        return x


class G:
    pass


_uid = [0]


def uid(p):
    _uid[0] += 1
    return f"{p}{_uid[0]}"


def sb(es, nc, shape, dt, name="t"):
    return es.enter_context(nc.sbuf_tensor(uid(name), list(shape), dt))


def pst(es, nc, shape, dt, name="p"):
    return es.enter_context(nc.psum_tensor(uid(name), list(shape), dt))


def sbufs(es, nc, n, shape, dt, name="t"):
    return [(sb(es, nc, shape, dt, name), Buf()) for _ in range(n)]


def psbufs(es, nc, n, shape, dt, name="p"):
    return [(pst(es, nc, shape, dt, name), Buf()) for _ in range(n)]


def fm(ap):
    return ap.rearrange("(k p) t -> p k t", p=128)


def ph_consts(g):
    nc = g.nc
    with ExitStack() as es:
        P = Prog(nc, es, uid("cst"))
        for t, src in ((g.identb, g.c_ident), (g.trib, g.c_tri), (g.trif, g.c_tri), (g.onesf, g.c_ones),
                       (g.pmf, g.c_pm)):
            P.dma("pool", lambda e, t=t, src=src: e.dma_start(out=t[:], in_=src), Buf(), writes=[])
        P.finish()


def ph_norm(g, src, gam_src, final_dst=None):
    nc, T = g.nc, g.T
    W = 256
    with ExitStack() as es:
        P = Prog(nc, es, uid("nrm"))
        hb = Rot(sbufs(es, nc, 2, [128, KC, W], F32, "hb"))
        sq = sb(es, nc, [128, KC, W], F32, "sq")
        sqA, sqB = Buf(), Buf()
        gm = sb(es, nc, [128, KC], F32, "gm")
        gmB = Buf()
        rs = Rot(sbufs(es, nc, 2, [128, W], F32, "rs"))
        rstd = Rot(sbufs(es, nc, 2, [128, W], F32, "rstd"))
        ps = Rot(psbufs(es, nc, 2, [128, W], F32, "nps"))
        ob = Rot(sbufs(es, nc, 2, [128, KC, W], F32, "ob")) if final_dst is not None else None
        P.dma("pool", lambda e: e.dma_start(out=gm[:], in_=gam_src), gmB, writes=[gmB])
        for j in range(T // W):
            ht, hB = hb.next()
            P.dma("pool", lambda e, ht=ht, j=j: e.dma_start(out=ht[:], in_=fm(src[:, j * W:(j + 1) * W])), hB,
                  writes=[hB])
            P.op("act", lambda e, ht=ht: e.activation(out=sq[:, 0:8, :], in_=ht[:, 0:8, :], func=AF.Square),
                 reads=[hB], writes=[sqA])
            P.op("dve", lambda e, ht=ht: e.tensor_tensor(out=sq[:, 8:16, :], in0=ht[:, 8:16, :], in1=ht[:, 8:16, :],
                                                         op=ALU.mult), reads=[hB], writes=[sqB])
            pt, pB = ps.next()

            def mm(e, pt=pt):
                for k in range(KC):
                    ins = e.matmul(pt[:], lhsT=g.onesf[:], rhs=sq[:, k, :], start=(k == 0), stop=(k == KC - 1))
                return ins
            P.op("pe", mm, reads=[sqA, sqB], writes=[pB])
            rt, rB = rs.next()
            P.op("act", lambda e, rt=rt, pt=pt: e.activation(out=rt[:], in_=pt[:], func=AF.Sqrt, bias=EPS,
                                                             scale=1.0 / D), reads=[pB], writes=[rB])
            st, sB = rstd.next()
            P.op("dve", lambda e, st=st, rt=rt: e.reciprocal(out=st[:], in_=rt[:]), reads=[rB], writes=[sB])
            if final_dst is None:
                def nrm(e, ht=ht, st=st, j=j):
                    for k in range(KC):
                        ins = e.scalar_tensor_tensor(out=g.hnT[:, k, j * W:(j + 1) * W], in0=ht[:, k, :],
                                                     scalar=gm[:, k:k + 1], in1=st[:], op0=ALU.mult, op1=ALU.mult)
                    return ins
                P.op("dve", nrm, reads=[hB, sB, gmB], writes=[Buf()])
            else:
                ot, oB = ob.next()

                def nrm(e, ht=ht, st=st, ot=ot):
                    for k in range(KC):
                        ins = e.scalar_tensor_tensor(out=ot[:, k, :], in0=ht[:, k, :],
                                                     scalar=gm[:, k:k + 1], in1=st[:], op0=ALU.mult, op1=ALU.mult)
                    return ins
                P.op("dve", nrm, reads=[hB, sB, gmB], writes=[oB])
                P.dma("sp", lambda e, ot=ot, j=j: e.dma_start(out=fm(final_dst[:, j * W:(j + 1) * W]), in_=ot[:]),
                      oB, reads=[oB])
        P.finish()


def slab_src(w2d, c0, cw):
    return w2d[:, c0:c0 + cw].rearrange("(k p) n -> p k n", p=128)


def ph_proj(g, l):
    nc, T = g.nc, g.T
    NQ, NT = T // 512, T // 128
    w = g.w_in[l]
    with ExitStack() as es:
        P = Prog(nc, es, uid("prj"))
        slabs = Rot(sbufs(es, nc, 3, [128, KC, 512], BF16, "wsl"))
        psr = Rot(psbufs(es, nc, 4, [128, 512], F32, "pp"))
        pxp = Rot(psbufs(es, nc, 2, [32, 512], F32, "px"))
        stg = Rot(sbufs(es, nc, 4, [128, 512], BF16, "stg"))
        xbr = Rot(sbufs(es, nc, 2, [128, 512], F32, "xb"))
        t1r = Rot(sbufs(es, nc, 2, [32, 512], F32, "t1"))
        t2r = Rot(sbufs(es, nc, 2, [32, 512], F32, "t2"))
        tmr = Rot(sbufs(es, nc, 2, [128, 512], F32, "tm"))
        cosT = sb(es, nc, [32, T], F32, "cos")
        sinT = sb(es, nc, [32, T], F32, "sin")
        bqk = sb(es, nc, [128, 48], F32, "bqk")
        brep = sb(es, nc, [128, 6144], F32, "brep")
        bif = sb(es, nc, [128, 16], F32, "bif")
        wif = sb(es, nc, [128, KC, 16], BF16, "wif")
        cB = Buf()
        for t, src in ((cosT, g.c_cos), (sinT, g.c_sin), (bqk, g.bqk[l]), (brep, g.btm[l].partition_broadcast(128)),
                       (bif, g.bif[l].partition_broadcast(128)), (wif, slab_src(w, O_MI, 16))):
            P.dma("pool", lambda e, t=t, src=src: e.dma_start(out=t[:], in_=src), cB, writes=[cB])
        jobs = []
        for s in range(2):
            jobs.append((O_MQ + s * 512, "fm", ("m", 0 + s * 4)))
        for s in range(2):
            jobs.append((O_MK + s * 512, "fm", ("m", 8 + s * 4)))
        for s in range(4):
            jobs.append((O_AQ + s * 512, "fm", ("aq", 16 + s * 4)))
        for s in range(4):
            jobs.append((O_AK + s * 512, "fm", ("ak", 32 + s * 4)))
        for s in range(4):
            jobs.append((O_MV + s * 512, "tm", ("mv", s)))
        for s in range(4):
            jobs.append((O_MO + s * 512, "tm", ("mo", s)))
        for s in range(4):
            jobs.append((O_AV + s * 512, "tm", ("av", s)))
        loaded = []

        def load(i):
            c0 = jobs[i][0]
            t, b = slabs.next()
            P.dma("pool", lambda e, t=t, c0=c0: e.dma_start(out=t[:], in_=slab_src(w, c0, 512)), b, writes=[b])
            loaded.append((t, b))
        load(0)
        load(1)
        for tt in range(NT):
            pt, pB = psr.next()

            def mm(e, pt=pt, tt=tt):
                for k in range(KC):
                    ins = e.matmul(pt[:, 0:16], lhsT=g.hnT[:, k, tt * 128:(tt + 1) * 128], rhs=wif[:, k, :],
                                   start=(k == 0), stop=(k == KC - 1))
                return ins
            P.op("pe", mm, reads=[cB], writes=[pB])
            P.op("dve", lambda e, pt=pt, tt=tt: e.tensor_tensor(out=g.IF[:, tt, :], in0=pt[:, 0:16], in1=bif[:],
                                                                op=ALU.add), reads=[pB, cB], writes=[Buf()])
        for i, (c0, kind, info) in enumerate(jobs):
            if i + 2 < len(jobs):
                load(i + 2)
            wt, wB = loaded[i]
            if kind == "fm":
                typ, ch0 = info
                for cc in range(4):
                    ch = ch0 + cc
                    for q in range(NQ):
                        pt, pB = psr.next()

                        def mm(e, pt=pt, wt=wt, cc=cc, q=q):
                            for k in range(KC):
                                ins = e.matmul(pt[:], lhsT=wt[:, k, cc * 128:(cc + 1) * 128],
                                               rhs=g.hnT[:, k, q * 512:(q + 1) * 512],
                                               start=(k == 0), stop=(k == KC - 1))
                            return ins
                        P.op("pe", mm, reads=[wB], writes=[pB])
                        st, sB = stg.next()
                        if typ == "m":
                            P.op("act", lambda e, st=st, pt=pt, ch=ch: e.activation(
                                out=st[:], in_=pt[:], func=AF.Identity, bias=bqk[:, ch:ch + 1]),
                                reads=[pB, cB], writes=[sB])
                        else:
                            xb, xB = xbr.next()
                            P.op("act", lambda e, xb=xb, pt=pt, ch=ch: e.activation(
                                out=xb[:], in_=pt[:], func=AF.Identity, bias=bqk[:, ch:ch + 1]),
                                reads=[pB, cB], writes=[xB])
                            px, pxB = pxp.next()
                            P.op("pe", lambda e, px=px, xb=xb: e.matmul(px[:], lhsT=g.pmf[:], rhs=xb[0:32, :],
                                                                        start=True, stop=True),
                                 reads=[xB], writes=[pxB])
                            t1, t1B = t1r.next()
                            t2, t2B = t2r.next()
                            P.op("dve", lambda e, t1=t1, px=px, q=q: e.tensor_tensor(
                                out=t1[:], in0=px[:], in1=sinT[:, q * 512:(q + 1) * 512], op=ALU.mult),
                                reads=[pxB, cB], writes=[t1B])
                            P.op("dve", lambda e, t2=t2, xb=xb, q=q: e.tensor_tensor(
                                out=t2[:], in0=xb[0:32, :], in1=cosT[:, q * 512:(q + 1) * 512], op=ALU.mult),
                                reads=[xB, cB], writes=[t2B])
                            P.op("dve", lambda e, t1=t1, t2=t2, xb=xb: e.tensor_tensor(
                                out=xb[0:32, :], in0=t1[:], in1=t2[:], op=ALU.add),
                                reads=[t1B, t2B], writes=[xB])
                            sc = (128.0 ** -0.5) if typ == "aq" else 1.0
                            P.op("act", lambda e, st=st, xb=xb, sc=sc: e.activation(
                                out=st[:], in_=xb[:], func=AF.Identity, scale=sc), reads=[xB], writes=[sB])
                        P.dma("sp", lambda e, st=st, ch=ch, q=q: e.dma_start(
                            out=g.QKs[ch * 128:(ch + 1) * 128, q * 512:(q + 1) * 512], in_=st[:]), sB, reads=[sB])
            else:
                typ, s = info
                dst = {"mv": g.MV, "mo": g.MO, "av": g.AV}[typ]
                boff = {"mv": 0, "mo": 2048, "av": 4096}[typ] + s * 512
                for tt in range(NT):
                    pt, pB = psr.next()

                    def mm(e, pt=pt, wt=wt, tt=tt):
                        for k in range(KC):
                            ins = e.matmul(pt[:], lhsT=g.hnT[:, k, tt * 128:(tt + 1) * 128], rhs=wt[:, k, :],
                                           start=(k == 0), stop=(k == KC - 1))
                        return ins
                    P.op("pe", mm, reads=[wB], writes=[pB])
                    st, sB = stg.next()
                    if typ == "mo":
                        tm, tB = tmr.next()
                        P.op("dve", lambda e, tm=tm, pt=pt, boff=boff: e.tensor_tensor(
                            out=tm[:], in0=pt[:], in1=brep[:, boff:boff + 512], op=ALU.add),
                            reads=[pB, cB], writes=[tB])
                        P.op("act", lambda e, st=st, tm=tm: e.activation(out=st[:], in_=tm[:], func=AF.Sigmoid),
                             reads=[tB], writes=[sB])
                    else:
                        P.op("dve", lambda e, st=st, pt=pt, boff=boff: e.tensor_tensor(
                            out=st[:], in0=pt[:], in1=brep[:, boff:boff + 512], op=ALU.add),
                            reads=[pB, cB], writes=[sB])
                    P.dma("sp", lambda e, st=st, dst=dst, tt=tt, s=s: e.dma_start(
                        out=dst[tt * 128:(tt + 1) * 128, s * 512:(s + 1) * 512], in_=st[:]), sB, reads=[sB])
        P.finish()


def tm_src(ap2d, h, NT):
    return ap2d[:, h * 256:(h + 1) * 256].rearrange("(t p) d -> p t d", p=128)


def ph_mlstm(g, l):
    nc, T = g.nc, g.T
    NT = T // 128
    with ExitStack() as es:
        P = Prog(nc, es, uid("mls"))
        NG = NT * 8
        def v3(ap):
            return ap.rearrange("p (c h) -> p c h", h=8)
        e1 = sb(es, nc, [128, NG], F32, "e1")
        l1 = sb(es, nc, [128, NG], F32, "l1")
        tg = sb(es, nc, [128, NG], F32, "tg")
        u = sb(es, nc, [128, NG], F32, "u")
        bnd = sb(es, nc, [128, NG], F32, "bnd")
        eg = sb(es, nc, [128, NG], F32, "eg")
        mrep = sb(es, nc, [128, 2048], F32, "mrep")
        cgp = pst(es, nc, [128, 2, NG], F32, "cgp")
        gB = Buf()
        P.dma("pool", lambda e: e.dma_start(out=mrep[:], in_=g.mnorm[l].partition_broadcast(128)), gB,
              writes=[gB])
        P.op("act", lambda e: e.activation(out=v3(e1[:]), in_=g.IF[:, :, 8:16], func=AF.Exp, scale=-1.0),
             writes=[gB])
        P.op("act", lambda e: e.activation(out=l1[:], in_=e1[:], func=AF.Ln, bias=1.0), reads=[gB], writes=[gB])

        def mmg(e):
            e.matmul(cgp[:, 0, :], lhsT=g.trif[:], rhs=l1[:], start=True, stop=True)
            return e.matmul(cgp[:, 1, :], lhsT=g.onesf[:], rhs=l1[:], start=True, stop=True)
        P.op("pe", mmg, reads=[gB], writes=[gB])
        P.op("dve", lambda e: e.tensor_tensor(out=v3(tg[:]), in0=v3(cgp[:, 0, :]), in1=g.IF[:, :, 0:8], op=ALU.add),
             reads=[gB], writes=[gB])

        def gex(e):
            e.activation(out=u[:], in_=tg[:], func=AF.Exp, bias=math.log(128.0 ** -0.5))
            e.activation(out=bnd[:], in_=cgp[:, 0, :], func=AF.Exp)
            return e.activation(out=eg[:], in_=cgp[:, 1, :], func=AF.Exp, scale=-1.0)
        P.op("act", gex, reads=[gB], writes=[gB])

        qkr = Rot([(sb(es, nc, [128, T], BF16, "qT"), sb(es, nc, [128, T], BF16, "kT"), Buf(), Buf())
                   for _ in range(2)])
        var = [(sb(es, nc, [128, NT, 257], BF16, "vA"), Buf()) for _ in range(2)]
        gr = Rot(sbufs(es, nc, 2, [128, NT, 256], BF16, "Gg"))
        hms = Rot(sbufs(es, nc, 2, [128, 2, T], BF16, "hms"))
        C = sb(es, nc, [128, 257], F32, "C")
        C1 = sb(es, nc, [128, 257], F32, "C1")
        Cb = sb(es, nc, [128, 257], BF16, "Cb")
        CB, C1B, CbB = Buf(), Buf(), Buf()
        ptr = Rot(sbufs(es, nc, 2, [128, 128], BF16, "PT"))
        kur = Rot(sbufs(es, nc, 2, [128, 128], BF16, "ku"))
        smr = Rot(sbufs(es, nc, 2, [128, 8], F32, "sm"))
        jkr = Rot(sbufs(es, nc, 2, [128, 256], F32, "jk"))
        hmr = Rot(sbufs(es, nc, 2, [128, 256], BF16, "hm"))
        stp = Rot(psbufs(es, nc, 2, [128, 128], F32, "stp"))
        ktp = Rot(psbufs(es, nc, 1, [128, 128], BF16, "ktp"))
        outp = Rot(psbufs(es, nc, 2, [128, 257], F32, "outp"))
        updp = Rot(psbufs(es, nc, 1, [128, 257], F32, "updp"))
        trp = Rot(psbufs(es, nc, 1, [128, 2, 128], BF16, "trp"))
        for vt, vB in var:
            P.op("dve", lambda e, vt=vt: e.memset(vt[:, :, 256:257], 1.0), writes=[vB])
        vr = Rot(var)
        for h in range(H):
            qT, kT, qB, kB = qkr.next()
            qkB = qB
            vA, vB = vr.next()
            Gt, GB = gr.next()
            hs, hsB = hms.next()
            P.dma("pool", lambda e, qT=qT, h=h: e.dma_start(out=qT[:], in_=g.QKs[h * 128:(h + 1) * 128, :]), qB,
                  writes=[qB])
            P.dma("pool", lambda e, kT=kT, h=h: e.dma_start(out=kT[:], in_=g.QKs[(8 + h) * 128:(9 + h) * 128, :]),
                  kB, writes=[kB])
            P.dma("pool", lambda e, vA=vA, h=h: e.dma_start(out=vA[:, :, 0:256], in_=tm_src(g.MV, h, NT)), vB,
                  writes=[vB])
            P.dma("pool", lambda e, Gt=Gt, h=h: e.dma_start(out=Gt[:], in_=tm_src(g.MO, h, NT)), GB,
                  writes=[GB])

            def gmul(e, Gt=Gt, h=h):
                for c in range(NT):
                    ins = e.tensor_tensor(out=Gt[:, c, :], in0=Gt[:, c, :], in1=mrep[:, h * 256:(h + 1) * 256],
                                          op=ALU.mult)
                return ins
            P.op("dve", gmul, reads=[gB], writes=[GB])
            for c in range(NT):
                cs = slice(c * 128, (c + 1) * 128)
                uc = u[:, c * 8 + h:c * 8 + h + 1]
                st, stB = stp.next()
                P.op("pe", lambda e, st=st, kT=kT, qT=qT, cs=cs: e.matmul(st[:], lhsT=kT[:, cs], rhs=qT[:, cs],
                                                                          start=True, stop=True),
                     reads=[qB, kB], writes=[stB])
                pt, ptB = ptr.next()
                P.op("dve", lambda e, pt=pt, st=st, uc=uc: e.scalar_tensor_tensor(
                    out=pt[:], in0=st[:], scalar=uc, in1=g.trif[:], op0=ALU.mult, op1=ALU.mult),
                    reads=[stB, gB], writes=[ptB])
                kt, ktB = ktp.next()
                P.op("pe", lambda e, kt=kt, kT=kT, cs=cs: e.transpose(kt[:], kT[:, cs], g.identb[:]),
                     reads=[kB], writes=[ktB])
                ku, kuB = kur.next()
                P.op("act", lambda e, ku=ku, kt=kt, uc=uc: e.activation(out=ku[:], in_=kt[:], func=AF.Identity,
                                                                        scale=uc),
                     reads=[ktB, gB], writes=[kuB])
                ot, oB = outp.next()

                def mmo(e, ot=ot, pt=pt, vA=vA, qT=qT, c=c, cs=cs):
                    ins = e.matmul(ot[:], lhsT=pt[:], rhs=vA[:, c, :], start=True, stop=(c == 0))
                    if c > 0:
                        ins = e.matmul(ot[:], lhsT=qT[:, cs], rhs=Cb[:], start=False, stop=True)
                    return ins
                P.op("pe", mmo, reads=[ptB, vB, qB] + ([CbB] if c > 0 else []), writes=[oB])
                if c < NT - 1:
                    up, upB = updp.next()
                    P.op("pe", lambda e, up=up, ku=ku, vA=vA, c=c: e.matmul(up[:], lhsT=ku[:], rhs=vA[:, c, :],
                                                                            start=True, stop=True),
                         reads=[kuB, vB], writes=[upB])
                    egc = eg[:, c * 8 + h:c * 8 + h + 1]
                    if c == 0:
                        P.op("dve", lambda e, up=up, egc=egc: e.tensor_scalar(
                            out=C[:], in0=up[:], scalar1=egc, scalar2=None, op0=ALU.mult),
                            reads=[upB, gB], writes=[CB])
                    else:
                        P.op("act", lambda e, egc=egc: e.activation(out=C1[:], in_=C[:], func=AF.Identity,
                                                                    scale=egc),
                             reads=[CB, gB], writes=[C1B])
                        P.op("dve", lambda e, up=up, egc=egc: e.scalar_tensor_tensor(
                            out=C[:], in0=up[:], scalar=egc, in1=C1[:], op0=ALU.mult, op1=ALU.add),
                            reads=[upB, C1B, gB], writes=[CB])
                    P.op("act", lambda e: e.activation(out=Cb[:], in_=C[:], func=AF.Identity),
                         reads=[CB], writes=[CbB])
                sm, smB = smr.next()
                P.op("act", lambda e, sm=sm, ot=ot: e.activation(out=sm[:, 0:1], in_=ot[:, 256:257], func=AF.Abs),
                     reads=[oB], writes=[smB])
                P.op("dve", lambda e, sm=sm, c=c, h=h: e.tensor_scalar(
                    out=sm[:, 1:2], in0=sm[:, 0:1], scalar1=bnd[:, c * 8 + h:c * 8 + h + 1], scalar2=None, op0=ALU.max),
                    reads=[smB, gB], writes=[smB])
                P.op("dve", lambda e, sm=sm: e.reciprocal(out=sm[:, 2:3], in_=sm[:, 1:2]),
                     reads=[smB], writes=[smB])
                jk, jkB = jkr.next()
                P.op("act", lambda e, jk=jk, ot=ot, sm=sm: e.activation(
                    out=jk[:], in_=ot[:, 0:256], func=AF.Square, scale=sm[:, 2:3], accum_out=sm[:, 3:4]),
                    reads=[oB, smB], writes=[jkB, smB])
                P.op("act", lambda e, sm=sm: e.activation(out=sm[:, 4:5], in_=sm[:, 3:4], func=AF.Ln, bias=EPS,
                                                          scale=1.0 / 256), reads=[smB], writes=[smB])
                P.op("act", lambda e, sm=sm: e.activation(out=sm[:, 5:6], in_=sm[:, 4:5], func=AF.Exp,
                                                          scale=-0.5), reads=[smB], writes=[smB])
                P.op("dve", lambda e, sm=sm: e.tensor_tensor(out=sm[:, 6:7], in0=sm[:, 5:6], in1=sm[:, 2:3],
                                                             op=ALU.mult), reads=[smB], writes=[smB])
                hm, hmB = hmr.next()
                P.op("dve", lambda e, hm=hm, ot=ot, sm=sm, Gt=Gt, c=c: e.scalar_tensor_tensor(
                    out=hm[:], in0=ot[:, 0:256], scalar=sm[:, 6:7], in1=Gt[:, c, :], op0=ALU.mult, op1=ALU.mult),
                    reads=[oB, smB, GB], writes=[hmB])
                tr, trB = trp.next()

                def trs(e, tr=tr, hm=hm):
                    e.transpose(tr[:, 0, :], hm[:, 0:128], g.identb[:])
                    return e.transpose(tr[:, 1, :], hm[:, 128:256], g.identb[:])
                P.op("pe", trs, reads=[hmB], writes=[trB])
                P.op("act", lambda e, hs=hs, tr=tr, cs=cs: e.activation(out=hs[:, :, cs], in_=tr[:],
                                                                        func=AF.Identity),
                     reads=[trB], writes=[hsB])
            P.dma("sp", lambda e, hs=hs, h=h: e.dma_start(
                out=g.HMT[h * 256:(h + 1) * 256, :].rearrange("(j p) t -> p j t", p=128), in_=hs[:]), hsB,
                reads=[hsB])
        P.finish()


def ph_attn(g, l):
    nc, T = g.nc, g.T
    NT, NG = T // 128, T // 256
    lam_init = 0.8 - 0.6 * math.exp(-0.3 * l)
    with ExitStack() as es:
        P = Prog(nc, es, uid("att"))
        lq = sb(es, nc, [128, 4, 128], F32, "lq")
        lp = sb(es, nc, [128, 2, 128], F32, "lp")
        ls = sb(es, nc, [128, 8], F32, "ls")
        anrep = sb(es, nc, [128, 2048], F32, "anrep")
        cB = Buf()
        P.dma("pool", lambda e: e.dma_start(out=lq[:].rearrange("p a b -> p (a b)"),
                                            in_=g.lamqk[l].partition_broadcast(128)), cB, writes=[cB])
        P.dma("pool", lambda e: e.dma_start(out=anrep[:], in_=g.anorm[l].partition_broadcast(128)), cB,
              writes=[cB])

        def lam1(e):
            e.tensor_tensor(out=lp[:, 0, :], in0=lq[:, 0, :], in1=lq[:, 1, :], op=ALU.mult)
            return e.tensor_tensor(out=lp[:, 1, :], in0=lq[:, 2, :], in1=lq[:, 3, :], op=ALU.mult)
        P.op("dve", lam1, reads=[cB], writes=[cB])

        def lam2(e):
            e.reduce_sum(out=ls[:, 0:1], in_=lp[:, 0, :], axis=mybir.AxisListType.X)
            return e.reduce_sum(out=ls[:, 1:2], in_=lp[:, 1, :], axis=mybir.AxisListType.X)
        P.op("dve", lam2, reads=[cB], writes=[cB])
        P.op("act", lambda e: e.activation(out=ls[:, 2:4], in_=ls[:, 0:2], func=AF.Exp), reads=[cB], writes=[cB])
        P.op("dve", lambda e: e.scalar_tensor_tensor(out=ls[:, 4:5], in0=ls[:, 3:4], scalar=-lam_init,
                                                     in1=ls[:, 2:3], op0=ALU.add, op1=ALU.subtract),
             reads=[cB], writes=[cB])
        P.op("dve", lambda e: e.tensor_scalar(out=anrep[:], in0=anrep[:], scalar1=(1.0 - lam_init), scalar2=None,
                                              op0=ALU.mult), reads=[cB], writes=[cB])
        nlam = ls[:, 4:5]

        inb = []
        for i in range(2):
            vt = sb(es, nc, [128, NT, 257], BF16, "vA")
            vB = Buf()
            P.op("dve", lambda e, vt=vt: e.memset(vt[:, :, 256:257], 1.0), writes=[vB])
            inb.append(dict(q2=sb(es, nc, [128, 2, T], BF16, "q2"), k2=sb(es, nc, [128, 2, T], BF16, "k2"),
                            qB=Buf(), kB=Buf(), vA=vt, vB=vB, hs=sb(es, nc, [128, 2, T], BF16, "has"), hsB=Buf()))
        inr = Rot(inb)
        ptr = Rot(sbufs(es, nc, 4, [128, 2, 256], BF16, "PT"))
        rrr = Rot(sbufs(es, nc, 5, [128, 8], F32, "rr"))
        c0r = Rot(sbufs(es, nc, 5, [128, 257], F32, "c0"))
        c1r = Rot(sbufs(es, nc, 5, [128, 257], F32, "c1"))
        a0r = Rot(sbufs(es, nc, 5, [128, 256], F32, "a0"))
        a1r = Rot(sbufs(es, nc, 5, [128, 256], F32, "a1"))
        jkr = Rot(sbufs(es, nc, 5, [128, 256], F32, "jk"))
        habr = Rot(sbufs(es, nc, 5, [128, 256], BF16, "hab"))
        stp = Rot(psbufs(es, nc, 3, [128, 2, 256], F32, "stp"))
        outp = [[(pst(es, nc, [128, 257], F32, "outp"), Buf()) for j in range(2)] for m in range(2)]
        trp = Rot(psbufs(es, nc, 1, [128, 2, 128], BF16, "trp"))

        hb = {}

        def ensure_head(h):
            if h in hb or h >= H:
                return
            b = inr.next()
            c0 = (16 + 2 * h) * 128
            c1 = (32 + 2 * h) * 128
            P.dma("pool", lambda e: e.dma_start(
                out=b["q2"][:], in_=g.QKs[c0:c0 + 256, :].rearrange("(c p) t -> p c t", p=128)), b["qB"],
                writes=[b["qB"]])
            P.dma("pool", lambda e: e.dma_start(
                out=b["k2"][:], in_=g.QKs[c1:c1 + 256, :].rearrange("(c p) t -> p c t", p=128)), b["kB"],
                writes=[b["kB"]])
            P.dma("pool", lambda e: e.dma_start(out=b["vA"][:, :, 0:256], in_=tm_src(g.AV, h, NT)), b["vB"],
                  writes=[b["vB"]])
            hb[h] = b

        steps = [(h, gq, kt) for h in range(H) for gq in range(NG) for kt in range(2 * gq + 2)]
        LA = 2
        stq = {}

        def rec_qk(i):
            h, gq, kt = steps[i]
            ensure_head(h)
            b = hb[h]
            q2, k2 = b["q2"], b["k2"]
            qs = slice(gq * 256, (gq + 1) * 256)
            ks = slice(kt * 128, (kt + 1) * 128)
            st, stB = stp.next()

            def mmq(e):
                e.matmul(st[:, 0, :], lhsT=k2[:, 0, ks], rhs=q2[:, 0, qs], start=True, stop=True)
                return e.matmul(st[:, 1, :], lhsT=k2[:, 1, ks], rhs=q2[:, 1, qs], start=True, stop=True)
            P.op("pe", mmq, reads=[b["qB"], b["kB"]], writes=[stB])
            stq[i] = (st, stB)

        def epilogue(h, gq, j, b):
            (o0, o0B), (o1, o1B) = outp[0][j], outp[1][j]
            hs, hsB = b["hs"], b["hsB"]
            ts = slice((gq * 2 + j) * 128, (gq * 2 + j + 1) * 128)
            c0, c0B = c0r.next()
            c1, c1B = c1r.next()
            P.op("act", lambda e: e.activation(out=c0[:], in_=o0[:], func=AF.Identity), reads=[o0B], writes=[c0B])
            P.op("dve", lambda e: e.tensor_copy(out=c1[:], in_=o1[:]), reads=[o1B], writes=[c1B])
            yield
            rr, rB = rrr.next()

            def rcp(e):
                e.reciprocal(out=rr[:, 0:1], in_=c0[:, 256:257])
                return e.reciprocal(out=rr[:, 1:2], in_=c1[:, 256:257])
            P.op("dve", rcp, reads=[c0B, c1B], writes=[rB])
            P.op("dve", lambda e: e.tensor_tensor(out=rr[:, 2:3], in0=rr[:, 1:2], in1=nlam, op=ALU.mult),
                 reads=[rB, cB], writes=[rB])
            yield
            a0, a0B = a0r.next()
            P.op("act", lambda e: e.activation(out=a0[:], in_=c0[:, 0:256], func=AF.Identity, scale=rr[:, 0:1]),
                 reads=[c0B, rB], writes=[a0B])
            yield
            a1, a1B = a1r.next()
            P.op("dve", lambda e: e.scalar_tensor_tensor(out=a1[:], in0=c1[:, 0:256], scalar=rr[:, 2:3], in1=a0[:],
                                                         op0=ALU.mult, op1=ALU.add),
                 reads=[c1B, rB, a0B], writes=[a1B])
            yield
            jk, jkB = jkr.next()
            P.op("act", lambda e: e.activation(out=jk[:], in_=a1[:], func=AF.Square, accum_out=rr[:, 3:4]),
                 reads=[a1B, rB], writes=[jkB, rB])
            yield
            P.op("act", lambda e: e.activation(out=rr[:, 4:5], in_=rr[:, 3:4], func=AF.Ln, bias=EPS,
                                               scale=1.0 / 256), reads=[rB], writes=[rB])
            yield
            P.op("act", lambda e: e.activation(out=rr[:, 5:6], in_=rr[:, 4:5], func=AF.Exp, scale=-0.5),
                 reads=[rB], writes=[rB])
            yield
            hab, habB = habr.next()
            P.op("dve", lambda e: e.scalar_tensor_tensor(out=hab[:], in0=a1[:], scalar=rr[:, 5:6],
                                                         in1=anrep[:, h * 256:(h + 1) * 256], op0=ALU.mult,
                                                         op1=ALU.mult), reads=[a1B, rB, cB], writes=[habB])
            yield
            tr, trB = trp.next()

            def trs(e):
                e.transpose(tr[:, 0, :], hab[:, 0:128], g.identb[:])
                return e.transpose(tr[:, 1, :], hab[:, 128:256], g.identb[:])
            P.op("pe", trs, reads=[habB], writes=[trB])
            yield
            P.op("act", lambda e: e.activation(out=hs[:, :, ts], in_=tr[:], func=AF.Identity),
                 reads=[trB], writes=[hsB])

        bg = []

        def advance():
            for gen in list(bg):
                try:
                    next(gen)
                except StopIteration:
                    bg.remove(gen)

        for i in range(min(LA, len(steps))):
            rec_qk(i)
        for i, (h, gq, kt) in enumerate(steps):
            if gq == 0 and kt == 0:
                ensure_head(h + 1)
            if i + LA < len(steps):
                rec_qk(i + LA)
            b = hb[h]
            vA, vB = b["vA"], b["vB"]
            st, stB = stq.pop(i)
            pt, ptB = ptr.next()
            P.op("act", lambda e, pt=pt, st=st: e.activation(out=pt[:], in_=st[:], func=AF.Exp),
                 reads=[stB], writes=[ptB])
            if kt >= 2 * gq:
                j0 = kt - 2 * gq

                def msk(e, pt=pt, j0=j0):
                    e.tensor_tensor(out=pt[:, 0, j0 * 128:(j0 + 1) * 128], in0=pt[:, 0, j0 * 128:(j0 + 1) * 128],
                                    in1=g.trib[:], op=ALU.mult)
                    return e.tensor_tensor(out=pt[:, 1, j0 * 128:(j0 + 1) * 128],
                                           in0=pt[:, 1, j0 * 128:(j0 + 1) * 128], in1=g.trib[:], op=ALU.mult)
                P.op("dve", msk, reads=[ptB], writes=[ptB])
            js = [j for j in range(2) if kt <= 2 * gq + j]

            def mmv(e, pt=pt, vA=vA, kt=kt, gq=gq, js=js):
                for j in js:
                    for m in range(2):
                        ins = e.matmul(outp[m][j][0][:], lhsT=pt[:, m, j * 128:(j + 1) * 128], rhs=vA[:, kt, :],
                                       start=(kt == 0), stop=(kt == 2 * gq + j), skip_group_check=True)
                return ins
            P.op("pe", mmv, reads=[ptB, vB], writes=[outp[m][j][1] for j in js for m in range(2)])
            advance()
            if kt == 2 * gq:
                gen = epilogue(h, gq, 0, b)
                next(gen)
                bg.append(gen)
            if kt == 2 * gq + 1:
                gen = epilogue(h, gq, 1, b)
                next(gen)
                bg.append(gen)
                if gq == NG - 1:
                    while bg:
                        advance()
                    P.dma("sp", lambda e, b=b, h=h: e.dma_start(
                        out=g.HAT[h * 256:(h + 1) * 256, :].rearrange("(j p) t -> p j t", p=128), in_=b["hs"][:]),
                        b["hsB"], reads=[b["hsB"]])
                    del hb[h]
        P.finish()


def ph_merge(g, l, which):
    nc, T = g.nc, g.T
    NQ = T // 512
    wa = (g.w_bm if which == 0 else g.w_bd)[l]
    wg = g.w_in[l]
    goff = O_GM if which == 0 else O_GD
    src = g.HMT if which == 0 else g.HAT
    with ExitStack() as es:
        P = Prog(nc, es, uid("mrg"))
        xT = sb(es, nc, [128, KC, T], BF16, "xT")
        xB = Buf()
        bg = sb(es, nc, [128, 32], F32, "bg")
        bgB = Buf()
        P.dma("pool", lambda e: e.dma_start(out=bg[:], in_=g.bgate[l]), bgB, writes=[bgB])
        P.dma("pool", lambda e: e.dma_start(out=xT[:], in_=fm(src)), xB, writes=[xB])
        SW = 256
        NS = 2048 // SW
        CPS = SW // 128
        slabs = Rot(sbufs(es, nc, 4, [128, KC, SW], BF16, "wsl"))
        psy = Rot(psbufs(es, nc, 3, [128, 512], F32, "py"))
        psg = Rot(psbufs(es, nc, 3, [128, 512], F32, "pg"))
        sgr = Rot(sbufs(es, nc, 2, [128, 512], F32, "sg"))
        y1r = Rot(sbufs(es, nc, 3, [128, 512], F32, "y1"))
        if which == 1:
            tr_ = Rot(sbufs(es, nc, 2, [128, 512], F32, "tt"))
            yor = Rot(sbufs(es, nc, 3, [128, 512], BF16, "yo"))
        loaded = []

        def load(s):
            ta, ba = slabs.next()
            P.dma("pool", lambda e, ta=ta, s=s: e.dma_start(out=ta[:], in_=slab_src(wa, s * SW, SW)), ba,
                  writes=[ba])
            tg, bgs = slabs.next()
            P.dma("pool", lambda e, tg=tg, s=s: e.dma_start(out=tg[:], in_=slab_src(wg, goff + s * SW, SW)), bgs,
                  writes=[bgs])
            loaded.append((ta, ba, tg, bgs))
        load(0)
        for s in range(NS):
            if s + 1 < NS:
                load(s + 1)
            ta, ba, tg, bgs = loaded[s]
            for cc in range(CPS):
                ch = s * CPS + cc
                for q in range(NQ):
                    qs = slice(q * 512, (q + 1) * 512)
                    py, pyB = psy.next()
                    pg, pgB = psg.next()

                    def mm(e, py=py, pg=pg, ta=ta, tg=tg, cc=cc, qs=qs):
                        for k in range(KC):
                            e.matmul(py[:], lhsT=ta[:, k, cc * 128:(cc + 1) * 128], rhs=xT[:, k, qs],
                                     start=(k == 0), stop=(k == KC - 1))
                        for k in range(KC):
                            ins = e.matmul(pg[:], lhsT=tg[:, k, cc * 128:(cc + 1) * 128], rhs=g.hnT[:, k, qs],
                                           start=(k == 0), stop=(k == KC - 1))
                        return ins
                    P.op("pe", mm, reads=[ba, bgs, xB], writes=[pyB, pgB])
                    sg, sgB = sgr.next()
                    bcol = which * 16 + ch
                    P.op("act", lambda e, sg=sg, pg=pg, bcol=bcol: e.activation(
                        out=sg[:], in_=pg[:], func=AF.Sigmoid, bias=bg[:, bcol:bcol + 1]),
                        reads=[pgB, bgB], writes=[sgB])
                    if which == 0:
                        y1, y1B = y1r.next()
                        P.op("dve", lambda e, y1=y1, py=py, sg=sg: e.tensor_tensor(out=y1[:], in0=py[:], in1=sg[:],
                                                                                   op=ALU.mult),
                             reads=[pyB, sgB], writes=[y1B])
                        P.dma("sp", lambda e, y1=y1, ch=ch, qs=qs: e.dma_start(
                            out=g.Y1[ch * 128:(ch + 1) * 128, qs], in_=y1[:]), y1B, reads=[y1B])
                    else:
                        y1, y1B = y1r.next()
                        P.dma("pool", lambda e, y1=y1, ch=ch, qs=qs: e.dma_start(
                            out=y1[:], in_=g.Y1[ch * 128:(ch + 1) * 128, qs]), y1B, writes=[y1B])
                        tt, ttB = tr_.next()
                        P.op("dve", lambda e, tt=tt, py=py, sg=sg: e.tensor_tensor(out=tt[:], in0=py[:], in1=sg[:],
                                                                                   op=ALU.mult),
                             reads=[pyB, sgB], writes=[ttB])
                        yo, yoB = yor.next()
                        P.op("dve", lambda e, yo=yo, tt=tt, y1=y1: e.tensor_tensor(out=yo[:], in0=tt[:], in1=y1[:],
                                                                                   op=ALU.add),
                             reads=[ttB, y1B], writes=[yoB])
                        P.dma("sp", lambda e, yo=yo, ch=ch, qs=qs: e.dma_start(
                            out=g.YT[ch * 128:(ch + 1) * 128, qs], in_=yo[:]), yoB, reads=[yoB])
        P.finish()


def ph_dump_hn(g, dst):
    nc = g.nc
    with ExitStack() as es:
        P = Prog(nc, es, uid("dmp"))
        b = Buf()
        for k in range(KC):
            P.dma("sp", lambda e, k=k: e.dma_start(out=dst[k * 128:(k + 1) * 128, :], in_=g.hnT[:, k, :]), b,
                  reads=[b])
        P.finish()


def ph_oproj(g, l, hsrc):
    nc, T = g.nc, g.T
    NQ = T // 512
    wo = g.w_o[l]
    with ExitStack() as es:
        P = Prog(nc, es, uid("opr"))
        xT = sb(es, nc, [128, KC, T], BF16, "xT")
        xB = Buf()
        P.dma("pool", lambda e: e.dma_start(out=xT[:], in_=fm(g.YT)), xB, writes=[xB])
        slabs = Rot(sbufs(es, nc, 3, [128, KC, 512], BF16, "wsl"))
        psr = Rot(psbufs(es, nc, 4, [128, 512], F32, "pp"))
        hir = Rot(sbufs(es, nc, 3, [128, 512], F32, "hi"))
        hor = Rot(sbufs(es, nc, 3, [128, 512], F32, "ho"))
        loaded = []

        def load(s):
            t, b = slabs.next()
            P.dma("pool", lambda e, t=t, s=s: e.dma_start(out=t[:], in_=slab_src(wo, s * 512, 512)), b, writes=[b])
            loaded.append((t, b))
        load(0)
        load(1)
        for s in range(4):
            if s + 2 < 4:
                load(s + 2)
            wt, wB = loaded[s]
            for cc in range(4):
                ch = s * 4 + cc
                for q in range(NQ):
                    qs = slice(q * 512, (q + 1) * 512)
                    hi, hiB = hir.next()
                    P.dma("pool", lambda e, hi=hi, ch=ch, qs=qs: e.dma_start(
                        out=hi[:], in_=hsrc[ch * 128:(ch + 1) * 128, qs]), hiB, writes=[hiB])
                    pt, pB = psr.next()

                    def mm(e, pt=pt, wt=wt, cc=cc, qs=qs):
                        for k in range(KC):
                            ins = e.matmul(pt[:], lhsT=wt[:, k, cc * 128:(cc + 1) * 128], rhs=xT[:, k, qs],
                                           start=(k == 0), stop=(k == KC - 1))
                        return ins
                    P.op("pe", mm, reads=[wB, xB], writes=[pB])
                    ho, hoB = hor.next()
                    P.op("dve", lambda e, ho=ho, pt=pt, hi=hi: e.tensor_tensor(out=ho[:], in0=pt[:], in1=hi[:],
                                                                               op=ALU.add),
                         reads=[pB, hiB], writes=[hoB])
                    P.dma("sp", lambda e, ho=ho, ch=ch, qs=qs: e.dma_start(
                        out=g.hT[ch * 128:(ch + 1) * 128, qs], in_=ho[:]), hoB, reads=[hoB])
                    if False:
                        P.dma("sp", lambda e, ho=ho, ch=ch, qs=qs: e.dma_start(
                            out=g.H1[ch * 128:(ch + 1) * 128, qs], in_=ho[:]), hoB, reads=[hoB])
        P.finish()


def ph_up(g, l):
    nc, T = g.nc, g.T
    NQ = T // 512
    wu = g.w_up[l]
    with ExitStack() as es:
        P = Prog(nc, es, uid("up"))
        cw = sb(es, nc, [128, FC, 3], F32, "cw")
        cb = sb(es, nc, [128, FC], F32, "cb")
        cB = Buf()
        P.dma("pool", lambda e: e.dma_start(out=cw[:], in_=g.cw[l]), cB, writes=[cB])
        P.dma("pool", lambda e: e.dma_start(out=cb[:], in_=g.cb[l]), cB, writes=[cB])
        slabs = Rot(sbufs(es, nc, 4, [128, KC, 512], BF16, "wsl"))
        psr = Rot(psbufs(es, nc, 6, [128, 512], F32, "pp"))
        gts = [(sb(es, nc, [128, T + 2], F32, "gt"), Buf()) for _ in range(2)]
        vsr = Rot(sbufs(es, nc, 2, [128, T], F32, "vs"))
        cvr = Rot(sbufs(es, nc, 2, [128, T], F32, "cv"))
        uor = Rot(sbufs(es, nc, 2, [128, T], BF16, "uo"))
        for gt, gB in gts:
            P.op("dve", lambda e, gt=gt: e.memset(gt[:, 0:2], 0.0), writes=[gB])
        gtr = Rot(gts)
        loaded = []

        def load(s):
            ta, ba = slabs.next()
            P.dma("pool", lambda e, ta=ta, s=s: e.dma_start(out=ta[:], in_=slab_src(wu, s * 512, 512)), ba,
                  writes=[ba])
            tv, bv = slabs.next()
            P.dma("pool", lambda e, tv=tv, s=s: e.dma_start(out=tv[:], in_=slab_src(wu, DFF + s * 512, 512)), bv,
                  writes=[bv])
            loaded.append((ta, ba, tv, bv))
        load(0)
        for s in range(11):
            if s + 1 < 11:
                load(s + 1)
            ta, ba, tv, bv = loaded[s]
            for cc in range(4):
                j = s * 4 + cc
                gt, gB = gtr.next()
                vs, vB = vsr.next()
                for q in range(NQ):
                    qs = slice(q * 512, (q + 1) * 512)
                    pa, paB = psr.next()
                    pv, pvB = psr.next()

                    def mm(e, pa=pa, pv=pv, ta=ta, tv=tv, cc=cc, qs=qs):
                        for k in range(KC):
                            e.matmul(pa[:], lhsT=ta[:, k, cc * 128:(cc + 1) * 128], rhs=g.hnT[:, k, qs],
                                     start=(k == 0), stop=(k == KC - 1))
                        for k in range(KC):
                            ins = e.matmul(pv[:], lhsT=tv[:, k, cc * 128:(cc + 1) * 128], rhs=g.hnT[:, k, qs],
                                           start=(k == 0), stop=(k == KC - 1))
                        return ins
                    P.op("pe", mm, reads=[ba, bv], writes=[paB, pvB])
                    P.op("act", lambda e, gt=gt, pa=pa, q=q: e.activation(
                        out=gt[:, 2 + q * 512:2 + (q + 1) * 512], in_=pa[:], func=AF.Identity),
                        reads=[paB], writes=[gB])
                    P.op("act", lambda e, vs=vs, pv=pv, qs=qs: e.activation(out=vs[:, qs], in_=pv[:],
                                                                            func=AF.Identity),
                         reads=[pvB], writes=[vB])
                cv, cvB = cvr.next()

                P.op("dve", lambda e, cv=cv, gt=gt, j=j: e.tensor_scalar(
                    out=cv[:], in0=gt[:, 0:T], scalar1=cw[:, j, 0:1], scalar2=cb[:, j:j + 1], op0=ALU.mult,
                    op1=ALU.add), reads=[gB, cB], writes=[cvB])
                P.op("dve", lambda e, cv=cv, gt=gt, j=j: e.scalar_tensor_tensor(
                    out=cv[:], in0=gt[:, 1:T + 1], scalar=cw[:, j, 1:2], in1=cv[:], op0=ALU.mult, op1=ALU.add),
                    reads=[gB, cB, cvB], writes=[cvB])
                P.op("dve", lambda e, cv=cv, gt=gt, j=j: e.scalar_tensor_tensor(
                    out=cv[:], in0=gt[:, 2:T + 2], scalar=cw[:, j, 2:3], in1=cv[:], op0=ALU.mult, op1=ALU.add),
                    reads=[gB, cB, cvB], writes=[cvB])
                P.op("act", lambda e, cv=cv: e.activation(out=cv[:], in_=cv[:], func=AF.Silu),
                     reads=[cvB], writes=[cvB])
                uo, uoB = uor.next()
                P.op("dve", lambda e, uo=uo, cv=cv, vs=vs: e.tensor_tensor(out=uo[:], in0=cv[:], in1=vs[:],
                                                                           op=ALU.mult),
                     reads=[cvB, vB], writes=[uoB])
                P.dma("sp", lambda e, uo=uo, j=j: e.dma_start(out=g.UT[j * 128:(j + 1) * 128, :], in_=uo[:]), uoB,
                      reads=[uoB])
        P.finish()


def ph_down(g, l):
    nc, T = g.nc, g.T
    TT = min(T, 1024)
    NQ = TT // 512
    wd = g.w_down[l]
    with ExitStack() as es:
        P = Prog(nc, es, uid("dwn"))
        uT = sb(es, nc, [128, FC, TT], BF16, "uT")
        uB = Buf()
        slabs = Rot(sbufs(es, nc, 3, [128, FC, 128], BF16, "wsl"))
        psr = Rot(psbufs(es, nc, 4, [128, 512], F32, "pp"))
        hir = Rot(sbufs(es, nc, 3, [128, 512], F32, "hi"))
        hor = Rot(sbufs(es, nc, 3, [128, 512], F32, "ho"))
        for half in range(T // TT):
            t0 = half * TT
            P.dma("pool", lambda e, t0=t0: e.dma_start(out=uT[:], in_=fm(g.UT[:, t0:t0 + TT])), uB, writes=[uB])
            loaded = []

            def load(c):
                t, b = slabs.next()
                P.dma("pool", lambda e, t=t, c=c: e.dma_start(
                    out=t[:], in_=wd[:, c * 128:(c + 1) * 128].rearrange("(k p) n -> p k n", p=128)), b,
                    writes=[b])
                loaded.append((t, b))
            load(0)
            load(1)
            for c in range(KC):
                if c + 2 < KC:
                    load(c + 2)
                wt, wB = loaded[c]
                for q in range(NQ):
                    qs = slice(q * 512, (q + 1) * 512)
                    gs = slice(t0 + q * 512, t0 + (q + 1) * 512)
                    hi, hiB = hir.next()
                    P.dma("pool", lambda e, hi=hi, c=c, gs=gs: e.dma_start(
                        out=hi[:], in_=g.hT[c * 128:(c + 1) * 128, gs]), hiB, writes=[hiB])
                    pt, pB = psr.next()

                    def mm(e, pt=pt, wt=wt, qs=qs):
                        for k in range(FC):
                            ins = e.matmul(pt[:], lhsT=wt[:, k, :], rhs=uT[:, k, qs], start=(k == 0),
                                           stop=(k == FC - 1))
                        return ins
                    P.op("pe", mm, reads=[wB, uB], writes=[pB])
                    ho, hoB = hor.next()
                    P.op("dve", lambda e, ho=ho, pt=pt, hi=hi: e.tensor_tensor(out=ho[:], in0=pt[:], in1=hi[:],
                                                                               op=ALU.add),
                         reads=[pB, hiB], writes=[hoB])
                    P.dma("sp", lambda e, ho=ho, c=c, gs=gs: e.dma_start(
                        out=g.hT[c * 128:(c + 1) * 128, gs], in_=ho[:]), hoB, reads=[hoB])
        P.finish()


def build(T, L, debug=False):
    nc = bass.Bass("TRN2", target_bir_lowering=False)
    g = G()
    g.nc, g.T, g.L = nc, T, L
    g.debug = debug

    def din(name, shape, dt=F32):
        return nc.dram_tensor(name, list(shape), dt, kind="ExternalInput").ap()

    def scr(name, shape, dt):
        kind = "ExternalOutput" if debug else "Internal"
        return nc.dram_tensor(name, list(shape), dt, kind=kind).ap()
    g.xT = din("xT", [D, T])
    g.w_in = din("w_in", [L, D, NIN])
    g.w_bm = din("w_bm", [L, D, D])
    g.w_bd = din("w_bd", [L, D, D])
    g.w_o = din("w_o", [L, D, D])
    g.w_up = din("w_up", [L, D, 2 * DFF])
    g.w_down = din("w_down", [L, DFF, D])
    g.gmix = din("gmix", [L, 128, KC])
    g.gffn = din("gffn", [L, 128, KC])
    g.gfin = din("gfin", [128, KC])
    g.bqk = din("bqk", [L, 128, 48])
    g.bgate = din("bgate", [L, 128, 32])
    g.btm = din("btm", [L, 6144])
    g.bif = din("bif", [L, 16])
    g.mnorm = din("mnorm", [L, 2048])
    g.anorm = din("anorm", [L, 2048])
    g.lamqk = din("lamqk", [L, 512])
    g.cw = din("cw", [L, 128, FC, 3])
    g.cb = din("cb", [L, 128, FC])
    g.c_ident = din("c_ident", [128, 128])
    g.c_tri = din("c_tri", [128, 128])
    g.c_ones = din("c_ones", [128, 128])
    g.c_pm = din("c_pm", [32, 32])
    g.c_cos = din("c_cos", [32, T])
    g.c_sin = din("c_sin", [32, T])
    g.outT = nc.dram_tensor("outT", [D, T], F32, kind="ExternalOutput").ap()
    g.hT = scr("hT", [D, T], F32)
    g.QKs = scr("QKs", [48 * 128, T], BF16)
    g.MV = scr("MV", [T, 2048], BF16)
    g.MO = scr("MO", [T, 2048], BF16)
    g.AV = scr("AV", [T, 2048], BF16)
    g.HMT = scr("HMT", [D, T], BF16)
    g.HAT = scr("HAT", [D, T], BF16)
    g.Y1 = scr("Y1", [D, T], F32)
    g.YT = scr("YT", [D, T], BF16)
    g.UT = scr("UT", [DFF, T], BF16)
    if debug:
        g.H1 = scr("H1", [D, T], F32)
        g.HN2 = scr("HN2", [D, T], BF16)
    with ExitStack() as es:
        g.hnT = sb(es, nc, [128, KC, T], BF16, "hnT")
        g.IF = sb(es, nc, [128, T // 128, 16], F32, "IF")
        g.identb = sb(es, nc, [128, 128], BF16, "identb")
        g.trib = sb(es, nc, [128, 128], BF16, "trib")
        g.trif = sb(es, nc, [128, 128], F32, "trif")
        g.onesf = sb(es, nc, [128, 128], F32, "onesf")
        g.pmf = sb(es, nc, [32, 32], F32, "pmf")
        ph_consts(g)
        for l in range(L):
            hsrc = g.xT if l == 0 else g.hT
            ph_norm(g, hsrc, g.gmix[l])
            ph_proj(g, l)
            ph_mlstm(g, l)
            ph_attn(g, l)
            ph_merge(g, l, 0)
            ph_merge(g, l, 1)
            ph_oproj(g, l, hsrc)
            if debug == 2:
                continue
            ph_norm(g, g.hT, g.gffn[l])
            ph_up(g, l)
            if debug == 3:
                continue
            ph_down(g, l)
        ph_norm(g, g.hT, g.gfin, final_dst=g.outT)
    return nc


def pcol(v, nch):
    v = np.asarray(v, dtype=np.float32)
    return np.ascontiguousarray(np.swapaxes(v.reshape(v.shape[:-1] + (nch, 128)), -1, -2))


def host_consts(T):
    s = np.arange(128)
    tri = (s[:, None] <= s[None, :]).astype(np.float32)
    pm = np.zeros((32, 32), np.float32)
    for j in range(16):
        pm[j + 16, j] = -1.0
        pm[j, j + 16] = 1.0
    half = 16
    inv = (np.float32(ROPE_THETA) ** (-np.arange(half, dtype=np.float32) / np.float32(half))).astype(np.float32)
    ang = np.arange(T, dtype=np.float32)[:, None] * inv[None, :]
    cos = np.cos(ang.astype(np.float64)).astype(np.float32).T
    sin = np.sin(ang.astype(np.float64)).astype(np.float32).T
    return {
        "c_ident": np.eye(128, dtype=np.float32), "c_tri": tri, "c_ones": np.ones((128, 128), np.float32),
        "c_pm": pm, "c_cos": np.ascontiguousarray(np.concatenate([cos, cos], 0)),
        "c_sin": np.ascontiguousarray(np.concatenate([sin, sin], 0)),
    }


def prep_shared(L, T, norm_mix, w_in, b_in, m_norm, a_norm, lam_qk, w_bm, w_bd, w_o, norm_ffn, w_up, conv_w, conv_b,
                w_down, norm_final):
    f = lambda a: np.ascontiguousarray(np.asarray(a, dtype=np.float32))
    b_in = f(b_in)
    bqk = np.concatenate([b_in[:, O_MQ:O_MQ + 2048], b_in[:, O_AQ:O_AQ + 4096]], axis=1)
    bgate = b_in[:, O_GM:O_GM + 4096]
    btm = np.concatenate([b_in[:, O_MV:O_MV + 4096], b_in[:, O_AV:O_AV + 2048]], axis=1)
    cwp = np.stack([pcol(f(conv_w)[:, i, :], FC) for i in range(3)], axis=-1)
    d = {
        "w_in": f(w_in)[:L], "w_bm": f(w_bm)[:L], "w_bd": f(w_bd)[:L], "w_o": f(w_o)[:L], "w_up": f(w_up)[:L],
        "w_down": f(w_down)[:L],
        "gmix": pcol(norm_mix, KC)[:L], "gffn": pcol(norm_ffn, KC)[:L], "gfin": pcol(norm_final, KC),
        "bqk": pcol(bqk, 48)[:L], "bgate": pcol(bgate, 32)[:L], "btm": f(btm)[:L],
        "bif": f(b_in[:, O_MI:O_MI + 16])[:L], "mnorm": f(m_norm)[:L], "anorm": f(a_norm)[:L],
        "lamqk": f(lam_qk).reshape(-1, 512)[:L], "cw": np.ascontiguousarray(cwp)[:L], "cb": pcol(conv_b, FC)[:L],
    }
    d.update(host_consts(T))
    return {k: np.ascontiguousarray(v) for k, v in d.items()}


def kernel(x, norm_mix, w_in, b_in, m_norm, a_norm, lam_qk, w_bm, w_bd, w_o, norm_ffn, w_up, conv_w, conv_b, w_down,
           norm_final):
    x = np.asarray(x, dtype=np.float32)
    B, S, _ = x.shape
    L = np.asarray(norm_mix).shape[0]
    shared = prep_shared(L, S, norm_mix, w_in, b_in, m_norm, a_norm, lam_qk, w_bm, w_bd, w_o, norm_ffn, w_up, conv_w,
                         conv_b, w_down, norm_final)
    nc = build(S, L)
    in_maps = []
    for b in range(B):
        m = dict(shared)
        m["xT"] = np.ascontiguousarray(x[b].T)
        in_maps.append(m)
    res = run_bass_kernel_spmd(nc, in_maps, core_ids=list(range(B)))
    out = np.stack([np.ascontiguousarray(res.results[b]["outT"].T) for b in range(B)], axis=0)
    return out.astype(np.float32)
```

```python
import math
from contextlib import ExitStack

import numpy as np
import concourse.bass as bass
import concourse.mybir as mybir
from concourse.bass_utils import run_bass_kernel_spmd

F32 = mybir.dt.float32
BF16 = mybir.dt.bfloat16
AF = mybir.ActivationFunctionType
ALU = mybir.AluOpType

D = 2048
KC = 16
H = 8
NIN = 16400
DFF = 5632
FC = 44
EPS = 1e-6
O_MQ, O_MK, O_MV, O_MO, O_MI, O_AQ, O_AK, O_AV, O_GM, O_GD = (
    0, 1024, 2048, 4096, 6144, 6160, 8208, 10256, 12304, 14352)
ROPE_THETA = 500000.0


class Buf:
    __slots__ = ("w", "r", "dsem", "dcnt")

    def __init__(self):
        self.w = None
        self.r = {}
        self.dsem = None
        self.dcnt = 0


class Prog:
    ENGS = ("pe", "act", "dve", "pool", "sp")

    def __init__(self, nc, es, name):
        self.nc, self.es, self.name = nc, es, name
        self.q = {e: [] for e in self.ENGS}
        self.allsems = []
        self.sem = {e: self._newsem(f"{name}_{e}") for e in self.ENGS}
        self.cnt = {e: 0 for e in self.ENGS}
        self.waited = {e: {} for e in self.ENGS}
        self.dsems = []
        self.nd = 0

    def _newsem(self, name):
        h = self.nc.alloc_semaphore(name=name)
        self.allsems.append(h)
        return h

    def _wait(self, eng, ev):
        sem, val = ev
        if eng == "pe" and sem is self.sem["pe"]:
            return
        w = self.waited[eng]
        k = id(sem)
        if w.get(k, 0) >= val:
            return
        w[k] = val
        self.q[eng].append((0, sem, val))

    def _deps(self, eng, reads, writes):
        for b in reads:
            if b.w is not None:
                self._wait(eng, b.w)
        for b in writes:
            if b.w is not None:
                self._wait(eng, b.w)
            for ev in b.r.values():
                self._wait(eng, ev)

    @staticmethod
    def _commit(ev, reads, writes):
        for b in reads:
            b.r[id(ev[0])] = ev
        for b in writes:
            b.w = ev
            b.r = {}

    def op(self, eng, fn, reads=(), writes=()):
        self._deps(eng, reads, writes)
        self.cnt[eng] += 1
        ev = (self.sem[eng], self.cnt[eng])
        self.q[eng].append((1, fn, self.sem[eng], 1))
        self._commit(ev, reads, writes)
        return ev

    def dma(self, qeng, fn, sb, reads=(), writes=()):
        self._deps(qeng, reads, writes)
        if sb.dsem is None:
            self.nd += 1
            sb.dsem = self._newsem(f"{self.name}_d{self.nd}")
            sb.dcnt = 0
            self.dsems.append(sb)
        sb.dcnt += 16
        ev = (sb.dsem, sb.dcnt)
        self.q[qeng].append((1, fn, sb.dsem, 16))
        self._commit(ev, reads, writes)
        return ev

    def finish(self):
        for e in self.ENGS:
            for e2 in self.ENGS:
                if e2 != e and self.cnt[e2] > 0:
                    self._wait(e, (self.sem[e2], self.cnt[e2]))
            for sb in self.dsems:
                self._wait(e, (sb.dsem, sb.dcnt))
        for sb in self.dsems:
            sb.dsem = None
            sb.dcnt = 0
            sb.w = None
            sb.r = {}
        with self.nc.Block() as blk:
            blk.tensor(lambda e: self._emit("pe", e))
            blk.scalar(lambda e: self._emit("act", e))
            blk.vector(lambda e: self._emit("dve", e))
            blk.gpsimd(lambda e: self._emit("pool", e))
            blk.sync(lambda e: self._emit("sp", e))
        self.nc.all_engine_barrier()
        self.nc.clear_and_free_semaphores(self.allsems)
        self.nc.all_engine_barrier()

    def _emit(self, eng, e):
        for it in self.q[eng]:
            if it[0] == 0:
                e.wait_ge(it[1], it[2])
            else:
                it[1](e).then_inc(it[2], it[3])


class Rot:
    def __init__(self, items):
        self.items = items
        self.i = 0

    def next(self):
        x = self.items[self.i]
        self.i = (self.i + 1) % len(self.items)
        return x


class G:
    pass


_uid = [0]


def uid(p):
    _uid[0] += 1
    return f"{p}{_uid[0]}"


def sb(es, nc, shape, dt, name="t"):
    return es.enter_context(nc.sbuf_tensor(uid(name), list(shape), dt))


def pst(es, nc, shape, dt, name="p"):
    return es.enter_context(nc.psum_tensor(uid(name), list(shape), dt))


def sbufs(es, nc, n, shape, dt, name="t"):
    return [(sb(es, nc, shape, dt, name), Buf()) for _ in range(n)]


def psbufs(es, nc, n, shape, dt, name="p"):
    return [(pst(es, nc, shape, dt, name), Buf()) for _ in range(n)]


def fm(ap):
    return ap.rearrange("(k p) t -> p k t", p=128)


def ph_consts(g):
    nc = g.nc
    with ExitStack() as es:
        P = Prog(nc, es, uid("cst"))
        for t, src in ((g.identb, g.c_ident), (g.trib, g.c_tri), (g.trif, g.c_tri), (g.onesf, g.c_ones),
                       (g.pmf, g.c_pm)):
            P.dma("pool", lambda e, t=t, src=src: e.dma_start(out=t[:], in_=src), Buf(), writes=[])
        P.finish()


def ph_norm(g, src, gam_src, final_dst=None):
    nc, T = g.nc, g.T
    W = 256
    with ExitStack() as es:
        P = Prog(nc, es, uid("nrm"))
        hb = Rot(sbufs(es, nc, 2, [128, KC, W], F32, "hb"))
        sq = sb(es, nc, [128, KC, W], F32, "sq")
        sqA, sqB = Buf(), Buf()
        gm = sb(es, nc, [128, KC], F32, "gm")
        gmB = Buf()
        rs = Rot(sbufs(es, nc, 2, [128, W], F32, "rs"))
        rstd = Rot(sbufs(es, nc, 2, [128, W], F32, "rstd"))
        ps = Rot(psbufs(es, nc, 2, [128, W], F32, "nps"))
        ob = Rot(sbufs(es, nc, 2, [128, KC, W], F32, "ob")) if final_dst is not None else None
        P.dma("pool", lambda e: e.dma_start(out=gm[:], in_=gam_src), gmB, writes=[gmB])
        for j in range(T // W):
            ht, hB = hb.next()
            P.dma("pool", lambda e, ht=ht, j=j: e.dma_start(out=ht[:], in_=fm(src[:, j * W:(j + 1) * W])), hB,
                  writes=[hB])
            P.op("act", lambda e, ht=ht: e.activation(out=sq[:, 0:8, :], in_=ht[:, 0:8, :], func=AF.Square),
                 reads=[hB], writes=[sqA])
            P.op("dve", lambda e, ht=ht: e.tensor_tensor(out=sq[:, 8:16, :], in0=ht[:, 8:16, :], in1=ht[:, 8:16, :],
                                                         op=ALU.mult), reads=[hB], writes=[sqB])
            pt, pB = ps.next()

            def mm(e, pt=pt):
                for k in range(KC):
                    ins = e.matmul(pt[:], lhsT=g.onesf[:], rhs=sq[:, k, :], start=(k == 0), stop=(k == KC - 1))
                return ins
            P.op("pe", mm, reads=[sqA, sqB], writes=[pB])
            rt, rB = rs.next()
            P.op("act", lambda e, rt=rt, pt=pt: e.activation(out=rt[:], in_=pt[:], func=AF.Sqrt, bias=EPS,
                                                             scale=1.0 / D), reads=[pB], writes=[rB])
            st, sB = rstd.next()
            P.op("dve", lambda e, st=st, rt=rt: e.reciprocal(out=st[:], in_=rt[:]), reads=[rB], writes=[sB])
            if final_dst is None:
                def nrm(e, ht=ht, st=st, j=j):
                    for k in range(KC):
                        ins = e.scalar_tensor_tensor(out=g.hnT[:, k, j * W:(j + 1) * W], in0=ht[:, k, :],
                                                     scalar=gm[:, k:k + 1], in1=st[:], op0=ALU.mult, op1=ALU.mult)
                    return ins
                P.op("dve", nrm, reads=[hB, sB, gmB], writes=[Buf()])
            else:
                ot, oB = ob.next()

                def nrm(e, ht=ht, st=st, ot=ot):
                    for k in range(KC):
                        ins = e.scalar_tensor_tensor(out=ot[:, k, :], in0=ht[:, k, :],
                                                     scalar=gm[:, k:k + 1], in1=st[:], op0=ALU.mult, op1=ALU.mult)
                    return ins
                P.op("dve", nrm, reads=[hB, sB, gmB], writes=[oB])
                P.dma("sp", lambda e, ot=ot, j=j: e.dma_start(out=fm(final_dst[:, j * W:(j + 1) * W]), in_=ot[:]),
                      oB, reads=[oB])
        P.finish()


def slab_src(w2d, c0, cw):
    return w2d[:, c0:c0 + cw].rearrange("(k p) n -> p k n", p=128)


def ph_proj(g, l):
    nc, T = g.nc, g.T
    NQ, NT = T // 512, T // 128
    w = g.w_in[l]
    with ExitStack() as es:
        P = Prog(nc, es, uid("prj"))
        slabs = Rot(sbufs(es, nc, 3, [128, KC, 512], BF16, "wsl"))
        psr = Rot(psbufs(es, nc, 4, [128, 512], F32, "pp"))
        pxp = Rot(psbufs(es, nc, 2, [32, 512], F32, "px"))
        stg = Rot(sbufs(es, nc, 4, [128, 512], BF16, "stg"))
        xbr = Rot(sbufs(es, nc, 2, [128, 512], F32, "xb"))
        t1r = Rot(sbufs(es, nc, 2, [32, 512], F32, "t1"))
        t2r = Rot(sbufs(es, nc, 2, [32, 512], F32, "t2"))
        tmr = Rot(sbufs(es, nc, 2, [128, 512], F32, "tm"))
        cosT = sb(es, nc, [32, T], F32, "cos")
        sinT = sb(es, nc, [32, T], F32, "sin")
        bqk = sb(es, nc, [128, 48], F32, "bqk")
        brep = sb(es, nc, [128, 6144], F32, "brep")
        bif = sb(es, nc, [128, 16], F32, "bif")
        wif = sb(es, nc, [128, KC, 16], BF16, "wif")
        cB = Buf()
        for t, src in ((cosT, g.c_cos), (sinT, g.c_sin), (bqk, g.bqk[l]), (brep, g.btm[l].partition_broadcast(128)),
                       (bif, g.bif[l].partition_broadcast(128)), (wif, slab_src(w, O_MI, 16))):
            P.dma("pool", lambda e, t=t, src=src: e.dma_start(out=t[:], in_=src), cB, writes=[cB])
        jobs = []
        for s in range(2):
            jobs.append((O_MQ + s * 512, "fm", ("m", 0 + s * 4)))
        for s in range(2):
            jobs.append((O_MK + s * 512, "fm", ("m", 8 + s * 4)))
        for s in range(4):
            jobs.append((O_AQ + s * 512, "fm", ("aq", 16 + s * 4)))
        for s in range(4):
            jobs.append((O_AK + s * 512, "fm", ("ak", 32 + s * 4)))
        for s in range(4):
            jobs.append((O_MV + s * 512, "tm", ("mv", s)))
        for s in range(4):
            jobs.append((O_MO + s * 512, "tm", ("mo", s)))
        for s in range(4):
            jobs.append((O_AV + s * 512, "tm", ("av", s)))
        loaded = []

        def load(i):
            c0 = jobs[i][0]
            t, b = slabs.next()
            P.dma("pool", lambda e, t=t, c0=c0: e.dma_start(out=t[:], in_=slab_src(w, c0, 512)), b, writes=[b])
            loaded.append((t, b))
        load(0)
        load(1)
        for tt in range(NT):
            pt, pB = psr.next()

            def mm(e, pt=pt, tt=tt):
                for k in range(KC):
                    ins = e.matmul(pt[:, 0:16], lhsT=g.hnT[:, k, tt * 128:(tt + 1) * 128], rhs=wif[:, k, :],
                                   start=(k == 0), stop=(k == KC - 1))
                return ins
            P.op("pe", mm, reads=[cB], writes=[pB])
            P.op("dve", lambda e, pt=pt, tt=tt: e.tensor_tensor(out=g.IF[:, tt, :], in0=pt[:, 0:16], in1=bif[:],
                                                                op=ALU.add), reads=[pB, cB], writes=[Buf()])
        for i, (c0, kind, info) in enumerate(jobs):
            if i + 2 < len(jobs):
                load(i + 2)
            wt, wB = loaded[i]
            if kind == "fm":
                typ, ch0 = info
                for cc in range(4):
                    ch = ch0 + cc
                    for q in range(NQ):
                        pt, pB = psr.next()

                        def mm(e, pt=pt, wt=wt, cc=cc, q=q):
                            for k in range(KC):
                                ins = e.matmul(pt[:], lhsT=wt[:, k, cc * 128:(cc + 1) * 128],
                                               rhs=g.hnT[:, k, q * 512:(q + 1) * 512],
                                               start=(k == 0), stop=(k == KC - 1))
                            return ins
                        P.op("pe", mm, reads=[wB], writes=[pB])
                        st, sB = stg.next()
                        if typ == "m":
                            P.op("act", lambda e, st=st, pt=pt, ch=ch: e.activation(
                                out=st[:], in_=pt[:], func=AF.Identity, bias=bqk[:, ch:ch + 1]),
                                reads=[pB, cB], writes=[sB])
                        else:
                            xb, xB = xbr.next()
                            P.op("act", lambda e, xb=xb, pt=pt, ch=ch: e.activation(
                                out=xb[:], in_=pt[:], func=AF.Identity, bias=bqk[:, ch:ch + 1]),
                                reads=[pB, cB], writes=[xB])
                            px, pxB = pxp.next()
                            P.op("pe", lambda e, px=px, xb=xb: e.matmul(px[:], lhsT=g.pmf[:], rhs=xb[0:32, :],
                                                                        start=True, stop=True),
                                 reads=[xB], writes=[pxB])
                            t1, t1B = t1r.next()
                            t2, t2B = t2r.next()
                            P.op("dve", lambda e, t1=t1, px=px, q=q: e.tensor_tensor(
                                out=t1[:], in0=px[:], in1=sinT[:, q * 512:(q + 1) * 512], op=ALU.mult),
                                reads=[pxB, cB], writes=[t1B])
                            P.op("dve", lambda e, t2=t2, xb=xb, q=q: e.tensor_tensor(
                                out=t2[:], in0=xb[0:32, :], in1=cosT[:, q * 512:(q + 1) * 512], op=ALU.mult),
                                reads=[xB, cB], writes=[t2B])
                            P.op("dve", lambda e, t1=t1, t2=t2, xb=xb: e.tensor_tensor(
                                out=xb[0:32, :], in0=t1[:], in1=t2[:], op=ALU.add),
                                reads=[t1B, t2B], writes=[xB])
                            sc = (128.0 ** -0.5) if typ == "aq" else 1.0
                            P.op("act", lambda e, st=st, xb=xb, sc=sc: e.activation(
                                out=st[:], in_=xb[:], func=AF.Identity, scale=sc), reads=[xB], writes=[sB])
                        P.dma("sp", lambda e, st=st, ch=ch, q=q: e.dma_start(
                            out=g.QKs[ch * 128:(ch + 1) * 128, q * 512:(q + 1) * 512], in_=st[:]), sB, reads=[sB])
            else:
                typ, s = info
                dst = {"mv": g.MV, "mo": g.MO, "av": g.AV}[typ]
                boff = {"mv": 0, "mo": 2048, "av": 4096}[typ] + s * 512
                for tt in range(NT):
                    pt, pB = psr.next()

                    def mm(e, pt=pt, wt=wt, tt=tt):
                        for k in range(KC):
                            ins = e.matmul(pt[:], lhsT=g.hnT[:, k, tt * 128:(tt + 1) * 128], rhs=wt[:, k, :],
                                           start=(k == 0), stop=(k == KC - 1))
                        return ins
                    P.op("pe", mm, reads=[wB], writes=[pB])
                    st, sB = stg.next()
                    if typ == "mo":
                        tm, tB = tmr.next()
                        P.op("dve", lambda e, tm=tm, pt=pt, boff=boff: e.tensor_tensor(
                            out=tm[:], in0=pt[:], in1=brep[:, boff:boff + 512], op=ALU.add),
                            reads=[pB, cB], writes=[tB])
                        P.op("act", lambda e, st=st, tm=tm: e.activation(out=st[:], in_=tm[:], func=AF.Sigmoid),
                             reads=[tB], writes=[sB])
                    else:
                        P.op("dve", lambda e, st=st, pt=pt, boff=boff: e.tensor_tensor(
                            out=st[:], in0=pt[:], in1=brep[:, boff:boff + 512], op=ALU.add),
                            reads=[pB, cB], writes=[sB])
                    P.dma("sp", lambda e, st=st, dst=dst, tt=tt, s=s: e.dma_start(
                        out=dst[tt * 128:(tt + 1) * 128, s * 512:(s + 1) * 512], in_=st[:]), sB, reads=[sB])
        P.finish()


def tm_src(ap2d, h, NT):
    return ap2d[:, h * 256:(h + 1) * 256].rearrange("(t p) d -> p t d", p=128)


def ph_mlstm(g, l):
    nc, T = g.nc, g.T
    NT = T // 128
    with ExitStack() as es:
        P = Prog(nc, es, uid("mls"))
        NG = NT * 8
        def v3(ap):
            return ap.rearrange("p (c h) -> p c h", h=8)
        e1 = sb(es, nc, [128, NG], F32, "e1")
        l1 = sb(es, nc, [128, NG], F32, "l1")
        tg = sb(es, nc, [128, NG], F32, "tg")
        u = sb(es, nc, [128, NG], F32, "u")
        bnd = sb(es, nc, [128, NG], F32, "bnd")
        eg = sb(es, nc, [128, NG], F32, "eg")
        mrep = sb(es, nc, [128, 2048], F32, "mrep")
        cgp = pst(es, nc, [128, 2, NG], F32, "cgp")
        gB = Buf()
        P.dma("pool", lambda e: e.dma_start(out=mrep[:], in_=g.mnorm[l].partition_broadcast(128)), gB,
              writes=[gB])
        P.op("act", lambda e: e.activation(out=v3(e1[:]), in_=g.IF[:, :, 8:16], func=AF.Exp, scale=-1.0),
             writes=[gB])
        P.op("act", lambda e: e.activation(out=l1[:], in_=e1[:], func=AF.Ln, bias=1.0), reads=[gB], writes=[gB])

        def mmg(e):
            e.matmul(cgp[:, 0, :], lhsT=g.trif[:], rhs=l1[:], start=True, stop=True)
            return e.matmul(cgp[:, 1, :], lhsT=g.onesf[:], rhs=l1[:], start=True, stop=True)
        P.op("pe", mmg, reads=[gB], writes=[gB])
        P.op("dve", lambda e: e.tensor_tensor(out=v3(tg[:]), in0=v3(cgp[:, 0, :]), in1=g.IF[:, :, 0:8], op=ALU.add),
             reads=[gB], writes=[gB])

        def gex(e):
            e.activation(out=u[:], in_=tg[:], func=AF.Exp, bias=math.log(128.0 ** -0.5))
            e.activation(out=bnd[:], in_=cgp[:, 0, :], func=AF.Exp)
            return e.activation(out=eg[:], in_=cgp[:, 1, :], func=AF.Exp, scale=-1.0)
        P.op("act", gex, reads=[gB], writes=[gB])

        qkr = Rot([(sb(es, nc, [128, T], BF16, "qT"), sb(es, nc, [128, T], BF16, "kT"), Buf(), Buf())
                   for _ in range(2)])
        var = [(sb(es, nc, [128, NT, 257], BF16, "vA"), Buf()) for _ in range(2)]
        gr = Rot(sbufs(es, nc, 2, [128, NT, 256], BF16, "Gg"))
        hms = Rot(sbufs(es, nc, 2, [128, 2, T], BF16, "hms"))
        C = sb(es, nc, [128, 257], F32, "C")
        C1 = sb(es, nc, [128, 257], F32, "C1")
        Cb = sb(es, nc, [128, 257], BF16, "Cb")
        CB, C1B, CbB = Buf(), Buf(), Buf()
        ptr = Rot(sbufs(es, nc, 2, [128, 128], BF16, "PT"))
        kur = Rot(sbufs(es, nc, 2, [128, 128], BF16, "ku"))
        smr = Rot(sbufs(es, nc, 2, [128, 8], F32, "sm"))
        jkr = Rot(sbufs(es, nc, 2, [128, 256], F32, "jk"))
        hmr = Rot(sbufs(es, nc, 2, [128, 256], BF16, "hm"))
        stp = Rot(psbufs(es, nc, 2, [128, 128], F32, "stp"))
        ktp = Rot(psbufs(es, nc, 1, [128, 128], BF16, "ktp"))
        outp = Rot(psbufs(es, nc, 2, [128, 257], F32, "outp"))
        updp = Rot(psbufs(es, nc, 1, [128, 257], F32, "updp"))
        trp = Rot(psbufs(es, nc, 1, [128, 2, 128], BF16, "trp"))
        for vt, vB in var:
            P.op("dve", lambda e, vt=vt: e.memset(vt[:, :, 256:257], 1.0), writes=[vB])
        vr = Rot(var)
        bg = []
        eps = {}

        def advance():
            for gen in list(bg):
                try:
                    next(gen)
                except StopIteration:
                    bg.remove(gen)

        for h in range(H):
            eps.clear()
            qT, kT, qB, kB = qkr.next()
            qkB = qB
            vA, vB = vr.next()
            Gt, GB = gr.next()
            hs, hsB = hms.next()
            P.dma("pool", lambda e, qT=qT, h=h: e.dma_start(out=qT[:], in_=g.QKs[h * 128:(h + 1) * 128, :]), qB,
                  writes=[qB])
            P.dma("pool", lambda e, kT=kT, h=h: e.dma_start(out=kT[:], in_=g.QKs[(8 + h) * 128:(9 + h) * 128, :]),
                  kB, writes=[kB])
            P.dma("pool", lambda e, vA=vA, h=h: e.dma_start(out=vA[:, :, 0:256], in_=tm_src(g.MV, h, NT)), vB,
                  writes=[vB])
            P.dma("pool", lambda e, Gt=Gt, h=h: e.dma_start(out=Gt[:], in_=tm_src(g.MO, h, NT)), GB,
                  writes=[GB])

            def gmul(e, Gt=Gt, h=h):
                for c in range(NT):
                    ins = e.tensor_tensor(out=Gt[:, c, :], in0=Gt[:, c, :], in1=mrep[:, h * 256:(h + 1) * 256],
                                          op=ALU.mult)
                return ins
            P.op("dve", gmul, reads=[gB], writes=[GB])
            for c in range(NT):
                cs = slice(c * 128, (c + 1) * 128)
                uc = u[:, c * 8 + h:c * 8 + h + 1]
                st, stB = stp.next()
                P.op("pe", lambda e, st=st, kT=kT, qT=qT, cs=cs: e.matmul(st[:], lhsT=kT[:, cs], rhs=qT[:, cs],
                                                                          start=True, stop=True),
                     reads=[qB, kB], writes=[stB])
                advance()
                pt, ptB = ptr.next()
                P.op("dve", lambda e, pt=pt, st=st, uc=uc: e.scalar_tensor_tensor(
                    out=pt[:], in0=st[:], scalar=uc, in1=g.trif[:], op0=ALU.mult, op1=ALU.mult),
                    reads=[stB, gB], writes=[ptB])
                kt, ktB = ktp.next()
                P.op("pe", lambda e, kt=kt, kT=kT, cs=cs: e.transpose(kt[:], kT[:, cs], g.identb[:]),
                     reads=[kB], writes=[ktB])
                advance()
                ku, kuB = kur.next()
                P.op("act", lambda e, ku=ku, kt=kt, uc=uc: e.activation(out=ku[:], in_=kt[:], func=AF.Identity,
                                                                        scale=uc),
                     reads=[ktB, gB], writes=[kuB])
                while eps.get(c - 2) in bg:
                    advance()
                ot, oB = outp.next()

                def mmo(e, ot=ot, pt=pt, vA=vA, qT=qT, c=c, cs=cs):
                    ins = e.matmul(ot[:], lhsT=pt[:], rhs=vA[:, c, :], start=True, stop=(c == 0))
                    if c > 0:
                        ins = e.matmul(ot[:], lhsT=qT[:, cs], rhs=Cb[:], start=False, stop=True)
                    return ins
                P.op("pe", mmo, reads=[ptB, vB, qB] + ([CbB] if c > 0 else []), writes=[oB])
                advance()
                if c < NT - 1:
                    up, upB = updp.next()
                    P.op("pe", lambda e, up=up, ku=ku, vA=vA, c=c: e.matmul(up[:], lhsT=ku[:], rhs=vA[:, c, :],
                                                                            start=True, stop=True),
                         reads=[kuB, vB], writes=[upB])
                    egc = eg[:, c * 8 + h:c * 8 + h + 1]
                    if c == 0:
                        P.op("dve", lambda e, up=up, egc=egc: e.tensor_scalar(
                            out=C[:], in0=up[:], scalar1=egc, scalar2=None, op0=ALU.mult),
                            reads=[upB, gB], writes=[CB])
                    else:
                        P.op("act", lambda e, egc=egc: e.activation(out=C1[:], in_=C[:], func=AF.Identity,
                                                                    scale=egc),
                             reads=[CB, gB], writes=[C1B])
                        P.op("dve", lambda e, up=up, egc=egc: e.scalar_tensor_tensor(
                            out=C[:], in0=up[:], scalar=egc, in1=C1[:], op0=ALU.mult, op1=ALU.add),
                            reads=[upB, C1B, gB], writes=[CB])
                    P.op("act", lambda e: e.activation(out=Cb[:], in_=C[:], func=AF.Identity),
                         reads=[CB], writes=[CbB])
                def epilogue(c=c, cs=cs, ot=ot, oB=oB):
                    sm, smB = smr.next()
                    P.op("act", lambda e, sm=sm, ot=ot: e.activation(out=sm[:, 0:1], in_=ot[:, 256:257], func=AF.Abs),
                         reads=[oB], writes=[smB])
                    yield
                    P.op("dve", lambda e, sm=sm, c=c, h=h: e.tensor_scalar(
                        out=sm[:, 1:2], in0=sm[:, 0:1], scalar1=bnd[:, c * 8 + h:c * 8 + h + 1], scalar2=None, op0=ALU.max),
                        reads=[smB, gB], writes=[smB])
                    yield
                    P.op("dve", lambda e, sm=sm: e.reciprocal(out=sm[:, 2:3], in_=sm[:, 1:2]),
                         reads=[smB], writes=[smB])
                    yield
                    jk, jkB = jkr.next()
                    P.op("act", lambda e, jk=jk, ot=ot, sm=sm: e.activation(
                        out=jk[:], in_=ot[:, 0:256], func=AF.Square, scale=sm[:, 2:3], accum_out=sm[:, 3:4]),
                        reads=[oB, smB], writes=[jkB, smB])
                    yield
                    P.op("act", lambda e, sm=sm: e.activation(out=sm[:, 4:5], in_=sm[:, 3:4], func=AF.Ln, bias=EPS,
                                                              scale=1.0 / 256), reads=[smB], writes=[smB])
                    yield
                    P.op("act", lambda e, sm=sm: e.activation(out=sm[:, 5:6], in_=sm[:, 4:5], func=AF.Exp,
                                                              scale=-0.5), reads=[smB], writes=[smB])
                    yield
                    P.op("dve", lambda e, sm=sm: e.tensor_tensor(out=sm[:, 6:7], in0=sm[:, 5:6], in1=sm[:, 2:3],
                                                                 op=ALU.mult), reads=[smB], writes=[smB])
                    yield
                    hm, hmB = hmr.next()
                    P.op("dve", lambda e, hm=hm, ot=ot, sm=sm, Gt=Gt, c=c: e.scalar_tensor_tensor(
                        out=hm[:], in0=ot[:, 0:256], scalar=sm[:, 6:7], in1=Gt[:, c, :], op0=ALU.mult, op1=ALU.mult),
                        reads=[oB, smB, GB], writes=[hmB])
                    yield
                    tr, trB = trp.next()

                    def trs(e, tr=tr, hm=hm):
                        e.transpose(tr[:, 0, :], hm[:, 0:128], g.identb[:])
                        return e.transpose(tr[:, 1, :], hm[:, 128:256], g.identb[:])
                    P.op("pe", trs, reads=[hmB], writes=[trB])
                    yield
                    P.op("act", lambda e, hs=hs, tr=tr, cs=cs: e.activation(out=hs[:, :, cs], in_=tr[:],
                                                                            func=AF.Identity),
                         reads=[trB], writes=[hsB])

                gen_ = epilogue()
                eps[c] = gen_
                bg.append(gen_)

            while bg:
                advance()
            P.dma("sp", lambda e, hs=hs, h=h: e.dma_start(
                out=g.HMT[h * 256:(h + 1) * 256, :].rearrange("(j p) t -> p j t", p=128), in_=hs[:]), hsB,
                reads=[hsB])
        P.finish()


def ph_attn(g, l):
    nc, T = g.nc, g.T
    NT, NG = T // 128, T // 256
    lam_init = 0.8 - 0.6 * math.exp(-0.3 * l)
    with ExitStack() as es:
        P = Prog(nc, es, uid("att"))
        lq = sb(es, nc, [128, 4, 128], F32, "lq")
        lp = sb(es, nc, [128, 2, 128], F32, "lp")
        ls = sb(es, nc, [128, 8], F32, "ls")
        anrep = sb(es, nc, [128, 2048], F32, "anrep")
        cB = Buf()
        P.dma("pool", lambda e: e.dma_start(out=lq[:].rearrange("p a b -> p (a b)"),
                                            in_=g.lamqk[l].partition_broadcast(128)), cB, writes=[cB])
        P.dma("pool", lambda e: e.dma_start(out=anrep[:], in_=g.anorm[l].partition_broadcast(128)), cB,
              writes=[cB])

        def lam1(e):
            e.tensor_tensor(out=lp[:, 0, :], in0=lq[:, 0, :], in1=lq[:, 1, :], op=ALU.mult)
            return e.tensor_tensor(out=lp[:, 1, :], in0=lq[:, 2, :], in1=lq[:, 3, :], op=ALU.mult)
        P.op("dve", lam1, reads=[cB], writes=[cB])

        def lam2(e):
            e.reduce_sum(out=ls[:, 0:1], in_=lp[:, 0, :], axis=mybir.AxisListType.X)
            return e.reduce_sum(out=ls[:, 1:2], in_=lp[:, 1, :], axis=mybir.AxisListType.X)
        P.op("dve", lam2, reads=[cB], writes=[cB])
        P.op("act", lambda e: e.activation(out=ls[:, 2:4], in_=ls[:, 0:2], func=AF.Exp), reads=[cB], writes=[cB])
        P.op("dve", lambda e: e.scalar_tensor_tensor(out=ls[:, 4:5], in0=ls[:, 3:4], scalar=-lam_init,
                                                     in1=ls[:, 2:3], op0=ALU.add, op1=ALU.subtract),
             reads=[cB], writes=[cB])
        P.op("dve", lambda e: e.tensor_scalar(out=anrep[:], in0=anrep[:], scalar1=(1.0 - lam_init), scalar2=None,
                                              op0=ALU.mult), reads=[cB], writes=[cB])
        nlam = ls[:, 4:5]

        inb = []
        for i in range(2):
            vt = sb(es, nc, [128, NT, 257], BF16, "vA")
            vB = Buf()
            P.op("dve", lambda e, vt=vt: e.memset(vt[:, :, 256:257], 1.0), writes=[vB])
            inb.append(dict(q2=sb(es, nc, [128, 2, T], BF16, "q2"), k2=sb(es, nc, [128, 2, T], BF16, "k2"),
                            qB=Buf(), kB=Buf(), vA=vt, vB=vB, hs=sb(es, nc, [128, 2, T], BF16, "has"), hsB=Buf()))
        inr = Rot(inb)
        ptr = Rot(sbufs(es, nc, 4, [128, 2, 256], BF16, "PT"))
        rrr = Rot(sbufs(es, nc, 5, [128, 8], F32, "rr"))
        c0r = Rot(sbufs(es, nc, 5, [128, 257], F32, "c0"))
        c1r = Rot(sbufs(es, nc, 5, [128, 257], F32, "c1"))
        a0r = Rot(sbufs(es, nc, 5, [128, 256], F32, "a0"))
        a1r = Rot(sbufs(es, nc, 5, [128, 256], F32, "a1"))
        jkr = Rot(sbufs(es, nc, 5, [128, 256], F32, "jk"))
        habr = Rot(sbufs(es, nc, 5, [128, 256], BF16, "hab"))
        stp = Rot(psbufs(es, nc, 3, [128, 2, 256], F32, "stp"))
        outp = [[(pst(es, nc, [128, 257], F32, "outp"), Buf()) for j in range(2)] for m in range(2)]
        trp = Rot(psbufs(es, nc, 1, [128, 2, 128], BF16, "trp"))

        hb = {}

        def ensure_head(h):
            if h in hb or h >= H:
                return
            b = inr.next()
            c0 = (16 + 2 * h) * 128
            c1 = (32 + 2 * h) * 128
            P.dma("pool", lambda e: e.dma_start(
                out=b["q2"][:], in_=g.QKs[c0:c0 + 256, :].rearrange("(c p) t -> p c t", p=128)), b["qB"],
                writes=[b["qB"]])
            P.dma("pool", lambda e: e.dma_start(
                out=b["k2"][:], in_=g.QKs[c1:c1 + 256, :].rearrange("(c p) t -> p c t", p=128)), b["kB"],
                writes=[b["kB"]])
            P.dma("pool", lambda e: e.dma_start(out=b["vA"][:, :, 0:256], in_=tm_src(g.AV, h, NT)), b["vB"],
                  writes=[b["vB"]])
            hb[h] = b

        steps = [(h, gq, kt) for h in range(H) for gq in range(NG) for kt in range(2 * gq + 2)]
        LA = 2
        stq = {}

        def rec_qk(i):
            h, gq, kt = steps[i]
            ensure_head(h)
            b = hb[h]
            q2, k2 = b["q2"], b["k2"]
            qs = slice(gq * 256, (gq + 1) * 256)
            ks = slice(kt * 128, (kt + 1) * 128)
            st, stB = stp.next()

            def mmq(e):
                e.matmul(st[:, 0, :], lhsT=k2[:, 0, ks], rhs=q2[:, 0, qs], start=True, stop=True)
                return e.matmul(st[:, 1, :], lhsT=k2[:, 1, ks], rhs=q2[:, 1, qs], start=True, stop=True)
            P.op("pe", mmq, reads=[b["qB"], b["kB"]], writes=[stB])
            stq[i] = (st, stB)

        def epilogue(h, gq, j, b):
            (o0, o0B), (o1, o1B) = outp[0][j], outp[1][j]
            hs, hsB = b["hs"], b["hsB"]
            ts = slice((gq * 2 + j) * 128, (gq * 2 + j + 1) * 128)
            c0, c0B = c0r.next()
            c1, c1B = c1r.next()
            P.op("act", lambda e: e.activation(out=c0[:], in_=o0[:], func=AF.Identity), reads=[o0B], writes=[c0B])
            P.op("dve", lambda e: e.tensor_copy(out=c1[:], in_=o1[:]), reads=[o1B], writes=[c1B])
            yield
            rr, rB = rrr.next()

            def rcp(e):
                e.reciprocal(out=rr[:, 0:1], in_=c0[:, 256:257])
                return e.reciprocal(out=rr[:, 1:2], in_=c1[:, 256:257])
            P.op("dve", rcp, reads=[c0B, c1B], writes=[rB])
            P.op("dve", lambda e: e.tensor_tensor(out=rr[:, 2:3], in0=rr[:, 1:2], in1=nlam, op=ALU.mult),
                 reads=[rB, cB], writes=[rB])
            yield
            a0, a0B = a0r.next()
            P.op("act", lambda e: e.activation(out=a0[:], in_=c0[:, 0:256], func=AF.Identity, scale=rr[:, 0:1]),
                 reads=[c0B, rB], writes=[a0B])
            yield
            a1, a1B = a1r.next()
            P.op("dve", lambda e: e.scalar_tensor_tensor(out=a1[:], in0=c1[:, 0:256], scalar=rr[:, 2:3], in1=a0[:],
                                                         op0=ALU.mult, op1=ALU.add),
                 reads=[c1B, rB, a0B], writes=[a1B])
            yield
            jk, jkB = jkr.next()
            P.op("act", lambda e: e.activation(out=jk[:], in_=a1[:], func=AF.Square, accum_out=rr[:, 3:4]),
                 reads=[a1B, rB], writes=[jkB, rB])
            yield
            P.op("act", lambda e: e.activation(out=rr[:, 4:5], in_=rr[:, 3:4], func=AF.Ln, bias=EPS,
                                               scale=1.0 / 256), reads=[rB], writes=[rB])
            yield
            P.op("act", lambda e: e.activation(out=rr[:, 5:6], in_=rr[:, 4:5], func=AF.Exp, scale=-0.5),
                 reads=[rB], writes=[rB])
            yield
            hab, habB = habr.next()
            P.op("dve", lambda e: e.scalar_tensor_tensor(out=hab[:], in0=a1[:], scalar=rr[:, 5:6],
                                                         in1=anrep[:, h * 256:(h + 1) * 256], op0=ALU.mult,
                                                         op1=ALU.mult), reads=[a1B, rB, cB], writes=[habB])
            yield
            tr, trB = trp.next()

            def trs(e):
                e.transpose(tr[:, 0, :], hab[:, 0:128], g.identb[:])
                return e.transpose(tr[:, 1, :], hab[:, 128:256], g.identb[:])
            P.op("pe", trs, reads=[habB], writes=[trB])
            yield
            P.op("act", lambda e: e.activation(out=hs[:, :, ts], in_=tr[:], func=AF.Identity),
                 reads=[trB], writes=[hsB])

        bg = []

        def advance():
            for gen in list(bg):
                try:
                    next(gen)
                except StopIteration:
                    bg.remove(gen)

        for i in range(min(LA, len(steps))):
            rec_qk(i)
        for i, (h, gq, kt) in enumerate(steps):
            if gq == 0 and kt == 0:
                ensure_head(h + 1)
            if i + LA < len(steps):
                rec_qk(i + LA)
            b = hb[h]
            vA, vB = b["vA"], b["vB"]
            st, stB = stq.pop(i)
            pt, ptB = ptr.next()
            P.op("act", lambda e, pt=pt, st=st: e.activation(out=pt[:], in_=st[:], func=AF.Exp),
                 reads=[stB], writes=[ptB])
            if kt >= 2 * gq:
                j0 = kt - 2 * gq

                def msk(e, pt=pt, j0=j0):
                    e.tensor_tensor(out=pt[:, 0, j0 * 128:(j0 + 1) * 128], in0=pt[:, 0, j0 * 128:(j0 + 1) * 128],
                                    in1=g.trib[:], op=ALU.mult)
                    return e.tensor_tensor(out=pt[:, 1, j0 * 128:(j0 + 1) * 128],
                                           in0=pt[:, 1, j0 * 128:(j0 + 1) * 128], in1=g.trib[:], op=ALU.mult)
                P.op("dve", msk, reads=[ptB], writes=[ptB])
            js = [j for j in range(2) if kt <= 2 * gq + j]

            def mmv(e, pt=pt, vA=vA, kt=kt, gq=gq, js=js):
                for j in js:
                    for m in range(2):
                        ins = e.matmul(outp[m][j][0][:], lhsT=pt[:, m, j * 128:(j + 1) * 128], rhs=vA[:, kt, :],
                                       start=(kt == 0), stop=(kt == 2 * gq + j), skip_group_check=True)
                return ins
            P.op("pe", mmv, reads=[ptB, vB], writes=[outp[m][j][1] for j in js for m in range(2)])
            advance()
            if kt == 2 * gq:
                gen = epilogue(h, gq, 0, b)
                next(gen)
                bg.append(gen)
            if kt == 2 * gq + 1:
                gen = epilogue(h, gq, 1, b)
                next(gen)
                bg.append(gen)
                if gq == NG - 1:
                    while bg:
                        advance()
                    P.dma("sp", lambda e, b=b, h=h: e.dma_start(
                        out=g.HAT[h * 256:(h + 1) * 256, :].rearrange("(j p) t -> p j t", p=128), in_=b["hs"][:]),
                        b["hsB"], reads=[b["hsB"]])
                    del hb[h]
        P.finish()


def ph_merge(g, l, which):
    nc, T = g.nc, g.T
    NQ = T // 512
    wa = (g.w_bm if which == 0 else g.w_bd)[l]
    wg = g.w_in[l]
    goff = O_GM if which == 0 else O_GD
    src = g.HMT if which == 0 else g.HAT
    with ExitStack() as es:
        P = Prog(nc, es, uid("mrg"))
        xT = sb(es, nc, [128, KC, T], BF16, "xT")
        xB = Buf()
        bg = sb(es, nc, [128, 32], F32, "bg")
        bgB = Buf()
        P.dma("pool", lambda e: e.dma_start(out=bg[:], in_=g.bgate[l]), bgB, writes=[bgB])
        P.dma("pool", lambda e: e.dma_start(out=xT[:], in_=fm(src)), xB, writes=[xB])
        SW = 256
        NS = 2048 // SW
        CPS = SW // 128
        slabs = Rot(sbufs(es, nc, 4, [128, KC, SW], BF16, "wsl"))
        psy = Rot(psbufs(es, nc, 3, [128, 512], F32, "py"))
        psg = Rot(psbufs(es, nc, 3, [128, 512], F32, "pg"))
        sgr = Rot(sbufs(es, nc, 2, [128, 512], F32, "sg"))
        y1r = Rot(sbufs(es, nc, 3, [128, 512], F32, "y1"))
        if which == 1:
            tr_ = Rot(sbufs(es, nc, 2, [128, 512], F32, "tt"))
            yor = Rot(sbufs(es, nc, 3, [128, 512], BF16, "yo"))
        loaded = []

        def load(s):
            ta, ba = slabs.next()
            P.dma("pool", lambda e, ta=ta, s=s: e.dma_start(out=ta[:], in_=slab_src(wa, s * SW, SW)), ba,
                  writes=[ba])
            tg, bgs = slabs.next()
            P.dma("pool", lambda e, tg=tg, s=s: e.dma_start(out=tg[:], in_=slab_src(wg, goff + s * SW, SW)), bgs,
                  writes=[bgs])
            loaded.append((ta, ba, tg, bgs))
        load(0)
        for s in range(NS):
            if s + 1 < NS:
                load(s + 1)
            ta, ba, tg, bgs = loaded[s]
            for cc in range(CPS):
                ch = s * CPS + cc
                for q in range(NQ):
                    qs = slice(q * 512, (q + 1) * 512)
                    py, pyB = psy.next()
                    pg, pgB = psg.next()

                    def mm(e, py=py, pg=pg, ta=ta, tg=tg, cc=cc, qs=qs):
                        for k in range(KC):
                            e.matmul(py[:], lhsT=ta[:, k, cc * 128:(cc + 1) * 128], rhs=xT[:, k, qs],
                                     start=(k == 0), stop=(k == KC - 1))
                        for k in range(KC):
                            ins = e.matmul(pg[:], lhsT=tg[:, k, cc * 128:(cc + 1) * 128], rhs=g.hnT[:, k, qs],
                                           start=(k == 0), stop=(k == KC - 1))
                        return ins
                    P.op("pe", mm, reads=[ba, bgs, xB], writes=[pyB, pgB])
                    sg, sgB = sgr.next()
                    bcol = which * 16 + ch
                    P.op("act", lambda e, sg=sg, pg=pg, bcol=bcol: e.activation(
                        out=sg[:], in_=pg[:], func=AF.Sigmoid, bias=bg[:, bcol:bcol + 1]),
                        reads=[pgB, bgB], writes=[sgB])
                    if which == 0:
                        y1, y1B = y1r.next()
                        P.op("dve", lambda e, y1=y1, py=py, sg=sg: e.tensor_tensor(out=y1[:], in0=py[:], in1=sg[:],
                                                                                   op=ALU.mult),
                             reads=[pyB, sgB], writes=[y1B])
                        P.dma("sp", lambda e, y1=y1, ch=ch, qs=qs: e.dma_start(
                            out=g.Y1[ch * 128:(ch + 1) * 128, qs], in_=y1[:]), y1B, reads=[y1B])
                    else:
                        y1, y1B = y1r.next()
                        P.dma("pool", lambda e, y1=y1, ch=ch, qs=qs: e.dma_start(
                            out=y1[:], in_=g.Y1[ch * 128:(ch + 1) * 128, qs]), y1B, writes=[y1B])
                        tt, ttB = tr_.next()
                        P.op("dve", lambda e, tt=tt, py=py, sg=sg: e.tensor_tensor(out=tt[:], in0=py[:], in1=sg[:],
                                                                                   op=ALU.mult),
                             reads=[pyB, sgB], writes=[ttB])
                        yo, yoB = yor.next()
                        P.op("dve", lambda e, yo=yo, tt=tt, y1=y1: e.tensor_tensor(out=yo[:], in0=tt[:], in1=y1[:],
                                                                                   op=ALU.add),
                             reads=[ttB, y1B], writes=[yoB])
                        P.dma("sp", lambda e, yo=yo, ch=ch, qs=qs: e.dma_start(
                            out=g.YT[ch * 128:(ch + 1) * 128, qs], in_=yo[:]), yoB, reads=[yoB])
        P.finish()


def ph_dump_hn(g, dst):
    nc = g.nc
    with ExitStack() as es:
        P = Prog(nc, es, uid("dmp"))
        b = Buf()
        for k in range(KC):
            P.dma("sp", lambda e, k=k: e.dma_start(out=dst[k * 128:(k + 1) * 128, :], in_=g.hnT[:, k, :]), b,
                  reads=[b])
        P.finish()


def ph_oproj(g, l, hsrc):
    nc, T = g.nc, g.T
    NQ = T // 512
    wo = g.w_o[l]
    with ExitStack() as es:
        P = Prog(nc, es, uid("opr"))
        xT = sb(es, nc, [128, KC, T], BF16, "xT")
        xB = Buf()
        P.dma("pool", lambda e: e.dma_start(out=xT[:], in_=fm(g.YT)), xB, writes=[xB])
        slabs = Rot(sbufs(es, nc, 3, [128, KC, 512], BF16, "wsl"))
        psr = Rot(psbufs(es, nc, 4, [128, 512], F32, "pp"))
        hir = Rot(sbufs(es, nc, 3, [128, 512], F32, "hi"))
        hor = Rot(sbufs(es, nc, 3, [128, 512], F32, "ho"))
        loaded = []

        def load(s):
            t, b = slabs.next()
            P.dma("pool", lambda e, t=t, s=s: e.dma_start(out=t[:], in_=slab_src(wo, s * 512, 512)), b, writes=[b])
            loaded.append((t, b))
        load(0)
        load(1)
        for s in range(4):
            if s + 2 < 4:
                load(s + 2)
            wt, wB = loaded[s]
            for cc in range(4):
                ch = s * 4 + cc
                for q in range(NQ):
                    qs = slice(q * 512, (q + 1) * 512)
                    hi, hiB = hir.next()
                    P.dma("pool", lambda e, hi=hi, ch=ch, qs=qs: e.dma_start(
                        out=hi[:], in_=hsrc[ch * 128:(ch + 1) * 128, qs]), hiB, writes=[hiB])
                    pt, pB = psr.next()

                    def mm(e, pt=pt, wt=wt, cc=cc, qs=qs):
                        for k in range(KC):
                            ins = e.matmul(pt[:], lhsT=wt[:, k, cc * 128:(cc + 1) * 128], rhs=xT[:, k, qs],
                                           start=(k == 0), stop=(k == KC - 1))
                        return ins
                    P.op("pe", mm, reads=[wB, xB], writes=[pB])
                    ho, hoB = hor.next()
                    P.op("dve", lambda e, ho=ho, pt=pt, hi=hi: e.tensor_tensor(out=ho[:], in0=pt[:], in1=hi[:],
                                                                               op=ALU.add),
                         reads=[pB, hiB], writes=[hoB])
                    P.dma("sp", lambda e, ho=ho, ch=ch, qs=qs: e.dma_start(
                        out=g.hT[ch * 128:(ch + 1) * 128, qs], in_=ho[:]), hoB, reads=[hoB])
                    if False:
                        P.dma("sp", lambda e, ho=ho, ch=ch, qs=qs: e.dma_start(
                            out=g.H1[ch * 128:(ch + 1) * 128, qs], in_=ho[:]), hoB, reads=[hoB])
        P.finish()


def ph_up(g, l):
    nc, T = g.nc, g.T
    NQ = T // 512
    wu = g.w_up[l]
    with ExitStack() as es:
        P = Prog(nc, es, uid("up"))
        cw = sb(es, nc, [128, FC, 3], F32, "cw")
        cb = sb(es, nc, [128, FC], F32, "cb")
        cB = Buf()
        P.dma("pool", lambda e: e.dma_start(out=cw[:], in_=g.cw[l]), cB, writes=[cB])
        P.dma("pool", lambda e: e.dma_start(out=cb[:], in_=g.cb[l]), cB, writes=[cB])
        slabs = Rot(sbufs(es, nc, 4, [128, KC, 512], BF16, "wsl"))
        psr = Rot(psbufs(es, nc, 6, [128, 512], F32, "pp"))
        gts = [(sb(es, nc, [128, T + 2], F32, "gt"), Buf()) for _ in range(2)]
        vsr = Rot(sbufs(es, nc, 2, [128, T], F32, "vs"))
        cvr = Rot(sbufs(es, nc, 2, [128, T], F32, "cv"))
        uor = Rot(sbufs(es, nc, 2, [128, T], BF16, "uo"))
        for gt, gB in gts:
            P.op("dve", lambda e, gt=gt: e.memset(gt[:, 0:2], 0.0), writes=[gB])
        gtr = Rot(gts)
        loaded = []

        def load(s):
            ta, ba = slabs.next()
            P.dma("pool", lambda e, ta=ta, s=s: e.dma_start(out=ta[:], in_=slab_src(wu, s * 512, 512)), ba,
                  writes=[ba])
            tv, bv = slabs.next()
            P.dma("pool", lambda e, tv=tv, s=s: e.dma_start(out=tv[:], in_=slab_src(wu, DFF + s * 512, 512)), bv,
                  writes=[bv])
            loaded.append((ta, ba, tv, bv))
        load(0)
        for s in range(11):
            if s + 1 < 11:
                load(s + 1)
            ta, ba, tv, bv = loaded[s]
            for cc in range(4):
                j = s * 4 + cc
                gt, gB = gtr.next()
                vs, vB = vsr.next()
                for q in range(NQ):
                    qs = slice(q * 512, (q + 1) * 512)
                    pa, paB = psr.next()
                    pv, pvB = psr.next()

                    def mm(e, pa=pa, pv=pv, ta=ta, tv=tv, cc=cc, qs=qs):
                        for k in range(KC):
                            e.matmul(pa[:], lhsT=ta[:, k, cc * 128:(cc + 1) * 128], rhs=g.hnT[:, k, qs],
                                     start=(k == 0), stop=(k == KC - 1))
                        for k in range(KC):
                            ins = e.matmul(pv[:], lhsT=tv[:, k, cc * 128:(cc + 1) * 128], rhs=g.hnT[:, k, qs],
                                           start=(k == 0), stop=(k == KC - 1))
                        return ins
                    P.op("pe", mm, reads=[ba, bv], writes=[paB, pvB])
                    P.op("act", lambda e, gt=gt, pa=pa, q=q: e.activation(
                        out=gt[:, 2 + q * 512:2 + (q + 1) * 512], in_=pa[:], func=AF.Identity),
                        reads=[paB], writes=[gB])
                    P.op("act", lambda e, vs=vs, pv=pv, qs=qs: e.activation(out=vs[:, qs], in_=pv[:],
                                                                            func=AF.Identity),
                         reads=[pvB], writes=[vB])
                cv, cvB = cvr.next()

                P.op("dve", lambda e, cv=cv, gt=gt, j=j: e.tensor_scalar(
                    out=cv[:], in0=gt[:, 0:T], scalar1=cw[:, j, 0:1], scalar2=cb[:, j:j + 1], op0=ALU.mult,
                    op1=ALU.add), reads=[gB, cB], writes=[cvB])
                P.op("dve", lambda e, cv=cv, gt=gt, j=j: e.scalar_tensor_tensor(
                    out=cv[:], in0=gt[:, 1:T + 1], scalar=cw[:, j, 1:2], in1=cv[:], op0=ALU.mult, op1=ALU.add),
                    reads=[gB, cB, cvB], writes=[cvB])
                P.op("dve", lambda e, cv=cv, gt=gt, j=j: e.scalar_tensor_tensor(
                    out=cv[:], in0=gt[:, 2:T + 2], scalar=cw[:, j, 2:3], in1=cv[:], op0=ALU.mult, op1=ALU.add),
                    reads=[gB, cB, cvB], writes=[cvB])
                P.op("act", lambda e, cv=cv: e.activation(out=cv[:], in_=cv[:], func=AF.Silu),
                     reads=[cvB], writes=[cvB])
                uo, uoB = uor.next()
                P.op("dve", lambda e, uo=uo, cv=cv, vs=vs: e.tensor_tensor(out=uo[:], in0=cv[:], in1=vs[:],
                                                                           op=ALU.mult),
                     reads=[cvB, vB], writes=[uoB])
                P.dma("sp", lambda e, uo=uo, j=j: e.dma_start(out=g.UT[j * 128:(j + 1) * 128, :], in_=uo[:]), uoB,
                      reads=[uoB])
        P.finish()


def ph_down(g, l):
    nc, T = g.nc, g.T
    TT = min(T, 1024)
    NQ = TT // 512
    wd = g.w_down[l]
    with ExitStack() as es:
        P = Prog(nc, es, uid("dwn"))
        uT = sb(es, nc, [128, FC, TT], BF16, "uT")
        uB = Buf()
        slabs = Rot(sbufs(es, nc, 3, [128, FC, 128], BF16, "wsl"))
        psr = Rot(psbufs(es, nc, 4, [128, 512], F32, "pp"))
        hir = Rot(sbufs(es, nc, 3, [128, 512], F32, "hi"))
        hor = Rot(sbufs(es, nc, 3, [128, 512], F32, "ho"))
        for half in range(T // TT):
            t0 = half * TT
            P.dma("pool", lambda e, t0=t0: e.dma_start(out=uT[:], in_=fm(g.UT[:, t0:t0 + TT])), uB, writes=[uB])
            loaded = []

            def load(c):
                t, b = slabs.next()
                P.dma("pool", lambda e, t=t, c=c: e.dma_start(
                    out=t[:], in_=wd[:, c * 128:(c + 1) * 128].rearrange("(k p) n -> p k n", p=128)), b,
                    writes=[b])
                loaded.append((t, b))
            load(0)
            load(1)
            for c in range(KC):
                if c + 2 < KC:
                    load(c + 2)
                wt, wB = loaded[c]
                for q in range(NQ):
                    qs = slice(q * 512, (q + 1) * 512)
                    gs = slice(t0 + q * 512, t0 + (q + 1) * 512)
                    hi, hiB = hir.next()
                    P.dma("pool", lambda e, hi=hi, c=c, gs=gs: e.dma_start(
                        out=hi[:], in_=g.hT[c * 128:(c + 1) * 128, gs]), hiB, writes=[hiB])
                    pt, pB = psr.next()

                    def mm(e, pt=pt, wt=wt, qs=qs):
                        for k in range(FC):
                            ins = e.matmul(pt[:], lhsT=wt[:, k, :], rhs=uT[:, k, qs], start=(k == 0),
                                           stop=(k == FC - 1))
                        return ins
                    P.op("pe", mm, reads=[wB, uB], writes=[pB])
                    ho, hoB = hor.next()
                    P.op("dve", lambda e, ho=ho, pt=pt, hi=hi: e.tensor_tensor(out=ho[:], in0=pt[:], in1=hi[:],
                                                                               op=ALU.add),
                         reads=[pB, hiB], writes=[hoB])
                    P.dma("sp", lambda e, ho=ho, c=c, gs=gs: e.dma_start(
                        out=g.hT[c * 128:(c + 1) * 128, gs], in_=ho[:]), hoB, reads=[hoB])
        P.finish()


def build(T, L, debug=False):
    nc = bass.Bass("TRN2", target_bir_lowering=False)
    g = G()
    g.nc, g.T, g.L = nc, T, L
    g.debug = debug

    def din(name, shape, dt=F32):
        return nc.dram_tensor(name, list(shape), dt, kind="ExternalInput").ap()

    def scr(name, shape, dt):
        kind = "ExternalOutput" if debug else "Internal"
        return nc.dram_tensor(name, list(shape), dt, kind=kind).ap()
    g.xT = din("xT", [D, T])
    g.w_in = din("w_in", [L, D, NIN])
    g.w_bm = din("w_bm", [L, D, D])
    g.w_bd = din("w_bd", [L, D, D])
    g.w_o = din("w_o", [L, D, D])
    g.w_up = din("w_up", [L, D, 2 * DFF])
    g.w_down = din("w_down", [L, DFF, D])
    g.gmix = din("gmix", [L, 128, KC])
    g.gffn = din("gffn", [L, 128, KC])
    g.gfin = din("gfin", [128, KC])
    g.bqk = din("bqk", [L, 128, 48])
    g.bgate = din("bgate", [L, 128, 32])
    g.btm = din("btm", [L, 6144])
    g.bif = din("bif", [L, 16])
    g.mnorm = din("mnorm", [L, 2048])
    g.anorm = din("anorm", [L, 2048])
    g.lamqk = din("lamqk", [L, 512])
    g.cw = din("cw", [L, 128, FC, 3])
    g.cb = din("cb", [L, 128, FC])
    g.c_ident = din("c_ident", [128, 128])
    g.c_tri = din("c_tri", [128, 128])
    g.c_ones = din("c_ones", [128, 128])
    g.c_pm = din("c_pm", [32, 32])
    g.c_cos = din("c_cos", [32, T])
    g.c_sin = din("c_sin", [32, T])
    g.outT = nc.dram_tensor("outT", [D, T], F32, kind="ExternalOutput").ap()
    g.hT = scr("hT", [D, T], F32)
    g.QKs = scr("QKs", [48 * 128, T], BF16)
    g.MV = scr("MV", [T, 2048], BF16)
    g.MO = scr("MO", [T, 2048], BF16)
    g.AV = scr("AV", [T, 2048], BF16)
    g.HMT = scr("HMT", [D, T], BF16)
    g.HAT = scr("HAT", [D, T], BF16)
    g.Y1 = scr("Y1", [D, T], F32)
    g.YT = scr("YT", [D, T], BF16)
    g.UT = scr("UT", [DFF, T], BF16)
    if debug:
        g.H1 = scr("H1", [D, T], F32)
        g.HN2 = scr("HN2", [D, T], BF16)
    with ExitStack() as es:
        g.hnT = sb(es, nc, [128, KC, T], BF16, "hnT")
        g.IF = sb(es, nc, [128, T // 128, 16], F32, "IF")
        g.identb = sb(es, nc, [128, 128], BF16, "identb")
        g.trib = sb(es, nc, [128, 128], BF16, "trib")
        g.trif = sb(es, nc, [128, 128], F32, "trif")
        g.onesf = sb(es, nc, [128, 128], F32, "onesf")
        g.pmf = sb(es, nc, [32, 32], F32, "pmf")
        ph_consts(g)
        for l in range(L):
            hsrc = g.xT if l == 0 else g.hT
            ph_norm(g, hsrc, g.gmix[l])
            ph_proj(g, l)
            ph_mlstm(g, l)
            ph_attn(g, l)
            ph_merge(g, l, 0)
            ph_merge(g, l, 1)
            ph_oproj(g, l, hsrc)
            if debug == 2:
                continue
            ph_norm(g, g.hT, g.gffn[l])
            ph_up(g, l)
            if debug == 3:
                continue
            ph_down(g, l)
        ph_norm(g, g.hT, g.gfin, final_dst=g.outT)
    return nc


def pcol(v, nch):
    v = np.asarray(v, dtype=np.float32)
    return np.ascontiguousarray(np.swapaxes(v.reshape(v.shape[:-1] + (nch, 128)), -1, -2))


def host_consts(T):
    s = np.arange(128)
    tri = (s[:, None] <= s[None, :]).astype(np.float32)
    pm = np.zeros((32, 32), np.float32)
    for j in range(16):
        pm[j + 16, j] = -1.0
        pm[j, j + 16] = 1.0
    half = 16
    inv = (np.float32(ROPE_THETA) ** (-np.arange(half, dtype=np.float32) / np.float32(half))).astype(np.float32)
    ang = np.arange(T, dtype=np.float32)[:, None] * inv[None, :]
    cos = np.cos(ang.astype(np.float64)).astype(np.float32).T
    sin = np.sin(ang.astype(np.float64)).astype(np.float32).T
    return {
        "c_ident": np.eye(128, dtype=np.float32), "c_tri": tri, "c_ones": np.ones((128, 128), np.float32),
        "c_pm": pm, "c_cos": np.ascontiguousarray(np.concatenate([cos, cos], 0)),
        "c_sin": np.ascontiguousarray(np.concatenate([sin, sin], 0)),
    }


def prep_shared(L, T, norm_mix, w_in, b_in, m_norm, a_norm, lam_qk, w_bm, w_bd, w_o, norm_ffn, w_up, conv_w, conv_b,
                w_down, norm_final):
    f = lambda a: np.ascontiguousarray(np.asarray(a, dtype=np.float32))
    b_in = f(b_in)
    bqk = np.concatenate([b_in[:, O_MQ:O_MQ + 2048], b_in[:, O_AQ:O_AQ + 4096]], axis=1)
    bgate = b_in[:, O_GM:O_GM + 4096]
    btm = np.concatenate([b_in[:, O_MV:O_MV + 4096], b_in[:, O_AV:O_AV + 2048]], axis=1)
    cwp = np.stack([pcol(f(conv_w)[:, i, :], FC) for i in range(3)], axis=-1)
    d = {
        "w_in": f(w_in)[:L], "w_bm": f(w_bm)[:L], "w_bd": f(w_bd)[:L], "w_o": f(w_o)[:L], "w_up": f(w_up)[:L],
        "w_down": f(w_down)[:L],
        "gmix": pcol(norm_mix, KC)[:L], "gffn": pcol(norm_ffn, KC)[:L], "gfin": pcol(norm_final, KC),
        "bqk": pcol(bqk, 48)[:L], "bgate": pcol(bgate, 32)[:L], "btm": f(btm)[:L],
        "bif": f(b_in[:, O_MI:O_MI + 16])[:L], "mnorm": f(m_norm)[:L], "anorm": f(a_norm)[:L],
        "lamqk": f(lam_qk).reshape(-1, 512)[:L], "cw": np.ascontiguousarray(cwp)[:L], "cb": pcol(conv_b, FC)[:L],
    }
    d.update(host_consts(T))
    return {k: np.ascontiguousarray(v) for k, v in d.items()}


def kernel(x, norm_mix, w_in, b_in, m_norm, a_norm, lam_qk, w_bm, w_bd, w_o, norm_ffn, w_up, conv_w, conv_b, w_down,
           norm_final):
    x = np.asarray(x, dtype=np.float32)
    B, S, _ = x.shape
    L = np.asarray(norm_mix).shape[0]
    shared = prep_shared(L, S, norm_mix, w_in, b_in, m_norm, a_norm, lam_qk, w_bm, w_bd, w_o, norm_ffn, w_up, conv_w,
                         conv_b, w_down, norm_final)
    nc = build(S, L)
    in_maps = []
    for b in range(B):
        m = dict(shared)
        m["xT"] = np.ascontiguousarray(x[b].T)
        in_maps.append(m)
    res = run_bass_kernel_spmd(nc, in_maps, core_ids=list(range(B)))
    out = np.stack([np.ascontiguousarray(res.results[b]["outT"].T) for b in range(B)], axis=0)
    return out.astype(np.float32)
```

```python
import math
from contextlib import ExitStack

import numpy as np
import concourse.bass as bass
import concourse.mybir as mybir
from concourse.bass_utils import run_bass_kernel_spmd

F32 = mybir.dt.float32
BF16 = mybir.dt.bfloat16
AF = mybir.ActivationFunctionType
ALU = mybir.AluOpType

D = 2048
KC = 16
H = 8
NIN = 16400
DFF = 5632
FC = 44
EPS = 1e-6
O_MQ, O_MK, O_MV, O_MO, O_MI, O_AQ, O_AK, O_AV, O_GM, O_GD = (
    0, 1024, 2048, 4096, 6144, 6160, 8208, 10256, 12304, 14352)
ROPE_THETA = 500000.0


class Buf:
    __slots__ = ("w", "r", "dsem", "dcnt")

    def __init__(self):
        self.w = None
        self.r = {}
        self.dsem = None
        self.dcnt = 0


class Prog:
    ENGS = ("pe", "act", "dve", "pool", "sp")

    def __init__(self, nc, es, name):
        self.nc, self.es, self.name = nc, es, name
        self.q = {e: [] for e in self.ENGS}
        self.allsems = []
        self.sem = {e: self._newsem(f"{name}_{e}") for e in self.ENGS}
        self.cnt = {e: 0 for e in self.ENGS}
        self.waited = {e: {} for e in self.ENGS}
        self.dsems = []
        self.nd = 0

    def _newsem(self, name):
        h = self.nc.alloc_semaphore(name=name)
        self.allsems.append(h)
        return h

    def _wait(self, eng, ev):
        sem, val = ev
        if eng == "pe" and sem is self.sem["pe"]:
            return
        w = self.waited[eng]
        k = id(sem)
        if w.get(k, 0) >= val:
            return
        w[k] = val
        self.q[eng].append((0, sem, val))

    def _deps(self, eng, reads, writes):
        for b in reads:
            if b.w is not None:
                self._wait(eng, b.w)
        for b in writes:
            if b.w is not None:
                self._wait(eng, b.w)
            for ev in b.r.values():
                self._wait(eng, ev)

    @staticmethod
    def _commit(ev, reads, writes):
        for b in reads:
            b.r[id(ev[0])] = ev
        for b in writes:
            b.w = ev
            b.r = {}

    def op(self, eng, fn, reads=(), writes=()):
        self._deps(eng, reads, writes)
        self.cnt[eng] += 1
        ev = (self.sem[eng], self.cnt[eng])
        self.q[eng].append((1, fn, self.sem[eng], 1))
        self._commit(ev, reads, writes)
        return ev

    def dma(self, qeng, fn, sb, reads=(), writes=()):
        self._deps(qeng, reads, writes)
        if sb.dsem is None:
            self.nd += 1
            sb.dsem = self._newsem(f"{self.name}_d{self.nd}")
            sb.dcnt = 0
            self.dsems.append(sb)
        sb.dcnt += 16
        ev = (sb.dsem, sb.dcnt)
        self.q[qeng].append((1, fn, sb.dsem, 16))
        self._commit(ev, reads, writes)
        return ev

    def finish(self):
        for e in self.ENGS:
            for e2 in self.ENGS:
                if e2 != e and self.cnt[e2] > 0:
                    self._wait(e, (self.sem[e2], self.cnt[e2]))
            for sb in self.dsems:
                self._wait(e, (sb.dsem, sb.dcnt))
        for sb in self.dsems:
            sb.dsem = None
            sb.dcnt = 0
            sb.w = None
            sb.r = {}
        with self.nc.Block() as blk:
            blk.tensor(lambda e: self._emit("pe", e))
            blk.scalar(lambda e: self._emit("act", e))
            blk.vector(lambda e: self._emit("dve", e))
            blk.gpsimd(lambda e: self._emit("pool", e))
            blk.sync(lambda e: self._emit("sp", e))
        self.nc.all_engine_barrier()
        self.nc.clear_and_free_semaphores(self.allsems)
        self.nc.all_engine_barrier()

    def _emit(self, eng, e):
        for it in self.q[eng]:
            if it[0] == 0:
                e.wait_ge(it[1], it[2])
            else:
                it[1](e).then_inc(it[2], it[3])


class Rot:
    def __init__(self, items):
        self.items = items
        self.i = 0

    def next(self):
        x = self.items[self.i]
        self.i = (self.i + 1) % len(self.items)
        return x


class G:
    pass


_uid = [0]


def uid(p):
    _uid[0] += 1
    return f"{p}{_uid[0]}"


def sb(es, nc, shape, dt, name="t"):
    return es.enter_context(nc.sbuf_tensor(uid(name), list(shape), dt))


def pst(es, nc, shape, dt, name="p"):
    return es.enter_context(nc.psum_tensor(uid(name), list(shape), dt))


def sbufs(es, nc, n, shape, dt, name="t"):
    return [(sb(es, nc, shape, dt, name), Buf()) for _ in range(n)]


def psbufs(es, nc, n, shape, dt, name="p"):
    return [(pst(es, nc, shape, dt, name), Buf()) for _ in range(n)]


def fm(ap):
    return ap.rearrange("(k p) t -> p k t", p=128)


def ph_consts(g):
    nc = g.nc
    with ExitStack() as es:
        P = Prog(nc, es, uid("cst"))
        for t, src in ((g.identb, g.c_ident), (g.trib, g.c_tri), (g.trif, g.c_tri), (g.onesf, g.c_ones),
                       (g.onesb, g.c_ones), (g.pmf, g.c_pm)):
            P.dma("pool", lambda e, t=t, src=src: e.dma_start(out=t[:], in_=src), Buf(), writes=[])
        P.finish()


def ph_norm(g, src, gam_src, final_dst=None):
    nc, T = g.nc, g.T
    W = 256
    with ExitStack() as es:
        P = Prog(nc, es, uid("nrm"))
        hb = Rot(sbufs(es, nc, 2, [128, KC, W], F32, "hb"))
        sq = sb(es, nc, [128, KC, W], BF16, "sq")
        sqA, sqB = Buf(), Buf()
        gm = sb(es, nc, [128, KC], F32, "gm")
        gmB = Buf()
        rs = Rot(sbufs(es, nc, 2, [128, W], F32, "rs"))
        rstd = Rot(sbufs(es, nc, 2, [128, W], F32, "rstd"))
        ps = Rot(psbufs(es, nc, 2, [128, W], F32, "nps"))
        ob = Rot(sbufs(es, nc, 2, [128, KC, W], F32, "ob")) if final_dst is not None else None
        P.dma("pool", lambda e: e.dma_start(out=gm[:], in_=gam_src), gmB, writes=[gmB])
        for j in range(T // W):
            ht, hB = hb.next()
            P.dma("pool", lambda e, ht=ht, j=j: e.dma_start(out=ht[:], in_=fm(src[:, j * W:(j + 1) * W])), hB,
                  writes=[hB])
            P.op("act", lambda e, ht=ht: e.activation(out=sq[:, 0:8, :], in_=ht[:, 0:8, :], func=AF.Square),
                 reads=[hB], writes=[sqA])
            P.op("dve", lambda e, ht=ht: e.tensor_tensor(out=sq[:, 8:16, :], in0=ht[:, 8:16, :], in1=ht[:, 8:16, :],
                                                         op=ALU.mult), reads=[hB], writes=[sqB])
            pt, pB = ps.next()

            def mm(e, pt=pt):
                for k in range(KC):
                    ins = e.matmul(pt[:], lhsT=g.onesb[:], rhs=sq[:, k, :], start=(k == 0), stop=(k == KC - 1))
                return ins
            P.op("pe", mm, reads=[sqA, sqB], writes=[pB])
            rt, rB = rs.next()
            P.op("act", lambda e, rt=rt, pt=pt: e.activation(out=rt[:], in_=pt[:], func=AF.Sqrt, bias=EPS,
                                                             scale=1.0 / D), reads=[pB], writes=[rB])
            st, sB = rstd.next()
            P.op("dve", lambda e, st=st, rt=rt: e.reciprocal(out=st[:], in_=rt[:]), reads=[rB], writes=[sB])
            if final_dst is None:
                def nrm(e, ht=ht, st=st, j=j):
                    for k in range(KC):
                        ins = e.scalar_tensor_tensor(out=g.hnT[:, k, j * W:(j + 1) * W], in0=ht[:, k, :],
                                                     scalar=gm[:, k:k + 1], in1=st[:], op0=ALU.mult, op1=ALU.mult)
                    return ins
                P.op("dve", nrm, reads=[hB, sB, gmB], writes=[Buf()])
            else:
                ot, oB = ob.next()

                def nrm(e, ht=ht, st=st, ot=ot):
                    for k in range(KC):
                        ins = e.scalar_tensor_tensor(out=ot[:, k, :], in0=ht[:, k, :],
                                                     scalar=gm[:, k:k + 1], in1=st[:], op0=ALU.mult, op1=ALU.mult)
                    return ins
                P.op("dve", nrm, reads=[hB, sB, gmB], writes=[oB])
                P.dma("sp", lambda e, ot=ot, j=j: e.dma_start(out=fm(final_dst[:, j * W:(j + 1) * W]), in_=ot[:]),
                      oB, reads=[oB])
        P.finish()


def slab_src(w2d, c0, cw):
    return w2d[:, c0:c0 + cw].rearrange("(k p) n -> p k n", p=128)


def ph_proj(g, l):
    nc, T = g.nc, g.T
    NQ, NT = T // 512, T // 128
    w = g.w_in[l]
    with ExitStack() as es:
        P = Prog(nc, es, uid("prj"))
        slabs = Rot(sbufs(es, nc, 3, [128, KC, 512], BF16, "wsl"))
        psr = Rot(psbufs(es, nc, 4, [128, 512], F32, "pp"))
        pxp = Rot(psbufs(es, nc, 2, [32, 512], F32, "px"))
        stg = Rot(sbufs(es, nc, 4, [128, 512], BF16, "stg"))
        xbr = Rot(sbufs(es, nc, 2, [128, 512], F32, "xb"))
        t1r = Rot(sbufs(es, nc, 2, [32, 512], F32, "t1"))
        t2r = Rot(sbufs(es, nc, 2, [32, 512], F32, "t2"))
        tmr = Rot(sbufs(es, nc, 2, [128, 512], F32, "tm"))
        cosT = sb(es, nc, [32, T], F32, "cos")
        sinT = sb(es, nc, [32, T], F32, "sin")
        bqk = sb(es, nc, [128, 48], F32, "bqk")
        brep = sb(es, nc, [128, 6144], F32, "brep")
        bif = sb(es, nc, [128, 16], F32, "bif")
        wif = sb(es, nc, [128, KC, 16], BF16, "wif")
        cB = Buf()
        for t, src in ((cosT, g.c_cos), (sinT, g.c_sin), (bqk, g.bqk[l]), (brep, g.btm[l].partition_broadcast(128)),
                       (bif, g.bif[l].partition_broadcast(128)), (wif, slab_src(w, O_MI, 16))):
            P.dma("pool", lambda e, t=t, src=src: e.dma_start(out=t[:], in_=src), cB, writes=[cB])
        jobs = []
        for s in range(2):
            jobs.append((O_MQ + s * 512, "fm", ("m", 0 + s * 4)))
        for s in range(2):
            jobs.append((O_MK + s * 512, "fm", ("m", 8 + s * 4)))
        for s in range(4):
            jobs.append((O_AQ + s * 512, "fm", ("aq", 16 + s * 4)))
        for s in range(4):
            jobs.append((O_AK + s * 512, "fm", ("ak", 32 + s * 4)))
        for s in range(4):
            jobs.append((O_MV + s * 512, "tm", ("mv", s)))
        for s in range(4):
            jobs.append((O_MO + s * 512, "tm", ("mo", s)))
        for s in range(4):
            jobs.append((O_AV + s * 512, "tm", ("av", s)))
        loaded = []

        def load(i):
            c0 = jobs[i][0]
            t, b = slabs.next()
            P.dma("pool", lambda e, t=t, c0=c0: e.dma_start(out=t[:], in_=slab_src(w, c0, 512)), b, writes=[b])
            loaded.append((t, b))
        load(0)
        load(1)
        for tt in range(NT):
            pt, pB = psr.next()

            def mm(e, pt=pt, tt=tt):
                for k in range(KC):
                    ins = e.matmul(pt[:, 0:16], lhsT=g.hnT[:, k, tt * 128:(tt + 1) * 128], rhs=wif[:, k, :],
                                   start=(k == 0), stop=(k == KC - 1))
                return ins
            P.op("pe", mm, reads=[cB], writes=[pB])
            P.op("dve", lambda e, pt=pt, tt=tt: e.tensor_tensor(out=g.IF[:, tt, :], in0=pt[:, 0:16], in1=bif[:],
                                                                op=ALU.add), reads=[pB, cB], writes=[Buf()])
        for i, (c0, kind, info) in enumerate(jobs):
            if i + 2 < len(jobs):
                load(i + 2)
            wt, wB = loaded[i]
            if kind == "fm":
                typ, ch0 = info
                for cc in range(4):
                    ch = ch0 + cc
                    for q in range(NQ):
                        pt, pB = psr.next()

                        def mm(e, pt=pt, wt=wt, cc=cc, q=q):
                            for k in range(KC):
                                ins = e.matmul(pt[:], lhsT=wt[:, k, cc * 128:(cc + 1) * 128],
                                               rhs=g.hnT[:, k, q * 512:(q + 1) * 512],
                                               start=(k == 0), stop=(k == KC - 1))
                            return ins
                        P.op("pe", mm, reads=[wB], writes=[pB])
                        st, sB = stg.next()
                        if typ == "m":
                            P.op("act", lambda e, st=st, pt=pt, ch=ch: e.activation(
                                out=st[:], in_=pt[:], func=AF.Identity, bias=bqk[:, ch:ch + 1]),
                                reads=[pB, cB], writes=[sB])
                        else:
                            xb, xB = xbr.next()
                            P.op("act", lambda e, xb=xb, pt=pt, ch=ch: e.activation(
                                out=xb[:], in_=pt[:], func=AF.Identity, bias=bqk[:, ch:ch + 1]),
                                reads=[pB, cB], writes=[xB])
                            px, pxB = pxp.next()
                            P.op("pe", lambda e, px=px, xb=xb: e.matmul(px[:], lhsT=g.pmf[:], rhs=xb[0:32, :],
                                                                        start=True, stop=True),
                                 reads=[xB], writes=[pxB])
                            t1, t1B = t1r.next()
                            t2, t2B = t2r.next()
                            P.op("dve", lambda e, t1=t1, px=px, q=q: e.tensor_tensor(
                                out=t1[:], in0=px[:], in1=sinT[:, q * 512:(q + 1) * 512], op=ALU.mult),
                                reads=[pxB, cB], writes=[t1B])
                            P.op("dve", lambda e, t2=t2, xb=xb, q=q: e.tensor_tensor(
                                out=t2[:], in0=xb[0:32, :], in1=cosT[:, q * 512:(q + 1) * 512], op=ALU.mult),
                                reads=[xB, cB], writes=[t2B])
                            P.op("dve", lambda e, t1=t1, t2=t2, xb=xb: e.tensor_tensor(
                                out=xb[0:32, :], in0=t1[:], in1=t2[:], op=ALU.add),
                                reads=[t1B, t2B], writes=[xB])
                            sc = (128.0 ** -0.5) if typ == "aq" else 1.0
                            P.op("act", lambda e, st=st, xb=xb, sc=sc: e.activation(
                                out=st[:], in_=xb[:], func=AF.Identity, scale=sc), reads=[xB], writes=[sB])
                        P.dma("sp", lambda e, st=st, ch=ch, q=q: e.dma_start(
                            out=g.QKs[ch * 128:(ch + 1) * 128, q * 512:(q + 1) * 512], in_=st[:]), sB, reads=[sB])
            else:
                typ, s = info
                dst = {"mv": g.MV, "mo": g.MO, "av": g.AV}[typ]
                boff = {"mv": 0, "mo": 2048, "av": 4096}[typ] + s * 512
                for tt in range(NT):
                    pt, pB = psr.next()

                    def mm(e, pt=pt, wt=wt, tt=tt):
                        for k in range(KC):
                            ins = e.matmul(pt[:], lhsT=g.hnT[:, k, tt * 128:(tt + 1) * 128], rhs=wt[:, k, :],
                                           start=(k == 0), stop=(k == KC - 1))
                        return ins
                    P.op("pe", mm, reads=[wB], writes=[pB])
                    st, sB = stg.next()
                    if typ == "mo":
                        tm, tB = tmr.next()
                        P.op("dve", lambda e, tm=tm, pt=pt, boff=boff: e.tensor_tensor(
                            out=tm[:], in0=pt[:], in1=brep[:, boff:boff + 512], op=ALU.add),
                            reads=[pB, cB], writes=[tB])
                        P.op("act", lambda e, st=st, tm=tm: e.activation(out=st[:], in_=tm[:], func=AF.Sigmoid),
                             reads=[tB], writes=[sB])
                    else:
                        P.op("dve", lambda e, st=st, pt=pt, boff=boff: e.tensor_tensor(
                            out=st[:], in0=pt[:], in1=brep[:, boff:boff + 512], op=ALU.add),
                            reads=[pB, cB], writes=[sB])
                    P.dma("sp", lambda e, st=st, dst=dst, tt=tt, s=s: e.dma_start(
                        out=dst[tt * 128:(tt + 1) * 128, s * 512:(s + 1) * 512], in_=st[:]), sB, reads=[sB])
        P.finish()


def tm_src(ap2d, h, NT):
    return ap2d[:, h * 256:(h + 1) * 256].rearrange("(t p) d -> p t d", p=128)


def ph_mlstm(g, l):
    nc, T = g.nc, g.T
    NT = T // 128
    with ExitStack() as es:
        P = Prog(nc, es, uid("mls"))
        NG = NT * 8
        def v3(ap):
            return ap.rearrange("p (c h) -> p c h", h=8)
        e1 = sb(es, nc, [128, NG], F32, "e1")
        l1 = sb(es, nc, [128, NG], F32, "l1")
        tg = sb(es, nc, [128, NG], F32, "tg")
        u = sb(es, nc, [128, NG], F32, "u")
        bnd = sb(es, nc, [128, NG], F32, "bnd")
        eg = sb(es, nc, [128, NG], F32, "eg")
        mrep = sb(es, nc, [128, 2048], F32, "mrep")
        cgp = pst(es, nc, [128, 2, NG], F32, "cgp")
        gB = Buf()
        P.dma("pool", lambda e: e.dma_start(out=mrep[:], in_=g.mnorm[l].partition_broadcast(128)), gB,
              writes=[gB])
        P.op("act", lambda e: e.activation(out=v3(e1[:]), in_=g.IF[:, :, 8:16], func=AF.Exp, scale=-1.0),
             writes=[gB])
        P.op("act", lambda e: e.activation(out=l1[:], in_=e1[:], func=AF.Ln, bias=1.0), reads=[gB], writes=[gB])

        def mmg(e):
            e.matmul(cgp[:, 0, :], lhsT=g.trif[:], rhs=l1[:], start=True, stop=True)
            return e.matmul(cgp[:, 1, :], lhsT=g.onesf[:], rhs=l1[:], start=True, stop=True)
        P.op("pe", mmg, reads=[gB], writes=[gB])
        P.op("dve", lambda e: e.tensor_tensor(out=v3(tg[:]), in0=v3(cgp[:, 0, :]), in1=g.IF[:, :, 0:8], op=ALU.add),
             reads=[gB], writes=[gB])

        def gex(e):
            e.activation(out=u[:], in_=tg[:], func=AF.Exp, bias=math.log(128.0 ** -0.5))
            e.activation(out=bnd[:], in_=cgp[:, 0, :], func=AF.Exp)
            return e.activation(out=eg[:], in_=cgp[:, 1, :], func=AF.Exp, scale=-1.0)
        P.op("act", gex, reads=[gB], writes=[gB])

        qkr = Rot([(sb(es, nc, [128, T], BF16, "qT"), sb(es, nc, [128, T], BF16, "kT"), Buf(), Buf())
                   for _ in range(2)])
        var = [(sb(es, nc, [128, NT, 257], BF16, "vA"), Buf()) for _ in range(2)]
        gr = Rot(sbufs(es, nc, 2, [128, NT, 256], BF16, "Gg"))
        hms = Rot(sbufs(es, nc, 2, [128, 2, T], BF16, "hms"))
        C = sb(es, nc, [128, 257], F32, "C")
        C1 = sb(es, nc, [128, 257], F32, "C1")
        Cb = sb(es, nc, [128, 257], BF16, "Cb")
        CB, C1B, CbB = Buf(), Buf(), Buf()
        ptr = Rot(sbufs(es, nc, 2, [128, 128], BF16, "PT"))
        kur = Rot(sbufs(es, nc, 2, [128, 128], BF16, "ku"))
        smr = Rot(sbufs(es, nc, 2, [128, 8], F32, "sm"))
        jkr = Rot(sbufs(es, nc, 2, [128, 256], F32, "jk"))
        hmr = Rot(sbufs(es, nc, 2, [128, 256], BF16, "hm"))
        stp = Rot(psbufs(es, nc, 2, [128, 128], F32, "stp"))
        ktp = Rot(psbufs(es, nc, 1, [128, 128], BF16, "ktp"))
        outp = Rot(psbufs(es, nc, 2, [128, 257], F32, "outp"))
        updp = Rot(psbufs(es, nc, 1, [128, 257], F32, "updp"))
        trp = Rot(psbufs(es, nc, 1, [128, 2, 128], BF16, "trp"))
        for vt, vB in var:
            P.op("dve", lambda e, vt=vt: e.memset(vt[:, :, 256:257], 1.0), writes=[vB])
        vr = Rot(var)
        bg = []
        eps = {}

        def advance():
            for gen in list(bg):
                try:
                    next(gen)
                except StopIteration:
                    bg.remove(gen)

        for h in range(H):
            eps.clear()
            qT, kT, qB, kB = qkr.next()
            qkB = qB
            vA, vB = vr.next()
            Gt, GB = gr.next()
            hs, hsB = hms.next()
            P.dma("pool", lambda e, qT=qT, h=h: e.dma_start(out=qT[:], in_=g.QKs[h * 128:(h + 1) * 128, :]), qB,
                  writes=[qB])
            P.dma("pool", lambda e, kT=kT, h=h: e.dma_start(out=kT[:], in_=g.QKs[(8 + h) * 128:(9 + h) * 128, :]),
                  kB, writes=[kB])
            P.dma("pool", lambda e, vA=vA, h=h: e.dma_start(out=vA[:, :, 0:256], in_=tm_src(g.MV, h, NT)), vB,
                  writes=[vB])
            P.dma("pool", lambda e, Gt=Gt, h=h: e.dma_start(out=Gt[:], in_=tm_src(g.MO, h, NT)), GB,
                  writes=[GB])

            def gmul(e, Gt=Gt, h=h):
                for c in range(NT):
                    ins = e.tensor_tensor(out=Gt[:, c, :], in0=Gt[:, c, :], in1=mrep[:, h * 256:(h + 1) * 256],
                                          op=ALU.mult)
                return ins
            P.op("dve", gmul, reads=[gB], writes=[GB])
            for c in range(NT):
                cs = slice(c * 128, (c + 1) * 128)
                uc = u[:, c * 8 + h:c * 8 + h + 1]
                st, stB = stp.next()
                P.op("pe", lambda e, st=st, kT=kT, qT=qT, cs=cs: e.matmul(st[:], lhsT=kT[:, cs], rhs=qT[:, cs],
                                                                          start=True, stop=True),
                     reads=[qB, kB], writes=[stB])
                advance()
                pt, ptB = ptr.next()
                P.op("dve", lambda e, pt=pt, st=st, uc=uc: e.scalar_tensor_tensor(
                    out=pt[:], in0=st[:], scalar=uc, in1=g.trif[:], op0=ALU.mult, op1=ALU.mult),
                    reads=[stB, gB], writes=[ptB])
                kt, ktB = ktp.next()
                P.op("pe", lambda e, kt=kt, kT=kT, cs=cs: e.transpose(kt[:], kT[:, cs], g.identb[:]),
                     reads=[kB], writes=[ktB])
                advance()
                ku, kuB = kur.next()
                P.op("act", lambda e, ku=ku, kt=kt, uc=uc: e.activation(out=ku[:], in_=kt[:], func=AF.Identity,
                                                                        scale=uc),
                     reads=[ktB, gB], writes=[kuB])
                while eps.get(c - 2) in bg:
                    advance()
                ot, oB = outp.next()

                def mmo(e, ot=ot, pt=pt, vA=vA, qT=qT, c=c, cs=cs):
                    ins = e.matmul(ot[:], lhsT=pt[:], rhs=vA[:, c, :], start=True, stop=(c == 0))
                    if c > 0:
                        ins = e.matmul(ot[:], lhsT=qT[:, cs], rhs=Cb[:], start=False, stop=True)
                    return ins
                P.op("pe", mmo, reads=[ptB, vB, qB] + ([CbB] if c > 0 else []), writes=[oB])
                advance()
                if c < NT - 1:
                    up, upB = updp.next()
                    P.op("pe", lambda e, up=up, ku=ku, vA=vA, c=c: e.matmul(up[:], lhsT=ku[:], rhs=vA[:, c, :],
                                                                            start=True, stop=True),
                         reads=[kuB, vB], writes=[upB])
                    egc = eg[:, c * 8 + h:c * 8 + h + 1]
                    if c == 0:
                        P.op("dve", lambda e, up=up, egc=egc: e.tensor_scalar(
                            out=C[:], in0=up[:], scalar1=egc, scalar2=None, op0=ALU.mult),
                            reads=[upB, gB], writes=[CB])
                    else:
                        P.op("act", lambda e, egc=egc: e.activation(out=C1[:], in_=C[:], func=AF.Identity,
                                                                    scale=egc),
                             reads=[CB, gB], writes=[C1B])
                        P.op("dve", lambda e, up=up, egc=egc: e.scalar_tensor_tensor(
                            out=C[:], in0=up[:], scalar=egc, in1=C1[:], op0=ALU.mult, op1=ALU.add),
                            reads=[upB, C1B, gB], writes=[CB])
                    P.op("act", lambda e: e.activation(out=Cb[:], in_=C[:], func=AF.Identity),
                         reads=[CB], writes=[CbB])
                def epilogue(c=c, cs=cs, ot=ot, oB=oB):
                    sm, smB = smr.next()
                    P.op("act", lambda e, sm=sm, ot=ot: e.activation(out=sm[:, 0:1], in_=ot[:, 256:257], func=AF.Abs),
                         reads=[oB], writes=[smB])
                    yield
                    P.op("dve", lambda e, sm=sm, c=c, h=h: e.tensor_scalar(
                        out=sm[:, 1:2], in0=sm[:, 0:1], scalar1=bnd[:, c * 8 + h:c * 8 + h + 1], scalar2=None, op0=ALU.max),
                        reads=[smB, gB], writes=[smB])
                    yield
                    P.op("dve", lambda e, sm=sm: e.reciprocal(out=sm[:, 2:3], in_=sm[:, 1:2]),
                         reads=[smB], writes=[smB])
                    yield
                    jk, jkB = jkr.next()
                    P.op("act", lambda e, jk=jk, ot=ot, sm=sm: e.activation(
                        out=jk[:], in_=ot[:, 0:256], func=AF.Square, scale=sm[:, 2:3], accum_out=sm[:, 3:4]),
                        reads=[oB, smB], writes=[jkB, smB])
                    yield
                    P.op("act", lambda e, sm=sm: e.activation(out=sm[:, 4:5], in_=sm[:, 3:4], func=AF.Ln, bias=EPS,
                                                              scale=1.0 / 256), reads=[smB], writes=[smB])
                    yield
                    P.op("act", lambda e, sm=sm: e.activation(out=sm[:, 5:6], in_=sm[:, 4:5], func=AF.Exp,
                                                              scale=-0.5), reads=[smB], writes=[smB])
                    yield
                    P.op("dve", lambda e, sm=sm: e.tensor_tensor(out=sm[:, 6:7], in0=sm[:, 5:6], in1=sm[:, 2:3],
                                                                 op=ALU.mult), reads=[smB], writes=[smB])
                    yield
                    hm, hmB = hmr.next()
                    P.op("dve", lambda e, hm=hm, ot=ot, sm=sm, Gt=Gt, c=c: e.scalar_tensor_tensor(
                        out=hm[:], in0=ot[:, 0:256], scalar=sm[:, 6:7], in1=Gt[:, c, :], op0=ALU.mult, op1=ALU.mult),
                        reads=[oB, smB, GB], writes=[hmB])
                    yield
                    tr, trB = trp.next()

                    def trs(e, tr=tr, hm=hm):
                        e.transpose(tr[:, 0, :], hm[:, 0:128], g.identb[:])
                        return e.transpose(tr[:, 1, :], hm[:, 128:256], g.identb[:])
                    P.op("pe", trs, reads=[hmB], writes=[trB])
                    yield
                    P.op("act", lambda e, hs=hs, tr=tr, cs=cs: e.activation(out=hs[:, :, cs], in_=tr[:],
                                                                            func=AF.Identity),
                         reads=[trB], writes=[hsB])

                gen_ = epilogue()
                eps[c] = gen_
                bg.append(gen_)

            while bg:
                advance()
            P.dma("sp", lambda e, hs=hs, h=h: e.dma_start(
                out=g.HMT[h * 256:(h + 1) * 256, :].rearrange("(j p) t -> p j t", p=128), in_=hs[:]), hsB,
                reads=[hsB])
        P.finish()


def ph_attn(g, l):
    nc, T = g.nc, g.T
    NT, NG = T // 128, T // 256
    lam_init = 0.8 - 0.6 * math.exp(-0.3 * l)
    with ExitStack() as es:
        P = Prog(nc, es, uid("att"))
        lq = sb(es, nc, [128, 4, 128], F32, "lq")
        lp = sb(es, nc, [128, 2, 128], F32, "lp")
        ls = sb(es, nc, [128, 8], F32, "ls")
        anrep = sb(es, nc, [128, 2048], F32, "anrep")
        cB = Buf()
        P.dma("pool", lambda e: e.dma_start(out=lq[:].rearrange("p a b -> p (a b)"),
                                            in_=g.lamqk[l].partition_broadcast(128)), cB, writes=[cB])
        P.dma("pool", lambda e: e.dma_start(out=anrep[:], in_=g.anorm[l].partition_broadcast(128)), cB,
              writes=[cB])

        def lam1(e):
            e.tensor_tensor(out=lp[:, 0, :], in0=lq[:, 0, :], in1=lq[:, 1, :], op=ALU.mult)
            return e.tensor_tensor(out=lp[:, 1, :], in0=lq[:, 2, :], in1=lq[:, 3, :], op=ALU.mult)
        P.op("dve", lam1, reads=[cB], writes=[cB])

        def lam2(e):
            e.reduce_sum(out=ls[:, 0:1], in_=lp[:, 0, :], axis=mybir.AxisListType.X)
            return e.reduce_sum(out=ls[:, 1:2], in_=lp[:, 1, :], axis=mybir.AxisListType.X)
        P.op("dve", lam2, reads=[cB], writes=[cB])
        P.op("act", lambda e: e.activation(out=ls[:, 2:4], in_=ls[:, 0:2], func=AF.Exp), reads=[cB], writes=[cB])
        P.op("dve", lambda e: e.scalar_tensor_tensor(out=ls[:, 4:5], in0=ls[:, 3:4], scalar=-lam_init,
                                                     in1=ls[:, 2:3], op0=ALU.add, op1=ALU.subtract),
             reads=[cB], writes=[cB])
        P.op("dve", lambda e: e.tensor_scalar(out=anrep[:], in0=anrep[:], scalar1=(1.0 - lam_init), scalar2=None,
                                              op0=ALU.mult), reads=[cB], writes=[cB])
        nlam = ls[:, 4:5]

        inb = []
        for i in range(2):
            vt = sb(es, nc, [128, NT, 257], BF16, "vA")
            vB = Buf()
            P.op("dve", lambda e, vt=vt: e.memset(vt[:, :, 256:257], 1.0), writes=[vB])
            inb.append(dict(q2=sb(es, nc, [128, 2, T], BF16, "q2"), k2=sb(es, nc, [128, 2, T], BF16, "k2"),
                            qB=Buf(), kB=Buf(), vA=vt, vB=vB, hs=sb(es, nc, [128, 2, T], BF16, "has"), hsB=Buf()))
        inr = Rot(inb)
        ptr = Rot(sbufs(es, nc, 4, [128, 2, 256], BF16, "PT"))
        rrr = Rot(sbufs(es, nc, 5, [128, 8], F32, "rr"))
        c0r = Rot(sbufs(es, nc, 5, [128, 257], F32, "c0"))
        c1r = Rot(sbufs(es, nc, 5, [128, 257], F32, "c1"))
        a0r = Rot(sbufs(es, nc, 5, [128, 256], F32, "a0"))
        a1r = Rot(sbufs(es, nc, 5, [128, 256], F32, "a1"))
        jkr = Rot(sbufs(es, nc, 5, [128, 256], F32, "jk"))
        habr = Rot(sbufs(es, nc, 5, [128, 256], BF16, "hab"))
        stp = Rot(psbufs(es, nc, 3, [128, 2, 256], F32, "stp"))
        outp = [[(pst(es, nc, [128, 257], F32, "outp"), Buf()) for j in range(2)] for m in range(2)]
        trp = Rot(psbufs(es, nc, 1, [128, 2, 128], BF16, "trp"))

        hb = {}

        def ensure_head(h):
            if h in hb or h >= H:
                return
            b = inr.next()
            c0 = (16 + 2 * h) * 128
            c1 = (32 + 2 * h) * 128
            P.dma("pool", lambda e: e.dma_start(
                out=b["q2"][:], in_=g.QKs[c0:c0 + 256, :].rearrange("(c p) t -> p c t", p=128)), b["qB"],
                writes=[b["qB"]])
            P.dma("pool", lambda e: e.dma_start(
                out=b["k2"][:], in_=g.QKs[c1:c1 + 256, :].rearrange("(c p) t -> p c t", p=128)), b["kB"],
                writes=[b["kB"]])
            P.dma("pool", lambda e: e.dma_start(out=b["vA"][:, :, 0:256], in_=tm_src(g.AV, h, NT)), b["vB"],
                  writes=[b["vB"]])
            hb[h] = b

        steps = [(h, gq, kt) for h in range(H) for gq in range(NG) for kt in range(2 * gq + 2)]
        LA = 2
        stq = {}

        def rec_qk(i):
            h, gq, kt = steps[i]
            ensure_head(h)
            b = hb[h]
            q2, k2 = b["q2"], b["k2"]
            qs = slice(gq * 256, (gq + 1) * 256)
            ks = slice(kt * 128, (kt + 1) * 128)
            st, stB = stp.next()

            def mmq(e):
                e.matmul(st[:, 0, :], lhsT=k2[:, 0, ks], rhs=q2[:, 0, qs], start=True, stop=True)
                return e.matmul(st[:, 1, :], lhsT=k2[:, 1, ks], rhs=q2[:, 1, qs], start=True, stop=True)
            P.op("pe", mmq, reads=[b["qB"], b["kB"]], writes=[stB])
            stq[i] = (st, stB)

        def epilogue(h, gq, j, b):
            (o0, o0B), (o1, o1B) = outp[0][j], outp[1][j]
            hs, hsB = b["hs"], b["hsB"]
            ts = slice((gq * 2 + j) * 128, (gq * 2 + j + 1) * 128)
            c0, c0B = c0r.next()
            c1, c1B = c1r.next()
            P.op("dve", lambda e: e.tensor_copy(out=c0[:], in_=o0[:]), reads=[o0B], writes=[c0B])
            P.op("dve", lambda e: e.tensor_copy(out=c1[:], in_=o1[:]), reads=[o1B], writes=[c1B])
            yield
            rr, rB = rrr.next()

            def rcp(e):
                e.reciprocal(out=rr[:, 0:1], in_=c0[:, 256:257])
                return e.reciprocal(out=rr[:, 1:2], in_=c1[:, 256:257])
            P.op("dve", rcp, reads=[c0B, c1B], writes=[rB])
            P.op("dve", lambda e: e.tensor_tensor(out=rr[:, 2:3], in0=rr[:, 1:2], in1=nlam, op=ALU.mult),
                 reads=[rB, cB], writes=[rB])
            yield
            a0, a0B = a0r.next()
            P.op("dve", lambda e: e.tensor_scalar(out=a0[:], in0=c0[:, 0:256], scalar1=rr[:, 0:1], scalar2=None,
                                                  op0=ALU.mult), reads=[c0B, rB], writes=[a0B])
            yield
            a1, a1B = a1r.next()
            P.op("dve", lambda e: e.scalar_tensor_tensor(out=a1[:], in0=c1[:, 0:256], scalar=rr[:, 2:3], in1=a0[:],
                                                         op0=ALU.mult, op1=ALU.add),
                 reads=[c1B, rB, a0B], writes=[a1B])
            yield
            jk, jkB = jkr.next()
            P.op("act", lambda e: e.activation(out=jk[:], in_=a1[:], func=AF.Square, accum_out=rr[:, 3:4]),
                 reads=[a1B, rB], writes=[jkB, rB])
            yield
            P.op("act", lambda e: e.activation(out=rr[:, 4:5], in_=rr[:, 3:4], func=AF.Ln, bias=EPS,
                                               scale=1.0 / 256), reads=[rB], writes=[rB])
            yield
            P.op("act", lambda e: e.activation(out=rr[:, 5:6], in_=rr[:, 4:5], func=AF.Exp, scale=-0.5),
                 reads=[rB], writes=[rB])
            yield
            hab, habB = habr.next()
            P.op("dve", lambda e: e.scalar_tensor_tensor(out=hab[:], in0=a1[:], scalar=rr[:, 5:6],
                                                         in1=anrep[:, h * 256:(h + 1) * 256], op0=ALU.mult,
                                                         op1=ALU.mult), reads=[a1B, rB, cB], writes=[habB])
            yield
            tr, trB = trp.next()

            def trs(e):
                e.transpose(tr[:, 0, :], hab[:, 0:128], g.identb[:])
                return e.transpose(tr[:, 1, :], hab[:, 128:256], g.identb[:])
            P.op("pe", trs, reads=[habB], writes=[trB])
            yield
            P.op("dve", lambda e: e.tensor_copy(out=hs[:, :, ts], in_=tr[:]), reads=[trB], writes=[hsB])

        bg = []

        def advance():
            for gen in list(bg):
                try:
                    next(gen)
                except StopIteration:
                    bg.remove(gen)

        for i in range(min(LA, len(steps))):
            rec_qk(i)
        for i, (h, gq, kt) in enumerate(steps):
            if gq == 0 and kt == 0:
                ensure_head(h + 1)
            if i + LA < len(steps):
                rec_qk(i + LA)
            b = hb[h]
            vA, vB = b["vA"], b["vB"]
            st, stB = stq.pop(i)
            pt, ptB = ptr.next()
            P.op("act", lambda e, pt=pt, st=st: e.activation(out=pt[:], in_=st[:], func=AF.Exp),
                 reads=[stB], writes=[ptB])
            if kt >= 2 * gq:
                j0 = kt - 2 * gq

                def msk(e, pt=pt, j0=j0):
                    e.tensor_tensor(out=pt[:, 0, j0 * 128:(j0 + 1) * 128], in0=pt[:, 0, j0 * 128:(j0 + 1) * 128],
                                    in1=g.trib[:], op=ALU.mult)
                    return e.tensor_tensor(out=pt[:, 1, j0 * 128:(j0 + 1) * 128],
                                           in0=pt[:, 1, j0 * 128:(j0 + 1) * 128], in1=g.trib[:], op=ALU.mult)
                P.op("dve", msk, reads=[ptB], writes=[ptB])
            js = [j for j in range(2) if kt <= 2 * gq + j]

            def mmv(e, pt=pt, vA=vA, kt=kt, gq=gq, js=js):
                for j in js:
                    for m in range(2):
                        ins = e.matmul(outp[m][j][0][:], lhsT=pt[:, m, j * 128:(j + 1) * 128], rhs=vA[:, kt, :],
                                       start=(kt == 0), stop=(kt == 2 * gq + j), skip_group_check=True)
                return ins
            P.op("pe", mmv, reads=[ptB, vB], writes=[outp[m][j][1] for j in js for m in range(2)])
            advance()
            if kt == 2 * gq:
                gen = epilogue(h, gq, 0, b)
                next(gen)
                bg.append(gen)
            if kt == 2 * gq + 1:
                gen = epilogue(h, gq, 1, b)
                next(gen)
                bg.append(gen)
                if gq == NG - 1:
                    while bg:
                        advance()
                    P.dma("sp", lambda e, b=b, h=h: e.dma_start(
                        out=g.HAT[h * 256:(h + 1) * 256, :].rearrange("(j p) t -> p j t", p=128), in_=b["hs"][:]),
                        b["hsB"], reads=[b["hsB"]])
                    del hb[h]
        P.finish()


def ph_merge(g, l, which):
    nc, T = g.nc, g.T
    NQ = T // 512
    wa = (g.w_bm if which == 0 else g.w_bd)[l]
    wg = g.w_in[l]
    goff = O_GM if which == 0 else O_GD
    src = g.HMT if which == 0 else g.HAT
    with ExitStack() as es:
        P = Prog(nc, es, uid("mrg"))
        xT = sb(es, nc, [128, KC, T], BF16, "xT")
        xB = Buf()
        bg = sb(es, nc, [128, 32], F32, "bg")
        bgB = Buf()
        P.dma("pool", lambda e: e.dma_start(out=bg[:], in_=g.bgate[l]), bgB, writes=[bgB])
        P.dma("pool", lambda e: e.dma_start(out=xT[:], in_=fm(src)), xB, writes=[xB])
        SW = 256
        NS = 2048 // SW
        CPS = SW // 128
        slabs = Rot(sbufs(es, nc, 4, [128, KC, SW], BF16, "wsl"))
        psy = Rot(psbufs(es, nc, 3, [128, 512], F32, "py"))
        psg = Rot(psbufs(es, nc, 3, [128, 512], F32, "pg"))
        sgr = Rot(sbufs(es, nc, 2, [128, 512], F32, "sg"))
        y1r = Rot(sbufs(es, nc, 3, [128, 512], F32, "y1"))
        if which == 1:
            tr_ = Rot(sbufs(es, nc, 2, [128, 512], F32, "tt"))
            yor = Rot(sbufs(es, nc, 3, [128, 512], BF16, "yo"))
        loaded = []

        def load(s):
            ta, ba = slabs.next()
            P.dma("pool", lambda e, ta=ta, s=s: e.dma_start(out=ta[:], in_=slab_src(wa, s * SW, SW)), ba,
                  writes=[ba])
            tg, bgs = slabs.next()
            P.dma("pool", lambda e, tg=tg, s=s: e.dma_start(out=tg[:], in_=slab_src(wg, goff + s * SW, SW)), bgs,
                  writes=[bgs])
            loaded.append((ta, ba, tg, bgs))
        load(0)
        for s in range(NS):
            if s + 1 < NS:
                load(s + 1)
            ta, ba, tg, bgs = loaded[s]
            for cc in range(CPS):
                ch = s * CPS + cc
                for q in range(NQ):
                    qs = slice(q * 512, (q + 1) * 512)
                    py, pyB = psy.next()
                    pg, pgB = psg.next()

                    def mm(e, py=py, pg=pg, ta=ta, tg=tg, cc=cc, qs=qs):
                        for k in range(KC):
                            e.matmul(py[:], lhsT=ta[:, k, cc * 128:(cc + 1) * 128], rhs=xT[:, k, qs],
                                     start=(k == 0), stop=(k == KC - 1))
                        for k in range(KC):
                            ins = e.matmul(pg[:], lhsT=tg[:, k, cc * 128:(cc + 1) * 128], rhs=g.hnT[:, k, qs],
                                           start=(k == 0), stop=(k == KC - 1))
                        return ins
                    P.op("pe", mm, reads=[ba, bgs, xB], writes=[pyB, pgB])
                    sg, sgB = sgr.next()
                    bcol = which * 16 + ch
                    P.op("act", lambda e, sg=sg, pg=pg, bcol=bcol: e.activation(
                        out=sg[:], in_=pg[:], func=AF.Sigmoid, bias=bg[:, bcol:bcol + 1]),
                        reads=[pgB, bgB], writes=[sgB])
                    if which == 0:
                        y1, y1B = y1r.next()
                        P.op("dve", lambda e, y1=y1, py=py, sg=sg: e.tensor_tensor(out=y1[:], in0=py[:], in1=sg[:],
                                                                                   op=ALU.mult),
                             reads=[pyB, sgB], writes=[y1B])
                        P.dma("sp", lambda e, y1=y1, ch=ch, qs=qs: e.dma_start(
                            out=g.Y1[ch * 128:(ch + 1) * 128, qs], in_=y1[:]), y1B, reads=[y1B])
                    else:
                        y1, y1B = y1r.next()
                        P.dma("pool", lambda e, y1=y1, ch=ch, qs=qs: e.dma_start(
                            out=y1[:], in_=g.Y1[ch * 128:(ch + 1) * 128, qs]), y1B, writes=[y1B])
                        tt, ttB = tr_.next()
                        P.op("dve", lambda e, tt=tt, py=py, sg=sg: e.tensor_tensor(out=tt[:], in0=py[:], in1=sg[:],
                                                                                   op=ALU.mult),
                             reads=[pyB, sgB], writes=[ttB])
                        yo, yoB = yor.next()
                        P.op("dve", lambda e, yo=yo, tt=tt, y1=y1: e.tensor_tensor(out=yo[:], in0=tt[:], in1=y1[:],
                                                                                   op=ALU.add),
                             reads=[ttB, y1B], writes=[yoB])
                        P.dma("sp", lambda e, yo=yo, ch=ch, qs=qs: e.dma_start(
                            out=g.YT[ch * 128:(ch + 1) * 128, qs], in_=yo[:]), yoB, reads=[yoB])
        P.finish()


def ph_dump_hn(g, dst):
    nc = g.nc
    with ExitStack() as es:
        P = Prog(nc, es, uid("dmp"))
        b = Buf()
        for k in range(KC):
            P.dma("sp", lambda e, k=k: e.dma_start(out=dst[k * 128:(k + 1) * 128, :], in_=g.hnT[:, k, :]), b,
                  reads=[b])
        P.finish()


def ph_oproj(g, l, hsrc):
    nc, T = g.nc, g.T
    NQ = T // 512
    wo = g.w_o[l]
    with ExitStack() as es:
        P = Prog(nc, es, uid("opr"))
        xT = sb(es, nc, [128, KC, T], BF16, "xT")
        xB = Buf()
        P.dma("pool", lambda e: e.dma_start(out=xT[:], in_=fm(g.YT)), xB, writes=[xB])
        slabs = Rot(sbufs(es, nc, 3, [128, KC, 512], BF16, "wsl"))
        psr = Rot(psbufs(es, nc, 4, [128, 512], F32, "pp"))
        hir = Rot(sbufs(es, nc, 3, [128, 512], F32, "hi"))
        hor = Rot(sbufs(es, nc, 3, [128, 512], F32, "ho"))
        loaded = []

        def load(s):
            t, b = slabs.next()
            P.dma("pool", lambda e, t=t, s=s: e.dma_start(out=t[:], in_=slab_src(wo, s * 512, 512)), b, writes=[b])
            loaded.append((t, b))
        load(0)
        load(1)
        for s in range(4):
            if s + 2 < 4:
                load(s + 2)
            wt, wB = loaded[s]
            for cc in range(4):
                ch = s * 4 + cc
                for q in range(NQ):
                    qs = slice(q * 512, (q + 1) * 512)
                    hi, hiB = hir.next()
                    P.dma("pool", lambda e, hi=hi, ch=ch, qs=qs: e.dma_start(
                        out=hi[:], in_=hsrc[ch * 128:(ch + 1) * 128, qs]), hiB, writes=[hiB])
                    pt, pB = psr.next()

                    def mm(e, pt=pt, wt=wt, cc=cc, qs=qs):
                        for k in range(KC):
                            ins = e.matmul(pt[:], lhsT=wt[:, k, cc * 128:(cc + 1) * 128], rhs=xT[:, k, qs],
                                           start=(k == 0), stop=(k == KC - 1))
                        return ins
                    P.op("pe", mm, reads=[wB, xB], writes=[pB])
                    ho, hoB = hor.next()
                    P.op("dve", lambda e, ho=ho, pt=pt, hi=hi: e.tensor_tensor(out=ho[:], in0=pt[:], in1=hi[:],
                                                                               op=ALU.add),
                         reads=[pB, hiB], writes=[hoB])
                    P.dma("sp", lambda e, ho=ho, ch=ch, qs=qs: e.dma_start(
                        out=g.hT[ch * 128:(ch + 1) * 128, qs], in_=ho[:]), hoB, reads=[hoB])
                    if False:
                        P.dma("sp", lambda e, ho=ho, ch=ch, qs=qs: e.dma_start(
                            out=g.H1[ch * 128:(ch + 1) * 128, qs], in_=ho[:]), hoB, reads=[hoB])
        P.finish()


def ph_up(g, l):
    nc, T = g.nc, g.T
    NQ = T // 512
    wu = g.w_up[l]
    with ExitStack() as es:
        P = Prog(nc, es, uid("up"))
        cw = sb(es, nc, [128, FC, 3], F32, "cw")
        cb = sb(es, nc, [128, FC], F32, "cb")
        cB = Buf()
        P.dma("pool", lambda e: e.dma_start(out=cw[:], in_=g.cw[l]), cB, writes=[cB])
        P.dma("pool", lambda e: e.dma_start(out=cb[:], in_=g.cb[l]), cB, writes=[cB])
        slabs = Rot(sbufs(es, nc, 4, [128, KC, 512], BF16, "wsl"))
        psr = Rot(psbufs(es, nc, 6, [128, 512], F32, "pp"))
        gts = [(sb(es, nc, [128, T + 2], F32, "gt"), Buf()) for _ in range(2)]
        vsr = Rot(sbufs(es, nc, 2, [128, T], F32, "vs"))
        cvr = Rot(sbufs(es, nc, 2, [128, T], F32, "cv"))
        uor = Rot(sbufs(es, nc, 2, [128, T], BF16, "uo"))
        for gt, gB in gts:
            P.op("dve", lambda e, gt=gt: e.memset(gt[:, 0:2], 0.0), writes=[gB])
        gtr = Rot(gts)
        loaded = []

        def load(s):
            ta, ba = slabs.next()
            P.dma("pool", lambda e, ta=ta, s=s: e.dma_start(out=ta[:], in_=slab_src(wu, s * 512, 512)), ba,
                  writes=[ba])
            tv, bv = slabs.next()
            P.dma("pool", lambda e, tv=tv, s=s: e.dma_start(out=tv[:], in_=slab_src(wu, DFF + s * 512, 512)), bv,
                  writes=[bv])
            loaded.append((ta, ba, tv, bv))
        load(0)
        for s in range(11):
            if s + 1 < 11:
                load(s + 1)
            ta, ba, tv, bv = loaded[s]
            for cc in range(4):
                j = s * 4 + cc
                gt, gB = gtr.next()
                vs, vB = vsr.next()
                for q in range(NQ):
                    qs = slice(q * 512, (q + 1) * 512)
                    pa, paB = psr.next()
                    pv, pvB = psr.next()

                    def mm(e, pa=pa, pv=pv, ta=ta, tv=tv, cc=cc, qs=qs):
                        for k in range(KC):
                            e.matmul(pa[:], lhsT=ta[:, k, cc * 128:(cc + 1) * 128], rhs=g.hnT[:, k, qs],
                                     start=(k == 0), stop=(k == KC - 1))
                        for k in range(KC):
                            ins = e.matmul(pv[:], lhsT=tv[:, k, cc * 128:(cc + 1) * 128], rhs=g.hnT[:, k, qs],
                                           start=(k == 0), stop=(k == KC - 1))
                        return ins
                    P.op("pe", mm, reads=[ba, bv], writes=[paB, pvB])
                    P.op("act", lambda e, gt=gt, pa=pa, q=q: e.activation(
                        out=gt[:, 2 + q * 512:2 + (q + 1) * 512], in_=pa[:], func=AF.Identity),
                        reads=[paB], writes=[gB])
                    P.op("act", lambda e, vs=vs, pv=pv, qs=qs: e.activation(out=vs[:, qs], in_=pv[:],
                                                                            func=AF.Identity),
                         reads=[pvB], writes=[vB])
                cv, cvB = cvr.next()

                P.op("dve", lambda e, cv=cv, gt=gt, j=j: e.tensor_scalar(
                    out=cv[:], in0=gt[:, 0:T], scalar1=cw[:, j, 0:1], scalar2=cb[:, j:j + 1], op0=ALU.mult,
                    op1=ALU.add), reads=[gB, cB], writes=[cvB])
                P.op("dve", lambda e, cv=cv, gt=gt, j=j: e.scalar_tensor_tensor(
                    out=cv[:], in0=gt[:, 1:T + 1], scalar=cw[:, j, 1:2], in1=cv[:], op0=ALU.mult, op1=ALU.add),
                    reads=[gB, cB, cvB], writes=[cvB])
                P.op("dve", lambda e, cv=cv, gt=gt, j=j: e.scalar_tensor_tensor(
                    out=cv[:], in0=gt[:, 2:T + 2], scalar=cw[:, j, 2:3], in1=cv[:], op0=ALU.mult, op1=ALU.add),
                    reads=[gB, cB, cvB], writes=[cvB])
                P.op("act", lambda e, cv=cv: e.activation(out=cv[:], in_=cv[:], func=AF.Silu),
                     reads=[cvB], writes=[cvB])
                uo, uoB = uor.next()
                P.op("dve", lambda e, uo=uo, cv=cv, vs=vs: e.tensor_tensor(out=uo[:], in0=cv[:], in1=vs[:],
                                                                           op=ALU.mult),
                     reads=[cvB, vB], writes=[uoB])
                P.dma("sp", lambda e, uo=uo, j=j: e.dma_start(out=g.UT[j * 128:(j + 1) * 128, :], in_=uo[:]), uoB,
                      reads=[uoB])
        P.finish()


def ph_down(g, l):
    nc, T = g.nc, g.T
    TT = min(T, 1024)
    NQ = TT // 512
    wd = g.w_down[l]
    with ExitStack() as es:
        P = Prog(nc, es, uid("dwn"))
        uT = sb(es, nc, [128, FC, TT], BF16, "uT")
        uB = Buf()
        slabs = Rot(sbufs(es, nc, 3, [128, FC, 128], BF16, "wsl"))
        psr = Rot(psbufs(es, nc, 4, [128, 512], F32, "pp"))
        hir = Rot(sbufs(es, nc, 3, [128, 512], F32, "hi"))
        hor = Rot(sbufs(es, nc, 3, [128, 512], F32, "ho"))
        for half in range(T // TT):
            t0 = half * TT
            P.dma("pool", lambda e, t0=t0: e.dma_start(out=uT[:], in_=fm(g.UT[:, t0:t0 + TT])), uB, writes=[uB])
            loaded = []

            def load(c):
                t, b = slabs.next()
                P.dma("pool", lambda e, t=t, c=c: e.dma_start(
                    out=t[:], in_=wd[:, c * 128:(c + 1) * 128].rearrange("(k p) n -> p k n", p=128)), b,
                    writes=[b])
                loaded.append((t, b))
            load(0)
            load(1)
            for c in range(KC):
                if c + 2 < KC:
                    load(c + 2)
                wt, wB = loaded[c]
                for q in range(NQ):
                    qs = slice(q * 512, (q + 1) * 512)
                    gs = slice(t0 + q * 512, t0 + (q + 1) * 512)
                    hi, hiB = hir.next()
                    P.dma("pool", lambda e, hi=hi, c=c, gs=gs: e.dma_start(
                        out=hi[:], in_=g.hT[c * 128:(c + 1) * 128, gs]), hiB, writes=[hiB])
                    pt, pB = psr.next()

                    def mm(e, pt=pt, wt=wt, qs=qs):
                        for k in range(FC):
                            ins = e.matmul(pt[:], lhsT=wt[:, k, :], rhs=uT[:, k, qs], start=(k == 0),
                                           stop=(k == FC - 1))
                        return ins
                    P.op("pe", mm, reads=[wB, uB], writes=[pB])
                    ho, hoB = hor.next()
                    P.op("dve", lambda e, ho=ho, pt=pt, hi=hi: e.tensor_tensor(out=ho[:], in0=pt[:], in1=hi[:],
                                                                               op=ALU.add),
                         reads=[pB, hiB], writes=[hoB])
                    P.dma("sp", lambda e, ho=ho, c=c, gs=gs: e.dma_start(
                        out=g.hT[c * 128:(c + 1) * 128, gs], in_=ho[:]), hoB, reads=[hoB])
        P.finish()


def build(T, L, debug=False):
    nc = bass.Bass("TRN2", target_bir_lowering=False)
    g = G()
    g.nc, g.T, g.L = nc, T, L
    g.debug = debug

    def din(name, shape, dt=F32):
        return nc.dram_tensor(name, list(shape), dt, kind="ExternalInput").ap()

    def scr(name, shape, dt):
        kind = "ExternalOutput" if debug else "Internal"
        return nc.dram_tensor(name, list(shape), dt, kind=kind).ap()
    g.xT = din("xT", [D, T])
    g.w_in = din("w_in", [L, D, NIN])
    g.w_bm = din("w_bm", [L, D, D])
    g.w_bd = din("w_bd", [L, D, D])
    g.w_o = din("w_o", [L, D, D])
    g.w_up = din("w_up", [L, D, 2 * DFF])
    g.w_down = din("w_down", [L, DFF, D])
    g.gmix = din("gmix", [L, 128, KC])
    g.gffn = din("gffn", [L, 128, KC])
    g.gfin = din("gfin", [128, KC])
    g.bqk = din("bqk", [L, 128, 48])
    g.bgate = din("bgate", [L, 128, 32])
    g.btm = din("btm", [L, 6144])
    g.bif = din("bif", [L, 16])
    g.mnorm = din("mnorm", [L, 2048])
    g.anorm = din("anorm", [L, 2048])
    g.lamqk = din("lamqk", [L, 512])
    g.cw = din("cw", [L, 128, FC, 3])
    g.cb = din("cb", [L, 128, FC])
    g.c_ident = din("c_ident", [128, 128])
    g.c_tri = din("c_tri", [128, 128])
    g.c_ones = din("c_ones", [128, 128])
    g.c_pm = din("c_pm", [32, 32])
    g.c_cos = din("c_cos", [32, T])
    g.c_sin = din("c_sin", [32, T])
    g.outT = nc.dram_tensor("outT", [D, T], F32, kind="ExternalOutput").ap()
    g.hT = scr("hT", [D, T], F32)
    g.QKs = scr("QKs", [48 * 128, T], BF16)
    g.MV = scr("MV", [T, 2048], BF16)
    g.MO = scr("MO", [T, 2048], BF16)
    g.AV = scr("AV", [T, 2048], BF16)
    g.HMT = scr("HMT", [D, T], BF16)
    g.HAT = scr("HAT", [D, T], BF16)
    g.Y1 = scr("Y1", [D, T], F32)
    g.YT = scr("YT", [D, T], BF16)
    g.UT = scr("UT", [DFF, T], BF16)
    if debug:
        g.H1 = scr("H1", [D, T], F32)
        g.HN2 = scr("HN2", [D, T], BF16)
    with ExitStack() as es:
        g.hnT = sb(es, nc, [128, KC, T], BF16, "hnT")
        g.IF = sb(es, nc, [128, T // 128, 16], F32, "IF")
        g.identb = sb(es, nc, [128, 128], BF16, "identb")
        g.trib = sb(es, nc, [128, 128], BF16, "trib")
        g.trif = sb(es, nc, [128, 128], F32, "trif")
        g.onesf = sb(es, nc, [128, 128], F32, "onesf")
        g.onesb = sb(es, nc, [128, 128], BF16, "onesb")
        g.pmf = sb(es, nc, [32, 32], F32, "pmf")
        ph_consts(g)
        for l in range(L):
            hsrc = g.xT if l == 0 else g.hT
            ph_norm(g, hsrc, g.gmix[l])
            ph_proj(g, l)
            ph_mlstm(g, l)
            ph_attn(g, l)
            ph_merge(g, l, 0)
            ph_merge(g, l, 1)
            ph_oproj(g, l, hsrc)
            if debug == 2:
                continue
            ph_norm(g, g.hT, g.gffn[l])
            ph_up(g, l)
            if debug == 3:
                continue
            ph_down(g, l)
        ph_norm(g, g.hT, g.gfin, final_dst=g.outT)
    return nc


def pcol(v, nch):
    v = np.asarray(v, dtype=np.float32)
    return np.ascontiguousarray(np.swapaxes(v.reshape(v.shape[:-1] + (nch, 128)), -1, -2))


def host_consts(T):
    s = np.arange(128)
    tri = (s[:, None] <= s[None, :]).astype(np.float32)
    pm = np.zeros((32, 32), np.float32)
    for j in range(16):
        pm[j + 16, j] = -1.0
        pm[j, j + 16] = 1.0
    half = 16
    inv = (np.float32(ROPE_THETA) ** (-np.arange(half, dtype=np.float32) / np.float32(half))).astype(np.float32)
    ang = np.arange(T, dtype=np.float32)[:, None] * inv[None, :]
    cos = np.cos(ang.astype(np.float64)).astype(np.float32).T
    sin = np.sin(ang.astype(np.float64)).astype(np.float32).T
    return {
        "c_ident": np.eye(128, dtype=np.float32), "c_tri": tri, "c_ones": np.ones((128, 128), np.float32),
        "c_pm": pm, "c_cos": np.ascontiguousarray(np.concatenate([cos, cos], 0)),
        "c_sin": np.ascontiguousarray(np.concatenate([sin, sin], 0)),
    }


def prep_shared(L, T, norm_mix, w_in, b_in, m_norm, a_norm, lam_qk, w_bm, w_bd, w_o, norm_ffn, w_up, conv_w, conv_b,
                w_down, norm_final):
    f = lambda a: np.ascontiguousarray(np.asarray(a, dtype=np.float32))
    b_in = f(b_in)
    bqk = np.concatenate([b_in[:, O_MQ:O_MQ + 2048], b_in[:, O_AQ:O_AQ + 4096]], axis=1)
    bgate = b_in[:, O_GM:O_GM + 4096]
    btm = np.concatenate([b_in[:, O_MV:O_MV + 4096], b_in[:, O_AV:O_AV + 2048]], axis=1)
    cwp = np.stack([pcol(f(conv_w)[:, i, :], FC) for i in range(3)], axis=-1)
    d = {
        "w_in": f(w_in)[:L], "w_bm": f(w_bm)[:L], "w_bd": f(w_bd)[:L], "w_o": f(w_o)[:L], "w_up": f(w_up)[:L],
        "w_down": f(w_down)[:L],
        "gmix": pcol(norm_mix, KC)[:L], "gffn": pcol(norm_ffn, KC)[:L], "gfin": pcol(norm_final, KC),
        "bqk": pcol(bqk, 48)[:L], "bgate": pcol(bgate, 32)[:L], "btm": f(btm)[:L],
        "bif": f(b_in[:, O_MI:O_MI + 16])[:L], "mnorm": f(m_norm)[:L], "anorm": f(a_norm)[:L],
        "lamqk": f(lam_qk).reshape(-1, 512)[:L], "cw": np.ascontiguousarray(cwp)[:L], "cb": pcol(conv_b, FC)[:L],
    }
    d.update(host_consts(T))
    return {k: np.ascontiguousarray(v) for k, v in d.items()}


def kernel(x, norm_mix, w_in, b_in, m_norm, a_norm, lam_qk, w_bm, w_bd, w_o, norm_ffn, w_up, conv_w, conv_b, w_down,
           norm_final):
    x = np.asarray(x, dtype=np.float32)
    B, S, _ = x.shape
    L = np.asarray(norm_mix).shape[0]
    shared = prep_shared(L, S, norm_mix, w_in, b_in, m_norm, a_norm, lam_qk, w_bm, w_bd, w_o, norm_ffn, w_up, conv_w,
                         conv_b, w_down, norm_final)
    nc = build(S, L)
    in_maps = []
    for b in range(B):
        m = dict(shared)
        m["xT"] = np.ascontiguousarray(x[b].T)
        in_maps.append(m)
    res = run_bass_kernel_spmd(nc, in_maps, core_ids=list(range(B)))
    out = np.stack([np.ascontiguousarray(res.results[b]["outT"].T) for b in range(B)], axis=0)
    return out.astype(np.float32)
```

```python
import math
from contextlib import ExitStack

import numpy as np
import concourse.bass as bass
import concourse.mybir as mybir
from concourse.bass_utils import run_bass_kernel_spmd

F32 = mybir.dt.float32
BF16 = mybir.dt.bfloat16
AF = mybir.ActivationFunctionType
ALU = mybir.AluOpType

D = 2048
KC = 16
H = 8
NIN = 16400
DFF = 5632
FC = 44
EPS = 1e-6
O_MQ, O_MK, O_MV, O_MO, O_MI, O_AQ, O_AK, O_AV, O_GM, O_GD = (
    0, 1024, 2048, 4096, 6144, 6160, 8208, 10256, 12304, 14352)
ROPE_THETA = 500000.0


class Buf:
    __slots__ = ("w", "r", "dsem", "dcnt")

    def __init__(self):
        self.w = None
        self.r = {}
        self.dsem = None
        self.dcnt = 0


class Prog:
    ENGS = ("pe", "act", "dve", "pool", "sp")

    def __init__(self, nc, es, name):
        self.nc, self.es, self.name = nc, es, name
        self.q = {e: [] for e in self.ENGS}
        self.allsems = []
        self.sem = {e: self._newsem(f"{name}_{e}") for e in self.ENGS}
        self.cnt = {e: 0 for e in self.ENGS}
        self.waited = {e: {} for e in self.ENGS}
        self.dsems = []
        self.nd = 0

    def _newsem(self, name):
        h = self.nc.alloc_semaphore(name=name)
        self.allsems.append(h)
        return h

    def _wait(self, eng, ev):
        sem, val = ev
        if eng == "pe" and sem is self.sem["pe"]:
            return
        w = self.waited[eng]
        k = id(sem)
        if w.get(k, 0) >= val:
            return
        w[k] = val
        self.q[eng].append((0, sem, val))

    def _deps(self, eng, reads, writes):
        for b in reads:
            if b.w is not None:
                self._wait(eng, b.w)
        for b in writes:
            if b.w is not None:
                self._wait(eng, b.w)
            for ev in b.r.values():
                self._wait(eng, ev)

    @staticmethod
    def _commit(ev, reads, writes):
        for b in reads:
            b.r[id(ev[0])] = ev
        for b in writes:
            b.w = ev
            b.r = {}

    def op(self, eng, fn, reads=(), writes=()):
        self._deps(eng, reads, writes)
        self.cnt[eng] += 1
        ev = (self.sem[eng], self.cnt[eng])
        self.q[eng].append((1, fn, self.sem[eng], 1))
        self._commit(ev, reads, writes)
        return ev

    def dma(self, qeng, fn, sb, reads=(), writes=()):
        self._deps(qeng, reads, writes)
        if sb.dsem is None:
            self.nd += 1
            sb.dsem = self._newsem(f"{self.name}_d{self.nd}")
            sb.dcnt = 0
            self.dsems.append(sb)
        sb.dcnt += 16
        ev = (sb.dsem, sb.dcnt)
        self.q[qeng].append((1, fn, sb.dsem, 16))
        self._commit(ev, reads, writes)
        return ev

    def finish(self):
        for e in self.ENGS:
            for e2 in self.ENGS:
                if e2 != e and self.cnt[e2] > 0:
                    self._wait(e, (self.sem[e2], self.cnt[e2]))
            for sb in self.dsems:
                self._wait(e, (sb.dsem, sb.dcnt))
        for sb in self.dsems:
            sb.dsem = None
            sb.dcnt = 0
            sb.w = None
            sb.r = {}
        with self.nc.Block() as blk:
            blk.tensor(lambda e: self._emit("pe", e))
            blk.scalar(lambda e: self._emit("act", e))
            blk.vector(lambda e: self._emit("dve", e))
            blk.gpsimd(lambda e: self._emit("pool", e))
            blk.sync(lambda e: self._emit("sp", e))
        self.nc.all_engine_barrier()
        self.nc.clear_and_free_semaphores(self.allsems)
        self.nc.all_engine_barrier()

    def _emit(self, eng, e):
        for it in self.q[eng]:
            if it[0] == 0:
                e.wait_ge(it[1], it[2])
            else:
                it[1](e).then_inc(it[2], it[3])


class Rot:
    def __init__(self, items):
        self.items = items
        self.i = 0

    def next(self):
        x = self.items[self.i]
        self.i = (self.i + 1) % len(self.items)
        return x


class G:
    pass


_uid = [0]


def uid(p):
    _uid[0] += 1
    return f"{p}{_uid[0]}"


def sb(es, nc, shape, dt, name="t"):
    return es.enter_context(nc.sbuf_tensor(uid(name), list(shape), dt))


def pst(es, nc, shape, dt, name="p"):
    return es.enter_context(nc.psum_tensor(uid(name), list(shape), dt))


def sbufs(es, nc, n, shape, dt, name="t"):
    return [(sb(es, nc, shape, dt, name), Buf()) for _ in range(n)]


def psbufs(es, nc, n, shape, dt, name="p"):
    return [(pst(es, nc, shape, dt, name), Buf()) for _ in range(n)]


def fm(ap):
    return ap.rearrange("(k p) t -> p k t", p=128)


def ph_consts(g):
    nc = g.nc
    with ExitStack() as es:
        P = Prog(nc, es, uid("cst"))
        for t, src in ((g.identb, g.c_ident), (g.trib, g.c_tri), (g.trif, g.c_tri), (g.onesf, g.c_ones),
                       (g.onesb, g.c_ones), (g.pmf, g.c_pm)):
            P.dma("pool", lambda e, t=t, src=src: e.dma_start(out=t[:], in_=src), Buf(), writes=[])
        P.finish()


def ph_norm(g, src, gam_src, final_dst=None):
    nc, T = g.nc, g.T
    W = 256
    with ExitStack() as es:
        P = Prog(nc, es, uid("nrm"))
        hb = Rot(sbufs(es, nc, 2, [128, KC, W], F32, "hb"))
        sq = sb(es, nc, [128, KC, W], BF16, "sq")
        sqA, sqB = Buf(), Buf()
        gm = sb(es, nc, [128, KC], F32, "gm")
        gmB = Buf()
        rs = Rot(sbufs(es, nc, 2, [128, W], F32, "rs"))
        rstd = Rot(sbufs(es, nc, 2, [128, W], F32, "rstd"))
        ps = Rot(psbufs(es, nc, 2, [128, W], F32, "nps"))
        ob = Rot(sbufs(es, nc, 2, [128, KC, W], F32, "ob")) if final_dst is not None else None
        P.dma("pool", lambda e: e.dma_start(out=gm[:], in_=gam_src), gmB, writes=[gmB])
        for j in range(T // W):
            ht, hB = hb.next()
            P.dma("pool", lambda e, ht=ht, j=j: e.dma_start(out=ht[:], in_=fm(src[:, j * W:(j + 1) * W])), hB,
                  writes=[hB])
            P.op("act", lambda e, ht=ht: e.activation(out=sq[:, 0:8, :], in_=ht[:, 0:8, :], func=AF.Square),
                 reads=[hB], writes=[sqA])
            P.op("dve", lambda e, ht=ht: e.tensor_tensor(out=sq[:, 8:16, :], in0=ht[:, 8:16, :], in1=ht[:, 8:16, :],
                                                         op=ALU.mult), reads=[hB], writes=[sqB])
            pt, pB = ps.next()

            def mm(e, pt=pt):
                for k in range(KC):
                    ins = e.matmul(pt[:], lhsT=g.onesb[:], rhs=sq[:, k, :], start=(k == 0), stop=(k == KC - 1))
                return ins
            P.op("pe", mm, reads=[sqA, sqB], writes=[pB])
            rt, rB = rs.next()
            P.op("act", lambda e, rt=rt, pt=pt: e.activation(out=rt[:], in_=pt[:], func=AF.Sqrt, bias=EPS,
                                                             scale=1.0 / D), reads=[pB], writes=[rB])
            st, sB = rstd.next()
            P.op("dve", lambda e, st=st, rt=rt: e.reciprocal(out=st[:], in_=rt[:]), reads=[rB], writes=[sB])
            if final_dst is None:
                def nrm(e, ht=ht, st=st, j=j):
                    for k in range(KC):
                        ins = e.scalar_tensor_tensor(out=g.hnT[:, k, j * W:(j + 1) * W], in0=ht[:, k, :],
                                                     scalar=gm[:, k:k + 1], in1=st[:], op0=ALU.mult, op1=ALU.mult)
                    return ins
                P.op("dve", nrm, reads=[hB, sB, gmB], writes=[Buf()])
            else:
                ot, oB = ob.next()

                def nrm(e, ht=ht, st=st, ot=ot):
                    for k in range(KC):
                        ins = e.scalar_tensor_tensor(out=ot[:, k, :], in0=ht[:, k, :],
                                                     scalar=gm[:, k:k + 1], in1=st[:], op0=ALU.mult, op1=ALU.mult)
                    return ins
                P.op("dve", nrm, reads=[hB, sB, gmB], writes=[oB])
                P.dma("sp", lambda e, ot=ot, j=j: e.dma_start(out=fm(final_dst[:, j * W:(j + 1) * W]), in_=ot[:]),
                      oB, reads=[oB])
        P.finish()


def slab_src(w2d, c0, cw):
    return w2d[:, c0:c0 + cw].rearrange("(k p) n -> p k n", p=128)


def ph_proj(g, l):
    nc, T = g.nc, g.T
    NQ, NT = T // 512, T // 128
    w = g.w_in[l]
    with ExitStack() as es:
        P = Prog(nc, es, uid("prj"))
        slabs = Rot(sbufs(es, nc, 3, [128, KC, 512], BF16, "wsl"))
        psr = Rot(psbufs(es, nc, 4, [128, 512], F32, "pp"))
        pxp = Rot(psbufs(es, nc, 2, [32, 512], F32, "px"))
        stg = Rot(sbufs(es, nc, 4, [128, 512], BF16, "stg"))
        xbr = Rot(sbufs(es, nc, 2, [128, 512], F32, "xb"))
        t1r = Rot(sbufs(es, nc, 2, [32, 512], F32, "t1"))
        t2r = Rot(sbufs(es, nc, 2, [32, 512], F32, "t2"))
        tmr = Rot(sbufs(es, nc, 2, [128, 512], F32, "tm"))
        cosT = sb(es, nc, [32, T], F32, "cos")
        sinT = sb(es, nc, [32, T], F32, "sin")
        bqk = sb(es, nc, [128, 48], F32, "bqk")
        brep = sb(es, nc, [128, 6144], F32, "brep")
        bif = sb(es, nc, [128, 16], F32, "bif")
        wif = sb(es, nc, [128, KC, 16], BF16, "wif")
        cB = Buf()
        for t, src in ((cosT, g.c_cos), (sinT, g.c_sin), (bqk, g.bqk[l]), (brep, g.btm[l].partition_broadcast(128)),
                       (bif, g.bif[l].partition_broadcast(128)), (wif, slab_src(w, O_MI, 16))):
            P.dma("pool", lambda e, t=t, src=src: e.dma_start(out=t[:], in_=src), cB, writes=[cB])
        jobs = []
        for s in range(2):
            jobs.append((O_MQ + s * 512, "fm", ("m", 0 + s * 4)))
        for s in range(2):
            jobs.append((O_MK + s * 512, "fm", ("m", 8 + s * 4)))
        for s in range(4):
            jobs.append((O_AQ + s * 512, "fm", ("aq", 16 + s * 4)))
        for s in range(4):
            jobs.append((O_AK + s * 512, "fm", ("ak", 32 + s * 4)))
        for s in range(4):
            jobs.append((O_MV + s * 512, "tm", ("mv", s)))
        for s in range(4):
            jobs.append((O_MO + s * 512, "tm", ("mo", s)))
        for s in range(4):
            jobs.append((O_AV + s * 512, "tm", ("av", s)))
        loaded = []

        def load(i):
            c0 = jobs[i][0]
            t, b = slabs.next()
            P.dma("pool", lambda e, t=t, c0=c0: e.dma_start(out=t[:], in_=slab_src(w, c0, 512)), b, writes=[b])
            loaded.append((t, b))
        load(0)
        load(1)
        for tt in range(NT):
            pt, pB = psr.next()

            def mm(e, pt=pt, tt=tt):
                for k in range(KC):
                    ins = e.matmul(pt[:, 0:16], lhsT=g.hnT[:, k, tt * 128:(tt + 1) * 128], rhs=wif[:, k, :],
                                   start=(k == 0), stop=(k == KC - 1))
                return ins
            P.op("pe", mm, reads=[cB], writes=[pB])
            P.op("dve", lambda e, pt=pt, tt=tt: e.tensor_tensor(out=g.IF[:, tt, :], in0=pt[:, 0:16], in1=bif[:],
                                                                op=ALU.add), reads=[pB, cB], writes=[Buf()])
        for i, (c0, kind, info) in enumerate(jobs):
            if i + 2 < len(jobs):
                load(i + 2)
            wt, wB = loaded[i]
            if kind == "fm":
                typ, ch0 = info
                for cc in range(4):
                    ch = ch0 + cc
                    for q in range(NQ):
                        pt, pB = psr.next()

                        def mm(e, pt=pt, wt=wt, cc=cc, q=q):
                            for k in range(KC):
                                ins = e.matmul(pt[:], lhsT=wt[:, k, cc * 128:(cc + 1) * 128],
                                               rhs=g.hnT[:, k, q * 512:(q + 1) * 512],
                                               start=(k == 0), stop=(k == KC - 1))
                            return ins
                        P.op("pe", mm, reads=[wB], writes=[pB])
                        st, sB = stg.next()
                        if typ == "m":
                            P.op("act", lambda e, st=st, pt=pt, ch=ch: e.activation(
                                out=st[:], in_=pt[:], func=AF.Identity, bias=bqk[:, ch:ch + 1]),
                                reads=[pB, cB], writes=[sB])
                        else:
                            xb, xB = xbr.next()
                            P.op("act", lambda e, xb=xb, pt=pt, ch=ch: e.activation(
                                out=xb[:], in_=pt[:], func=AF.Identity, bias=bqk[:, ch:ch + 1]),
                                reads=[pB, cB], writes=[xB])
                            px, pxB = pxp.next()
                            P.op("pe", lambda e, px=px, xb=xb: e.matmul(px[:], lhsT=g.pmf[:], rhs=xb[0:32, :],
                                                                        start=True, stop=True),
                                 reads=[xB], writes=[pxB])
                            t1, t1B = t1r.next()
                            t2, t2B = t2r.next()
                            P.op("dve", lambda e, t1=t1, px=px, q=q: e.tensor_tensor(
                                out=t1[:], in0=px[:], in1=sinT[:, q * 512:(q + 1) * 512], op=ALU.mult),
                                reads=[pxB, cB], writes=[t1B])
                            P.op("dve", lambda e, t2=t2, xb=xb, q=q: e.tensor_tensor(
                                out=t2[:], in0=xb[0:32, :], in1=cosT[:, q * 512:(q + 1) * 512], op=ALU.mult),
                                reads=[xB, cB], writes=[t2B])
                            P.op("dve", lambda e, t1=t1, t2=t2, xb=xb: e.tensor_tensor(
                                out=xb[0:32, :], in0=t1[:], in1=t2[:], op=ALU.add),
                                reads=[t1B, t2B], writes=[xB])
                            sc = (128.0 ** -0.5) if typ == "aq" else 1.0
                            P.op("act", lambda e, st=st, xb=xb, sc=sc: e.activation(
                                out=st[:], in_=xb[:], func=AF.Identity, scale=sc), reads=[xB], writes=[sB])
                        P.dma("sp", lambda e, st=st, ch=ch, q=q: e.dma_start(
                            out=g.QKs[ch * 128:(ch + 1) * 128, q * 512:(q + 1) * 512], in_=st[:]), sB, reads=[sB])
            else:
                typ, s = info
                dst = {"mv": g.MV, "mo": g.MO, "av": g.AV}[typ]
                boff = {"mv": 0, "mo": 2048, "av": 4096}[typ] + s * 512
                for tt in range(NT):
                    pt, pB = psr.next()

                    def mm(e, pt=pt, wt=wt, tt=tt):
                        for k in range(KC):
                            ins = e.matmul(pt[:], lhsT=g.hnT[:, k, tt * 128:(tt + 1) * 128], rhs=wt[:, k, :],
                                           start=(k == 0), stop=(k == KC - 1))
                        return ins
                    P.op("pe", mm, reads=[wB], writes=[pB])
                    st, sB = stg.next()
                    if typ == "mo":
                        tm, tB = tmr.next()
                        P.op("dve", lambda e, tm=tm, pt=pt, boff=boff: e.tensor_tensor(
                            out=tm[:], in0=pt[:], in1=brep[:, boff:boff + 512], op=ALU.add),
                            reads=[pB, cB], writes=[tB])
                        P.op("act", lambda e, st=st, tm=tm: e.activation(out=st[:], in_=tm[:], func=AF.Sigmoid),
                             reads=[tB], writes=[sB])
                    else:
                        P.op("dve", lambda e, st=st, pt=pt, boff=boff: e.tensor_tensor(
                            out=st[:], in0=pt[:], in1=brep[:, boff:boff + 512], op=ALU.add),
                            reads=[pB, cB], writes=[sB])
                    P.dma("sp", lambda e, st=st, dst=dst, tt=tt, s=s: e.dma_start(
                        out=dst[tt * 128:(tt + 1) * 128, s * 512:(s + 1) * 512], in_=st[:]), sB, reads=[sB])
        P.finish()


def tm_src(ap2d, h, NT):
    return ap2d[:, h * 256:(h + 1) * 256].rearrange("(t p) d -> p t d", p=128)


def ph_mlstm(g, l):
    nc, T = g.nc, g.T
    NT = T // 128
    with ExitStack() as es:
        P = Prog(nc, es, uid("mls"))
        NG = NT * 8
        def v3(ap):
            return ap.rearrange("p (c h) -> p c h", h=8)
        e1 = sb(es, nc, [128, NG], F32, "e1")
        l1 = sb(es, nc, [128, NG], F32, "l1")
        tg = sb(es, nc, [128, NG], F32, "tg")
        u = sb(es, nc, [128, NG], F32, "u")
        bnd = sb(es, nc, [128, NG], F32, "bnd")
        eg = sb(es, nc, [128, NG], F32, "eg")
        mrep = sb(es, nc, [128, 2048], F32, "mrep")
        cgp = pst(es, nc, [128, 2, NG], F32, "cgp")
        gB = Buf()
        P.dma("pool", lambda e: e.dma_start(out=mrep[:], in_=g.mnorm[l].partition_broadcast(128)), gB,
              writes=[gB])
        P.op("act", lambda e: e.activation(out=v3(e1[:]), in_=g.IF[:, :, 8:16], func=AF.Exp, scale=-1.0),
             writes=[gB])
        P.op("act", lambda e: e.activation(out=l1[:], in_=e1[:], func=AF.Ln, bias=1.0), reads=[gB], writes=[gB])

        def mmg(e):
            e.matmul(cgp[:, 0, :], lhsT=g.trif[:], rhs=l1[:], start=True, stop=True)
            return e.matmul(cgp[:, 1, :], lhsT=g.onesf[:], rhs=l1[:], start=True, stop=True)
        P.op("pe", mmg, reads=[gB], writes=[gB])
        P.op("dve", lambda e: e.tensor_tensor(out=v3(tg[:]), in0=v3(cgp[:, 0, :]), in1=g.IF[:, :, 0:8], op=ALU.add),
             reads=[gB], writes=[gB])

        def gex(e):
            e.activation(out=u[:], in_=tg[:], func=AF.Exp, bias=math.log(128.0 ** -0.5))
            e.activation(out=bnd[:], in_=cgp[:, 0, :], func=AF.Exp)
            return e.activation(out=eg[:], in_=cgp[:, 1, :], func=AF.Exp, scale=-1.0)
        P.op("act", gex, reads=[gB], writes=[gB])

        qkr = Rot([(sb(es, nc, [128, T], BF16, "qT"), sb(es, nc, [128, T], BF16, "kT"), Buf(), Buf())
                   for _ in range(2)])
        var = [(sb(es, nc, [128, NT, 257], BF16, "vA"), Buf()) for _ in range(2)]
        gr = Rot(sbufs(es, nc, 2, [128, NT, 256], BF16, "Gg"))
        hms = Rot(sbufs(es, nc, 2, [128, 2, T], BF16, "hms"))
        C = sb(es, nc, [128, 257], F32, "C")
        C1 = sb(es, nc, [128, 257], F32, "C1")
        Cb = sb(es, nc, [128, 257], BF16, "Cb")
        CB, C1B, CbB = Buf(), Buf(), Buf()
        ptr = Rot(sbufs(es, nc, 2, [128, 128], BF16, "PT"))
        kur = Rot(sbufs(es, nc, 2, [128, 128], BF16, "ku"))
        smr = Rot(sbufs(es, nc, 2, [128, 8], F32, "sm"))
        jkr = Rot(sbufs(es, nc, 2, [128, 256], F32, "jk"))
        hmr = Rot(sbufs(es, nc, 2, [128, 256], BF16, "hm"))
        stp = Rot(psbufs(es, nc, 2, [128, 128], F32, "stp"))
        ktp = Rot(psbufs(es, nc, 1, [128, 128], BF16, "ktp"))
        outp = Rot(psbufs(es, nc, 2, [128, 257], F32, "outp"))
        updp = Rot(psbufs(es, nc, 1, [128, 257], F32, "updp"))
        trp = Rot(psbufs(es, nc, 1, [128, 2, 128], BF16, "trp"))
        for vt, vB in var:
            P.op("dve", lambda e, vt=vt: e.memset(vt[:, :, 256:257], 1.0), writes=[vB])
        vr = Rot(var)
        bg = []
        eps = {}

        def advance():
            for gen in list(bg):
                try:
                    next(gen)
                except StopIteration:
                    bg.remove(gen)

        for h in range(H):
            eps.clear()
            qT, kT, qB, kB = qkr.next()
            qkB = qB
            vA, vB = vr.next()
            Gt, GB = gr.next()
            hs, hsB = hms.next()
            P.dma("pool", lambda e, qT=qT, h=h: e.dma_start(out=qT[:], in_=g.QKs[h * 128:(h + 1) * 128, :]), qB,
                  writes=[qB])
            P.dma("pool", lambda e, kT=kT, h=h: e.dma_start(out=kT[:], in_=g.QKs[(8 + h) * 128:(9 + h) * 128, :]),
                  kB, writes=[kB])
            P.dma("pool", lambda e, vA=vA, h=h: e.dma_start(out=vA[:, :, 0:256], in_=tm_src(g.MV, h, NT)), vB,
                  writes=[vB])
            P.dma("pool", lambda e, Gt=Gt, h=h: e.dma_start(out=Gt[:], in_=tm_src(g.MO, h, NT)), GB,
                  writes=[GB])

            def gmul(e, Gt=Gt, h=h):
                for c in range(NT):
                    ins = e.tensor_tensor(out=Gt[:, c, :], in0=Gt[:, c, :], in1=mrep[:, h * 256:(h + 1) * 256],
                                          op=ALU.mult)
                return ins
            P.op("dve", gmul, reads=[gB], writes=[GB])
            for c in range(NT):
                cs = slice(c * 128, (c + 1) * 128)
                uc = u[:, c * 8 + h:c * 8 + h + 1]
                st, stB = stp.next()
                P.op("pe", lambda e, st=st, kT=kT, qT=qT, cs=cs: e.matmul(st[:], lhsT=kT[:, cs], rhs=qT[:, cs],
                                                                          start=True, stop=True),
                     reads=[qB, kB], writes=[stB])
                advance()
                pt, ptB = ptr.next()
                P.op("dve", lambda e, pt=pt, st=st, uc=uc: e.scalar_tensor_tensor(
                    out=pt[:], in0=st[:], scalar=uc, in1=g.trif[:], op0=ALU.mult, op1=ALU.mult),
                    reads=[stB, gB], writes=[ptB])
                kt, ktB = ktp.next()
                P.op("pe", lambda e, kt=kt, kT=kT, cs=cs: e.transpose(kt[:], kT[:, cs], g.identb[:]),
                     reads=[kB], writes=[ktB])
                advance()
                ku, kuB = kur.next()
                P.op("act", lambda e, ku=ku, kt=kt, uc=uc: e.activation(out=ku[:], in_=kt[:], func=AF.Identity,
                                                                        scale=uc),
                     reads=[ktB, gB], writes=[kuB])
                while eps.get(c - 2) in bg:
                    advance()
                ot, oB = outp.next()

                def mmo(e, ot=ot, pt=pt, vA=vA, qT=qT, c=c, cs=cs):
                    ins = e.matmul(ot[:], lhsT=pt[:], rhs=vA[:, c, :], start=True, stop=(c == 0))
                    if c > 0:
                        ins = e.matmul(ot[:], lhsT=qT[:, cs], rhs=Cb[:], start=False, stop=True)
                    return ins
                P.op("pe", mmo, reads=[ptB, vB, qB] + ([CbB] if c > 0 else []), writes=[oB])
                advance()
                if c < NT - 1:
                    up, upB = updp.next()
                    P.op("pe", lambda e, up=up, ku=ku, vA=vA, c=c: e.matmul(up[:], lhsT=ku[:], rhs=vA[:, c, :],
                                                                            start=True, stop=True),
                         reads=[kuB, vB], writes=[upB])
                    egc = eg[:, c * 8 + h:c * 8 + h + 1]
                    if c == 0:
                        P.op("dve", lambda e, up=up, egc=egc: e.tensor_scalar(
                            out=C[:], in0=up[:], scalar1=egc, scalar2=None, op0=ALU.mult),
                            reads=[upB, gB], writes=[CB])
                    else:
                        P.op("act", lambda e, egc=egc: e.activation(out=C1[:], in_=C[:], func=AF.Identity,
                                                                    scale=egc),
                             reads=[CB, gB], writes=[C1B])
                        P.op("dve", lambda e, up=up, egc=egc: e.scalar_tensor_tensor(
                            out=C[:], in0=up[:], scalar=egc, in1=C1[:], op0=ALU.mult, op1=ALU.add),
                            reads=[upB, C1B, gB], writes=[CB])
                    P.op("act", lambda e: e.activation(out=Cb[:], in_=C[:], func=AF.Identity),
                         reads=[CB], writes=[CbB])
                def epilogue(c=c, cs=cs, ot=ot, oB=oB):
                    sm, smB = smr.next()
                    P.op("act", lambda e, sm=sm, ot=ot: e.activation(out=sm[:, 0:1], in_=ot[:, 256:257], func=AF.Abs),
                         reads=[oB], writes=[smB])
                    yield
                    P.op("dve", lambda e, sm=sm, c=c, h=h: e.tensor_scalar(
                        out=sm[:, 1:2], in0=sm[:, 0:1], scalar1=bnd[:, c * 8 + h:c * 8 + h + 1], scalar2=None, op0=ALU.max),
                        reads=[smB, gB], writes=[smB])
                    yield
                    P.op("dve", lambda e, sm=sm: e.reciprocal(out=sm[:, 2:3], in_=sm[:, 1:2]),
                         reads=[smB], writes=[smB])
                    yield
                    jk, jkB = jkr.next()
                    P.op("act", lambda e, jk=jk, ot=ot, sm=sm: e.activation(
                        out=jk[:], in_=ot[:, 0:256], func=AF.Square, scale=sm[:, 2:3], accum_out=sm[:, 3:4]),
                        reads=[oB, smB], writes=[jkB, smB])
                    yield
                    P.op("act", lambda e, sm=sm: e.activation(out=sm[:, 4:5], in_=sm[:, 3:4], func=AF.Ln, bias=EPS,
                                                              scale=1.0 / 256), reads=[smB], writes=[smB])
                    yield
                    P.op("act", lambda e, sm=sm: e.activation(out=sm[:, 5:6], in_=sm[:, 4:5], func=AF.Exp,
                                                              scale=-0.5), reads=[smB], writes=[smB])
                    yield
                    P.op("dve", lambda e, sm=sm: e.tensor_tensor(out=sm[:, 6:7], in0=sm[:, 5:6], in1=sm[:, 2:3],
                                                                 op=ALU.mult), reads=[smB], writes=[smB])
                    yield
                    hm, hmB = hmr.next()
                    P.op("dve", lambda e, hm=hm, ot=ot, sm=sm, Gt=Gt, c=c: e.scalar_tensor_tensor(
                        out=hm[:], in0=ot[:, 0:256], scalar=sm[:, 6:7], in1=Gt[:, c, :], op0=ALU.mult, op1=ALU.mult),
                        reads=[oB, smB, GB], writes=[hmB])
                    yield
                    tr, trB = trp.next()

                    def trs(e, tr=tr, hm=hm):
                        e.transpose(tr[:, 0, :], hm[:, 0:128], g.identb[:])
                        return e.transpose(tr[:, 1, :], hm[:, 128:256], g.identb[:])
                    P.op("pe", trs, reads=[hmB], writes=[trB])
                    yield
                    P.op("act", lambda e, hs=hs, tr=tr, cs=cs: e.activation(out=hs[:, :, cs], in_=tr[:],
                                                                            func=AF.Identity),
                         reads=[trB], writes=[hsB])

                gen_ = epilogue()
                eps[c] = gen_
                bg.append(gen_)

            while bg:
                advance()
            P.dma("sp", lambda e, hs=hs, h=h: e.dma_start(
                out=g.HMT[h * 256:(h + 1) * 256, :].rearrange("(j p) t -> p j t", p=128), in_=hs[:]), hsB,
                reads=[hsB])
        P.finish()


def ph_attn(g, l):
    nc, T = g.nc, g.T
    NT, NG = T // 128, T // 256
    lam_init = 0.8 - 0.6 * math.exp(-0.3 * l)
    with ExitStack() as es:
        P = Prog(nc, es, uid("att"))
        lq = sb(es, nc, [128, 4, 128], F32, "lq")
        lp = sb(es, nc, [128, 2, 128], F32, "lp")
        ls = sb(es, nc, [128, 8], F32, "ls")
        anrep = sb(es, nc, [128, 2048], F32, "anrep")
        cB = Buf()
        P.dma("pool", lambda e: e.dma_start(out=lq[:].rearrange("p a b -> p (a b)"),
                                            in_=g.lamqk[l].partition_broadcast(128)), cB, writes=[cB])
        P.dma("pool", lambda e: e.dma_start(out=anrep[:], in_=g.anorm[l].partition_broadcast(128)), cB,
              writes=[cB])

        def lam1(e):
            e.tensor_tensor(out=lp[:, 0, :], in0=lq[:, 0, :], in1=lq[:, 1, :], op=ALU.mult)
            return e.tensor_tensor(out=lp[:, 1, :], in0=lq[:, 2, :], in1=lq[:, 3, :], op=ALU.mult)
        P.op("dve", lam1, reads=[cB], writes=[cB])

        def lam2(e):
            e.reduce_sum(out=ls[:, 0:1], in_=lp[:, 0, :], axis=mybir.AxisListType.X)
            return e.reduce_sum(out=ls[:, 1:2], in_=lp[:, 1, :], axis=mybir.AxisListType.X)
        P.op("dve", lam2, reads=[cB], writes=[cB])
        P.op("act", lambda e: e.activation(out=ls[:, 2:4], in_=ls[:, 0:2], func=AF.Exp), reads=[cB], writes=[cB])
        P.op("dve", lambda e: e.scalar_tensor_tensor(out=ls[:, 4:5], in0=ls[:, 3:4], scalar=-lam_init,
                                                     in1=ls[:, 2:3], op0=ALU.add, op1=ALU.subtract),
             reads=[cB], writes=[cB])
        P.op("dve", lambda e: e.tensor_scalar(out=anrep[:], in0=anrep[:], scalar1=(1.0 - lam_init), scalar2=None,
                                              op0=ALU.mult), reads=[cB], writes=[cB])
        nlam = ls[:, 4:5]

        inb = []
        for i in range(2):
            vt = sb(es, nc, [128, NT, 257], BF16, "vA")
            vB = Buf()
            P.op("dve", lambda e, vt=vt: e.memset(vt[:, :, 256:257], 1.0), writes=[vB])
            inb.append(dict(q2=sb(es, nc, [128, 2, T], BF16, "q2"), k2=sb(es, nc, [128, 2, T], BF16, "k2"),
                            qB=Buf(), kB=Buf(), vA=vt, vB=vB, hs=sb(es, nc, [128, 2, T], BF16, "has"), hsB=Buf()))
        inr = Rot(inb)
        ptr = Rot(sbufs(es, nc, 4, [128, 2, 256], BF16, "PT"))
        rrr = Rot(sbufs(es, nc, 5, [128, 8], F32, "rr"))
        c0r = Rot(sbufs(es, nc, 5, [128, 257], F32, "c0"))
        c1r = Rot(sbufs(es, nc, 5, [128, 257], F32, "c1"))
        a0r = Rot(sbufs(es, nc, 5, [128, 256], F32, "a0"))
        a1r = Rot(sbufs(es, nc, 5, [128, 256], F32, "a1"))
        jkr = Rot(sbufs(es, nc, 5, [128, 256], F32, "jk"))
        habr = Rot(sbufs(es, nc, 5, [128, 256], BF16, "hab"))
        stp = Rot(psbufs(es, nc, 3, [128, 2, 256], F32, "stp"))
        outp = [[(pst(es, nc, [128, 257], F32, "outp"), Buf()) for j in range(2)] for m in range(2)]
        trp = Rot(psbufs(es, nc, 1, [128, 2, 128], BF16, "trp"))

        hb = {}

        def ensure_head(h):
            if h in hb or h >= H:
                return
            b = inr.next()
            c0 = (16 + 2 * h) * 128
            c1 = (32 + 2 * h) * 128
            P.dma("pool", lambda e: e.dma_start(
                out=b["q2"][:], in_=g.QKs[c0:c0 + 256, :].rearrange("(c p) t -> p c t", p=128)), b["qB"],
                writes=[b["qB"]])
            P.dma("pool", lambda e: e.dma_start(
                out=b["k2"][:], in_=g.QKs[c1:c1 + 256, :].rearrange("(c p) t -> p c t", p=128)), b["kB"],
                writes=[b["kB"]])
            P.dma("pool", lambda e: e.dma_start(out=b["vA"][:, :, 0:256], in_=tm_src(g.AV, h, NT)), b["vB"],
                  writes=[b["vB"]])
            hb[h] = b

        steps = [(h, gq, kt) for h in range(H) for gq in range(NG) for kt in range(2 * gq + 2)]
        LA = 2
        stq = {}

        def rec_qk(i):
            h, gq, kt = steps[i]
            ensure_head(h)
            b = hb[h]
            q2, k2 = b["q2"], b["k2"]
            qs = slice(gq * 256, (gq + 1) * 256)
            ks = slice(kt * 128, (kt + 1) * 128)
            st, stB = stp.next()

            def mmq(e):
                e.matmul(st[:, 0, :], lhsT=k2[:, 0, ks], rhs=q2[:, 0, qs], start=True, stop=True)
                return e.matmul(st[:, 1, :], lhsT=k2[:, 1, ks], rhs=q2[:, 1, qs], start=True, stop=True)
            P.op("pe", mmq, reads=[b["qB"], b["kB"]], writes=[stB])
            stq[i] = (st, stB)

        def epilogue(h, gq, j, b):
            (o0, o0B), (o1, o1B) = outp[0][j], outp[1][j]
            hs, hsB = b["hs"], b["hsB"]
            ts = slice((gq * 2 + j) * 128, (gq * 2 + j + 1) * 128)
            c0, c0B = c0r.next()
            c1, c1B = c1r.next()
            P.op("dve", lambda e: e.tensor_copy(out=c0[:], in_=o0[:]), reads=[o0B], writes=[c0B])
            P.op("dve", lambda e: e.tensor_copy(out=c1[:], in_=o1[:]), reads=[o1B], writes=[c1B])
            yield
            rr, rB = rrr.next()

            def rcp(e):
                e.reciprocal(out=rr[:, 0:1], in_=c0[:, 256:257])
                return e.reciprocal(out=rr[:, 1:2], in_=c1[:, 256:257])
            P.op("dve", rcp, reads=[c0B, c1B], writes=[rB])
            P.op("dve", lambda e: e.tensor_tensor(out=rr[:, 2:3], in0=rr[:, 1:2], in1=nlam, op=ALU.mult),
                 reads=[rB, cB], writes=[rB])
            yield
            a0, a0B = a0r.next()
            P.op("dve", lambda e: e.tensor_scalar(out=a0[:], in0=c0[:, 0:256], scalar1=rr[:, 0:1], scalar2=None,
                                                  op0=ALU.mult), reads=[c0B, rB], writes=[a0B])
            yield
            a1, a1B = a1r.next()
            P.op("dve", lambda e: e.scalar_tensor_tensor(out=a1[:], in0=c1[:, 0:256], scalar=rr[:, 2:3], in1=a0[:],
                                                         op0=ALU.mult, op1=ALU.add),
                 reads=[c1B, rB, a0B], writes=[a1B])
            yield
            jk, jkB = jkr.next()
            P.op("act", lambda e: e.activation(out=jk[:], in_=a1[:], func=AF.Square, accum_out=rr[:, 3:4]),
                 reads=[a1B, rB], writes=[jkB, rB])
            yield
            P.op("act", lambda e: e.activation(out=rr[:, 4:5], in_=rr[:, 3:4], func=AF.Ln, bias=EPS,
                                               scale=1.0 / 256), reads=[rB], writes=[rB])
            yield
            P.op("act", lambda e: e.activation(out=rr[:, 5:6], in_=rr[:, 4:5], func=AF.Exp, scale=-0.5),
                 reads=[rB], writes=[rB])
            yield
            hab, habB = habr.next()
            P.op("dve", lambda e: e.scalar_tensor_tensor(out=hab[:], in0=a1[:], scalar=rr[:, 5:6],
                                                         in1=anrep[:, h * 256:(h + 1) * 256], op0=ALU.mult,
                                                         op1=ALU.mult), reads=[a1B, rB, cB], writes=[habB])
            yield
            tr, trB = trp.next()

            def trs(e):
                e.transpose(tr[:, 0, :], hab[:, 0:128], g.identb[:])
                return e.transpose(tr[:, 1, :], hab[:, 128:256], g.identb[:])
            P.op("pe", trs, reads=[habB], writes=[trB])
            yield
            P.op("dve", lambda e: e.tensor_copy(out=hs[:, :, ts], in_=tr[:]), reads=[trB], writes=[hsB])

        bg = []

        def advance():
            for gen in list(bg):
                try:
                    next(gen)
                except StopIteration:
                    bg.remove(gen)

        for i in range(min(LA, len(steps))):
            rec_qk(i)
        for i, (h, gq, kt) in enumerate(steps):
            if gq == 0 and kt == 0:
                ensure_head(h + 1)
            if i + LA < len(steps):
                rec_qk(i + LA)
            b = hb[h]
            vA, vB = b["vA"], b["vB"]
            st, stB = stq.pop(i)
            pt, ptB = ptr.next()
            P.op("act", lambda e, pt=pt, st=st: e.activation(out=pt[:], in_=st[:], func=AF.Exp),
                 reads=[stB], writes=[ptB])
            if kt >= 2 * gq:
                j0 = kt - 2 * gq

                def msk(e, pt=pt, j0=j0):
                    e.tensor_tensor(out=pt[:, 0, j0 * 128:(j0 + 1) * 128], in0=pt[:, 0, j0 * 128:(j0 + 1) * 128],
                                    in1=g.trib[:], op=ALU.mult)
                    return e.tensor_tensor(out=pt[:, 1, j0 * 128:(j0 + 1) * 128],
                                           in0=pt[:, 1, j0 * 128:(j0 + 1) * 128], in1=g.trib[:], op=ALU.mult)
                P.op("dve", msk, reads=[ptB], writes=[ptB])
            js = [j for j in range(2) if kt <= 2 * gq + j]

            def mmv(e, pt=pt, vA=vA, kt=kt, gq=gq, js=js):
                for j in js:
                    for m in range(2):
                        ins = e.matmul(outp[m][j][0][:], lhsT=pt[:, m, j * 128:(j + 1) * 128], rhs=vA[:, kt, :],
                                       start=(kt == 0), stop=(kt == 2 * gq + j), skip_group_check=True)
                return ins
            P.op("pe", mmv, reads=[ptB, vB], writes=[outp[m][j][1] for j in js for m in range(2)])
            advance()
            if kt == 2 * gq:
                gen = epilogue(h, gq, 0, b)
                next(gen)
                bg.append(gen)
            if kt == 2 * gq + 1:
                gen = epilogue(h, gq, 1, b)
                next(gen)
                bg.append(gen)
                if gq == NG - 1:
                    while bg:
                        advance()
                    P.dma("sp", lambda e, b=b, h=h: e.dma_start(
                        out=g.HAT[h * 256:(h + 1) * 256, :].rearrange("(j p) t -> p j t", p=128), in_=b["hs"][:]),
                        b["hsB"], reads=[b["hsB"]])
                    del hb[h]
        P.finish()


def ph_merge(g, l, which):
    nc, T = g.nc, g.T
    NQ = T // 512
    wa = (g.w_bm if which == 0 else g.w_bd)[l]
    wg = g.w_in[l]
    goff = O_GM if which == 0 else O_GD
    src = g.HMT if which == 0 else g.HAT
    with ExitStack() as es:
        P = Prog(nc, es, uid("mrg"))
        xT = sb(es, nc, [128, KC, T], BF16, "xT")
        xB = Buf()
        bg = sb(es, nc, [128, 32], F32, "bg")
        bgB = Buf()
        P.dma("pool", lambda e: e.dma_start(out=bg[:], in_=g.bgate[l]), bgB, writes=[bgB])
        P.dma("pool", lambda e: e.dma_start(out=xT[:], in_=fm(src)), xB, writes=[xB])
        SW = 256
        NS = 2048 // SW
        CPS = SW // 128
        slabs = Rot(sbufs(es, nc, 4, [128, KC, SW], BF16, "wsl"))
        psy = Rot(psbufs(es, nc, 3, [128, 512], F32, "py"))
        psg = Rot(psbufs(es, nc, 3, [128, 512], F32, "pg"))
        sgr = Rot(sbufs(es, nc, 2, [128, 512], F32, "sg"))
        y1r = Rot(sbufs(es, nc, 3, [128, 512], F32, "y1"))
        if which == 1:
            tr_ = Rot(sbufs(es, nc, 2, [128, 512], F32, "tt"))
            yor = Rot(sbufs(es, nc, 3, [128, 512], BF16, "yo"))
        loaded = []

        def load(s):
            ta, ba = slabs.next()
            P.dma("pool", lambda e, ta=ta, s=s: e.dma_start(out=ta[:], in_=slab_src(wa, s * SW, SW)), ba,
                  writes=[ba])
            tg, bgs = slabs.next()
            P.dma("pool", lambda e, tg=tg, s=s: e.dma_start(out=tg[:], in_=slab_src(wg, goff + s * SW, SW)), bgs,
                  writes=[bgs])
            loaded.append((ta, ba, tg, bgs))
        load(0)
        for s in range(NS):
            if s + 1 < NS:
                load(s + 1)
            ta, ba, tg, bgs = loaded[s]
            for cc in range(CPS):
                ch = s * CPS + cc
                for q in range(NQ):
                    qs = slice(q * 512, (q + 1) * 512)
                    py, pyB = psy.next()
                    pg, pgB = psg.next()

                    def mm(e, py=py, pg=pg, ta=ta, tg=tg, cc=cc, qs=qs):
                        for k in range(KC):
                            e.matmul(py[:], lhsT=ta[:, k, cc * 128:(cc + 1) * 128], rhs=xT[:, k, qs],
                                     start=(k == 0), stop=(k == KC - 1))
                        for k in range(KC):
                            ins = e.matmul(pg[:], lhsT=tg[:, k, cc * 128:(cc + 1) * 128], rhs=g.hnT[:, k, qs],
                                           start=(k == 0), stop=(k == KC - 1))
                        return ins
                    P.op("pe", mm, reads=[ba, bgs, xB], writes=[pyB, pgB])
                    sg, sgB = sgr.next()
                    bcol = which * 16 + ch
                    P.op("act", lambda e, sg=sg, pg=pg, bcol=bcol: e.activation(
                        out=sg[:], in_=pg[:], func=AF.Sigmoid, bias=bg[:, bcol:bcol + 1]),
                        reads=[pgB, bgB], writes=[sgB])
                    if which == 0:
                        y1, y1B = y1r.next()
                        P.op("dve", lambda e, y1=y1, py=py, sg=sg: e.tensor_tensor(out=y1[:], in0=py[:], in1=sg[:],
                                                                                   op=ALU.mult),
                             reads=[pyB, sgB], writes=[y1B])
                        P.dma("sp", lambda e, y1=y1, ch=ch, qs=qs: e.dma_start(
                            out=g.Y1[ch * 128:(ch + 1) * 128, qs], in_=y1[:]), y1B, reads=[y1B])
                    else:
                        y1, y1B = y1r.next()
                        P.dma("pool", lambda e, y1=y1, ch=ch, qs=qs: e.dma_start(
                            out=y1[:], in_=g.Y1[ch * 128:(ch + 1) * 128, qs]), y1B, writes=[y1B])
                        tt, ttB = tr_.next()
                        P.op("dve", lambda e, tt=tt, py=py, sg=sg: e.tensor_tensor(out=tt[:], in0=py[:], in1=sg[:],
                                                                                   op=ALU.mult),
                             reads=[pyB, sgB], writes=[ttB])
                        yo, yoB = yor.next()
                        P.op("dve", lambda e, yo=yo, tt=tt, y1=y1: e.tensor_tensor(out=yo[:], in0=tt[:], in1=y1[:],
                                                                                   op=ALU.add),
                             reads=[ttB, y1B], writes=[yoB])
                        P.dma("sp", lambda e, yo=yo, ch=ch, qs=qs: e.dma_start(
                            out=g.YT[ch * 128:(ch + 1) * 128, qs], in_=yo[:]), yoB, reads=[yoB])
        P.finish()


def ph_dump_hn(g, dst):
    nc = g.nc
    with ExitStack() as es:
        P = Prog(nc, es, uid("dmp"))
        b = Buf()
        for k in range(KC):
            P.dma("sp", lambda e, k=k: e.dma_start(out=dst[k * 128:(k + 1) * 128, :], in_=g.hnT[:, k, :]), b,
                  reads=[b])
        P.finish()


def ph_oproj(g, l, hsrc):
    nc, T = g.nc, g.T
    NQ = T // 512
    wo = g.w_o[l]
    with ExitStack() as es:
        P = Prog(nc, es, uid("opr"))
        xT = sb(es, nc, [128, KC, T], BF16, "xT")
        xB = Buf()
        P.dma("pool", lambda e: e.dma_start(out=xT[:], in_=fm(g.YT)), xB, writes=[xB])
        slabs = Rot(sbufs(es, nc, 3, [128, KC, 512], BF16, "wsl"))
        psr = Rot(psbufs(es, nc, 4, [128, 512], F32, "pp"))
        hir = Rot(sbufs(es, nc, 3, [128, 512], F32, "hi"))
        hor = Rot(sbufs(es, nc, 3, [128, 512], F32, "ho"))
        loaded = []

        def load(s):
            t, b = slabs.next()
            P.dma("pool", lambda e, t=t, s=s: e.dma_start(out=t[:], in_=slab_src(wo, s * 512, 512)), b, writes=[b])
            loaded.append((t, b))
        load(0)
        load(1)
        for s in range(4):
            if s + 2 < 4:
                load(s + 2)
            wt, wB = loaded[s]
            for cc in range(4):
                ch = s * 4 + cc
                for q in range(NQ):
                    qs = slice(q * 512, (q + 1) * 512)
                    hi, hiB = hir.next()
                    P.dma("pool", lambda e, hi=hi, ch=ch, qs=qs: e.dma_start(
                        out=hi[:], in_=hsrc[ch * 128:(ch + 1) * 128, qs]), hiB, writes=[hiB])
                    pt, pB = psr.next()

                    def mm(e, pt=pt, wt=wt, cc=cc, qs=qs):
                        for k in range(KC):
                            ins = e.matmul(pt[:], lhsT=wt[:, k, cc * 128:(cc + 1) * 128], rhs=xT[:, k, qs],
                                           start=(k == 0), stop=(k == KC - 1))
                        return ins
                    P.op("pe", mm, reads=[wB, xB], writes=[pB])
                    ho, hoB = hor.next()
                    P.op("dve", lambda e, ho=ho, pt=pt, hi=hi: e.tensor_tensor(out=ho[:], in0=pt[:], in1=hi[:],
                                                                               op=ALU.add),
                         reads=[pB, hiB], writes=[hoB])
                    P.dma("sp", lambda e, ho=ho, ch=ch, qs=qs: e.dma_start(
                        out=g.hT[ch * 128:(ch + 1) * 128, qs], in_=ho[:]), hoB, reads=[hoB])
                    if False:
                        P.dma("sp", lambda e, ho=ho, ch=ch, qs=qs: e.dma_start(
                            out=g.H1[ch * 128:(ch + 1) * 128, qs], in_=ho[:]), hoB, reads=[hoB])
        P.finish()


def ph_up(g, l):
    nc, T = g.nc, g.T
    NQ = T // 512
    wu = g.w_up[l]
    with ExitStack() as es:
        P = Prog(nc, es, uid("up"))
        cw = sb(es, nc, [128, FC, 3], F32, "cw")
        cb = sb(es, nc, [128, FC], F32, "cb")
        cB = Buf()
        P.dma("pool", lambda e: e.dma_start(out=cw[:], in_=g.cw[l]), cB, writes=[cB])
        P.dma("pool", lambda e: e.dma_start(out=cb[:], in_=g.cb[l]), cB, writes=[cB])
        slabs = Rot(sbufs(es, nc, 4, [128, KC, 512], BF16, "wsl"))
        psr = Rot(psbufs(es, nc, 6, [128, 512], F32, "pp"))
        gts = [(sb(es, nc, [128, T + 2], F32, "gt"), Buf()) for _ in range(2)]
        vsr = Rot(sbufs(es, nc, 2, [128, T], F32, "vs"))
        cvr = Rot(sbufs(es, nc, 2, [128, T], F32, "cv"))
        uor = Rot(sbufs(es, nc, 2, [128, T], BF16, "uo"))
        for gt, gB in gts:
            P.op("dve", lambda e, gt=gt: e.memset(gt[:, 0:2], 0.0), writes=[gB])
        gtr = Rot(gts)
        loaded = []

        def load(s):
            ta, ba = slabs.next()
            P.dma("pool", lambda e, ta=ta, s=s: e.dma_start(out=ta[:], in_=slab_src(wu, s * 512, 512)), ba,
                  writes=[ba])
            tv, bv = slabs.next()
            P.dma("pool", lambda e, tv=tv, s=s: e.dma_start(out=tv[:], in_=slab_src(wu, DFF + s * 512, 512)), bv,
                  writes=[bv])
            loaded.append((ta, ba, tv, bv))
        load(0)
        for s in range(11):
            if s + 1 < 11:
                load(s + 1)
            ta, ba, tv, bv = loaded[s]
            for cc in range(4):
                j = s * 4 + cc
                gt, gB = gtr.next()
                vs, vB = vsr.next()
                for q in range(NQ):
                    qs = slice(q * 512, (q + 1) * 512)
                    pa, paB = psr.next()
                    pv, pvB = psr.next()

                    def mm(e, pa=pa, pv=pv, ta=ta, tv=tv, cc=cc, qs=qs):
                        for k in range(KC):
                            e.matmul(pa[:], lhsT=ta[:, k, cc * 128:(cc + 1) * 128], rhs=g.hnT[:, k, qs],
                                     start=(k == 0), stop=(k == KC - 1))
                        for k in range(KC):
                            ins = e.matmul(pv[:], lhsT=tv[:, k, cc * 128:(cc + 1) * 128], rhs=g.hnT[:, k, qs],
                                           start=(k == 0), stop=(k == KC - 1))
                        return ins
                    P.op("pe", mm, reads=[ba, bv], writes=[paB, pvB])
                    P.op("act", lambda e, gt=gt, pa=pa, q=q: e.activation(
                        out=gt[:, 2 + q * 512:2 + (q + 1) * 512], in_=pa[:], func=AF.Identity),
                        reads=[paB], writes=[gB])
                    P.op("act", lambda e, vs=vs, pv=pv, qs=qs: e.activation(out=vs[:, qs], in_=pv[:],
                                                                            func=AF.Identity),
                         reads=[pvB], writes=[vB])
                cv, cvB = cvr.next()

                P.op("dve", lambda e, cv=cv, gt=gt, j=j: e.tensor_scalar(
                    out=cv[:], in0=gt[:, 0:T], scalar1=cw[:, j, 0:1], scalar2=cb[:, j:j + 1], op0=ALU.mult,
                    op1=ALU.add), reads=[gB, cB], writes=[cvB])
                P.op("dve", lambda e, cv=cv, gt=gt, j=j: e.scalar_tensor_tensor(
                    out=cv[:], in0=gt[:, 1:T + 1], scalar=cw[:, j, 1:2], in1=cv[:], op0=ALU.mult, op1=ALU.add),
                    reads=[gB, cB, cvB], writes=[cvB])
                P.op("dve", lambda e, cv=cv, gt=gt, j=j: e.scalar_tensor_tensor(
                    out=cv[:], in0=gt[:, 2:T + 2], scalar=cw[:, j, 2:3], in1=cv[:], op0=ALU.mult, op1=ALU.add),
                    reads=[gB, cB, cvB], writes=[cvB])
                P.op("act", lambda e, cv=cv: e.activation(out=cv[:], in_=cv[:], func=AF.Silu),
                     reads=[cvB], writes=[cvB])
                uo, uoB = uor.next()
                P.op("dve", lambda e, uo=uo, cv=cv, vs=vs: e.tensor_tensor(out=uo[:], in0=cv[:], in1=vs[:],
                                                                           op=ALU.mult),
                     reads=[cvB, vB], writes=[uoB])
                P.dma("sp", lambda e, uo=uo, j=j: e.dma_start(out=g.UT[j * 128:(j + 1) * 128, :], in_=uo[:]), uoB,
                      reads=[uoB])
        P.finish()


def ph_down(g, l):
    nc, T = g.nc, g.T
    TT = min(T, 1024)
    NQ = TT // 512
    wd = g.w_down[l]
    with ExitStack() as es:
        P = Prog(nc, es, uid("dwn"))
        uT = sb(es, nc, [128, FC, TT], BF16, "uT")
        uB = Buf()
        slabs = Rot(sbufs(es, nc, 3, [128, FC, 128], BF16, "wsl"))
        psr = Rot(psbufs(es, nc, 4, [128, 512], F32, "pp"))
        hir = Rot(sbufs(es, nc, 3, [128, 512], F32, "hi"))
        hor = Rot(sbufs(es, nc, 3, [128, 512], F32, "ho"))
        for half in range(T // TT):
            t0 = half * TT
            P.dma("pool", lambda e, t0=t0: e.dma_start(out=uT[:], in_=fm(g.UT[:, t0:t0 + TT])), uB, writes=[uB])
            loaded = []

            def load(c):
                t, b = slabs.next()
                P.dma("pool", lambda e, t=t, c=c: e.dma_start(
                    out=t[:], in_=wd[:, c * 128:(c + 1) * 128].rearrange("(k p) n -> p k n", p=128)), b,
                    writes=[b])
                loaded.append((t, b))
            load(0)
            load(1)
            for c in range(KC):
                if c + 2 < KC:
                    load(c + 2)
                wt, wB = loaded[c]
                for q in range(NQ):
                    qs = slice(q * 512, (q + 1) * 512)
                    gs = slice(t0 + q * 512, t0 + (q + 1) * 512)
                    hi, hiB = hir.next()
                    P.dma("pool", lambda e, hi=hi, c=c, gs=gs: e.dma_start(
                        out=hi[:], in_=g.hT[c * 128:(c + 1) * 128, gs]), hiB, writes=[hiB])
                    pt, pB = psr.next()

                    def mm(e, pt=pt, wt=wt, qs=qs):
                        for k in range(FC):
                            ins = e.matmul(pt[:], lhsT=wt[:, k, :], rhs=uT[:, k, qs], start=(k == 0),
                                           stop=(k == FC - 1))
                        return ins
                    P.op("pe", mm, reads=[wB, uB], writes=[pB])
                    ho, hoB = hor.next()
                    P.op("dve", lambda e, ho=ho, pt=pt, hi=hi: e.tensor_tensor(out=ho[:], in0=pt[:], in1=hi[:],
                                                                               op=ALU.add),
                         reads=[pB, hiB], writes=[hoB])
                    P.dma("sp", lambda e, ho=ho, c=c, gs=gs: e.dma_start(
                        out=g.hT[c * 128:(c + 1) * 128, gs], in_=ho[:]), hoB, reads=[hoB])
        P.finish()


def build(T, L, debug=False):
    nc = bass.Bass("TRN2", target_bir_lowering=False)
    g = G()
    g.nc, g.T, g.L = nc, T, L
    g.debug = debug

    def din(name, shape, dt=F32):
        return nc.dram_tensor(name, list(shape), dt, kind="ExternalInput").ap()

    def scr(name, shape, dt):
        kind = "ExternalOutput" if debug else "Internal"
        return nc.dram_tensor(name, list(shape), dt, kind=kind).ap()
    g.xT = din("xT", [D, T])
    g.w_in = din("w_in", [L, D, NIN])
    g.w_bm = din("w_bm", [L, D, D])
    g.w_bd = din("w_bd", [L, D, D])
    g.w_o = din("w_o", [L, D, D])
    g.w_up = din("w_up", [L, D, 2 * DFF])
    g.w_down = din("w_down", [L, DFF, D])
    g.gmix = din("gmix", [L, 128, KC])
    g.gffn = din("gffn", [L, 128, KC])
    g.gfin = din("gfin", [128, KC])
    g.bqk = din("bqk", [L, 128, 48])
    g.bgate = din("bgate", [L, 128, 32])
    g.btm = din("btm", [L, 6144])
    g.bif = din("bif", [L, 16])
    g.mnorm = din("mnorm", [L, 2048])
    g.anorm = din("anorm", [L, 2048])
    g.lamqk = din("lamqk", [L, 512])
    g.cw = din("cw", [L, 128, FC, 3])
    g.cb = din("cb", [L, 128, FC])
    g.c_ident = din("c_ident", [128, 128])
    g.c_tri = din("c_tri", [128, 128])
    g.c_ones = din("c_ones", [128, 128])
    g.c_pm = din("c_pm", [32, 32])
    g.c_cos = din("c_cos", [32, T])
    g.c_sin = din("c_sin", [32, T])
    g.outT = nc.dram_tensor("outT", [D, T], F32, kind="ExternalOutput").ap()
    g.hT = scr("hT", [D, T], F32)
    g.QKs = scr("QKs", [48 * 128, T], BF16)
    g.MV = scr("MV", [T, 2048], BF16)
    g.MO = scr("MO", [T, 2048], BF16)
    g.AV = scr("AV", [T, 2048], BF16)
    g.HMT = scr("HMT", [D, T], BF16)
    g.HAT = scr("HAT", [D, T], BF16)
    g.Y1 = scr("Y1", [D, T], F32)
    g.YT = scr("YT", [D, T], BF16)
    g.UT = scr("UT", [DFF, T], BF16)
    if debug:
        g.H1 = scr("H1", [D, T], F32)
        g.HN2 = scr("HN2", [D, T], BF16)
    with ExitStack() as es:
        g.hnT = sb(es, nc, [128, KC, T], BF16, "hnT")
        g.IF = sb(es, nc, [128, T // 128, 16], F32, "IF")
        g.identb = sb(es, nc, [128, 128], BF16, "identb")
        g.trib = sb(es, nc, [128, 128], BF16, "trib")
        g.trif = sb(es, nc, [128, 128], F32, "trif")
        g.onesf = sb(es, nc, [128, 128], F32, "onesf")
        g.onesb = sb(es, nc, [128, 128], BF16, "onesb")
        g.pmf = sb(es, nc, [32, 32], F32, "pmf")
        ph_consts(g)
        for l in range(L):
            hsrc = g.xT if l == 0 else g.hT
            ph_norm(g, hsrc, g.gmix[l])
            ph_proj(g, l)
            ph_mlstm(g, l)
            ph_attn(g, l)
            ph_merge(g, l, 0)
            ph_merge(g, l, 1)
            ph_oproj(g, l, hsrc)
            if debug == 2:
                continue
            ph_norm(g, g.hT, g.gffn[l])
            ph_up(g, l)
            if debug == 3:
                continue
            ph_down(g, l)
        ph_norm(g, g.hT, g.gfin, final_dst=g.outT)
    return nc


def pcol(v, nch):
    v = np.asarray(v, dtype=np.float32)
    return np.ascontiguousarray(np.swapaxes(v.reshape(v.shape[:-1] + (nch, 128)), -1, -2))


def host_consts(T):
    s = np.arange(128)
    tri = (s[:, None] <= s[None, :]).astype(np.float32)
    pm = np.zeros((32, 32), np.float32)
    for j in range(16):
        pm[j + 16, j] = -1.0
        pm[j, j + 16] = 1.0
    half = 16
    inv = (np.float32(ROPE_THETA) ** (-np.arange(half, dtype=np.float32) / np.float32(half))).astype(np.float32)
    ang = np.arange(T, dtype=np.float32)[:, None] * inv[None, :]
    cos = np.cos(ang.astype(np.float64)).astype(np.float32).T
    sin = np.sin(ang.astype(np.float64)).astype(np.float32).T
    return {
        "c_ident": np.eye(128, dtype=np.float32), "c_tri": tri, "c_ones": np.ones((128, 128), np.float32),
        "c_pm": pm, "c_cos": np.ascontiguousarray(np.concatenate([cos, cos], 0)),
        "c_sin": np.ascontiguousarray(np.concatenate([sin, sin], 0)),
    }


def prep_shared(L, T, norm_mix, w_in, b_in, m_norm, a_norm, lam_qk, w_bm, w_bd, w_o, norm_ffn, w_up, conv_w, conv_b,
                w_down, norm_final):
    f = lambda a: np.ascontiguousarray(np.asarray(a, dtype=np.float32))
    b_in = f(b_in)
    bqk = np.concatenate([b_in[:, O_MQ:O_MQ + 2048], b_in[:, O_AQ:O_AQ + 4096]], axis=1)
    bgate = b_in[:, O_GM:O_GM + 4096]
    btm = np.concatenate([b_in[:, O_MV:O_MV + 4096], b_in[:, O_AV:O_AV + 2048]], axis=1)
    cwp = np.stack([pcol(f(conv_w)[:, i, :], FC) for i in range(3)], axis=-1)
    d = {
        "w_in": f(w_in)[:L], "w_bm": f(w_bm)[:L], "w_bd": f(w_bd)[:L], "w_o": f(w_o)[:L], "w_up": f(w_up)[:L],
        "w_down": f(w_down)[:L],
        "gmix": pcol(norm_mix, KC)[:L], "gffn": pcol(norm_ffn, KC)[:L], "gfin": pcol(norm_final, KC),
        "bqk": pcol(bqk, 48)[:L], "bgate": pcol(bgate, 32)[:L], "btm": f(btm)[:L],
        "bif": f(b_in[:, O_MI:O_MI + 16])[:L], "mnorm": f(m_norm)[:L], "anorm": f(a_norm)[:L],
        "lamqk": f(lam_qk).reshape(-1, 512)[:L], "cw": np.ascontiguousarray(cwp)[:L], "cb": pcol(conv_b, FC)[:L],
    }
    d.update(host_consts(T))
    return {k: np.ascontiguousarray(v) for k, v in d.items()}


def kernel(x, norm_mix, w_in, b_in, m_norm, a_norm, lam_qk, w_bm, w_bd, w_o, norm_ffn, w_up, conv_w, conv_b, w_down,
           norm_final):
    x = np.asarray(x, dtype=np.float32)
    B, S, _ = x.shape
    L = np.asarray(norm_mix).shape[0]
    shared = prep_shared(L, S, norm_mix, w_in, b_in, m_norm, a_norm, lam_qk, w_bm, w_bd, w_o, norm_ffn, w_up, conv_w,
                         conv_b, w_down, norm_final)
    nc = build(S, L)
    NCORES = 8
    real = [0, 1, 4, 5][:B]
    zero_shared = {k: (v if k.startswith("c_") else np.zeros_like(v)) for k, v in shared.items()}
    zx = np.zeros((D, S), np.float32)
    in_maps = []
    for c in range(NCORES):
        if c in real:
            m = dict(shared)
            m["xT"] = np.ascontiguousarray(x[real.index(c)].T)
        else:
            m = dict(zero_shared)
            m["xT"] = zx
        in_maps.append(m)
    res = run_bass_kernel_spmd(nc, in_maps, core_ids=list(range(NCORES)))
    out = np.stack([np.ascontiguousarray(res.results[real[b]]["outT"].T) for b in range(B)], axis=0)
    return out.astype(np.float32)
```

```python
import math
from contextlib import ExitStack

import numpy as np
import concourse.bass as bass
import concourse.mybir as mybir
from concourse.bass_utils import run_bass_kernel_spmd

F32 = mybir.dt.float32
BF16 = mybir.dt.bfloat16
AF = mybir.ActivationFunctionType
ALU = mybir.AluOpType

D = 2048
KC = 16
H = 8
NIN = 16400
DFF = 5632
FC = 44
EPS = 1e-6
O_MQ, O_MK, O_MV, O_MO, O_MI, O_AQ, O_AK, O_AV, O_GM, O_GD = (
    0, 1024, 2048, 4096, 6144, 6160, 8208, 10256, 12304, 14352)
ROPE_THETA = 500000.0


class Buf:
    __slots__ = ("w", "r", "dsem", "dcnt")

    def __init__(self):
        self.w = None
        self.r = {}
        self.dsem = None
        self.dcnt = 0


class Prog:
    ENGS = ("pe", "act", "dve", "pool", "sp")

    def __init__(self, nc, es, name):
        self.nc, self.es, self.name = nc, es, name
        self.q = {e: [] for e in self.ENGS}
        self.allsems = []
        self.sem = {e: self._newsem(f"{name}_{e}") for e in self.ENGS}
        self.cnt = {e: 0 for e in self.ENGS}
        self.waited = {e: {} for e in self.ENGS}
        self.dsems = []
        self.nd = 0

    def _newsem(self, name):
        h = self.nc.alloc_semaphore(name=name)
        self.allsems.append(h)
        return h

    def _wait(self, eng, ev):
        sem, val = ev
        if eng == "pe" and sem is self.sem["pe"]:
            return
        w = self.waited[eng]
        k = id(sem)
        if w.get(k, 0) >= val:
            return
        w[k] = val
        self.q[eng].append((0, sem, val))

    def _deps(self, eng, reads, writes):
        for b in reads:
            if b.w is not None:
                self._wait(eng, b.w)
        for b in writes:
            if b.w is not None:
                self._wait(eng, b.w)
            for ev in b.r.values():
                self._wait(eng, ev)

    @staticmethod
    def _commit(ev, reads, writes):
        for b in reads:
            b.r[id(ev[0])] = ev
        for b in writes:
            b.w = ev
            b.r = {}

    def op(self, eng, fn, reads=(), writes=()):
        self._deps(eng, reads, writes)
        self.cnt[eng] += 1
        ev = (self.sem[eng], self.cnt[eng])
        self.q[eng].append((1, fn, self.sem[eng], 1))
        self._commit(ev, reads, writes)
        return ev

    def dma(self, qeng, fn, sb, reads=(), writes=()):
        self._deps(qeng, reads, writes)
        if sb.dsem is None:
            self.nd += 1
            sb.dsem = self._newsem(f"{self.name}_d{self.nd}")
            sb.dcnt = 0
            self.dsems.append(sb)
        sb.dcnt += 16
        ev = (sb.dsem, sb.dcnt)
        self.q[qeng].append((1, fn, sb.dsem, 16))
        self._commit(ev, reads, writes)
        return ev

    def finish(self):
        for e in self.ENGS:
            for e2 in self.ENGS:
                if e2 != e and self.cnt[e2] > 0:
                    self._wait(e, (self.sem[e2], self.cnt[e2]))
            for sb in self.dsems:
                self._wait(e, (sb.dsem, sb.dcnt))
        for sb in self.dsems:
            sb.dsem = None
            sb.dcnt = 0
            sb.w = None
            sb.r = {}
        with self.nc.Block() as blk:
            blk.tensor(lambda e: self._emit("pe", e))
            blk.scalar(lambda e: self._emit("act", e))
            blk.vector(lambda e: self._emit("dve", e))
            blk.gpsimd(lambda e: self._emit("pool", e))
            blk.sync(lambda e: self._emit("sp", e))
        self.nc.all_engine_barrier()
        self.nc.clear_and_free_semaphores(self.allsems)
        self.nc.all_engine_barrier()

    def _emit(self, eng, e):
        for it in self.q[eng]:
            if it[0] == 0:
                e.wait_ge(it[1], it[2])
            else:
                it[1](e).then_inc(it[2], it[3])


class Rot:
    def __init__(self, items):
        self.items = items
        self.i = 0

    def next(self):
        x = self.items[self.i]
        self.i = (self.i + 1) % len(self.items)
        return x


class G:
    pass


_uid = [0]


def uid(p):
    _uid[0] += 1
    return f"{p}{_uid[0]}"


def sb(es, nc, shape, dt, name="t"):
    return es.enter_context(nc.sbuf_tensor(uid(name), list(shape), dt))


def pst(es, nc, shape, dt, name="p"):
    return es.enter_context(nc.psum_tensor(uid(name), list(shape), dt))


def sbufs(es, nc, n, shape, dt, name="t"):
    return [(sb(es, nc, shape, dt, name), Buf()) for _ in range(n)]


def psbufs(es, nc, n, shape, dt, name="p"):
    return [(pst(es, nc, shape, dt, name), Buf()) for _ in range(n)]


def fm(ap):
    return ap.rearrange("(k p) t -> p k t", p=128)


def ph_consts(g):
    nc = g.nc
    with ExitStack() as es:
        P = Prog(nc, es, uid("cst"))
        for t, src in ((g.identb, g.c_ident), (g.trib, g.c_tri), (g.trif, g.c_tri), (g.onesf, g.c_ones),
                       (g.onesb, g.c_ones), (g.pmf, g.c_pm), (g.pmb, g.c_pm)):
            P.dma("pool", lambda e, t=t, src=src: e.dma_start(out=t[:], in_=src), Buf(), writes=[])
        P.finish()


def ph_norm(g, src, gam_src, final_dst=None):
    nc, T = g.nc, g.T
    W = 256
    with ExitStack() as es:
        P = Prog(nc, es, uid("nrm"))
        hb = Rot(sbufs(es, nc, 2, [128, KC, W], F32, "hb"))
        sq = sb(es, nc, [128, KC, W], BF16, "sq")
        sqA, sqB = Buf(), Buf()
        gm = sb(es, nc, [128, KC], F32, "gm")
        gmB = Buf()
        rs = Rot(sbufs(es, nc, 2, [128, W], F32, "rs"))
        rstd = Rot(sbufs(es, nc, 2, [128, W], F32, "rstd"))
        ps = Rot(psbufs(es, nc, 2, [128, W], F32, "nps"))
        ob = Rot(sbufs(es, nc, 2, [128, KC, W], F32, "ob")) if final_dst is not None else None
        P.dma("pool", lambda e: e.dma_start(out=gm[:], in_=gam_src), gmB, writes=[gmB])
        for j in range(T // W):
            ht, hB = hb.next()
            P.dma("pool", lambda e, ht=ht, j=j: e.dma_start(out=ht[:], in_=fm(src[:, j * W:(j + 1) * W])), hB,
                  writes=[hB])
            P.op("act", lambda e, ht=ht: e.activation(out=sq[:, 0:8, :], in_=ht[:, 0:8, :], func=AF.Square),
                 reads=[hB], writes=[sqA])
            P.op("dve", lambda e, ht=ht: e.tensor_tensor(out=sq[:, 8:16, :], in0=ht[:, 8:16, :], in1=ht[:, 8:16, :],
                                                         op=ALU.mult), reads=[hB], writes=[sqB])
            pt, pB = ps.next()

            def mm(e, pt=pt):
                for k in range(KC):
                    ins = e.matmul(pt[:], lhsT=g.onesb[:], rhs=sq[:, k, :], start=(k == 0), stop=(k == KC - 1))
                return ins
            P.op("pe", mm, reads=[sqA, sqB], writes=[pB])
            rt, rB = rs.next()
            P.op("act", lambda e, rt=rt, pt=pt: e.activation(out=rt[:], in_=pt[:], func=AF.Sqrt, bias=EPS,
                                                             scale=1.0 / D), reads=[pB], writes=[rB])
            st, sB = rstd.next()
            P.op("dve", lambda e, st=st, rt=rt: e.reciprocal(out=st[:], in_=rt[:]), reads=[rB], writes=[sB])
            if final_dst is None:
                def nrm(e, ht=ht, st=st, j=j):
                    for k in range(KC):
                        ins = e.scalar_tensor_tensor(out=g.hnT[:, k, j * W:(j + 1) * W], in0=ht[:, k, :],
                                                     scalar=gm[:, k:k + 1], in1=st[:], op0=ALU.mult, op1=ALU.mult)
                    return ins
                P.op("dve", nrm, reads=[hB, sB, gmB], writes=[Buf()])
            else:
                ot, oB = ob.next()

                def nrm(e, ht=ht, st=st, ot=ot):
                    for k in range(KC):
                        ins = e.scalar_tensor_tensor(out=ot[:, k, :], in0=ht[:, k, :],
                                                     scalar=gm[:, k:k + 1], in1=st[:], op0=ALU.mult, op1=ALU.mult)
                    return ins
                P.op("dve", nrm, reads=[hB, sB, gmB], writes=[oB])
                P.dma("sp", lambda e, ot=ot, j=j: e.dma_start(out=fm(final_dst[:, j * W:(j + 1) * W]), in_=ot[:]),
                      oB, reads=[oB])
        P.finish()


def slab_src(w2d, c0, cw):
    return w2d[:, c0:c0 + cw].rearrange("(k p) n -> p k n", p=128)


def ph_proj(g, l):
    nc, T = g.nc, g.T
    NQ, NT = T // 512, T // 128
    w = g.w_in[l]
    with ExitStack() as es:
        P = Prog(nc, es, uid("prj"))
        slabs = Rot(sbufs(es, nc, 3, [128, KC, 512], BF16, "wsl"))
        psr = Rot(psbufs(es, nc, 4, [128, 512], F32, "pp"))
        pxp = Rot(psbufs(es, nc, 2, [32, 512], F32, "px"))
        stg = Rot(sbufs(es, nc, 4, [128, 512], BF16, "stg"))
        xbr = Rot(sbufs(es, nc, 2, [128, 512], F32, "xb"))
        x16r = Rot(sbufs(es, nc, 2, [32, 512], BF16, "x16"))
        t1r = Rot(sbufs(es, nc, 2, [32, 512], F32, "t1"))
        t2r = Rot(sbufs(es, nc, 2, [32, 512], F32, "t2"))
        tmr = Rot(sbufs(es, nc, 2, [128, 512], F32, "tm"))
        cosT = sb(es, nc, [32, T], F32, "cos")
        sinT = sb(es, nc, [32, T], F32, "sin")
        bqk = sb(es, nc, [128, 48], F32, "bqk")
        brep = sb(es, nc, [128, 6144], F32, "brep")
        bif = sb(es, nc, [128, 16], F32, "bif")
        wif = sb(es, nc, [128, KC, 16], BF16, "wif")
        cB = Buf()
        for t, src in ((cosT, g.c_cos), (sinT, g.c_sin), (bqk, g.bqk[l]), (brep, g.btm[l].partition_broadcast(128)),
                       (bif, g.bif[l].partition_broadcast(128)), (wif, slab_src(w, O_MI, 16))):
            P.dma("pool", lambda e, t=t, src=src: e.dma_start(out=t[:], in_=src), cB, writes=[cB])
        jobs = []
        for s in range(2):
            jobs.append((O_MQ + s * 512, "fm", ("m", 0 + s * 4)))
        for s in range(2):
            jobs.append((O_MK + s * 512, "fm", ("m", 8 + s * 4)))
        for s in range(4):
            jobs.append((O_AQ + s * 512, "fm", ("aq", 16 + s * 4)))
        for s in range(4):
            jobs.append((O_AK + s * 512, "fm", ("ak", 32 + s * 4)))
        for s in range(4):
            jobs.append((O_MV + s * 512, "tm", ("mv", s)))
        for s in range(4):
            jobs.append((O_MO + s * 512, "tm", ("mo", s)))
        for s in range(4):
            jobs.append((O_AV + s * 512, "tm", ("av", s)))
        loaded = []

        def load(i):
            c0 = jobs[i][0]
            t, b = slabs.next()
            P.dma("pool", lambda e, t=t, c0=c0: e.dma_start(out=t[:], in_=slab_src(w, c0, 512)), b, writes=[b])
            loaded.append((t, b))
        load(0)
        load(1)
        for tt in range(NT):
            pt, pB = psr.next()

            def mm(e, pt=pt, tt=tt):
                for k in range(KC):
                    ins = e.matmul(pt[:, 0:16], lhsT=g.hnT[:, k, tt * 128:(tt + 1) * 128], rhs=wif[:, k, :],
                                   start=(k == 0), stop=(k == KC - 1))
                return ins
            P.op("pe", mm, reads=[cB], writes=[pB])
            P.op("dve", lambda e, pt=pt, tt=tt: e.tensor_tensor(out=g.IF[:, tt, :], in0=pt[:, 0:16], in1=bif[:],
                                                                op=ALU.add), reads=[pB, cB], writes=[Buf()])
        for i, (c0, kind, info) in enumerate(jobs):
            if i + 2 < len(jobs):
                load(i + 2)
            wt, wB = loaded[i]
            if kind == "fm":
                typ, ch0 = info
                for cc in range(4):
                    ch = ch0 + cc
                    for q in range(NQ):
                        pt, pB = psr.next()

                        def mm(e, pt=pt, wt=wt, cc=cc, q=q):
                            for k in range(KC):
                                ins = e.matmul(pt[:], lhsT=wt[:, k, cc * 128:(cc + 1) * 128],
                                               rhs=g.hnT[:, k, q * 512:(q + 1) * 512],
                                               start=(k == 0), stop=(k == KC - 1))
                            return ins
                        P.op("pe", mm, reads=[wB], writes=[pB])
                        st, sB = stg.next()
                        if typ == "m":
                            P.op("act", lambda e, st=st, pt=pt, ch=ch: e.activation(
                                out=st[:], in_=pt[:], func=AF.Identity, bias=bqk[:, ch:ch + 1]),
                                reads=[pB, cB], writes=[sB])
                        else:
                            xb, xB = xbr.next()
                            P.op("act", lambda e, xb=xb, pt=pt, ch=ch: e.activation(
                                out=xb[:], in_=pt[:], func=AF.Identity, bias=bqk[:, ch:ch + 1]),
                                reads=[pB, cB], writes=[xB])
                            px, pxB = pxp.next()
                            x16, x16B = x16r.next()
                            P.op("dve", lambda e, x16=x16, xb=xb: e.tensor_copy(out=x16[:], in_=xb[0:32, :]),
                                 reads=[xB], writes=[x16B])
                            P.op("pe", lambda e, px=px, x16=x16: e.matmul(px[:], lhsT=g.pmb[:], rhs=x16[:],
                                                                          start=True, stop=True),
                                 reads=[x16B], writes=[pxB])
                            t1, t1B = t1r.next()
                            t2, t2B = t2r.next()
                            P.op("dve", lambda e, t1=t1, px=px, q=q: e.tensor_tensor(
                                out=t1[:], in0=px[:], in1=sinT[:, q * 512:(q + 1) * 512], op=ALU.mult),
                                reads=[pxB, cB], writes=[t1B])
                            P.op("dve", lambda e, t2=t2, xb=xb, q=q: e.tensor_tensor(
                                out=t2[:], in0=xb[0:32, :], in1=cosT[:, q * 512:(q + 1) * 512], op=ALU.mult),
                                reads=[xB, cB], writes=[t2B])
                            P.op("dve", lambda e, t1=t1, t2=t2, xb=xb: e.tensor_tensor(
                                out=xb[0:32, :], in0=t1[:], in1=t2[:], op=ALU.add),
                                reads=[t1B, t2B], writes=[xB])
                            sc = (128.0 ** -0.5) if typ == "aq" else 1.0
                            P.op("act", lambda e, st=st, xb=xb, sc=sc: e.activation(
                                out=st[:], in_=xb[:], func=AF.Identity, scale=sc), reads=[xB], writes=[sB])
                        P.dma("sp", lambda e, st=st, ch=ch, q=q: e.dma_start(
                            out=g.QKs[ch * 128:(ch + 1) * 128, q * 512:(q + 1) * 512], in_=st[:]), sB, reads=[sB])
            else:
                typ, s = info
                dst = {"mv": g.MV, "mo": g.MO, "av": g.AV}[typ]
                boff = {"mv": 0, "mo": 2048, "av": 4096}[typ] + s * 512
                for tt in range(NT):
                    pt, pB = psr.next()

                    def mm(e, pt=pt, wt=wt, tt=tt):
                        for k in range(KC):
                            ins = e.matmul(pt[:], lhsT=g.hnT[:, k, tt * 128:(tt + 1) * 128], rhs=wt[:, k, :],
                                           start=(k == 0), stop=(k == KC - 1))
                        return ins
                    P.op("pe", mm, reads=[wB], writes=[pB])
                    st, sB = stg.next()
                    if typ == "mo":
                        tm, tB = tmr.next()
                        P.op("dve", lambda e, tm=tm, pt=pt, boff=boff: e.tensor_tensor(
                            out=tm[:], in0=pt[:], in1=brep[:, boff:boff + 512], op=ALU.add),
                            reads=[pB, cB], writes=[tB])
                        P.op("act", lambda e, st=st, tm=tm: e.activation(out=st[:], in_=tm[:], func=AF.Sigmoid),
                             reads=[tB], writes=[sB])
                    else:
                        P.op("dve", lambda e, st=st, pt=pt, boff=boff: e.tensor_tensor(
                            out=st[:], in0=pt[:], in1=brep[:, boff:boff + 512], op=ALU.add),
                            reads=[pB, cB], writes=[sB])
                    P.dma("sp", lambda e, st=st, dst=dst, tt=tt, s=s: e.dma_start(
                        out=dst[tt * 128:(tt + 1) * 128, s * 512:(s + 1) * 512], in_=st[:]), sB, reads=[sB])
        P.finish()


def tm_src(ap2d, h, NT):
    return ap2d[:, h * 256:(h + 1) * 256].rearrange("(t p) d -> p t d", p=128)


def ph_mlstm(g, l):
    nc, T = g.nc, g.T
    NT = T // 128
    with ExitStack() as es:
        P = Prog(nc, es, uid("mls"))
        NG = NT * 8
        def v3(ap):
            return ap.rearrange("p (c h) -> p c h", h=8)
        e1 = sb(es, nc, [128, NG], F32, "e1")
        l1 = sb(es, nc, [128, NG], F32, "l1")
        tg = sb(es, nc, [128, NG], F32, "tg")
        u = sb(es, nc, [128, NG], F32, "u")
        bnd = sb(es, nc, [128, NG], F32, "bnd")
        eg = sb(es, nc, [128, NG], F32, "eg")
        mrep = sb(es, nc, [128, 2048], F32, "mrep")
        cgp = pst(es, nc, [128, 2, NG], F32, "cgp")
        gB = Buf()
        P.dma("pool", lambda e: e.dma_start(out=mrep[:], in_=g.mnorm[l].partition_broadcast(128)), gB,
              writes=[gB])
        P.op("act", lambda e: e.activation(out=v3(e1[:]), in_=g.IF[:, :, 8:16], func=AF.Exp, scale=-1.0),
             writes=[gB])
        P.op("act", lambda e: e.activation(out=l1[:], in_=e1[:], func=AF.Ln, bias=1.0), reads=[gB], writes=[gB])

        def mmg(e):
            e.matmul(cgp[:, 0, :], lhsT=g.trif[:], rhs=l1[:], start=True, stop=True)
            return e.matmul(cgp[:, 1, :], lhsT=g.onesf[:], rhs=l1[:], start=True, stop=True)
        P.op("pe", mmg, reads=[gB], writes=[gB])
        P.op("dve", lambda e: e.tensor_tensor(out=v3(tg[:]), in0=v3(cgp[:, 0, :]), in1=g.IF[:, :, 0:8], op=ALU.add),
             reads=[gB], writes=[gB])

        def gex(e):
            e.activation(out=u[:], in_=tg[:], func=AF.Exp, bias=math.log(128.0 ** -0.5))
            e.activation(out=bnd[:], in_=cgp[:, 0, :], func=AF.Exp)
            return e.activation(out=eg[:], in_=cgp[:, 1, :], func=AF.Exp, scale=-1.0)
        P.op("act", gex, reads=[gB], writes=[gB])

        qkr = Rot([(sb(es, nc, [128, T], BF16, "qT"), sb(es, nc, [128, T], BF16, "kT"), Buf(), Buf())
                   for _ in range(2)])
        var = [(sb(es, nc, [128, NT, 257], BF16, "vA"), Buf()) for _ in range(2)]
        gr = Rot(sbufs(es, nc, 2, [128, NT, 256], BF16, "Gg"))
        hms = Rot(sbufs(es, nc, 2, [128, 2, T], BF16, "hms"))
        C = sb(es, nc, [128, 257], F32, "C")
        C1 = sb(es, nc, [128, 257], F32, "C1")
        Cb = sb(es, nc, [128, 257], BF16, "Cb")
        CB, C1B, CbB = Buf(), Buf(), Buf()
        ptr = Rot(sbufs(es, nc, 2, [128, 128], BF16, "PT"))
        kur = Rot(sbufs(es, nc, 2, [128, 128], BF16, "ku"))
        smr = Rot(sbufs(es, nc, 2, [128, 8], F32, "sm"))
        jkr = Rot(sbufs(es, nc, 2, [128, 256], F32, "jk"))
        hmr = Rot(sbufs(es, nc, 2, [128, 256], BF16, "hm"))
        stp = Rot(psbufs(es, nc, 2, [128, 128], F32, "stp"))
        ktp = Rot(psbufs(es, nc, 1, [128, 128], BF16, "ktp"))
        outp = Rot(psbufs(es, nc, 2, [128, 257], F32, "outp"))
        updp = Rot(psbufs(es, nc, 1, [128, 257], F32, "updp"))
        trp = Rot(psbufs(es, nc, 1, [128, 2, 128], BF16, "trp"))
        for vt, vB in var:
            P.op("dve", lambda e, vt=vt: e.memset(vt[:, :, 256:257], 1.0), writes=[vB])
        vr = Rot(var)
        bg = []
        eps = {}

        def advance():
            for gen in list(bg):
                try:
                    next(gen)
                except StopIteration:
                    bg.remove(gen)

        for h in range(H):
            eps.clear()
            qT, kT, qB, kB = qkr.next()
            qkB = qB
            vA, vB = vr.next()
            Gt, GB = gr.next()
            hs, hsB = hms.next()
            P.dma("pool", lambda e, qT=qT, h=h: e.dma_start(out=qT[:], in_=g.QKs[h * 128:(h + 1) * 128, :]), qB,
                  writes=[qB])
            P.dma("pool", lambda e, kT=kT, h=h: e.dma_start(out=kT[:], in_=g.QKs[(8 + h) * 128:(9 + h) * 128, :]),
                  kB, writes=[kB])
            P.dma("pool", lambda e, vA=vA, h=h: e.dma_start(out=vA[:, :, 0:256], in_=tm_src(g.MV, h, NT)), vB,
                  writes=[vB])
            P.dma("pool", lambda e, Gt=Gt, h=h: e.dma_start(out=Gt[:], in_=tm_src(g.MO, h, NT)), GB,
                  writes=[GB])

            def gmul(e, Gt=Gt, h=h):
                for c in range(NT):
                    ins = e.tensor_tensor(out=Gt[:, c, :], in0=Gt[:, c, :], in1=mrep[:, h * 256:(h + 1) * 256],
                                          op=ALU.mult)
                return ins
            P.op("dve", gmul, reads=[gB], writes=[GB])
            for c in range(NT):
                cs = slice(c * 128, (c + 1) * 128)
                uc = u[:, c * 8 + h:c * 8 + h + 1]
                st, stB = stp.next()
                P.op("pe", lambda e, st=st, kT=kT, qT=qT, cs=cs: e.matmul(st[:], lhsT=kT[:, cs], rhs=qT[:, cs],
                                                                          start=True, stop=True),
                     reads=[qB, kB], writes=[stB])
                advance()
                pt, ptB = ptr.next()
                P.op("dve", lambda e, pt=pt, st=st, uc=uc: e.scalar_tensor_tensor(
                    out=pt[:], in0=st[:], scalar=uc, in1=g.trif[:], op0=ALU.mult, op1=ALU.mult),
                    reads=[stB, gB], writes=[ptB])
                kt, ktB = ktp.next()
                P.op("pe", lambda e, kt=kt, kT=kT, cs=cs: e.transpose(kt[:], kT[:, cs], g.identb[:]),
                     reads=[kB], writes=[ktB])
                advance()
                ku, kuB = kur.next()
                P.op("act", lambda e, ku=ku, kt=kt, uc=uc: e.activation(out=ku[:], in_=kt[:], func=AF.Identity,
                                                                        scale=uc),
                     reads=[ktB, gB], writes=[kuB])
                while eps.get(c - 2) in bg:
                    advance()
                ot, oB = outp.next()

                def mmo(e, ot=ot, pt=pt, vA=vA, qT=qT, c=c, cs=cs):
                    ins = e.matmul(ot[:], lhsT=pt[:], rhs=vA[:, c, :], start=True, stop=(c == 0))
                    if c > 0:
                        ins = e.matmul(ot[:], lhsT=qT[:, cs], rhs=Cb[:], start=False, stop=True)
                    return ins
                P.op("pe", mmo, reads=[ptB, vB, qB] + ([CbB] if c > 0 else []), writes=[oB])
                advance()
                if c < NT - 1:
                    up, upB = updp.next()
                    P.op("pe", lambda e, up=up, ku=ku, vA=vA, c=c: e.matmul(up[:], lhsT=ku[:], rhs=vA[:, c, :],
                                                                            start=True, stop=True),
                         reads=[kuB, vB], writes=[upB])
                    egc = eg[:, c * 8 + h:c * 8 + h + 1]
                    if c == 0:
                        P.op("dve", lambda e, up=up, egc=egc: e.tensor_scalar(
                            out=C[:], in0=up[:], scalar1=egc, scalar2=None, op0=ALU.mult),
                            reads=[upB, gB], writes=[CB])
                    else:
                        P.op("act", lambda e, egc=egc: e.activation(out=C1[:], in_=C[:], func=AF.Identity,
                                                                    scale=egc),
                             reads=[CB, gB], writes=[C1B])
                        P.op("dve", lambda e, up=up, egc=egc: e.scalar_tensor_tensor(
                            out=C[:], in0=up[:], scalar=egc, in1=C1[:], op0=ALU.mult, op1=ALU.add),
                            reads=[upB, C1B, gB], writes=[CB])
                    P.op("act", lambda e: e.activation(out=Cb[:], in_=C[:], func=AF.Identity),
                         reads=[CB], writes=[CbB])
                def epilogue(c=c, cs=cs, ot=ot, oB=oB):
                    sm, smB = smr.next()
                    P.op("act", lambda e, sm=sm, ot=ot: e.activation(out=sm[:, 0:1], in_=ot[:, 256:257], func=AF.Abs),
                         reads=[oB], writes=[smB])
                    yield
                    P.op("dve", lambda e, sm=sm, c=c, h=h: e.tensor_scalar(
                        out=sm[:, 1:2], in0=sm[:, 0:1], scalar1=bnd[:, c * 8 + h:c * 8 + h + 1], scalar2=None, op0=ALU.max),
                        reads=[smB, gB], writes=[smB])
                    yield
                    P.op("dve", lambda e, sm=sm: e.reciprocal(out=sm[:, 2:3], in_=sm[:, 1:2]),
                         reads=[smB], writes=[smB])
                    yield
                    jk, jkB = jkr.next()
                    P.op("act", lambda e, jk=jk, ot=ot, sm=sm: e.activation(
                        out=jk[:], in_=ot[:, 0:256], func=AF.Square, scale=sm[:, 2:3], accum_out=sm[:, 3:4]),
                        reads=[oB, smB], writes=[jkB, smB])
                    yield
                    P.op("act", lambda e, sm=sm: e.activation(out=sm[:, 4:5], in_=sm[:, 3:4], func=AF.Ln, bias=EPS,
                                                              scale=1.0 / 256), reads=[smB], writes=[smB])
                    yield
                    P.op("act", lambda e, sm=sm: e.activation(out=sm[:, 5:6], in_=sm[:, 4:5], func=AF.Exp,
                                                              scale=-0.5), reads=[smB], writes=[smB])
                    yield
                    P.op("dve", lambda e, sm=sm: e.tensor_tensor(out=sm[:, 6:7], in0=sm[:, 5:6], in1=sm[:, 2:3],
                                                                 op=ALU.mult), reads=[smB], writes=[smB])
                    yield
                    hm, hmB = hmr.next()
                    P.op("dve", lambda e, hm=hm, ot=ot, sm=sm, Gt=Gt, c=c: e.scalar_tensor_tensor(
                        out=hm[:], in0=ot[:, 0:256], scalar=sm[:, 6:7], in1=Gt[:, c, :], op0=ALU.mult, op1=ALU.mult),
                        reads=[oB, smB, GB], writes=[hmB])
                    yield
                    tr, trB = trp.next()

                    def trs(e, tr=tr, hm=hm):
                        e.transpose(tr[:, 0, :], hm[:, 0:128], g.identb[:])
                        return e.transpose(tr[:, 1, :], hm[:, 128:256], g.identb[:])
                    P.op("pe", trs, reads=[hmB], writes=[trB])
                    yield
                    P.op("act", lambda e, hs=hs, tr=tr, cs=cs: e.activation(out=hs[:, :, cs], in_=tr[:],
                                                                            func=AF.Identity),
                         reads=[trB], writes=[hsB])

                gen_ = epilogue()
                eps[c] = gen_
                bg.append(gen_)

            while bg:
                advance()
            P.dma("sp", lambda e, hs=hs, h=h: e.dma_start(
                out=g.HMT[h * 256:(h + 1) * 256, :].rearrange("(j p) t -> p j t", p=128), in_=hs[:]), hsB,
                reads=[hsB])
        P.finish()


def ph_attn(g, l):
    nc, T = g.nc, g.T
    NT, NG = T // 128, T // 256
    lam_init = 0.8 - 0.6 * math.exp(-0.3 * l)
    with ExitStack() as es:
        P = Prog(nc, es, uid("att"))
        lq = sb(es, nc, [128, 4, 128], F32, "lq")
        lp = sb(es, nc, [128, 2, 128], F32, "lp")
        ls = sb(es, nc, [128, 8], F32, "ls")
        anrep = sb(es, nc, [128, 2048], F32, "anrep")
        cB = Buf()
        P.dma("pool", lambda e: e.dma_start(out=lq[:].rearrange("p a b -> p (a b)"),
                                            in_=g.lamqk[l].partition_broadcast(128)), cB, writes=[cB])
        P.dma("pool", lambda e: e.dma_start(out=anrep[:], in_=g.anorm[l].partition_broadcast(128)), cB,
              writes=[cB])

        def lam1(e):
            e.tensor_tensor(out=lp[:, 0, :], in0=lq[:, 0, :], in1=lq[:, 1, :], op=ALU.mult)
            return e.tensor_tensor(out=lp[:, 1, :], in0=lq[:, 2, :], in1=lq[:, 3, :], op=ALU.mult)
        P.op("dve", lam1, reads=[cB], writes=[cB])

        def lam2(e):
            e.reduce_sum(out=ls[:, 0:1], in_=lp[:, 0, :], axis=mybir.AxisListType.X)
            return e.reduce_sum(out=ls[:, 1:2], in_=lp[:, 1, :], axis=mybir.AxisListType.X)
        P.op("dve", lam2, reads=[cB], writes=[cB])
        P.op("act", lambda e: e.activation(out=ls[:, 2:4], in_=ls[:, 0:2], func=AF.Exp), reads=[cB], writes=[cB])
        P.op("dve", lambda e: e.scalar_tensor_tensor(out=ls[:, 4:5], in0=ls[:, 3:4], scalar=-lam_init,
                                                     in1=ls[:, 2:3], op0=ALU.add, op1=ALU.subtract),
             reads=[cB], writes=[cB])
        P.op("dve", lambda e: e.tensor_scalar(out=anrep[:], in0=anrep[:], scalar1=(1.0 - lam_init), scalar2=None,
                                              op0=ALU.mult), reads=[cB], writes=[cB])
        nlam = ls[:, 4:5]

        inb = []
        for i in range(2):
            vt = sb(es, nc, [128, NT, 257], BF16, "vA")
            vB = Buf()
            P.op("dve", lambda e, vt=vt: e.memset(vt[:, :, 256:257], 1.0), writes=[vB])
            inb.append(dict(q2=sb(es, nc, [128, 2, T], BF16, "q2"), k2=sb(es, nc, [128, 2, T], BF16, "k2"),
                            qB=Buf(), kB=Buf(), vA=vt, vB=vB, hs=sb(es, nc, [128, 2, T], BF16, "has"), hsB=Buf()))
        inr = Rot(inb)
        ptr = Rot(sbufs(es, nc, 4, [128, 2, 256], BF16, "PT"))
        rrr = Rot(sbufs(es, nc, 5, [128, 8], F32, "rr"))
        c0r = Rot(sbufs(es, nc, 5, [128, 257], F32, "c0"))
        c1r = Rot(sbufs(es, nc, 5, [128, 257], F32, "c1"))
        a0r = Rot(sbufs(es, nc, 5, [128, 256], F32, "a0"))
        a1r = Rot(sbufs(es, nc, 5, [128, 256], F32, "a1"))
        jkr = Rot(sbufs(es, nc, 5, [128, 256], F32, "jk"))
        habr = Rot(sbufs(es, nc, 5, [128, 256], BF16, "hab"))
        stp = Rot(psbufs(es, nc, 3, [128, 2, 256], F32, "stp"))
        outp = [[(pst(es, nc, [128, 257], F32, "outp"), Buf()) for j in range(2)] for m in range(2)]
        trp = Rot(psbufs(es, nc, 1, [128, 2, 128], BF16, "trp"))

        hb = {}

        def ensure_head(h):
            if h in hb or h >= H:
                return
            b = inr.next()
            c0 = (16 + 2 * h) * 128
            c1 = (32 + 2 * h) * 128
            P.dma("pool", lambda e: e.dma_start(
                out=b["q2"][:], in_=g.QKs[c0:c0 + 256, :].rearrange("(c p) t -> p c t", p=128)), b["qB"],
                writes=[b["qB"]])
            P.dma("pool", lambda e: e.dma_start(
                out=b["k2"][:], in_=g.QKs[c1:c1 + 256, :].rearrange("(c p) t -> p c t", p=128)), b["kB"],
                writes=[b["kB"]])
            P.dma("pool", lambda e: e.dma_start(out=b["vA"][:, :, 0:256], in_=tm_src(g.AV, h, NT)), b["vB"],
                  writes=[b["vB"]])
            hb[h] = b

        steps = [(h, gq, kt) for h in range(H) for gq in range(NG) for kt in range(2 * gq + 2)]
        LA = 2
        stq = {}

        def rec_qk(i):
            h, gq, kt = steps[i]
            ensure_head(h)
            b = hb[h]
            q2, k2 = b["q2"], b["k2"]
            qs = slice(gq * 256, (gq + 1) * 256)
            ks = slice(kt * 128, (kt + 1) * 128)
            st, stB = stp.next()

            def mmq(e):
                e.matmul(st[:, 0, :], lhsT=k2[:, 0, ks], rhs=q2[:, 0, qs], start=True, stop=True)
                return e.matmul(st[:, 1, :], lhsT=k2[:, 1, ks], rhs=q2[:, 1, qs], start=True, stop=True)
            P.op("pe", mmq, reads=[b["qB"], b["kB"]], writes=[stB])
            stq[i] = (st, stB)

        def epilogue(h, gq, j, b):
            (o0, o0B), (o1, o1B) = outp[0][j], outp[1][j]
            hs, hsB = b["hs"], b["hsB"]
            ts = slice((gq * 2 + j) * 128, (gq * 2 + j + 1) * 128)
            c0, c0B = c0r.next()
            c1, c1B = c1r.next()
            P.op("dve", lambda e: e.tensor_copy(out=c0[:], in_=o0[:]), reads=[o0B], writes=[c0B])
            P.op("dve", lambda e: e.tensor_copy(out=c1[:], in_=o1[:]), reads=[o1B], writes=[c1B])
            yield
            rr, rB = rrr.next()

            def rcp(e):
                e.reciprocal(out=rr[:, 0:1], in_=c0[:, 256:257])
                return e.reciprocal(out=rr[:, 1:2], in_=c1[:, 256:257])
            P.op("dve", rcp, reads=[c0B, c1B], writes=[rB])
            P.op("dve", lambda e: e.tensor_tensor(out=rr[:, 2:3], in0=rr[:, 1:2], in1=nlam, op=ALU.mult),
                 reads=[rB, cB], writes=[rB])
            yield
            a0, a0B = a0r.next()
            P.op("dve", lambda e: e.tensor_scalar(out=a0[:], in0=c0[:, 0:256], scalar1=rr[:, 0:1], scalar2=None,
                                                  op0=ALU.mult), reads=[c0B, rB], writes=[a0B])
            yield
            a1, a1B = a1r.next()
            P.op("dve", lambda e: e.scalar_tensor_tensor(out=a1[:], in0=c1[:, 0:256], scalar=rr[:, 2:3], in1=a0[:],
                                                         op0=ALU.mult, op1=ALU.add),
                 reads=[c1B, rB, a0B], writes=[a1B])
            yield
            jk, jkB = jkr.next()
            P.op("act", lambda e: e.activation(out=jk[:], in_=a1[:], func=AF.Square, accum_out=rr[:, 3:4]),
                 reads=[a1B, rB], writes=[jkB, rB])
            yield
            P.op("act", lambda e: e.activation(out=rr[:, 4:5], in_=rr[:, 3:4], func=AF.Ln, bias=EPS,
                                               scale=1.0 / 256), reads=[rB], writes=[rB])
            yield
            P.op("act", lambda e: e.activation(out=rr[:, 5:6], in_=rr[:, 4:5], func=AF.Exp, scale=-0.5),
                 reads=[rB], writes=[rB])
            yield
            hab, habB = habr.next()
            P.op("dve", lambda e: e.scalar_tensor_tensor(out=hab[:], in0=a1[:], scalar=rr[:, 5:6],
                                                         in1=anrep[:, h * 256:(h + 1) * 256], op0=ALU.mult,
                                                         op1=ALU.mult), reads=[a1B, rB, cB], writes=[habB])
            yield
            tr, trB = trp.next()

            def trs(e):
                e.transpose(tr[:, 0, :], hab[:, 0:128], g.identb[:])
                return e.transpose(tr[:, 1, :], hab[:, 128:256], g.identb[:])
            P.op("pe", trs, reads=[habB], writes=[trB])
            yield
            P.op("dve", lambda e: e.tensor_copy(out=hs[:, :, ts], in_=tr[:]), reads=[trB], writes=[hsB])

        bg = []

        def advance():
            for gen in list(bg):
                try:
                    next(gen)
                except StopIteration:
                    bg.remove(gen)

        for i in range(min(LA, len(steps))):
            rec_qk(i)
        for i, (h, gq, kt) in enumerate(steps):
            if gq == 0 and kt == 0:
                ensure_head(h + 1)
            if i + LA < len(steps):
                rec_qk(i + LA)
            b = hb[h]
            vA, vB = b["vA"], b["vB"]
            st, stB = stq.pop(i)
            pt, ptB = ptr.next()
            P.op("act", lambda e, pt=pt, st=st: e.activation(out=pt[:], in_=st[:], func=AF.Exp),
                 reads=[stB], writes=[ptB])
            if kt >= 2 * gq:
                j0 = kt - 2 * gq

                def msk(e, pt=pt, j0=j0):
                    e.tensor_tensor(out=pt[:, 0, j0 * 128:(j0 + 1) * 128], in0=pt[:, 0, j0 * 128:(j0 + 1) * 128],
                                    in1=g.trib[:], op=ALU.mult)
                    return e.tensor_tensor(out=pt[:, 1, j0 * 128:(j0 + 1) * 128],
                                           in0=pt[:, 1, j0 * 128:(j0 + 1) * 128], in1=g.trib[:], op=ALU.mult)
                P.op("dve", msk, reads=[ptB], writes=[ptB])
            js = [j for j in range(2) if kt <= 2 * gq + j]

            def mmv(e, pt=pt, vA=vA, kt=kt, gq=gq, js=js):
                for j in js:
                    for m in range(2):
                        ins = e.matmul(outp[m][j][0][:], lhsT=pt[:, m, j * 128:(j + 1) * 128], rhs=vA[:, kt, :],
                                       start=(kt == 0), stop=(kt == 2 * gq + j), skip_group_check=True)
                return ins
            P.op("pe", mmv, reads=[ptB, vB], writes=[outp[m][j][1] for j in js for m in range(2)])
            advance()
            if kt == 2 * gq:
                gen = epilogue(h, gq, 0, b)
                next(gen)
                bg.append(gen)
            if kt == 2 * gq + 1:
                gen = epilogue(h, gq, 1, b)
                next(gen)
                bg.append(gen)
                if gq == NG - 1:
                    while bg:
                        advance()
                    P.dma("sp", lambda e, b=b, h=h: e.dma_start(
                        out=g.HAT[h * 256:(h + 1) * 256, :].rearrange("(j p) t -> p j t", p=128), in_=b["hs"][:]),
                        b["hsB"], reads=[b["hsB"]])
                    del hb[h]
        P.finish()


def ph_merge(g, l, which):
    nc, T = g.nc, g.T
    NQ = T // 512
    wa = (g.w_bm if which == 0 else g.w_bd)[l]
    wg = g.w_in[l]
    goff = O_GM if which == 0 else O_GD
    src = g.HMT if which == 0 else g.HAT
    with ExitStack() as es:
        P = Prog(nc, es, uid("mrg"))
        xT = sb(es, nc, [128, KC, T], BF16, "xT")
        xB = Buf()
        bg = sb(es, nc, [128, 32], F32, "bg")
        bgB = Buf()
        P.dma("pool", lambda e: e.dma_start(out=bg[:], in_=g.bgate[l]), bgB, writes=[bgB])
        P.dma("pool", lambda e: e.dma_start(out=xT[:], in_=fm(src)), xB, writes=[xB])
        SW = 256
        NS = 2048 // SW
        CPS = SW // 128
        slabs = Rot(sbufs(es, nc, 4, [128, KC, SW], BF16, "wsl"))
        psy = Rot(psbufs(es, nc, 3, [128, 512], F32, "py"))
        psg = Rot(psbufs(es, nc, 3, [128, 512], F32, "pg"))
        sgr = Rot(sbufs(es, nc, 2, [128, 512], F32, "sg"))
        y1r = Rot(sbufs(es, nc, 3, [128, 512], F32, "y1"))
        if which == 1:
            tr_ = Rot(sbufs(es, nc, 2, [128, 512], F32, "tt"))
            yor = Rot(sbufs(es, nc, 3, [128, 512], BF16, "yo"))
        loaded = []

        def load(s):
            ta, ba = slabs.next()
            P.dma("pool", lambda e, ta=ta, s=s: e.dma_start(out=ta[:], in_=slab_src(wa, s * SW, SW)), ba,
                  writes=[ba])
            tg, bgs = slabs.next()
            P.dma("pool", lambda e, tg=tg, s=s: e.dma_start(out=tg[:], in_=slab_src(wg, goff + s * SW, SW)), bgs,
                  writes=[bgs])
            loaded.append((ta, ba, tg, bgs))
        load(0)
        for s in range(NS):
            if s + 1 < NS:
                load(s + 1)
            ta, ba, tg, bgs = loaded[s]
            for cc in range(CPS):
                ch = s * CPS + cc
                for q in range(NQ):
                    qs = slice(q * 512, (q + 1) * 512)
                    py, pyB = psy.next()
                    pg, pgB = psg.next()

                    def mm(e, py=py, pg=pg, ta=ta, tg=tg, cc=cc, qs=qs):
                        for k in range(KC):
                            e.matmul(py[:], lhsT=ta[:, k, cc * 128:(cc + 1) * 128], rhs=xT[:, k, qs],
                                     start=(k == 0), stop=(k == KC - 1))
                        for k in range(KC):
                            ins = e.matmul(pg[:], lhsT=tg[:, k, cc * 128:(cc + 1) * 128], rhs=g.hnT[:, k, qs],
                                           start=(k == 0), stop=(k == KC - 1))
                        return ins
                    P.op("pe", mm, reads=[ba, bgs, xB], writes=[pyB, pgB])
                    sg, sgB = sgr.next()
                    bcol = which * 16 + ch
                    P.op("act", lambda e, sg=sg, pg=pg, bcol=bcol: e.activation(
                        out=sg[:], in_=pg[:], func=AF.Sigmoid, bias=bg[:, bcol:bcol + 1]),
                        reads=[pgB, bgB], writes=[sgB])
                    if which == 0:
                        y1, y1B = y1r.next()
                        P.op("dve", lambda e, y1=y1, py=py, sg=sg: e.tensor_tensor(out=y1[:], in0=py[:], in1=sg[:],
                                                                                   op=ALU.mult),
                             reads=[pyB, sgB], writes=[y1B])
                        P.dma("sp", lambda e, y1=y1, ch=ch, qs=qs: e.dma_start(
                            out=g.Y1[ch * 128:(ch + 1) * 128, qs], in_=y1[:]), y1B, reads=[y1B])
                    else:
                        y1, y1B = y1r.next()
                        P.dma("pool", lambda e, y1=y1, ch=ch, qs=qs: e.dma_start(
                            out=y1[:], in_=g.Y1[ch * 128:(ch + 1) * 128, qs]), y1B, writes=[y1B])
                        tt, ttB = tr_.next()
                        P.op("dve", lambda e, tt=tt, py=py, sg=sg: e.tensor_tensor(out=tt[:], in0=py[:], in1=sg[:],
                                                                                   op=ALU.mult),
                             reads=[pyB, sgB], writes=[ttB])
                        yo, yoB = yor.next()
                        P.op("dve", lambda e, yo=yo, tt=tt, y1=y1: e.tensor_tensor(out=yo[:], in0=tt[:], in1=y1[:],
                                                                                   op=ALU.add),
                             reads=[ttB, y1B], writes=[yoB])
                        P.dma("sp", lambda e, yo=yo, ch=ch, qs=qs: e.dma_start(
                            out=g.YT[ch * 128:(ch + 1) * 128, qs], in_=yo[:]), yoB, reads=[yoB])
        P.finish()


def ph_dump_hn(g, dst):
    nc = g.nc
    with ExitStack() as es:
        P = Prog(nc, es, uid("dmp"))
        b = Buf()
        for k in range(KC):
            P.dma("sp", lambda e, k=k: e.dma_start(out=dst[k * 128:(k + 1) * 128, :], in_=g.hnT[:, k, :]), b,
                  reads=[b])
        P.finish()


def ph_oproj(g, l, hsrc):
    nc, T = g.nc, g.T
    NQ = T // 512
    wo = g.w_o[l]
    with ExitStack() as es:
        P = Prog(nc, es, uid("opr"))
        xT = sb(es, nc, [128, KC, T], BF16, "xT")
        xB = Buf()
        P.dma("pool", lambda e: e.dma_start(out=xT[:], in_=fm(g.YT)), xB, writes=[xB])
        slabs = Rot(sbufs(es, nc, 3, [128, KC, 512], BF16, "wsl"))
        psr = Rot(psbufs(es, nc, 4, [128, 512], F32, "pp"))
        hir = Rot(sbufs(es, nc, 3, [128, 512], F32, "hi"))
        hor = Rot(sbufs(es, nc, 3, [128, 512], F32, "ho"))
        loaded = []

        def load(s):
            t, b = slabs.next()
            P.dma("pool", lambda e, t=t, s=s: e.dma_start(out=t[:], in_=slab_src(wo, s * 512, 512)), b, writes=[b])
            loaded.append((t, b))
        load(0)
        load(1)
        for s in range(4):
            if s + 2 < 4:
                load(s + 2)
            wt, wB = loaded[s]
            for cc in range(4):
                ch = s * 4 + cc
                for q in range(NQ):
                    qs = slice(q * 512, (q + 1) * 512)
                    hi, hiB = hir.next()
                    P.dma("pool", lambda e, hi=hi, ch=ch, qs=qs: e.dma_start(
                        out=hi[:], in_=hsrc[ch * 128:(ch + 1) * 128, qs]), hiB, writes=[hiB])
                    pt, pB = psr.next()

                    def mm(e, pt=pt, wt=wt, cc=cc, qs=qs):
                        for k in range(KC):
                            ins = e.matmul(pt[:], lhsT=wt[:, k, cc * 128:(cc + 1) * 128], rhs=xT[:, k, qs],
                                           start=(k == 0), stop=(k == KC - 1))
                        return ins
                    P.op("pe", mm, reads=[wB, xB], writes=[pB])
                    ho, hoB = hor.next()
                    P.op("dve", lambda e, ho=ho, pt=pt, hi=hi: e.tensor_tensor(out=ho[:], in0=pt[:], in1=hi[:],
                                                                               op=ALU.add),
                         reads=[pB, hiB], writes=[hoB])
                    P.dma("sp", lambda e, ho=ho, ch=ch, qs=qs: e.dma_start(
                        out=g.hT[ch * 128:(ch + 1) * 128, qs], in_=ho[:]), hoB, reads=[hoB])
                    if False:
                        P.dma("sp", lambda e, ho=ho, ch=ch, qs=qs: e.dma_start(
                            out=g.H1[ch * 128:(ch + 1) * 128, qs], in_=ho[:]), hoB, reads=[hoB])
        P.finish()


def ph_up(g, l):
    nc, T = g.nc, g.T
    NQ = T // 512
    wu = g.w_up[l]
    with ExitStack() as es:
        P = Prog(nc, es, uid("up"))
        cw = sb(es, nc, [128, FC, 3], F32, "cw")
        cb = sb(es, nc, [128, FC], F32, "cb")
        cB = Buf()
        P.dma("pool", lambda e: e.dma_start(out=cw[:], in_=g.cw[l]), cB, writes=[cB])
        P.dma("pool", lambda e: e.dma_start(out=cb[:], in_=g.cb[l]), cB, writes=[cB])
        slabs = Rot(sbufs(es, nc, 4, [128, KC, 512], BF16, "wsl"))
        psr = Rot(psbufs(es, nc, 6, [128, 512], F32, "pp"))
        gts = [(sb(es, nc, [128, T + 2], F32, "gt"), Buf()) for _ in range(2)]
        vsr = Rot(sbufs(es, nc, 2, [128, T], F32, "vs"))
        cvr = Rot(sbufs(es, nc, 2, [128, T], F32, "cv"))
        uor = Rot(sbufs(es, nc, 2, [128, T], BF16, "uo"))
        for gt, gB in gts:
            P.op("dve", lambda e, gt=gt: e.memset(gt[:, 0:2], 0.0), writes=[gB])
        gtr = Rot(gts)
        loaded = []

        def load(s):
            ta, ba = slabs.next()
            P.dma("pool", lambda e, ta=ta, s=s: e.dma_start(out=ta[:], in_=slab_src(wu, s * 512, 512)), ba,
                  writes=[ba])
            tv, bv = slabs.next()
            P.dma("pool", lambda e, tv=tv, s=s: e.dma_start(out=tv[:], in_=slab_src(wu, DFF + s * 512, 512)), bv,
                  writes=[bv])
            loaded.append((ta, ba, tv, bv))
        load(0)
        for s in range(11):
            if s + 1 < 11:
                load(s + 1)
            ta, ba, tv, bv = loaded[s]
            for cc in range(4):
                j = s * 4 + cc
                gt, gB = gtr.next()
                vs, vB = vsr.next()
                for q in range(NQ):
                    qs = slice(q * 512, (q + 1) * 512)
                    pa, paB = psr.next()
                    pv, pvB = psr.next()

                    def mm(e, pa=pa, pv=pv, ta=ta, tv=tv, cc=cc, qs=qs):
                        for k in range(KC):
                            e.matmul(pa[:], lhsT=ta[:, k, cc * 128:(cc + 1) * 128], rhs=g.hnT[:, k, qs],
                                     start=(k == 0), stop=(k == KC - 1))
                        for k in range(KC):
                            ins = e.matmul(pv[:], lhsT=tv[:, k, cc * 128:(cc + 1) * 128], rhs=g.hnT[:, k, qs],
                                           start=(k == 0), stop=(k == KC - 1))
                        return ins
                    P.op("pe", mm, reads=[ba, bv], writes=[paB, pvB])
                    P.op("act", lambda e, gt=gt, pa=pa, q=q: e.activation(
                        out=gt[:, 2 + q * 512:2 + (q + 1) * 512], in_=pa[:], func=AF.Identity),
                        reads=[paB], writes=[gB])
                    P.op("act", lambda e, vs=vs, pv=pv, qs=qs: e.activation(out=vs[:, qs], in_=pv[:],
                                                                            func=AF.Identity),
                         reads=[pvB], writes=[vB])
                cv, cvB = cvr.next()

                P.op("dve", lambda e, cv=cv, gt=gt, j=j: e.tensor_scalar(
                    out=cv[:], in0=gt[:, 0:T], scalar1=cw[:, j, 0:1], scalar2=cb[:, j:j + 1], op0=ALU.mult,
                    op1=ALU.add), reads=[gB, cB], writes=[cvB])
                P.op("dve", lambda e, cv=cv, gt=gt, j=j: e.scalar_tensor_tensor(
                    out=cv[:], in0=gt[:, 1:T + 1], scalar=cw[:, j, 1:2], in1=cv[:], op0=ALU.mult, op1=ALU.add),
                    reads=[gB, cB, cvB], writes=[cvB])
                P.op("dve", lambda e, cv=cv, gt=gt, j=j: e.scalar_tensor_tensor(
                    out=cv[:], in0=gt[:, 2:T + 2], scalar=cw[:, j, 2:3], in1=cv[:], op0=ALU.mult, op1=ALU.add),
                    reads=[gB, cB, cvB], writes=[cvB])
                P.op("act", lambda e, cv=cv: e.activation(out=cv[:], in_=cv[:], func=AF.Silu),
                     reads=[cvB], writes=[cvB])
                uo, uoB = uor.next()
                P.op("dve", lambda e, uo=uo, cv=cv, vs=vs: e.tensor_tensor(out=uo[:], in0=cv[:], in1=vs[:],
                                                                           op=ALU.mult),
                     reads=[cvB, vB], writes=[uoB])
                P.dma("sp", lambda e, uo=uo, j=j: e.dma_start(out=g.UT[j * 128:(j + 1) * 128, :], in_=uo[:]), uoB,
                      reads=[uoB])
        P.finish()


def ph_down(g, l):
    nc, T = g.nc, g.T
    TT = min(T, 1024)
    NQ = TT // 512
    wd = g.w_down[l]
    with ExitStack() as es:
        P = Prog(nc, es, uid("dwn"))
        uT = sb(es, nc, [128, FC, TT], BF16, "uT")
        uB = Buf()
        slabs = Rot(sbufs(es, nc, 3, [128, FC, 128], BF16, "wsl"))
        psr = Rot(psbufs(es, nc, 4, [128, 512], F32, "pp"))
        hir = Rot(sbufs(es, nc, 3, [128, 512], F32, "hi"))
        hor = Rot(sbufs(es, nc, 3, [128, 512], F32, "ho"))
        for half in range(T // TT):
            t0 = half * TT
            P.dma("pool", lambda e, t0=t0: e.dma_start(out=uT[:], in_=fm(g.UT[:, t0:t0 + TT])), uB, writes=[uB])
            loaded = []

            def load(c):
                t, b = slabs.next()
                P.dma("pool", lambda e, t=t, c=c: e.dma_start(
                    out=t[:], in_=wd[:, c * 128:(c + 1) * 128].rearrange("(k p) n -> p k n", p=128)), b,
                    writes=[b])
                loaded.append((t, b))
            load(0)
            load(1)
            for c in range(KC):
                if c + 2 < KC:
                    load(c + 2)
                wt, wB = loaded[c]
                for q in range(NQ):
                    qs = slice(q * 512, (q + 1) * 512)
                    gs = slice(t0 + q * 512, t0 + (q + 1) * 512)
                    hi, hiB = hir.next()
                    P.dma("pool", lambda e, hi=hi, c=c, gs=gs: e.dma_start(
                        out=hi[:], in_=g.hT[c * 128:(c + 1) * 128, gs]), hiB, writes=[hiB])
                    pt, pB = psr.next()

                    def mm(e, pt=pt, wt=wt, qs=qs):
                        for k in range(FC):
                            ins = e.matmul(pt[:], lhsT=wt[:, k, :], rhs=uT[:, k, qs], start=(k == 0),
                                           stop=(k == FC - 1))
                        return ins
                    P.op("pe", mm, reads=[wB, uB], writes=[pB])
                    ho, hoB = hor.next()
                    P.op("dve", lambda e, ho=ho, pt=pt, hi=hi: e.tensor_tensor(out=ho[:], in0=pt[:], in1=hi[:],
                                                                               op=ALU.add),
                         reads=[pB, hiB], writes=[hoB])
                    P.dma("sp", lambda e, ho=ho, c=c, gs=gs: e.dma_start(
                        out=g.hT[c * 128:(c + 1) * 128, gs], in_=ho[:]), hoB, reads=[hoB])
        P.finish()


def build(T, L, debug=False):
    nc = bass.Bass("TRN2", target_bir_lowering=False)
    g = G()
    g.nc, g.T, g.L = nc, T, L
    g.debug = debug

    def din(name, shape, dt=F32):
        return nc.dram_tensor(name, list(shape), dt, kind="ExternalInput").ap()

    def scr(name, shape, dt):
        kind = "ExternalOutput" if debug else "Internal"
        return nc.dram_tensor(name, list(shape), dt, kind=kind).ap()
    g.xT = din("xT", [D, T])
    g.w_in = din("w_in", [L, D, NIN])
    g.w_bm = din("w_bm", [L, D, D])
    g.w_bd = din("w_bd", [L, D, D])
    g.w_o = din("w_o", [L, D, D])
    g.w_up = din("w_up", [L, D, 2 * DFF])
    g.w_down = din("w_down", [L, DFF, D])
    g.gmix = din("gmix", [L, 128, KC])
    g.gffn = din("gffn", [L, 128, KC])
    g.gfin = din("gfin", [128, KC])
    g.bqk = din("bqk", [L, 128, 48])
    g.bgate = din("bgate", [L, 128, 32])
    g.btm = din("btm", [L, 6144])
    g.bif = din("bif", [L, 16])
    g.mnorm = din("mnorm", [L, 2048])
    g.anorm = din("anorm", [L, 2048])
    g.lamqk = din("lamqk", [L, 512])
    g.cw = din("cw", [L, 128, FC, 3])
    g.cb = din("cb", [L, 128, FC])
    g.c_ident = din("c_ident", [128, 128])
    g.c_tri = din("c_tri", [128, 128])
    g.c_ones = din("c_ones", [128, 128])
    g.c_pm = din("c_pm", [32, 32])
    g.c_cos = din("c_cos", [32, T])
    g.c_sin = din("c_sin", [32, T])
    g.outT = nc.dram_tensor("outT", [D, T], F32, kind="ExternalOutput").ap()
    g.hT = scr("hT", [D, T], F32)
    g.QKs = scr("QKs", [48 * 128, T], BF16)
    g.MV = scr("MV", [T, 2048], BF16)
    g.MO = scr("MO", [T, 2048], BF16)
    g.AV = scr("AV", [T, 2048], BF16)
    g.HMT = scr("HMT", [D, T], BF16)
    g.HAT = scr("HAT", [D, T], BF16)
    g.Y1 = scr("Y1", [D, T], F32)
    g.YT = scr("YT", [D, T], BF16)
    g.UT = scr("UT", [DFF, T], BF16)
    if debug:
        g.H1 = scr("H1", [D, T], F32)
        g.HN2 = scr("HN2", [D, T], BF16)
    with ExitStack() as es:
        g.hnT = sb(es, nc, [128, KC, T], BF16, "hnT")
        g.IF = sb(es, nc, [128, T // 128, 16], F32, "IF")
        g.identb = sb(es, nc, [128, 128], BF16, "identb")
        g.trib = sb(es, nc, [128, 128], BF16, "trib")
        g.trif = sb(es, nc, [128, 128], F32, "trif")
        g.onesf = sb(es, nc, [128, 128], F32, "onesf")
        g.onesb = sb(es, nc, [128, 128], BF16, "onesb")
        g.pmf = sb(es, nc, [32, 32], F32, "pmf")
        g.pmb = sb(es, nc, [32, 32], BF16, "pmb")
        ph_consts(g)
        for l in range(L):
            hsrc = g.xT if l == 0 else g.hT
            ph_norm(g, hsrc, g.gmix[l])
            ph_proj(g, l)
            ph_mlstm(g, l)
            ph_attn(g, l)
            ph_merge(g, l, 0)
            ph_merge(g, l, 1)
            ph_oproj(g, l, hsrc)
            if debug == 2:
                continue
            ph_norm(g, g.hT, g.gffn[l])
            ph_up(g, l)
            if debug == 3:
                continue
            ph_down(g, l)
        ph_norm(g, g.hT, g.gfin, final_dst=g.outT)
    return nc


def pcol(v, nch):
    v = np.asarray(v, dtype=np.float32)
    return np.ascontiguousarray(np.swapaxes(v.reshape(v.shape[:-1] + (nch, 128)), -1, -2))


def host_consts(T):
    s = np.arange(128)
    tri = (s[:, None] <= s[None, :]).astype(np.float32)
    pm = np.zeros((32, 32), np.float32)
    for j in range(16):
        pm[j + 16, j] = -1.0
        pm[j, j + 16] = 1.0
    half = 16
    inv = (np.float32(ROPE_THETA) ** (-np.arange(half, dtype=np.float32) / np.float32(half))).astype(np.float32)
    ang = np.arange(T, dtype=np.float32)[:, None] * inv[None, :]
    cos = np.cos(ang.astype(np.float64)).astype(np.float32).T
    sin = np.sin(ang.astype(np.float64)).astype(np.float32).T
    return {
        "c_ident": np.eye(128, dtype=np.float32), "c_tri": tri, "c_ones": np.ones((128, 128), np.float32),
        "c_pm": pm, "c_cos": np.ascontiguousarray(np.concatenate([cos, cos], 0)),
        "c_sin": np.ascontiguousarray(np.concatenate([sin, sin], 0)),
    }


def prep_shared(L, T, norm_mix, w_in, b_in, m_norm, a_norm, lam_qk, w_bm, w_bd, w_o, norm_ffn, w_up, conv_w, conv_b,
                w_down, norm_final):
    f = lambda a: np.ascontiguousarray(np.asarray(a, dtype=np.float32))
    b_in = f(b_in)
    bqk = np.concatenate([b_in[:, O_MQ:O_MQ + 2048], b_in[:, O_AQ:O_AQ + 4096]], axis=1)
    bgate = b_in[:, O_GM:O_GM + 4096]
    btm = np.concatenate([b_in[:, O_MV:O_MV + 4096], b_in[:, O_AV:O_AV + 2048]], axis=1)
    cwp = np.stack([pcol(f(conv_w)[:, i, :], FC) for i in range(3)], axis=-1)
    d = {
        "w_in": f(w_in)[:L], "w_bm": f(w_bm)[:L], "w_bd": f(w_bd)[:L], "w_o": f(w_o)[:L], "w_up": f(w_up)[:L],
        "w_down": f(w_down)[:L],
        "gmix": pcol(norm_mix, KC)[:L], "gffn": pcol(norm_ffn, KC)[:L], "gfin": pcol(norm_final, KC),
        "bqk": pcol(bqk, 48)[:L], "bgate": pcol(bgate, 32)[:L], "btm": f(btm)[:L],
        "bif": f(b_in[:, O_MI:O_MI + 16])[:L], "mnorm": f(m_norm)[:L], "anorm": f(a_norm)[:L],
        "lamqk": f(lam_qk).reshape(-1, 512)[:L], "cw": np.ascontiguousarray(cwp)[:L], "cb": pcol(conv_b, FC)[:L],
    }
    d.update(host_consts(T))
    return {k: np.ascontiguousarray(v) for k, v in d.items()}


def kernel(x, norm_mix, w_in, b_in, m_norm, a_norm, lam_qk, w_bm, w_bd, w_o, norm_ffn, w_up, conv_w, conv_b, w_down,
           norm_final):
    x = np.asarray(x, dtype=np.float32)
    B, S, _ = x.shape
    L = np.asarray(norm_mix).shape[0]
    shared = prep_shared(L, S, norm_mix, w_in, b_in, m_norm, a_norm, lam_qk, w_bm, w_bd, w_o, norm_ffn, w_up, conv_w,
                         conv_b, w_down, norm_final)
    nc = build(S, L)
    NCORES = 8
    real = [0, 1, 4, 5][:B]
    zero_shared = {k: (v if k.startswith("c_") else np.zeros_like(v)) for k, v in shared.items()}
    zx = np.zeros((D, S), np.float32)
    in_maps = []
    for c in range(NCORES):
        if c in real:
            m = dict(shared)
            m["xT"] = np.ascontiguousarray(x[real.index(c)].T)
        else:
            m = dict(zero_shared)
            m["xT"] = zx
        in_maps.append(m)
    res = run_bass_kernel_spmd(nc, in_maps, core_ids=list(range(NCORES)))
    out = np.stack([np.ascontiguousarray(res.results[real[b]]["outT"].T) for b in range(B)], axis=0)
    return out.astype(np.float32)
```

```python
import math
from contextlib import ExitStack

import numpy as np
import concourse.bass as bass
import concourse.mybir as mybir
from concourse.bass_utils import run_bass_kernel_spmd

F32 = mybir.dt.float32
BF16 = mybir.dt.bfloat16
AF = mybir.ActivationFunctionType
ALU = mybir.AluOpType

D = 2048
KC = 16
H = 8
NIN = 16400
DFF = 5632
FC = 44
EPS = 1e-6
O_MQ, O_MK, O_MV, O_MO, O_MI, O_AQ, O_AK, O_AV, O_GM, O_GD = (
    0, 1024, 2048, 4096, 6144, 6160, 8208, 10256, 12304, 14352)
ROPE_THETA = 500000.0


class Buf:
    __slots__ = ("w", "r", "dsem", "dcnt")

    def __init__(self):
        self.w = None
        self.r = {}
        self.dsem = None
        self.dcnt = 0


class Prog:
    ENGS = ("pe", "act", "dve", "pool", "sp")

    def __init__(self, nc, es, name):
        self.nc, self.es, self.name = nc, es, name
        self.q = {e: [] for e in self.ENGS}
        self.allsems = []
        self.sem = {e: self._newsem(f"{name}_{e}") for e in self.ENGS}
        self.cnt = {e: 0 for e in self.ENGS}
        self.waited = {e: {} for e in self.ENGS}
        self.dsems = []
        self.nd = 0

    def _newsem(self, name):
        h = self.nc.alloc_semaphore(name=name)
        self.allsems.append(h)
        return h

    def _wait(self, eng, ev):
        sem, val = ev
        if eng == "pe" and sem is self.sem["pe"]:
            return
        w = self.waited[eng]
        k = id(sem)
        if w.get(k, 0) >= val:
            return
        w[k] = val
        self.q[eng].append((0, sem, val))

    def _deps(self, eng, reads, writes):
        for b in reads:
            if b.w is not None:
                self._wait(eng, b.w)
        for b in writes:
            if b.w is not None:
                self._wait(eng, b.w)
            for ev in b.r.values():
                self._wait(eng, ev)

    @staticmethod
    def _commit(ev, reads, writes):
        for b in reads:
            b.r[id(ev[0])] = ev
        for b in writes:
            b.w = ev
            b.r = {}

    def op(self, eng, fn, reads=(), writes=()):
        self._deps(eng, reads, writes)
        self.cnt[eng] += 1
        ev = (self.sem[eng], self.cnt[eng])
        self.q[eng].append((1, fn, self.sem[eng], 1))
        self._commit(ev, reads, writes)
        return ev

    def dma(self, qeng, fn, sb, reads=(), writes=()):
        self._deps(qeng, reads, writes)
        if sb.dsem is None:
            self.nd += 1
            sb.dsem = self._newsem(f"{self.name}_d{self.nd}")
            sb.dcnt = 0
            self.dsems.append(sb)
        sb.dcnt += 16
        ev = (sb.dsem, sb.dcnt)
        self.q[qeng].append((1, fn, sb.dsem, 16))
        self._commit(ev, reads, writes)
        return ev

    def finish(self):
        for e in self.ENGS:
            for e2 in self.ENGS:
                if e2 != e and self.cnt[e2] > 0:
                    self._wait(e, (self.sem[e2], self.cnt[e2]))
            for sb in self.dsems:
                self._wait(e, (sb.dsem, sb.dcnt))
        for sb in self.dsems:
            sb.dsem = None
            sb.dcnt = 0
            sb.w = None
            sb.r = {}
        with self.nc.Block() as blk:
            blk.tensor(lambda e: self._emit("pe", e))
            blk.scalar(lambda e: self._emit("act", e))
            blk.vector(lambda e: self._emit("dve", e))
            blk.gpsimd(lambda e: self._emit("pool", e))
            blk.sync(lambda e: self._emit("sp", e))
        self.nc.all_engine_barrier()
        self.nc.clear_and_free_semaphores(self.allsems)
        self.nc.all_engine_barrier()

    def _emit(self, eng, e):
        for it in self.q[eng]:
            if it[0] == 0:
                e.wait_ge(it[1], it[2])
            else:
                it[1](e).then_inc(it[2], it[3])


class Rot:
    def __init__(self, items):
        self.items = items
        self.i = 0

    def next(self):
        x = self.items[self.i]
        self.i = (self.i + 1) % len(self.items)
        return x


class G:
    pass


_uid = [0]


def uid(p):
    _uid[0] += 1
    return f"{p}{_uid[0]}"


def sb(es, nc, shape, dt, name="t"):
    return es.enter_context(nc.sbuf_tensor(uid(name), list(shape), dt))


def pst(es, nc, shape, dt, name="p"):
    return es.enter_context(nc.psum_tensor(uid(name), list(shape), dt))


def sbufs(es, nc, n, shape, dt, name="t"):
    return [(sb(es, nc, shape, dt, name), Buf()) for _ in range(n)]


def psbufs(es, nc, n, shape, dt, name="p"):
    return [(pst(es, nc, shape, dt, name), Buf()) for _ in range(n)]


def fm(ap):
    return ap.rearrange("(k p) t -> p k t", p=128)


def ph_consts(g):
    nc = g.nc
    with ExitStack() as es:
        P = Prog(nc, es, uid("cst"))
        for t, src in ((g.identb, g.c_ident), (g.trib, g.c_tri), (g.trif, g.c_tri), (g.onesf, g.c_ones),
                       (g.onesb, g.c_ones), (g.pmf, g.c_pm), (g.pmb, g.c_pm)):
            P.dma("pool", lambda e, t=t, src=src: e.dma_start(out=t[:], in_=src), Buf(), writes=[])
        P.finish()


def ph_norm(g, src, gam_src, final_dst=None):
    nc, T = g.nc, g.T
    W = 256
    with ExitStack() as es:
        P = Prog(nc, es, uid("nrm"))
        hb = Rot(sbufs(es, nc, 2, [128, KC, W], F32, "hb"))
        sq = sb(es, nc, [128, KC, W], BF16, "sq")
        sqA, sqB = Buf(), Buf()
        gm = sb(es, nc, [128, KC], F32, "gm")
        gmB = Buf()
        rs = Rot(sbufs(es, nc, 2, [128, W], F32, "rs"))
        rstd = Rot(sbufs(es, nc, 2, [128, W], F32, "rstd"))
        ps = Rot(psbufs(es, nc, 2, [128, W], F32, "nps"))
        ob = Rot(sbufs(es, nc, 2, [128, KC, W], F32, "ob")) if final_dst is not None else None
        P.dma("pool", lambda e: e.dma_start(out=gm[:], in_=gam_src), gmB, writes=[gmB])
        for j in range(T // W):
            ht, hB = hb.next()
            P.dma("pool", lambda e, ht=ht, j=j: e.dma_start(out=ht[:], in_=fm(src[:, j * W:(j + 1) * W])), hB,
                  writes=[hB])
            P.op("act", lambda e, ht=ht: e.activation(out=sq[:, 0:8, :], in_=ht[:, 0:8, :], func=AF.Square),
                 reads=[hB], writes=[sqA])
            P.op("dve", lambda e, ht=ht: e.tensor_tensor(out=sq[:, 8:16, :], in0=ht[:, 8:16, :], in1=ht[:, 8:16, :],
                                                         op=ALU.mult), reads=[hB], writes=[sqB])
            pt, pB = ps.next()

            def mm(e, pt=pt):
                for k in range(KC):
                    ins = e.matmul(pt[:], lhsT=g.onesb[:], rhs=sq[:, k, :], start=(k == 0), stop=(k == KC - 1))
                return ins
            P.op("pe", mm, reads=[sqA, sqB], writes=[pB])
            rt, rB = rs.next()
            P.op("act", lambda e, rt=rt, pt=pt: e.activation(out=rt[:], in_=pt[:], func=AF.Sqrt, bias=EPS,
                                                             scale=1.0 / D), reads=[pB], writes=[rB])
            st, sB = rstd.next()
            P.op("dve", lambda e, st=st, rt=rt: e.reciprocal(out=st[:], in_=rt[:]), reads=[rB], writes=[sB])
            if final_dst is None:
                def nrm(e, ht=ht, st=st, j=j):
                    for k in range(KC):
                        ins = e.scalar_tensor_tensor(out=g.hnT[:, k, j * W:(j + 1) * W], in0=ht[:, k, :],
                                                     scalar=gm[:, k:k + 1], in1=st[:], op0=ALU.mult, op1=ALU.mult)
                    return ins
                P.op("dve", nrm, reads=[hB, sB, gmB], writes=[Buf()])
            else:
                ot, oB = ob.next()

                def nrm(e, ht=ht, st=st, ot=ot):
                    for k in range(KC):
                        ins = e.scalar_tensor_tensor(out=ot[:, k, :], in0=ht[:, k, :],
                                                     scalar=gm[:, k:k + 1], in1=st[:], op0=ALU.mult, op1=ALU.mult)
                    return ins
                P.op("dve", nrm, reads=[hB, sB, gmB], writes=[oB])
                P.dma("sp", lambda e, ot=ot, j=j: e.dma_start(out=fm(final_dst[:, j * W:(j + 1) * W]), in_=ot[:]),
                      oB, reads=[oB])
        P.finish()


def slab_src(w2d, c0, cw):
    return w2d[:, c0:c0 + cw].rearrange("(k p) n -> p k n", p=128)


def ph_proj(g, l):
    nc, T = g.nc, g.T
    NQ, NT = T // 512, T // 128
    w = g.w_in[l]
    with ExitStack() as es:
        P = Prog(nc, es, uid("prj"))
        slabs = Rot(sbufs(es, nc, 3, [128, KC, 512], BF16, "wsl"))
        psr = Rot(psbufs(es, nc, 4, [128, 512], F32, "pp"))
        pxp = Rot(psbufs(es, nc, 2, [32, 512], F32, "px"))
        stg = Rot(sbufs(es, nc, 4, [128, 512], BF16, "stg"))
        xbr = Rot(sbufs(es, nc, 3, [128, 512], F32, "xb"))
        x16r = Rot(sbufs(es, nc, 2, [32, 512], BF16, "x16"))
        t1r = Rot(sbufs(es, nc, 2, [32, 512], F32, "t1"))
        t2r = Rot(sbufs(es, nc, 2, [32, 512], F32, "t2"))
        tmr = Rot(sbufs(es, nc, 2, [128, 512], F32, "tm"))
        cosT = sb(es, nc, [32, T], F32, "cos")
        sinT = sb(es, nc, [32, T], F32, "sin")
        bqk = sb(es, nc, [128, 48], F32, "bqk")
        brep = sb(es, nc, [128, 6144], F32, "brep")
        bif = sb(es, nc, [128, 16], F32, "bif")
        wif = sb(es, nc, [128, KC, 16], BF16, "wif")
        cB = Buf()
        for t, src in ((cosT, g.c_cos), (sinT, g.c_sin), (bqk, g.bqk[l]), (brep, g.btm[l].partition_broadcast(128)),
                       (bif, g.bif[l].partition_broadcast(128)), (wif, slab_src(w, O_MI, 16))):
            P.dma("pool", lambda e, t=t, src=src: e.dma_start(out=t[:], in_=src), cB, writes=[cB])
        jobs = []
        for s in range(2):
            jobs.append((O_MQ + s * 512, "fm", ("m", 0 + s * 4)))
        for s in range(2):
            jobs.append((O_MK + s * 512, "fm", ("m", 8 + s * 4)))
        for s in range(4):
            jobs.append((O_AQ + s * 512, "fm", ("aq", 16 + s * 4)))
        for s in range(4):
            jobs.append((O_AK + s * 512, "fm", ("ak", 32 + s * 4)))
        for s in range(4):
            jobs.append((O_MV + s * 512, "tm", ("mv", s)))
        for s in range(4):
            jobs.append((O_MO + s * 512, "tm", ("mo", s)))
        for s in range(4):
            jobs.append((O_AV + s * 512, "tm", ("av", s)))
        loaded = []

        def load(i):
            c0 = jobs[i][0]
            t, b = slabs.next()
            P.dma("pool", lambda e, t=t, c0=c0: e.dma_start(out=t[:], in_=slab_src(w, c0, 512)), b, writes=[b])
            loaded.append((t, b))
        load(0)
        load(1)
        for tt in range(NT):
            pt, pB = psr.next()

            def mm(e, pt=pt, tt=tt):
                for k in range(KC):
                    ins = e.matmul(pt[:, 0:16], lhsT=g.hnT[:, k, tt * 128:(tt + 1) * 128], rhs=wif[:, k, :],
                                   start=(k == 0), stop=(k == KC - 1))
                return ins
            P.op("pe", mm, reads=[cB], writes=[pB])
            P.op("dve", lambda e, pt=pt, tt=tt: e.tensor_tensor(out=g.IF[:, tt, :], in0=pt[:, 0:16], in1=bif[:],
                                                                op=ALU.add), reads=[pB, cB], writes=[Buf()])
        pend = []
        for i, (c0, kind, info) in enumerate(jobs):
            if i + 2 < len(jobs):
                load(i + 2)
            wt, wB = loaded[i]
            if kind != "fm" and pend:
                for t_ in pend:
                    t_()
                del pend[:]
            if kind == "fm":
                typ, ch0 = info
                for cc in range(4):
                    ch = ch0 + cc
                    for q in range(NQ):
                        pt, pB = psr.next()

                        def mm(e, pt=pt, wt=wt, cc=cc, q=q):
                            for k in range(KC):
                                ins = e.matmul(pt[:], lhsT=wt[:, k, cc * 128:(cc + 1) * 128],
                                               rhs=g.hnT[:, k, q * 512:(q + 1) * 512],
                                               start=(k == 0), stop=(k == KC - 1))
                            return ins
                        P.op("pe", mm, reads=[wB], writes=[pB])
                        prev_tail = pend[:]
                        del pend[:]
                        st, sB = stg.next()
                        if typ == "m":
                            P.op("act", lambda e, st=st, pt=pt, ch=ch: e.activation(
                                out=st[:], in_=pt[:], func=AF.Identity, bias=bqk[:, ch:ch + 1]),
                                reads=[pB, cB], writes=[sB])
                            P.dma("sp", lambda e, st=st, ch=ch, q=q: e.dma_start(
                                out=g.QKs[ch * 128:(ch + 1) * 128, q * 512:(q + 1) * 512], in_=st[:]), sB, reads=[sB])
                        else:
                            xb, xB = xbr.next()
                            P.op("act", lambda e, xb=xb, pt=pt, ch=ch: e.activation(
                                out=xb[:], in_=pt[:], func=AF.Identity, bias=bqk[:, ch:ch + 1]),
                                reads=[pB, cB], writes=[xB])
                            def tail(xb=xb, xB=xB, st=st, sB=sB, ch=ch, q=q, typ=typ):
                                px, pxB = pxp.next()
                                x16, x16B = x16r.next()
                                P.op("dve", lambda e, x16=x16, xb=xb: e.tensor_copy(out=x16[:], in_=xb[0:32, :]),
                                     reads=[xB], writes=[x16B])
                                P.op("pe", lambda e, px=px, x16=x16: e.matmul(px[:], lhsT=g.pmb[:], rhs=x16[:],
                                                                              start=True, stop=True),
                                     reads=[x16B], writes=[pxB])
                                t1, t1B = t1r.next()
                                t2, t2B = t2r.next()
                                P.op("dve", lambda e, t1=t1, px=px, q=q: e.tensor_tensor(
                                    out=t1[:], in0=px[:], in1=sinT[:, q * 512:(q + 1) * 512], op=ALU.mult),
                                    reads=[pxB, cB], writes=[t1B])
                                P.op("dve", lambda e, t2=t2, xb=xb, q=q: e.tensor_tensor(
                                    out=t2[:], in0=xb[0:32, :], in1=cosT[:, q * 512:(q + 1) * 512], op=ALU.mult),
                                    reads=[xB, cB], writes=[t2B])
                                P.op("dve", lambda e, t1=t1, t2=t2, xb=xb: e.tensor_tensor(
                                    out=xb[0:32, :], in0=t1[:], in1=t2[:], op=ALU.add),
                                    reads=[t1B, t2B], writes=[xB])
                                sc = (128.0 ** -0.5) if typ == "aq" else 1.0
                                P.op("act", lambda e, st=st, xb=xb, sc=sc: e.activation(
                                    out=st[:], in_=xb[:], func=AF.Identity, scale=sc), reads=[xB], writes=[sB])

                                P.dma("sp", lambda e, st=st, ch=ch, q=q: e.dma_start(
                                    out=g.QKs[ch * 128:(ch + 1) * 128, q * 512:(q + 1) * 512], in_=st[:]), sB, reads=[sB])
                            pend.append(tail)
                        for t_ in prev_tail:
                            t_()
            else:
                typ, s = info
                dst = {"mv": g.MV, "mo": g.MO, "av": g.AV}[typ]
                boff = {"mv": 0, "mo": 2048, "av": 4096}[typ] + s * 512
                for tt in range(NT):
                    pt, pB = psr.next()

                    def mm(e, pt=pt, wt=wt, tt=tt):
                        for k in range(KC):
                            ins = e.matmul(pt[:], lhsT=g.hnT[:, k, tt * 128:(tt + 1) * 128], rhs=wt[:, k, :],
                                           start=(k == 0), stop=(k == KC - 1))
                        return ins
                    P.op("pe", mm, reads=[wB], writes=[pB])
                    st, sB = stg.next()
                    if typ == "mo":
                        tm, tB = tmr.next()
                        P.op("dve", lambda e, tm=tm, pt=pt, boff=boff: e.tensor_tensor(
                            out=tm[:], in0=pt[:], in1=brep[:, boff:boff + 512], op=ALU.add),
                            reads=[pB, cB], writes=[tB])
                        P.op("act", lambda e, st=st, tm=tm: e.activation(out=st[:], in_=tm[:], func=AF.Sigmoid),
                             reads=[tB], writes=[sB])
                    else:
                        P.op("dve", lambda e, st=st, pt=pt, boff=boff: e.tensor_tensor(
                            out=st[:], in0=pt[:], in1=brep[:, boff:boff + 512], op=ALU.add),
                            reads=[pB, cB], writes=[sB])
                    P.dma("sp", lambda e, st=st, dst=dst, tt=tt, s=s: e.dma_start(
                        out=dst[tt * 128:(tt + 1) * 128, s * 512:(s + 1) * 512], in_=st[:]), sB, reads=[sB])
        P.finish()


def tm_src(ap2d, h, NT):
    return ap2d[:, h * 256:(h + 1) * 256].rearrange("(t p) d -> p t d", p=128)


def ph_mlstm(g, l):
    nc, T = g.nc, g.T
    NT = T // 128
    with ExitStack() as es:
        P = Prog(nc, es, uid("mls"))
        NG = NT * 8
        def v3(ap):
            return ap.rearrange("p (c h) -> p c h", h=8)
        e1 = sb(es, nc, [128, NG], F32, "e1")
        l1 = sb(es, nc, [128, NG], F32, "l1")
        tg = sb(es, nc, [128, NG], F32, "tg")
        u = sb(es, nc, [128, NG], F32, "u")
        bnd = sb(es, nc, [128, NG], F32, "bnd")
        eg = sb(es, nc, [128, NG], F32, "eg")
        mrep = sb(es, nc, [128, 2048], F32, "mrep")
        cgp = pst(es, nc, [128, 2, NG], F32, "cgp")
        gB = Buf()
        P.dma("pool", lambda e: e.dma_start(out=mrep[:], in_=g.mnorm[l].partition_broadcast(128)), gB,
              writes=[gB])
        P.op("act", lambda e: e.activation(out=v3(e1[:]), in_=g.IF[:, :, 8:16], func=AF.Exp, scale=-1.0),
             writes=[gB])
        P.op("act", lambda e: e.activation(out=l1[:], in_=e1[:], func=AF.Ln, bias=1.0), reads=[gB], writes=[gB])

        def mmg(e):
            e.matmul(cgp[:, 0, :], lhsT=g.trif[:], rhs=l1[:], start=True, stop=True)
            return e.matmul(cgp[:, 1, :], lhsT=g.onesf[:], rhs=l1[:], start=True, stop=True)
        P.op("pe", mmg, reads=[gB], writes=[gB])
        P.op("dve", lambda e: e.tensor_tensor(out=v3(tg[:]), in0=v3(cgp[:, 0, :]), in1=g.IF[:, :, 0:8], op=ALU.add),
             reads=[gB], writes=[gB])

        def gex(e):
            e.activation(out=u[:], in_=tg[:], func=AF.Exp, bias=math.log(128.0 ** -0.5))
            e.activation(out=bnd[:], in_=cgp[:, 0, :], func=AF.Exp)
            return e.activation(out=eg[:], in_=cgp[:, 1, :], func=AF.Exp, scale=-1.0)
        P.op("act", gex, reads=[gB], writes=[gB])

        qkr = Rot([(sb(es, nc, [128, T], BF16, "qT"), sb(es, nc, [128, T], BF16, "kT"), Buf(), Buf())
                   for _ in range(2)])
        var = [(sb(es, nc, [128, NT, 257], BF16, "vA"), Buf()) for _ in range(2)]
        gr = Rot(sbufs(es, nc, 2, [128, NT, 256], BF16, "Gg"))
        hms = Rot(sbufs(es, nc, 2, [128, 2, T], BF16, "hms"))
        C = sb(es, nc, [128, 257], F32, "C")
        C1 = sb(es, nc, [128, 257], F32, "C1")
        Cb = sb(es, nc, [128, 257], BF16, "Cb")
        CB, C1B, CbB = Buf(), Buf(), Buf()
        ptr = Rot(sbufs(es, nc, 2, [128, 128], BF16, "PT"))
        kur = Rot(sbufs(es, nc, 2, [128, 128], BF16, "ku"))
        smr = Rot(sbufs(es, nc, 2, [128, 8], F32, "sm"))
        jkr = Rot(sbufs(es, nc, 2, [128, 256], F32, "jk"))
        hmr = Rot(sbufs(es, nc, 2, [128, 256], BF16, "hm"))
        stp = Rot(psbufs(es, nc, 2, [128, 128], F32, "stp"))
        ktp = Rot(psbufs(es, nc, 1, [128, 128], BF16, "ktp"))
        outp = Rot(psbufs(es, nc, 2, [128, 257], F32, "outp"))
        updp = Rot(psbufs(es, nc, 1, [128, 257], F32, "updp"))
        trp = Rot(psbufs(es, nc, 1, [128, 2, 128], BF16, "trp"))
        for vt, vB in var:
            P.op("dve", lambda e, vt=vt: e.memset(vt[:, :, 256:257], 1.0), writes=[vB])
        vr = Rot(var)
        bg = []
        eps = {}

        def advance():
            for gen in list(bg):
                try:
                    next(gen)
                except StopIteration:
                    bg.remove(gen)

        for h in range(H):
            eps.clear()
            qT, kT, qB, kB = qkr.next()
            qkB = qB
            vA, vB = vr.next()
            Gt, GB = gr.next()
            hs, hsB = hms.next()
            P.dma("pool", lambda e, qT=qT, h=h: e.dma_start(out=qT[:], in_=g.QKs[h * 128:(h + 1) * 128, :]), qB,
                  writes=[qB])
            P.dma("pool", lambda e, kT=kT, h=h: e.dma_start(out=kT[:], in_=g.QKs[(8 + h) * 128:(9 + h) * 128, :]),
                  kB, writes=[kB])
            P.dma("pool", lambda e, vA=vA, h=h: e.dma_start(out=vA[:, :, 0:256], in_=tm_src(g.MV, h, NT)), vB,
                  writes=[vB])
            P.dma("pool", lambda e, Gt=Gt, h=h: e.dma_start(out=Gt[:], in_=tm_src(g.MO, h, NT)), GB,
                  writes=[GB])

            def gmul(e, Gt=Gt, h=h):
                for c in range(NT):
                    ins = e.tensor_tensor(out=Gt[:, c, :], in0=Gt[:, c, :], in1=mrep[:, h * 256:(h + 1) * 256],
                                          op=ALU.mult)
                return ins
            P.op("dve", gmul, reads=[gB], writes=[GB])
            for c in range(NT):
                cs = slice(c * 128, (c + 1) * 128)
                uc = u[:, c * 8 + h:c * 8 + h + 1]
                st, stB = stp.next()
                P.op("pe", lambda e, st=st, kT=kT, qT=qT, cs=cs: e.matmul(st[:], lhsT=kT[:, cs], rhs=qT[:, cs],
                                                                          start=True, stop=True),
                     reads=[qB, kB], writes=[stB])
                advance()
                pt, ptB = ptr.next()
                P.op("dve", lambda e, pt=pt, st=st, uc=uc: e.scalar_tensor_tensor(
                    out=pt[:], in0=st[:], scalar=uc, in1=g.trif[:], op0=ALU.mult, op1=ALU.mult),
                    reads=[stB, gB], writes=[ptB])
                kt, ktB = ktp.next()
                P.op("pe", lambda e, kt=kt, kT=kT, cs=cs: e.transpose(kt[:], kT[:, cs], g.identb[:]),
                     reads=[kB], writes=[ktB])
                advance()
                ku, kuB = kur.next()
                P.op("act", lambda e, ku=ku, kt=kt, uc=uc: e.activation(out=ku[:], in_=kt[:], func=AF.Identity,
                                                                        scale=uc),
                     reads=[ktB, gB], writes=[kuB])
                while eps.get(c - 2) in bg:
                    advance()
                ot, oB = outp.next()

                def mmo(e, ot=ot, pt=pt, vA=vA, qT=qT, c=c, cs=cs):
                    ins = e.matmul(ot[:], lhsT=pt[:], rhs=vA[:, c, :], start=True, stop=(c == 0))
                    if c > 0:
                        ins = e.matmul(ot[:], lhsT=qT[:, cs], rhs=Cb[:], start=False, stop=True)
                    return ins
                P.op("pe", mmo, reads=[ptB, vB, qB] + ([CbB] if c > 0 else []), writes=[oB])
                advance()
                if c < NT - 1:
                    up, upB = updp.next()
                    P.op("pe", lambda e, up=up, ku=ku, vA=vA, c=c: e.matmul(up[:], lhsT=ku[:], rhs=vA[:, c, :],
                                                                            start=True, stop=True),
                         reads=[kuB, vB], writes=[upB])
                    egc = eg[:, c * 8 + h:c * 8 + h + 1]
                    if c == 0:
                        P.op("dve", lambda e, up=up, egc=egc: e.tensor_scalar(
                            out=C[:], in0=up[:], scalar1=egc, scalar2=None, op0=ALU.mult),
                            reads=[upB, gB], writes=[CB])
                    else:
                        P.op("act", lambda e, egc=egc: e.activation(out=C1[:], in_=C[:], func=AF.Identity,
                                                                    scale=egc),
                             reads=[CB, gB], writes=[C1B])
                        P.op("dve", lambda e, up=up, egc=egc: e.scalar_tensor_tensor(
                            out=C[:], in0=up[:], scalar=egc, in1=C1[:], op0=ALU.mult, op1=ALU.add),
                            reads=[upB, C1B, gB], writes=[CB])
                    P.op("act", lambda e: e.activation(out=Cb[:], in_=C[:], func=AF.Identity),
                         reads=[CB], writes=[CbB])
                def epilogue(c=c, cs=cs, ot=ot, oB=oB):
                    sm, smB = smr.next()
                    P.op("act", lambda e, sm=sm, ot=ot: e.activation(out=sm[:, 0:1], in_=ot[:, 256:257], func=AF.Abs),
                         reads=[oB], writes=[smB])
                    yield
                    P.op("dve", lambda e, sm=sm, c=c, h=h: e.tensor_scalar(
                        out=sm[:, 1:2], in0=sm[:, 0:1], scalar1=bnd[:, c * 8 + h:c * 8 + h + 1], scalar2=None, op0=ALU.max),
                        reads=[smB, gB], writes=[smB])
                    yield
                    P.op("dve", lambda e, sm=sm: e.reciprocal(out=sm[:, 2:3], in_=sm[:, 1:2]),
                         reads=[smB], writes=[smB])
                    yield
                    jk, jkB = jkr.next()
                    P.op("act", lambda e, jk=jk, ot=ot, sm=sm: e.activation(
                        out=jk[:], in_=ot[:, 0:256], func=AF.Square, scale=sm[:, 2:3], accum_out=sm[:, 3:4]),
                        reads=[oB, smB], writes=[jkB, smB])
                    yield
                    P.op("act", lambda e, sm=sm: e.activation(out=sm[:, 4:5], in_=sm[:, 3:4], func=AF.Ln, bias=EPS,
                                                              scale=1.0 / 256), reads=[smB], writes=[smB])
                    yield
                    P.op("act", lambda e, sm=sm: e.activation(out=sm[:, 5:6], in_=sm[:, 4:5], func=AF.Exp,
                                                              scale=-0.5), reads=[smB], writes=[smB])
                    yield
                    P.op("dve", lambda e, sm=sm: e.tensor_tensor(out=sm[:, 6:7], in0=sm[:, 5:6], in1=sm[:, 2:3],
                                                                 op=ALU.mult), reads=[smB], writes=[smB])
                    yield
                    hm, hmB = hmr.next()
                    P.op("dve", lambda e, hm=hm, ot=ot, sm=sm, Gt=Gt, c=c: e.scalar_tensor_tensor(
                        out=hm[:], in0=ot[:, 0:256], scalar=sm[:, 6:7], in1=Gt[:, c, :], op0=ALU.mult, op1=ALU.mult),
                        reads=[oB, smB, GB], writes=[hmB])
                    yield
                    tr, trB = trp.next()

                    def trs(e, tr=tr, hm=hm):
                        e.transpose(tr[:, 0, :], hm[:, 0:128], g.identb[:])
                        return e.transpose(tr[:, 1, :], hm[:, 128:256], g.identb[:])
                    P.op("pe", trs, reads=[hmB], writes=[trB])
                    yield
                    P.op("act", lambda e, hs=hs, tr=tr, cs=cs: e.activation(out=hs[:, :, cs], in_=tr[:],
                                                                            func=AF.Identity),
                         reads=[trB], writes=[hsB])

                gen_ = epilogue()
                eps[c] = gen_
                bg.append(gen_)

            while bg:
                advance()
            P.dma("sp", lambda e, hs=hs, h=h: e.dma_start(
                out=g.HMT[h * 256:(h + 1) * 256, :].rearrange("(j p) t -> p j t", p=128), in_=hs[:]), hsB,
                reads=[hsB])
        P.finish()


def ph_attn(g, l):
    nc, T = g.nc, g.T
    NT, NG = T // 128, T // 256
    lam_init = 0.8 - 0.6 * math.exp(-0.3 * l)
    with ExitStack() as es:
        P = Prog(nc, es, uid("att"))
        lq = sb(es, nc, [128, 4, 128], F32, "lq")
        lp = sb(es, nc, [128, 2, 128], F32, "lp")
        ls = sb(es, nc, [128, 8], F32, "ls")
        anrep = sb(es, nc, [128, 2048], F32, "anrep")
        cB = Buf()
        P.dma("pool", lambda e: e.dma_start(out=lq[:].rearrange("p a b -> p (a b)"),
                                            in_=g.lamqk[l].partition_broadcast(128)), cB, writes=[cB])
        P.dma("pool", lambda e: e.dma_start(out=anrep[:], in_=g.anorm[l].partition_broadcast(128)), cB,
              writes=[cB])

        def lam1(e):
            e.tensor_tensor(out=lp[:, 0, :], in0=lq[:, 0, :], in1=lq[:, 1, :], op=ALU.mult)
            return e.tensor_tensor(out=lp[:, 1, :], in0=lq[:, 2, :], in1=lq[:, 3, :], op=ALU.mult)
        P.op("dve", lam1, reads=[cB], writes=[cB])

        def lam2(e):
            e.reduce_sum(out=ls[:, 0:1], in_=lp[:, 0, :], axis=mybir.AxisListType.X)
            return e.reduce_sum(out=ls[:, 1:2], in_=lp[:, 1, :], axis=mybir.AxisListType.X)
        P.op("dve", lam2, reads=[cB], writes=[cB])
        P.op("act", lambda e: e.activation(out=ls[:, 2:4], in_=ls[:, 0:2], func=AF.Exp), reads=[cB], writes=[cB])
        P.op("dve", lambda e: e.scalar_tensor_tensor(out=ls[:, 4:5], in0=ls[:, 3:4], scalar=-lam_init,
                                                     in1=ls[:, 2:3], op0=ALU.add, op1=ALU.subtract),
             reads=[cB], writes=[cB])
        P.op("dve", lambda e: e.tensor_scalar(out=anrep[:], in0=anrep[:], scalar1=(1.0 - lam_init), scalar2=None,
                                              op0=ALU.mult), reads=[cB], writes=[cB])
        nlam = ls[:, 4:5]

        inb = []
        for i in range(2):
            vt = sb(es, nc, [128, NT, 257], BF16, "vA")
            vB = Buf()
            P.op("dve", lambda e, vt=vt: e.memset(vt[:, :, 256:257], 1.0), writes=[vB])
            inb.append(dict(q2=sb(es, nc, [128, 2, T], BF16, "q2"), k2=sb(es, nc, [128, 2, T], BF16, "k2"),
                            qB=Buf(), kB=Buf(), vA=vt, vB=vB, hs=sb(es, nc, [128, 2, T], BF16, "has"), hsB=Buf()))
        inr = Rot(inb)
        ptr = Rot(sbufs(es, nc, 4, [128, 2, 256], BF16, "PT"))
        rrr = Rot(sbufs(es, nc, 5, [128, 8], F32, "rr"))
        c0r = Rot(sbufs(es, nc, 5, [128, 257], F32, "c0"))
        c1r = Rot(sbufs(es, nc, 5, [128, 257], F32, "c1"))
        a0r = Rot(sbufs(es, nc, 5, [128, 256], F32, "a0"))
        a1r = Rot(sbufs(es, nc, 5, [128, 256], F32, "a1"))
        jkr = Rot(sbufs(es, nc, 5, [128, 256], F32, "jk"))
        habr = Rot(sbufs(es, nc, 5, [128, 256], BF16, "hab"))
        stp = Rot(psbufs(es, nc, 3, [128, 2, 256], F32, "stp"))
        outp = [[(pst(es, nc, [128, 257], F32, "outp"), Buf()) for j in range(2)] for m in range(2)]
        trp = Rot(psbufs(es, nc, 1, [128, 2, 128], BF16, "trp"))

        hb = {}

        def ensure_head(h):
            if h in hb or h >= H:
                return
            b = inr.next()
            c0 = (16 + 2 * h) * 128
            c1 = (32 + 2 * h) * 128
            P.dma("pool", lambda e: e.dma_start(
                out=b["q2"][:], in_=g.QKs[c0:c0 + 256, :].rearrange("(c p) t -> p c t", p=128)), b["qB"],
                writes=[b["qB"]])
            P.dma("pool", lambda e: e.dma_start(
                out=b["k2"][:], in_=g.QKs[c1:c1 + 256, :].rearrange("(c p) t -> p c t", p=128)), b["kB"],
                writes=[b["kB"]])
            P.dma("pool", lambda e: e.dma_start(out=b["vA"][:, :, 0:256], in_=tm_src(g.AV, h, NT)), b["vB"],
                  writes=[b["vB"]])
            hb[h] = b

        steps = [(h, gq, kt) for h in range(H) for gq in range(NG) for kt in range(2 * gq + 2)]
        LA = 2
        stq = {}

        def rec_qk(i):
            h, gq, kt = steps[i]
            ensure_head(h)
            b = hb[h]
            q2, k2 = b["q2"], b["k2"]
            qs = slice(gq * 256, (gq + 1) * 256)
            ks = slice(kt * 128, (kt + 1) * 128)
            st, stB = stp.next()

            def mmq(e):
                e.matmul(st[:, 0, :], lhsT=k2[:, 0, ks], rhs=q2[:, 0, qs], start=True, stop=True)
                return e.matmul(st[:, 1, :], lhsT=k2[:, 1, ks], rhs=q2[:, 1, qs], start=True, stop=True)
            P.op("pe", mmq, reads=[b["qB"], b["kB"]], writes=[stB])
            stq[i] = (st, stB)

        def epilogue(h, gq, j, b):
            (o0, o0B), (o1, o1B) = outp[0][j], outp[1][j]
            hs, hsB = b["hs"], b["hsB"]
            ts = slice((gq * 2 + j) * 128, (gq * 2 + j + 1) * 128)
            c0, c0B = c0r.next()
            c1, c1B = c1r.next()
            P.op("dve", lambda e: e.tensor_copy(out=c0[:], in_=o0[:]), reads=[o0B], writes=[c0B])
            P.op("dve", lambda e: e.tensor_copy(out=c1[:], in_=o1[:]), reads=[o1B], writes=[c1B])
            yield
            rr, rB = rrr.next()

            def rcp(e):
                e.reciprocal(out=rr[:, 0:1], in_=c0[:, 256:257])
                return e.reciprocal(out=rr[:, 1:2], in_=c1[:, 256:257])
            P.op("dve", rcp, reads=[c0B, c1B], writes=[rB])
            P.op("dve", lambda e: e.tensor_tensor(out=rr[:, 2:3], in0=rr[:, 1:2], in1=nlam, op=ALU.mult),
                 reads=[rB, cB], writes=[rB])
            yield
            a0, a0B = a0r.next()
            P.op("dve", lambda e: e.tensor_scalar(out=a0[:], in0=c0[:, 0:256], scalar1=rr[:, 0:1], scalar2=None,
                                                  op0=ALU.mult), reads=[c0B, rB], writes=[a0B])
            yield
            a1, a1B = a1r.next()
            P.op("dve", lambda e: e.scalar_tensor_tensor(out=a1[:], in0=c1[:, 0:256], scalar=rr[:, 2:3], in1=a0[:],
                                                         op0=ALU.mult, op1=ALU.add),
                 reads=[c1B, rB, a0B], writes=[a1B])
            yield
            jk, jkB = jkr.next()
            P.op("act", lambda e: e.activation(out=jk[:], in_=a1[:], func=AF.Square, accum_out=rr[:, 3:4]),
                 reads=[a1B, rB], writes=[jkB, rB])
            yield
            P.op("act", lambda e: e.activation(out=rr[:, 4:5], in_=rr[:, 3:4], func=AF.Ln, bias=EPS,
                                               scale=1.0 / 256), reads=[rB], writes=[rB])
            yield
            P.op("act", lambda e: e.activation(out=rr[:, 5:6], in_=rr[:, 4:5], func=AF.Exp, scale=-0.5),
                 reads=[rB], writes=[rB])
            yield
            hab, habB = habr.next()
            P.op("dve", lambda e: e.scalar_tensor_tensor(out=hab[:], in0=a1[:], scalar=rr[:, 5:6],
                                                         in1=anrep[:, h * 256:(h + 1) * 256], op0=ALU.mult,
                                                         op1=ALU.mult), reads=[a1B, rB, cB], writes=[habB])
            yield
            tr, trB = trp.next()

            def trs(e):
                e.transpose(tr[:, 0, :], hab[:, 0:128], g.identb[:])
                return e.transpose(tr[:, 1, :], hab[:, 128:256], g.identb[:])
            P.op("pe", trs, reads=[habB], writes=[trB])
            yield
            P.op("dve", lambda e: e.tensor_copy(out=hs[:, :, ts], in_=tr[:]), reads=[trB], writes=[hsB])

        bg = []

        def advance():
            for gen in list(bg):
                try:
                    next(gen)
                except StopIteration:
                    bg.remove(gen)

        for i in range(min(LA, len(steps))):
            rec_qk(i)
        for i, (h, gq, kt) in enumerate(steps):
            if gq == 0 and kt == 0:
                ensure_head(h + 1)
            if i + LA < len(steps):
                rec_qk(i + LA)
            b = hb[h]
            vA, vB = b["vA"], b["vB"]
            st, stB = stq.pop(i)
            pt, ptB = ptr.next()
            P.op("act", lambda e, pt=pt, st=st: e.activation(out=pt[:], in_=st[:], func=AF.Exp),
                 reads=[stB], writes=[ptB])
            if kt >= 2 * gq:
                j0 = kt - 2 * gq

                def msk(e, pt=pt, j0=j0):
                    e.tensor_tensor(out=pt[:, 0, j0 * 128:(j0 + 1) * 128], in0=pt[:, 0, j0 * 128:(j0 + 1) * 128],
                                    in1=g.trib[:], op=ALU.mult)
                    return e.tensor_tensor(out=pt[:, 1, j0 * 128:(j0 + 1) * 128],
                                           in0=pt[:, 1, j0 * 128:(j0 + 1) * 128], in1=g.trib[:], op=ALU.mult)
                P.op("dve", msk, reads=[ptB], writes=[ptB])
            js = [j for j in range(2) if kt <= 2 * gq + j]

            def mmv(e, pt=pt, vA=vA, kt=kt, gq=gq, js=js):
                for j in js:
                    for m in range(2):
                        ins = e.matmul(outp[m][j][0][:], lhsT=pt[:, m, j * 128:(j + 1) * 128], rhs=vA[:, kt, :],
                                       start=(kt == 0), stop=(kt == 2 * gq + j), skip_group_check=True)
                return ins
            P.op("pe", mmv, reads=[ptB, vB], writes=[outp[m][j][1] for j in js for m in range(2)])
            advance()
            if kt == 2 * gq:
                gen = epilogue(h, gq, 0, b)
                next(gen)
                bg.append(gen)
            if kt == 2 * gq + 1:
                gen = epilogue(h, gq, 1, b)
                next(gen)
                bg.append(gen)
                if gq == NG - 1:
                    while bg:
                        advance()
                    P.dma("sp", lambda e, b=b, h=h: e.dma_start(
                        out=g.HAT[h * 256:(h + 1) * 256, :].rearrange("(j p) t -> p j t", p=128), in_=b["hs"][:]),
                        b["hsB"], reads=[b["hsB"]])
                    del hb[h]
        P.finish()


def ph_merge(g, l, which):
    nc, T = g.nc, g.T
    NQ = T // 512
    wa = (g.w_bm if which == 0 else g.w_bd)[l]
    wg = g.w_in[l]
    goff = O_GM if which == 0 else O_GD
    src = g.HMT if which == 0 else g.HAT
    with ExitStack() as es:
        P = Prog(nc, es, uid("mrg"))
        xT = sb(es, nc, [128, KC, T], BF16, "xT")
        xB = Buf()
        bg = sb(es, nc, [128, 32], F32, "bg")
        bgB = Buf()
        P.dma("pool", lambda e: e.dma_start(out=bg[:], in_=g.bgate[l]), bgB, writes=[bgB])
        P.dma("pool", lambda e: e.dma_start(out=xT[:], in_=fm(src)), xB, writes=[xB])
        SW = 256
        NS = 2048 // SW
        CPS = SW // 128
        slabs = Rot(sbufs(es, nc, 4, [128, KC, SW], BF16, "wsl"))
        psy = Rot(psbufs(es, nc, 3, [128, 512], F32, "py"))
        psg = Rot(psbufs(es, nc, 3, [128, 512], F32, "pg"))
        sgr = Rot(sbufs(es, nc, 2, [128, 512], F32, "sg"))
        y1r = Rot(sbufs(es, nc, 3, [128, 512], F32, "y1"))
        if which == 1:
            tr_ = Rot(sbufs(es, nc, 2, [128, 512], F32, "tt"))
            yor = Rot(sbufs(es, nc, 3, [128, 512], BF16, "yo"))
        loaded = []

        def load(s):
            ta, ba = slabs.next()
            P.dma("pool", lambda e, ta=ta, s=s: e.dma_start(out=ta[:], in_=slab_src(wa, s * SW, SW)), ba,
                  writes=[ba])
            tg, bgs = slabs.next()
            P.dma("pool", lambda e, tg=tg, s=s: e.dma_start(out=tg[:], in_=slab_src(wg, goff + s * SW, SW)), bgs,
                  writes=[bgs])
            loaded.append((ta, ba, tg, bgs))
        load(0)
        for s in range(NS):
            if s + 1 < NS:
                load(s + 1)
            ta, ba, tg, bgs = loaded[s]
            for cc in range(CPS):
                ch = s * CPS + cc
                for q in range(NQ):
                    qs = slice(q * 512, (q + 1) * 512)
                    py, pyB = psy.next()
                    pg, pgB = psg.next()

                    def mm(e, py=py, pg=pg, ta=ta, tg=tg, cc=cc, qs=qs):
                        for k in range(KC):
                            e.matmul(py[:], lhsT=ta[:, k, cc * 128:(cc + 1) * 128], rhs=xT[:, k, qs],
                                     start=(k == 0), stop=(k == KC - 1))
                        for k in range(KC):
                            ins = e.matmul(pg[:], lhsT=tg[:, k, cc * 128:(cc + 1) * 128], rhs=g.hnT[:, k, qs],
                                           start=(k == 0), stop=(k == KC - 1))
                        return ins
                    P.op("pe", mm, reads=[ba, bgs, xB], writes=[pyB, pgB])
                    sg, sgB = sgr.next()
                    bcol = which * 16 + ch
                    P.op("act", lambda e, sg=sg, pg=pg, bcol=bcol: e.activation(
                        out=sg[:], in_=pg[:], func=AF.Sigmoid, bias=bg[:, bcol:bcol + 1]),
                        reads=[pgB, bgB], writes=[sgB])
                    if which == 0:
                        y1, y1B = y1r.next()
                        P.op("dve", lambda e, y1=y1, py=py, sg=sg: e.tensor_tensor(out=y1[:], in0=py[:], in1=sg[:],
                                                                                   op=ALU.mult),
                             reads=[pyB, sgB], writes=[y1B])
                        P.dma("sp", lambda e, y1=y1, ch=ch, qs=qs: e.dma_start(
                            out=g.Y1[ch * 128:(ch + 1) * 128, qs], in_=y1[:]), y1B, reads=[y1B])
                    else:
                        y1, y1B = y1r.next()
                        P.dma("pool", lambda e, y1=y1, ch=ch, qs=qs: e.dma_start(
                            out=y1[:], in_=g.Y1[ch * 128:(ch + 1) * 128, qs]), y1B, writes=[y1B])
                        tt, ttB = tr_.next()
                        P.op("dve", lambda e, tt=tt, py=py, sg=sg: e.tensor_tensor(out=tt[:], in0=py[:], in1=sg[:],
                                                                                   op=ALU.mult),
                             reads=[pyB, sgB], writes=[ttB])
                        yo, yoB = yor.next()
                        P.op("dve", lambda e, yo=yo, tt=tt, y1=y1: e.tensor_tensor(out=yo[:], in0=tt[:], in1=y1[:],
                                                                                   op=ALU.add),
                             reads=[ttB, y1B], writes=[yoB])
                        P.dma("sp", lambda e, yo=yo, ch=ch, qs=qs: e.dma_start(
                            out=g.YT[ch * 128:(ch + 1) * 128, qs], in_=yo[:]), yoB, reads=[yoB])
        P.finish()


def ph_dump_hn(g, dst):
    nc = g.nc
    with ExitStack() as es:
        P = Prog(nc, es, uid("dmp"))
        b = Buf()
        for k in range(KC):
            P.dma("sp", lambda e, k=k: e.dma_start(out=dst[k * 128:(k + 1) * 128, :], in_=g.hnT[:, k, :]), b,
                  reads=[b])
        P.finish()


def ph_oproj(g, l, hsrc):
    nc, T = g.nc, g.T
    NQ = T // 512
    wo = g.w_o[l]
    with ExitStack() as es:
        P = Prog(nc, es, uid("opr"))
        xT = sb(es, nc, [128, KC, T], BF16, "xT")
        xB = Buf()
        P.dma("pool", lambda e: e.dma_start(out=xT[:], in_=fm(g.YT)), xB, writes=[xB])
        slabs = Rot(sbufs(es, nc, 3, [128, KC, 512], BF16, "wsl"))
        psr = Rot(psbufs(es, nc, 4, [128, 512], F32, "pp"))
        hir = Rot(sbufs(es, nc, 3, [128, 512], F32, "hi"))
        hor = Rot(sbufs(es, nc, 3, [128, 512], F32, "ho"))
        loaded = []

        def load(s):
            t, b = slabs.next()
            P.dma("pool", lambda e, t=t, s=s: e.dma_start(out=t[:], in_=slab_src(wo, s * 512, 512)), b, writes=[b])
            loaded.append((t, b))
        load(0)
        load(1)
        for s in range(4):
            if s + 2 < 4:
                load(s + 2)
            wt, wB = loaded[s]
            for cc in range(4):
                ch = s * 4 + cc
                for q in range(NQ):
                    qs = slice(q * 512, (q + 1) * 512)
                    hi, hiB = hir.next()
                    P.dma("pool", lambda e, hi=hi, ch=ch, qs=qs: e.dma_start(
                        out=hi[:], in_=hsrc[ch * 128:(ch + 1) * 128, qs]), hiB, writes=[hiB])
                    pt, pB = psr.next()

                    def mm(e, pt=pt, wt=wt, cc=cc, qs=qs):
                        for k in range(KC):
                            ins = e.matmul(pt[:], lhsT=wt[:, k, cc * 128:(cc + 1) * 128], rhs=xT[:, k, qs],
                                           start=(k == 0), stop=(k == KC - 1))
                        return ins
                    P.op("pe", mm, reads=[wB, xB], writes=[pB])
                    ho, hoB = hor.next()
                    P.op("dve", lambda e, ho=ho, pt=pt, hi=hi: e.tensor_tensor(out=ho[:], in0=pt[:], in1=hi[:],
                                                                               op=ALU.add),
                         reads=[pB, hiB], writes=[hoB])
                    P.dma("sp", lambda e, ho=ho, ch=ch, qs=qs: e.dma_start(
                        out=g.hT[ch * 128:(ch + 1) * 128, qs], in_=ho[:]), hoB, reads=[hoB])
                    if False:
                        P.dma("sp", lambda e, ho=ho, ch=ch, qs=qs: e.dma_start(
                            out=g.H1[ch * 128:(ch + 1) * 128, qs], in_=ho[:]), hoB, reads=[hoB])
        P.finish()


def ph_up(g, l):
    nc, T = g.nc, g.T
    NQ = T // 512
    wu = g.w_up[l]
    with ExitStack() as es:
        P = Prog(nc, es, uid("up"))
        cw = sb(es, nc, [128, FC, 3], F32, "cw")
        cb = sb(es, nc, [128, FC], F32, "cb")
        cB = Buf()
        P.dma("pool", lambda e: e.dma_start(out=cw[:], in_=g.cw[l]), cB, writes=[cB])
        P.dma("pool", lambda e: e.dma_start(out=cb[:], in_=g.cb[l]), cB, writes=[cB])
        slabs = Rot(sbufs(es, nc, 4, [128, KC, 512], BF16, "wsl"))
        psr = Rot(psbufs(es, nc, 6, [128, 512], F32, "pp"))
        gts = [(sb(es, nc, [128, T + 2], F32, "gt"), Buf()) for _ in range(2)]
        vsr = Rot(sbufs(es, nc, 2, [128, T], F32, "vs"))
        cvr = Rot(sbufs(es, nc, 2, [128, T], F32, "cv"))
        uor = Rot(sbufs(es, nc, 2, [128, T], BF16, "uo"))
        for gt, gB in gts:
            P.op("dve", lambda e, gt=gt: e.memset(gt[:, 0:2], 0.0), writes=[gB])
        gtr = Rot(gts)
        loaded = []

        def load(s):
            ta, ba = slabs.next()
            P.dma("pool", lambda e, ta=ta, s=s: e.dma_start(out=ta[:], in_=slab_src(wu, s * 512, 512)), ba,
                  writes=[ba])
            tv, bv = slabs.next()
            P.dma("pool", lambda e, tv=tv, s=s: e.dma_start(out=tv[:], in_=slab_src(wu, DFF + s * 512, 512)), bv,
                  writes=[bv])
            loaded.append((ta, ba, tv, bv))
        load(0)
        for s in range(11):
            if s + 1 < 11:
                load(s + 1)
            ta, ba, tv, bv = loaded[s]
            for cc in range(4):
                j = s * 4 + cc
                gt, gB = gtr.next()
                vs, vB = vsr.next()
                for q in range(NQ):
                    qs = slice(q * 512, (q + 1) * 512)
                    pa, paB = psr.next()
                    pv, pvB = psr.next()

                    def mm(e, pa=pa, pv=pv, ta=ta, tv=tv, cc=cc, qs=qs):
                        for k in range(KC):
                            e.matmul(pa[:], lhsT=ta[:, k, cc * 128:(cc + 1) * 128], rhs=g.hnT[:, k, qs],
                                     start=(k == 0), stop=(k == KC - 1))
                        for k in range(KC):
                            ins = e.matmul(pv[:], lhsT=tv[:, k, cc * 128:(cc + 1) * 128], rhs=g.hnT[:, k, qs],
                                           start=(k == 0), stop=(k == KC - 1))
                        return ins
                    P.op("pe", mm, reads=[ba, bv], writes=[paB, pvB])
                    P.op("act", lambda e, gt=gt, pa=pa, q=q: e.activation(
                        out=gt[:, 2 + q * 512:2 + (q + 1) * 512], in_=pa[:], func=AF.Identity),
                        reads=[paB], writes=[gB])
                    P.op("act", lambda e, vs=vs, pv=pv, qs=qs: e.activation(out=vs[:, qs], in_=pv[:],
                                                                            func=AF.Identity),
                         reads=[pvB], writes=[vB])
                cv, cvB = cvr.next()

                P.op("dve", lambda e, cv=cv, gt=gt, j=j: e.tensor_scalar(
                    out=cv[:], in0=gt[:, 0:T], scalar1=cw[:, j, 0:1], scalar2=cb[:, j:j + 1], op0=ALU.mult,
                    op1=ALU.add), reads=[gB, cB], writes=[cvB])
                P.op("dve", lambda e, cv=cv, gt=gt, j=j: e.scalar_tensor_tensor(
                    out=cv[:], in0=gt[:, 1:T + 1], scalar=cw[:, j, 1:2], in1=cv[:], op0=ALU.mult, op1=ALU.add),
                    reads=[gB, cB, cvB], writes=[cvB])
                P.op("dve", lambda e, cv=cv, gt=gt, j=j: e.scalar_tensor_tensor(
                    out=cv[:], in0=gt[:, 2:T + 2], scalar=cw[:, j, 2:3], in1=cv[:], op0=ALU.mult, op1=ALU.add),
                    reads=[gB, cB, cvB], writes=[cvB])
                P.op("act", lambda e, cv=cv: e.activation(out=cv[:], in_=cv[:], func=AF.Silu),
                     reads=[cvB], writes=[cvB])
                uo, uoB = uor.next()
                P.op("dve", lambda e, uo=uo, cv=cv, vs=vs: e.tensor_tensor(out=uo[:], in0=cv[:], in1=vs[:],
                                                                           op=ALU.mult),
                     reads=[cvB, vB], writes=[uoB])
                P.dma("sp", lambda e, uo=uo, j=j: e.dma_start(out=g.UT[j * 128:(j + 1) * 128, :], in_=uo[:]), uoB,
                      reads=[uoB])
        P.finish()


def ph_down(g, l):
    nc, T = g.nc, g.T
    TT = min(T, 1024)
    NQ = TT // 512
    wd = g.w_down[l]
    with ExitStack() as es:
        P = Prog(nc, es, uid("dwn"))
        uT = sb(es, nc, [128, FC, TT], BF16, "uT")
        uB = Buf()
        slabs = Rot(sbufs(es, nc, 3, [128, FC, 128], BF16, "wsl"))
        psr = Rot(psbufs(es, nc, 4, [128, 512], F32, "pp"))
        hir = Rot(sbufs(es, nc, 3, [128, 512], F32, "hi"))
        hor = Rot(sbufs(es, nc, 3, [128, 512], F32, "ho"))
        for half in range(T // TT):
            t0 = half * TT
            P.dma("pool", lambda e, t0=t0: e.dma_start(out=uT[:], in_=fm(g.UT[:, t0:t0 + TT])), uB, writes=[uB])
            loaded = []

            def load(c):
                t, b = slabs.next()
                P.dma("pool", lambda e, t=t, c=c: e.dma_start(
                    out=t[:], in_=wd[:, c * 128:(c + 1) * 128].rearrange("(k p) n -> p k n", p=128)), b,
                    writes=[b])
                loaded.append((t, b))
            load(0)
            load(1)
            for c in range(KC):
                if c + 2 < KC:
                    load(c + 2)
                wt, wB = loaded[c]
                for q in range(NQ):
                    qs = slice(q * 512, (q + 1) * 512)
                    gs = slice(t0 + q * 512, t0 + (q + 1) * 512)
                    hi, hiB = hir.next()
                    P.dma("pool", lambda e, hi=hi, c=c, gs=gs: e.dma_start(
                        out=hi[:], in_=g.hT[c * 128:(c + 1) * 128, gs]), hiB, writes=[hiB])
                    pt, pB = psr.next()

                    def mm(e, pt=pt, wt=wt, qs=qs):
                        for k in range(FC):
                            ins = e.matmul(pt[:], lhsT=wt[:, k, :], rhs=uT[:, k, qs], start=(k == 0),
                                           stop=(k == FC - 1))
                        return ins
                    P.op("pe", mm, reads=[wB, uB], writes=[pB])
                    ho, hoB = hor.next()
                    P.op("dve", lambda e, ho=ho, pt=pt, hi=hi: e.tensor_tensor(out=ho[:], in0=pt[:], in1=hi[:],
                                                                               op=ALU.add),
                         reads=[pB, hiB], writes=[hoB])
                    P.dma("sp", lambda e, ho=ho, c=c, gs=gs: e.dma_start(
                        out=g.hT[c * 128:(c + 1) * 128, gs], in_=ho[:]), hoB, reads=[hoB])
        P.finish()


def build(T, L, debug=False):
    nc = bass.Bass("TRN2", target_bir_lowering=False)
    g = G()
    g.nc, g.T, g.L = nc, T, L
    g.debug = debug

    def din(name, shape, dt=F32):
        return nc.dram_tensor(name, list(shape), dt, kind="ExternalInput").ap()

    def scr(name, shape, dt):
        kind = "ExternalOutput" if debug else "Internal"
        return nc.dram_tensor(name, list(shape), dt, kind=kind).ap()
    g.xT = din("xT", [D, T])
    g.w_in = din("w_in", [L, D, NIN])
    g.w_bm = din("w_bm", [L, D, D])
    g.w_bd = din("w_bd", [L, D, D])
    g.w_o = din("w_o", [L, D, D])
    g.w_up = din("w_up", [L, D, 2 * DFF])
    g.w_down = din("w_down", [L, DFF, D])
    g.gmix = din("gmix", [L, 128, KC])
    g.gffn = din("gffn", [L, 128, KC])
    g.gfin = din("gfin", [128, KC])
    g.bqk = din("bqk", [L, 128, 48])
    g.bgate = din("bgate", [L, 128, 32])
    g.btm = din("btm", [L, 6144])
    g.bif = din("bif", [L, 16])
    g.mnorm = din("mnorm", [L, 2048])
    g.anorm = din("anorm", [L, 2048])
    g.lamqk = din("lamqk", [L, 512])
    g.cw = din("cw", [L, 128, FC, 3])
    g.cb = din("cb", [L, 128, FC])
    g.c_ident = din("c_ident", [128, 128])
    g.c_tri = din("c_tri", [128, 128])
    g.c_ones = din("c_ones", [128, 128])
    g.c_pm = din("c_pm", [32, 32])
    g.c_cos = din("c_cos", [32, T])
    g.c_sin = din("c_sin", [32, T])
    g.outT = nc.dram_tensor("outT", [D, T], F32, kind="ExternalOutput").ap()
    g.hT = scr("hT", [D, T], F32)
    g.QKs = scr("QKs", [48 * 128, T], BF16)
    g.MV = scr("MV", [T, 2048], BF16)
    g.MO = scr("MO", [T, 2048], BF16)
    g.AV = scr("AV", [T, 2048], BF16)
    g.HMT = scr("HMT", [D, T], BF16)
    g.HAT = scr("HAT", [D, T], BF16)
    g.Y1 = scr("Y1", [D, T], F32)
    g.YT = scr("YT", [D, T], BF16)
    g.UT = scr("UT", [DFF, T], BF16)
    if debug:
        g.H1 = scr("H1", [D, T], F32)
        g.HN2 = scr("HN2", [D, T], BF16)
    with ExitStack() as es:
        g.hnT = sb(es, nc, [128, KC, T], BF16, "hnT")
        g.IF = sb(es, nc, [128, T // 128, 16], F32, "IF")
        g.identb = sb(es, nc, [128, 128], BF16, "identb")
        g.trib = sb(es, nc, [128, 128], BF16, "trib")
        g.trif = sb(es, nc, [128, 128], F32, "trif")
        g.onesf = sb(es, nc, [128, 128], F32, "onesf")
        g.onesb = sb(es, nc, [128, 128], BF16, "onesb")
        g.pmf = sb(es, nc, [32, 32], F32, "pmf")
        g.pmb = sb(es, nc, [32, 32], BF16, "pmb")
        ph_consts(g)
        for l in range(L):
            hsrc = g.xT if l == 0 else g.hT
            ph_norm(g, hsrc, g.gmix[l])
            ph_proj(g, l)
            ph_mlstm(g, l)
            ph_attn(g, l)
            ph_merge(g, l, 0)
            ph_merge(g, l, 1)
            ph_oproj(g, l, hsrc)
            if debug == 2:
                continue
            ph_norm(g, g.hT, g.gffn[l])
            ph_up(g, l)
            if debug == 3:
                continue
            ph_down(g, l)
        ph_norm(g, g.hT, g.gfin, final_dst=g.outT)
    return nc


def pcol(v, nch):
    v = np.asarray(v, dtype=np.float32)
    return np.ascontiguousarray(np.swapaxes(v.reshape(v.shape[:-1] + (nch, 128)), -1, -2))


def host_consts(T):
    s = np.arange(128)
    tri = (s[:, None] <= s[None, :]).astype(np.float32)
    pm = np.zeros((32, 32), np.float32)
    for j in range(16):
        pm[j + 16, j] = -1.0
        pm[j, j + 16] = 1.0
    half = 16
    inv = (np.float32(ROPE_THETA) ** (-np.arange(half, dtype=np.float32) / np.float32(half))).astype(np.float32)
    ang = np.arange(T, dtype=np.float32)[:, None] * inv[None, :]
    cos = np.cos(ang.astype(np.float64)).astype(np.float32).T
    sin = np.sin(ang.astype(np.float64)).astype(np.float32).T
    return {
        "c_ident": np.eye(128, dtype=np.float32), "c_tri": tri, "c_ones": np.ones((128, 128), np.float32),
        "c_pm": pm, "c_cos": np.ascontiguousarray(np.concatenate([cos, cos], 0)),
        "c_sin": np.ascontiguousarray(np.concatenate([sin, sin], 0)),
    }


def prep_shared(L, T, norm_mix, w_in, b_in, m_norm, a_norm, lam_qk, w_bm, w_bd, w_o, norm_ffn, w_up, conv_w, conv_b,
                w_down, norm_final):
    f = lambda a: np.ascontiguousarray(np.asarray(a, dtype=np.float32))
    b_in = f(b_in)
    bqk = np.concatenate([b_in[:, O_MQ:O_MQ + 2048], b_in[:, O_AQ:O_AQ + 4096]], axis=1)
    bgate = b_in[:, O_GM:O_GM + 4096]
    btm = np.concatenate([b_in[:, O_MV:O_MV + 4096], b_in[:, O_AV:O_AV + 2048]], axis=1)
    cwp = np.stack([pcol(f(conv_w)[:, i, :], FC) for i in range(3)], axis=-1)
    d = {
        "w_in": f(w_in)[:L], "w_bm": f(w_bm)[:L], "w_bd": f(w_bd)[:L], "w_o": f(w_o)[:L], "w_up": f(w_up)[:L],
        "w_down": f(w_down)[:L],
        "gmix": pcol(norm_mix, KC)[:L], "gffn": pcol(norm_ffn, KC)[:L], "gfin": pcol(norm_final, KC),
        "bqk": pcol(bqk, 48)[:L], "bgate": pcol(bgate, 32)[:L], "btm": f(btm)[:L],
        "bif": f(b_in[:, O_MI:O_MI + 16])[:L], "mnorm": f(m_norm)[:L], "anorm": f(a_norm)[:L],
        "lamqk": f(lam_qk).reshape(-1, 512)[:L], "cw": np.ascontiguousarray(cwp)[:L], "cb": pcol(conv_b, FC)[:L],
    }
    d.update(host_consts(T))
    return {k: np.ascontiguousarray(v) for k, v in d.items()}


def kernel(x, norm_mix, w_in, b_in, m_norm, a_norm, lam_qk, w_bm, w_bd, w_o, norm_ffn, w_up, conv_w, conv_b, w_down,
           norm_final):
    x = np.asarray(x, dtype=np.float32)
    B, S, _ = x.shape
    L = np.asarray(norm_mix).shape[0]
    shared = prep_shared(L, S, norm_mix, w_in, b_in, m_norm, a_norm, lam_qk, w_bm, w_bd, w_o, norm_ffn, w_up, conv_w,
                         conv_b, w_down, norm_final)
    nc = build(S, L)
    NCORES = 8
    real = [0, 1, 4, 5][:B]
    zero_shared = {k: (v if k.startswith("c_") else np.zeros_like(v)) for k, v in shared.items()}
    zx = np.zeros((D, S), np.float32)
    in_maps = []
    for c in range(NCORES):
        if c in real:
            m = dict(shared)
            m["xT"] = np.ascontiguousarray(x[real.index(c)].T)
        else:
            m = dict(zero_shared)
            m["xT"] = zx
        in_maps.append(m)
    res = run_bass_kernel_spmd(nc, in_maps, core_ids=list(range(NCORES)))
    out = np.stack([np.ascontiguousarray(res.results[real[b]]["outT"].T) for b in range(B)], axis=0)
    return out.astype(np.float32)
```
